# Optimizing a Trainium2 kernel written in Bass

```python
import math
import jax, jax.numpy as jnp
from jax import lax
import numpy as np

D_MODEL = 1024
BATCH = 8
SEQ = 4096
DEPTH = 4

GDN_HEADS = 4
GDN_DK = 128
GDN_DV = 128
GDN_CONV = 4
GDN_CHUNK = 64
SWA_HQ = 8
SWA_HKV = 2
SWA_DH = 64
WINDOW = 128
POOL_WINDOWS = (2, 4, 8, 16)
POOL_GROUPS = 4
POOL_GDIM = 128
N_BRANCH = 3
N_EXPERTS = 32
TOP_K = 4
D_FF = 1024
ROUTE_BLOCK = 256
SWIGLU_ALPHA = 1.702
SWIGLU_LIMIT = 7.0
LN_EPS = 1e-5
RMS_EPS = 1e-6
DEEPNORM_ALPHA = (2 * DEPTH) ** 0.25
DEEPNORM_BETA = (8 * DEPTH) ** -0.25

GDN_QK = GDN_HEADS * GDN_DK
GDN_V = GDN_HEADS * GDN_DV
SWA_Q = SWA_HQ * SWA_DH
SWA_KV = SWA_HKV * SWA_DH
POOL_DIM = POOL_GROUPS * POOL_GDIM
IN_SIZES = (GDN_QK, GDN_QK, GDN_V, GDN_V, GDN_HEADS, GDN_HEADS, SWA_Q, SWA_KV, SWA_KV, POOL_DIM, N_BRANCH * D_MODEL)
IN_WIDTH = sum(IN_SIZES)

kernel_name = "hybrid_gdn_swa_pool_moe_deepnorm"


def layer_norm(x):
    xf = x.astype(jnp.float32)
    mu = jnp.mean(xf, -1, keepdims=True)
    var = jnp.mean(jnp.square(xf - mu), -1, keepdims=True)
    return ((xf - mu) * lax.rsqrt(var + LN_EPS)).astype(x.dtype)


def layer_norm_affine(x, g, b):
    xf = x.astype(jnp.float32)
    mu = jnp.mean(xf, -1, keepdims=True)
    var = jnp.mean(jnp.square(xf - mu), -1, keepdims=True)
    y = (xf - mu) * lax.rsqrt(var + LN_EPS) * g.astype(jnp.float32) + b.astype(jnp.float32)
    return y.astype(x.dtype)


def causal_depthwise_conv(x, w):
    k_width, chans = w.shape
    return lax.conv_general_dilated(x, w[:, None, :], window_strides=(1,), padding=((k_width - 1, 0),),
                                    dimension_numbers=('NWC', 'WIO', 'NWC'), feature_group_count=chans)


def l2norm(t):
    return t * lax.rsqrt(jnp.sum(t * t, -1, keepdims=True) + 1e-6)


def gated_delta_rule(q, k, v, g, beta):
    B, S, H, DK = q.shape
    DV = v.shape[-1]
    C = GDN_CHUNK
    N = S // C
    f32 = jnp.float32

    def chunks(t):
        t = t.astype(f32).reshape((B, N, C, H) + t.shape[3:])
        return jnp.moveaxis(t, 3, 1)

    q, k, v, g, beta = chunks(q), chunks(k), chunks(v), chunks(g), chunks(beta)
    q = l2norm(q) * (DK ** -0.5)
    k = l2norm(k)
    gc = jnp.cumsum(g, axis=-1)
    causal = jnp.tril(jnp.ones((C, C), bool))
    strict = jnp.tril(jnp.ones((C, C), bool), -1)
    diff = gc[..., :, None] - gc[..., None, :]
    decay = jnp.where(causal, jnp.exp(jnp.where(causal, diff, 0.0)), 0.0)
    k_beta = k * beta[..., None]
    lower = jnp.where(strict, jnp.einsum('bhnck,bhnsk->bhncs', k_beta, k) * decay, 0.0)
    a_mat = lower + jnp.eye(C, dtype=f32)
    rhs = jnp.concatenate([v * beta[..., None], k_beta * jnp.exp(gc)[..., None]], -1)
    sol = lax.linalg.triangular_solve(a_mat, rhs, left_side=True, lower=True, unit_diagonal=True)
    u, w = sol[..., :DV], sol[..., DV:]
    attn = jnp.einsum('bhnck,bhnsk->bhncs', q, k) * decay
    q_g = q * jnp.exp(gc)[..., None]
    k_g = k * jnp.exp(gc[..., -1:] - gc)[..., None]
    g_last = jnp.exp(gc[..., -1])

    def step(state, inp):
        qg_i, kg_i, u_i, w_i, attn_i, gl_i = inp
        v_new = u_i - jnp.einsum('bhck,bhkv->bhcv', w_i, state)
        out = jnp.einsum('bhck,bhkv->bhcv', qg_i, state) + jnp.einsum('bhcs,bhsv->bhcv', attn_i, v_new)
        state = state * gl_i[..., None, None] + jnp.einsum('bhck,bhcv->bhkv', kg_i, v_new)
        return state, out

    xs = tuple(jnp.moveaxis(t, 2, 0) for t in (q_g, k_g, u, w, attn, g_last))
    s0 = jnp.zeros((B, H, DK, DV), f32)
    _, outs = lax.scan(step, s0, xs)
    return jnp.transpose(outs, (1, 0, 3, 2, 4)).reshape(B, S, H, DV)


def sliding_window_attention(q, k, v, sinks):
    B, S, _ = q.shape
    G = SWA_HQ // SWA_HKV
    NB = S // WINDOW
    f32 = jnp.float32
    qb = q.reshape(B, NB, WINDOW, SWA_HKV, G, SWA_DH)
    kb = k.reshape(B, NB, WINDOW, SWA_HKV, SWA_DH)
    vb = v.reshape(B, NB, WINDOW, SWA_HKV, SWA_DH)

    def with_prev(t):
        prev = jnp.pad(t, ((0, 0), (1, 0), (0, 0), (0, 0), (0, 0)))[:, :-1]
        return jnp.concatenate([prev, t], axis=2)

    kk, vv = with_prev(kb), with_prev(vb)
    s = jnp.einsum('bnqhgd,bnkhd->bnhgqk', qb, kk).astype(f32) * (SWA_DH ** -0.5)
    qi = jnp.arange(WINDOW)[:, None]
    ki = jnp.arange(2 * WINDOW)[None, :]
    dist = qi + WINDOW - ki
    blk = jnp.arange(NB)[:, None, None]
    valid = (dist >= 0) & (dist < WINDOW) & (blk * WINDOW + ki - WINDOW >= 0)
    slopes = 2.0 ** (-8.0 * jnp.arange(1, SWA_HQ + 1, dtype=f32) / SWA_HQ)
    alibi = -slopes.reshape(SWA_HKV, G, 1, 1) * dist.astype(f32)
    s = jnp.where(valid[None, :, None, None], s + alibi, -jnp.inf)
    sink = jnp.broadcast_to(sinks.astype(f32).reshape(1, 1, SWA_HKV, G, 1, 1), s.shape[:-1] + (1,))
    p = jax.nn.softmax(jnp.concatenate([s, sink], -1), axis=-1)[..., :-1]
    o = jnp.einsum('bnhgqk,bnkhd->bnqhgd', p.astype(v.dtype), vv)
    return o.reshape(B, S, SWA_Q)


def multiscale_pool(u, pool_w, pool_scale):
    B, S, _ = u.shape
    f32 = jnp.float32
    uf = u.astype(f32).reshape(B, S, POOL_GROUPS, POOL_GDIM)
    cs = jnp.pad(jnp.cumsum(uf, axis=1), ((0, 0), (1, 0), (0, 0), (0, 0)))
    t = jnp.arange(S)
    outs = []
    for gi, win in enumerate(POOL_WINDOWS):
        hi = cs[:, 1:, gi]
        lo = jnp.concatenate([jnp.zeros((B, win - 1, POOL_GDIM), f32), cs[:, :S - win + 1, gi]], axis=1)
        cnt = jnp.minimum(t + 1, win).astype(f32)[None, :, None]
        outs.append((hi - lo) / cnt - uf[:, :, gi])
    d = jnp.stack(outs, axis=2)
    y = jnp.einsum('bsgc,gcd->bsgd', d, pool_w.astype(f32)).reshape(B, S, POOL_DIM)
    return (y * pool_scale.astype(f32)).astype(u.dtype)


def hybrid_mixer(h, w_in, conv_w, a_log, dt_bias, gdn_norm_w, sinks, pool_w, pool_scale, w_pa, w_pb, w_pc, w_o):
    B, S, _ = h.shape
    f32 = jnp.float32
    points = [int(p) for p in np.cumsum(IN_SIZES)[:-1]]
    proj = h @ w_in
    q_a, k_a, v_a, z_a, a_a, b_a, q_b, k_b, v_b, u_c, gate_logits = jnp.split(proj, points, axis=-1)
    qkv = jax.nn.silu(causal_depthwise_conv(jnp.concatenate([q_a, k_a, v_a], -1), conv_w))
    q_a, k_a, v_a = jnp.split(qkv, [GDN_QK, 2 * GDN_QK], axis=-1)
    beta = jax.nn.sigmoid(b_a.astype(f32))
    g = -jnp.exp(a_log.astype(f32)) * jax.nn.softplus(a_a.astype(f32) + dt_bias.astype(f32))
    o_a = gated_delta_rule(q_a.reshape(B, S, GDN_HEADS, GDN_DK), k_a.reshape(B, S, GDN_HEADS, GDN_DK),
                           v_a.reshape(B, S, GDN_HEADS, GDN_DV), g, beta)
    o_a = (o_a * lax.rsqrt(jnp.mean(o_a * o_a, -1, keepdims=True) + RMS_EPS) * gdn_norm_w.astype(f32)
           * jax.nn.silu(z_a.astype(f32).reshape(B, S, GDN_HEADS, GDN_DV)))
    y_a = o_a.reshape(B, S, GDN_V).astype(h.dtype)
    y_b = sliding_window_attention(q_b, k_b, v_b, sinks)
    y_c = multiscale_pool(u_c, pool_w, pool_scale)
    g_a, g_b, g_c = jnp.split(jax.nn.sigmoid(gate_logits), N_BRANCH, axis=-1)
    merged = g_a * (y_a @ w_pa) + g_b * (y_b @ w_pb) + g_c * (y_c @ w_pc)
    return merged @ w_o


def moe_ffn(h, router_w, router_b, exp_w1, exp_b1, exp_w2, exp_b2):
    B, S, D = h.shape
    T = B * S
    TK = T * TOP_K
    xt = h.reshape(T, D)
    logits = (xt @ router_w + router_b).astype(jnp.float32)
    top_v, top_i = lax.top_k(logits, TOP_K)
    gate = jax.nn.softmax(top_v, axis=-1)
    flat_e = top_i.reshape(-1)
    order = jnp.argsort(flat_e)
    e_sorted = flat_e[order]
    tok_sorted = (order // TOP_K).astype(jnp.int32)
    gate_sorted = gate.reshape(-1)[order]
    counts = jnp.bincount(flat_e, length=N_EXPERTS)
    padded = (counts + ROUTE_BLOCK - 1) // ROUTE_BLOCK * ROUTE_BLOCK
    start = jnp.cumsum(counts) - counts
    ends_p = jnp.cumsum(padded)
    pstart = ends_p - padded
    dest = pstart[e_sorted] + (jnp.arange(TK) - start[e_sorted])
    n_blocks = (TK + ROUTE_BLOCK - 1) // ROUTE_BLOCK + N_EXPERTS
    rows = n_blocks * ROUTE_BLOCK
    buf_tok = jnp.zeros((rows,), jnp.int32).at[dest].set(tok_sorted)
    buf_gate = jnp.zeros((rows,), jnp.float32).at[dest].set(gate_sorted)
    block_e = jnp.minimum(jnp.searchsorted(ends_p, jnp.arange(n_blocks) * ROUTE_BLOCK, side='right'), N_EXPERTS - 1)
    xs = xt[buf_tok].reshape(n_blocks, ROUTE_BLOCK, D)

    def expert_block(args):
        xb, e = args
        gu = xb @ exp_w1[e] + exp_b1[e]
        glu, lin = gu[:, :D_FF], gu[:, D_FF:]
        glu = jnp.minimum(glu, SWIGLU_LIMIT)
        lin = jnp.clip(lin, -SWIGLU_LIMIT, SWIGLU_LIMIT)
        act = glu * jax.nn.sigmoid(SWIGLU_ALPHA * glu) * (lin + 1.0)
        return act @ exp_w2[e] + exp_b2[e]

    ys = lax.map(expert_block, (xs, block_e)).reshape(rows, D)
    out = jax.ops.segment_sum(ys * buf_gate[:, None], buf_tok, num_segments=T)
    return out.reshape(B, S, D).astype(h.dtype)


def setup_inputs(seed: int = 0) -> dict:
    key = jax.random.key(seed)
    ks = jax.random.split(key, 26)
    f32 = jnp.float32

    def nrm(k, shape, scale):
        return jax.random.normal(k, shape, f32) * scale

    L, D, E = DEPTH, D_MODEL, N_EXPERTS
    col_scale = jnp.concatenate([jnp.full((n,), DEEPNORM_BETA if i in (2, 8) else 1.0, f32)
                                 for i, n in enumerate(IN_SIZES)])
    dt = jnp.exp(jax.random.uniform(ks[7], (L, GDN_HEADS), f32, math.log(1e-3), math.log(1e-1)))
    return {
        'x': nrm(ks[0], (BATCH, SEQ, D), 1.0),
        'c': nrm(ks[1], (BATCH, D), 1.0),
        'ada_w': nrm(ks[2], (L, D, 6 * D), 0.2 * D ** -0.5),
        'ada_b': nrm(ks[3], (L, 6 * D), 0.02),
        'w_in': nrm(ks[4], (L, D, IN_WIDTH), D ** -0.5) * col_scale,
        'conv_w': nrm(ks[5], (L, GDN_CONV, 2 * GDN_QK + GDN_V), GDN_CONV ** -0.5),
        'a_log': jnp.log(jax.random.uniform(ks[6], (L, GDN_HEADS), f32, 1.0, 16.0)),
        'dt_bias': dt + jnp.log(-jnp.expm1(-dt)),
        'gdn_norm_w': 1.0 + nrm(ks[8], (L, GDN_DV), 0.1),
        'sinks': nrm(ks[9], (L, SWA_HQ), 0.5),
        'pool_w': nrm(ks[10], (L, POOL_GROUPS, POOL_GDIM, POOL_GDIM), POOL_GDIM ** -0.5),
        'pool_scale': 1.0 + nrm(ks[11], (L, POOL_DIM), 0.1),
        'w_pa': nrm(ks[12], (L, GDN_V, D), GDN_V ** -0.5),
        'w_pb': nrm(ks[13], (L, SWA_Q, D), SWA_Q ** -0.5),
        'w_pc': nrm(ks[14], (L, POOL_DIM, D), POOL_DIM ** -0.5),
        'w_o': nrm(ks[15], (L, D, D), DEEPNORM_BETA * D ** -0.5),
        'ln1_g': 1.0 + nrm(ks[16], (L, D), 0.1),
        'ln1_b': nrm(ks[17], (L, D), 0.02),
        'ln2_g': 1.0 + nrm(ks[18], (L, D), 0.1),
        'ln2_b': nrm(ks[19], (L, D), 0.02),
        'router_w': nrm(ks[20], (L, D, E), D ** -0.5),
        'router_b': nrm(ks[21], (L, E), 0.01),
        'exp_w1': nrm(ks[22], (L, E, D, 2 * D_FF), D ** -0.5),
        'exp_b1': nrm(ks[23], (L, E, 2 * D_FF), 0.02),
        'exp_w2': nrm(ks[24], (L, E, D_FF, D), DEEPNORM_BETA * D_FF ** -0.5),
        'exp_b2': nrm(ks[25], (L, E, D), 0.02),
    }


def reference(x, c, ada_w, ada_b, w_in, conv_w, a_log, dt_bias, gdn_norm_w, sinks, pool_w, pool_scale,
              w_pa, w_pb, w_pc, w_o, ln1_g, ln1_b, ln2_g, ln2_b, router_w, router_b,
              exp_w1, exp_b1, exp_w2, exp_b2):
    cond = jax.nn.silu(c)
    for l in range(DEPTH):
        mod = cond @ ada_w[l] + ada_b[l]
        sh1, sc1, gt1, sh2, sc2, gt2 = jnp.split(mod[:, None, :], 6, axis=-1)
        h = layer_norm(x) * (1.0 + sc1) + sh1
        y = hybrid_mixer(h, w_in[l], conv_w[l], a_log[l], dt_bias[l], gdn_norm_w[l], sinks[l], pool_w[l],
                         pool_scale[l], w_pa[l], w_pb[l], w_pc[l], w_o[l])
        x = layer_norm_affine(DEEPNORM_ALPHA * x + (1.0 + gt1) * y, ln1_g[l], ln1_b[l])
        h = layer_norm(x) * (1.0 + sc2) + sh2
        y = moe_ffn(h, router_w[l], router_b[l], exp_w1[l], exp_b1[l], exp_w2[l], exp_b2[l])
        x = layer_norm_affine(DEEPNORM_ALPHA * x + (1.0 + gt2) * y, ln2_g[l], ln2_b[l])
    return x
```

```python
import numpy as np
from contextlib import ExitStack
import concourse.bass as bass
import concourse.mybir as mybir
from concourse.bass_utils import run_bass_kernel_spmd
from concourse.alu_op_type import AluOpType as ALU

F32, BF16 = mybir.dt.float32, mybir.dt.bfloat16
I32 = mybir.dt.int32
CAP = 1536
BIG = 1.0e6
AF = mybir.ActivationFunctionType
AX = mybir.AxisListType

D = 1024
S = 4096
NT = S // 128
DEPTH = 4
INW = 6408
NE = 32
DFF = 1024
ALPHA = (2 * DEPTH) ** 0.25
NEG = -30000.0
DBG = {}


class KB:
    def __init__(self, nc, es, ndma=48):
        self.nc = nc
        self.eng = {'pe': nc.tensor, 'dve': nc.vector, 'act': nc.scalar, 'pool': nc.gpsimd, 'sp': nc.sync}
        self.sems = []
        self.psid = {}
        for e in self.eng:
            self.psid[e] = len(self.sems)
            self.sems.append(es.enter_context(nc.semaphore("ps_" + e)))
        self.cnt = {e: 0 for e in self.eng}
        self.dsid = []
        for i in range(ndma):
            self.dsid.append(len(self.sems))
            self.sems.append(es.enter_context(nc.semaphore("ds%d" % i)))
        self.dcum = [0] * ndma
        self.dnext = 0
        self.seen = {e: {} for e in self.eng}
        self.lastw = {}
        self.reads = {}
        self.nins = 0
        self.excl = set()

    def need(self, E, tok):
        sid, val, _ = tok
        if self.seen[E].get(sid, 0) < val:
            self.eng[E].wait_ge(self.sems[sid], val)
            self.seen[E][sid] = val
            if 'log' in DBG:
                DBG['log'].append("%s WAIT sem%d>=%d" % (E, sid, val))

    def _deps(self, E, r, w, isdma):
        for k in r:
            t = self.lastw.get(k)
            if t is not None:
                self.need(E, t)
            if k in self.excl:
                for t in self.reads.get(k, {}).values():
                    if t[2] != E:
                        self.need(E, t)
        for k in w:
            t = self.lastw.get(k)
            if t is not None and (isdma or t[2] != E):
                self.need(E, t)
            for t in self.reads.get(k, {}).values():
                if isdma or t[2] != E:
                    self.need(E, t)

    def _commit(self, tok, r, w):
        for k in r:
            self.reads.setdefault(k, {})[tok[0]] = tok
        for k in w:
            self.lastw[k] = tok
            self.reads[k] = {}

    def op(self, E, fn, r=(), w=()):
        self._deps(E, r, w, False)
        ins = fn(self.eng[E])
        ins.then_inc(self.sems[self.psid[E]], 1)
        self.cnt[E] += 1
        self.nins += 1
        tok = (self.psid[E], self.cnt[E], E)
        if 'log' in DBG:
            DBG['log'].append("%s OP#%d r=%s w=%s" % (E, self.cnt[E], list(r), list(w)))
        self._commit(tok, r, w)
        return tok

    def dma(self, Q, out, in_, r=(), w=(), **kw):
        self._deps(Q, r, w, True)
        i = self.dnext
        self.dnext = (i + 1) % len(self.dsid)
        if self.dcum[i] > 0:
            self.need(Q, (self.dsid[i], self.dcum[i], None))
        self.eng[Q].dma_start(out=out, in_=in_, **kw).then_inc(self.sems[self.dsid[i]], 16)
        self.dcum[i] += 16
        self.nins += 1
        tok = (self.dsid[i], self.dcum[i], None)
        if 'log' in DBG:
            DBG['log'].append("%s DMA sem%d->%d r=%s w=%s" % (Q, self.dsid[i], self.dcum[i], list(r), list(w)))
        self._commit(tok, r, w)
        return tok

    def ind(self, Q, r, w, **kw):
        self._deps(Q, r, w, True)
        i = self.dnext
        self.dnext = (i + 1) % len(self.dsid)
        if self.dcum[i] > 0:
            self.need(Q, (self.dsid[i], self.dcum[i], None))
        self.eng[Q].indirect_dma_start(**kw).then_inc(self.sems[self.dsid[i]], 16)
        self.dcum[i] += 16
        self.nins += 1
        tok = (self.dsid[i], self.dcum[i], None)
        self._commit(tok, r, w)
        return tok

    def barrier(self):
        for E in self.eng:
            for F in self.eng:
                if F != E and self.cnt[F] > 0:
                    self.need(E, (self.psid[F], self.cnt[F], F))
            for i, s in enumerate(self.dsid):
                if self.dcum[i] > 0:
                    self.need(E, (s, self.dcum[i], None))


_UID = [0]


def _uname(name):
    _UID[0] += 1
    return "%s_u%d" % (name, _UID[0])


def sbt(nc, st, name, shape, dt):
    return st.enter_context(nc.sbuf_tensor(_uname(name), shape, dt))


def pst(nc, st, name, shape, dt):
    return st.enter_context(nc.psum_tensor(_uname(name), shape, dt))


def stage_mod(kb, nc, l, A, mod_bc, cst):
    with ExitStack() as st:
        cc = sbt(nc, st, "m_cc", [128, 8], F32)
        cond = sbt(nc, st, "m_cond", [128, 8], F32)
        crep = sbt(nc, st, "m_crep", [128, 8, 128], F32)
        brow = sbt(nc, st, "m_brow", [1, 6144], F32)
        wa = [sbt(nc, st, "m_wa%d" % i, [128, 8, 512], F32) for i in range(2)]
        ps = [pst(nc, st, "m_ps%d" % i, [128, 512], F32) for i in range(2)]
        kb.dma('sp', cc[:], A['c_col'][:, :], w=['m_cc'])
        kb.dma('sp', brow[:], A['ada_b'][l:l + 1, :], w=['m_brow'])
        kb.op('act', lambda e: e.activation(out=cond[:], in_=cc[:], func=AF.Silu), r=['m_cc'], w=['m_cond'])
        for kc in range(8):
            kb.op('dve', lambda e, kc=kc: e.tensor_scalar(out=crep[:, kc, :], in0=cst['ones_f'][:, :],
                                                         scalar1=cond[:, kc:kc + 1], scalar2=None, op0=ALU.mult),
                  r=['m_cond', 'cst'], w=['m_crep'])
        for j in range(12):
            b = j % 2
            kb.dma('sp' if j % 2 == 0 else 'pool', wa[b][:],
                   A['ada_w'][l, :, j * 512:(j + 1) * 512].rearrange("(kc p) n -> p kc n", p=128),
                   w=['m_wa%d' % b])
            for kc in range(8):
                kb.op('pe', lambda e, kc=kc, b=b: e.matmul(ps[b][:], lhsT=crep[:, kc, :], rhs=wa[b][:, kc, :],
                                                           start=(kc == 0), stop=False),
                      r=['m_crep', 'm_wa%d' % b], w=['m_ps%d' % b])
            kb.op('pe', lambda e, b=b, j=j: e.matmul(ps[b][:], lhsT=cst['ones_f'][0:1, :],
                                                     rhs=brow[0:1, j * 512:(j + 1) * 512], start=False, stop=True),
                  r=['m_brow', 'cst'], w=['m_ps%d' % b])
            addone = 1.0 if (j // 2) in (1, 2, 4, 5) else 0.0
            kb.op('act', lambda e, b=b, j=j, addone=addone: e.activation(
                out=mod_bc[:, j * 512:(j + 1) * 512], in_=ps[b][:], func=AF.Identity, bias=addone, scale=1.0),
                r=['m_ps%d' % b], w=['mod_bc'])
        kb.barrier()


def ln_mod_tile(kb, nc, xt, xk, bufs, sc_ap, sh_ap, h_out, hk, sfx):
    stt, mv, rstd, nb, xn = bufs
    kb.op('dve', lambda e: e.bn_stats(out=stt[:, 0, :], in_=xt[:, 0:512]), r=[xk], w=['ln_st' + sfx])
    kb.op('dve', lambda e: e.bn_stats(out=stt[:, 1, :], in_=xt[:, 512:1024]), r=[xk], w=['ln_st' + sfx])
    kb.op('dve', lambda e: e.bn_aggr(out=mv[:], in_=stt[:].rearrange("p a b -> p (a b)")),
          r=['ln_st' + sfx], w=['ln_mv' + sfx])
    kb.op('act', lambda e: e.activation(out=rstd[:], in_=mv[:, 1:2], func=AF.Sqrt, bias=1e-5, scale=1.0),
          r=['ln_mv' + sfx], w=['ln_rstd' + sfx])
    kb.op('dve', lambda e: e.reciprocal(out=rstd[:], in_=rstd[:]), r=['ln_rstd' + sfx], w=['ln_rstd' + sfx])
    kb.op('dve', lambda e: e.scalar_tensor_tensor(out=nb[:], in0=mv[:, 0:1], scalar=-1.0, in1=rstd[:],
                                                  op0=ALU.mult, op1=ALU.mult),
          r=['ln_mv' + sfx, 'ln_rstd' + sfx], w=['ln_nb' + sfx])
    kb.op('act', lambda e: e.activation(out=xn[:], in_=xt[:], func=AF.Identity, bias=nb[:], scale=rstd[:]),
          r=[xk, 'ln_nb' + sfx, 'ln_rstd' + sfx], w=['ln_xn' + sfx])
    kb.op('dve', lambda e: e.tensor_tensor(out=xn[:], in0=xn[:], in1=sc_ap, op=ALU.mult),
          r=['ln_xn' + sfx, 'mod_bc'], w=['ln_xn' + sfx])
    kb.op('dve', lambda e: e.tensor_tensor(out=h_out, in0=xn[:], in1=sh_ap, op=ALU.add),
          r=['ln_xn' + sfx, 'mod_bc'], w=[hk])


def build_hT(kb, nc, st, x_src, mod_bc, sc_off, sh_off, hT, cst, pfx):
    xts = [sbt(nc, st, pfx + "xt%d" % i, [128, D], F32) for i in range(2)]
    hbs = [sbt(nc, st, pfx + "hb%d" % i, [128, D], BF16) for i in range(2)]
    bufs = []
    for i in range(2):
        bufs.append((sbt(nc, st, pfx + "st%d" % i, [128, 2, 6], F32), sbt(nc, st, pfx + "mv%d" % i, [128, 2], F32),
                     sbt(nc, st, pfx + "rs%d" % i, [128, 1], F32), sbt(nc, st, pfx + "nb%d" % i, [128, 1], F32),
                     sbt(nc, st, pfx + "xn%d" % i, [128, D], F32)))
    pT = [pst(nc, st, pfx + "pT%d" % i, [128, 8, 128], BF16) for i in range(2)]
    for t in range(NT):
        b = t % 2
        kb.dma('sp', xts[b][:], x_src[t * 128:(t + 1) * 128, :], r=[pfx + 'xsrc'], w=[pfx + 'xt%d' % b])
        ln_mod_tile(kb, nc, xts[b], pfx + 'xt%d' % b, bufs[b], mod_bc[:, sc_off:sc_off + D],
                    mod_bc[:, sh_off:sh_off + D], hbs[b][:], pfx + 'hb%d' % b, pfx + str(b))
        for kc in range(8):
            kb.op('pe', lambda e, kc=kc, b=b: e.transpose(out=pT[b][:, kc, :], in_=hbs[b][:, kc * 128:(kc + 1) * 128],
                                                          identity=cst['ident_b'][:, :]),
                  r=[pfx + 'hb%d' % b, 'cst'], w=[pfx + 'pT%d' % b])
        eng = 'act' if t % 2 == 0 else 'dve'
        if eng == 'act':
            kb.op('act', lambda e, b=b, t=t: e.copy(out=hT[:, :, t * 128:(t + 1) * 128], in_=pT[b][:]),
                  r=[pfx + 'pT%d' % b], w=['hT'])
        else:
            kb.op('dve', lambda e, b=b, t=t: e.tensor_copy(out=hT[:, :, t * 128:(t + 1) * 128], in_=pT[b][:]),
                  r=[pfx + 'pT%d' % b], w=['hT'])


def stage_proj(kb, nc, l, A, SC, x_src, mod_bc, cst):
    with ExitStack() as st:
        hT = sbt(nc, st, "hT", [128, 8, S], BF16)
        with ExitStack() as st2:
            build_hT(kb, nc, st2, x_src, mod_bc, 1 * D, 0, hT, cst, "p1_")
            kb.barrier()
        wts = [sbt(nc, st, "wt%d" % i, [128, 8, 512], BF16) for i in range(2)]
        raw = sbt(nc, st, "raw", [128, S + 4], F32)
        acc = sbt(nc, st, "acc", [128, S], F32)
        tmp = [sbt(nc, st, "tmp%d" % i, [128, 512], F32) for i in range(2)]
        gb = [sbt(nc, st, "gb%d" % i, [128, S], BF16) for i in range(2)]
        cw = sbt(nc, st, "cw", [128, 12, 4], F32)
        alog = sbt(nc, st, "alog", [128, 4], F32)
        dtb = sbt(nc, st, "dtb", [128, 4], F32)
        sm = sbt(nc, st, "sm", [128, 6, 4], F32)
        g_all = sbt(nc, st, "g_all", [128, NT, 4], F32)
        b_all = sbt(nc, st, "b_all", [128, NT, 4], F32)
        vb_all = sbt(nc, st, "vb_all", [128, NT, 128], BF16)
        zt = [sbt(nc, st, "zt%d" % i, [128, 512], F32) for i in range(2)]
        ps = [pst(nc, st, "pj_ps%d" % i, [128, 512], F32) for i in range(4)]
        kb.dma('sp', cw[:], A['conv_wl'][l], w=['cw'])
        kb.dma('sp', alog[:], A['alog_bc'][l], w=['alog'])
        kb.dma('sp', dtb[:], A['dtb_bc'][l], w=['dtb'])
        kb.op('act', lambda e: e.activation(out=alog[:], in_=alog[:], func=AF.Exp), r=['alog'], w=['alog'])
        kb.op('dve', lambda e: e.memset(raw[:, 0:4], 0.0), w=['raw'])
        W = A['w_in']
        state = {'g': 0, 'ps': 0, 'gb': 0}

        def load_group(pieces):
            b = state['g'] % 2
            state['g'] += 1
            for (off, c0, n) in pieces:
                kb.dma('pool', wts[b][:, :, off:off + n],
                       W[l, :, c0:c0 + n].rearrange("(kc p) n -> p kc n", p=128), w=['wt%d' % b])
            return b

        def fm_chunk(b, off, evac):
            for tc in range(8):
                p = state['ps'] % 4
                state['ps'] += 1
                for kc in range(8):
                    kb.op('pe', lambda e, kc=kc, p=p, tc=tc: e.matmul(
                        ps[p][:], lhsT=wts[b][:, kc, off:off + 128], rhs=hT[:, kc, tc * 512:(tc + 1) * 512],
                        start=(kc == 0), stop=(kc == 7)), r=['wt%d' % b, 'hT'], w=['pj_ps%d' % p])
                evac(tc, ps[p], 'pj_ps%d' % p)

        def conv_chunk(b, off, ch):
            def ev(tc, p, pk):
                eng = 'act' if tc % 2 == 0 else 'dve'
                if eng == 'act':
                    kb.op('act', lambda e: e.copy(out=raw[:, 4 + tc * 512: 4 + (tc + 1) * 512], in_=p[:]),
                          r=[pk], w=['raw'])
                else:
                    kb.op('dve', lambda e: e.tensor_copy(out=raw[:, 4 + tc * 512: 4 + (tc + 1) * 512], in_=p[:]),
                          r=[pk], w=['raw'])
            fm_chunk(b, off, ev)
            kb.op('dve', lambda e: e.tensor_scalar(out=acc[:], in0=raw[:, 1:1 + S], scalar1=cw[:, ch, 0:1],
                                                   scalar2=None, op0=ALU.mult), r=['raw', 'cw'], w=['acc'])
            for j in range(1, 4):
                kb.op('dve', lambda e, j=j: e.scalar_tensor_tensor(out=acc[:], in0=raw[:, 1 + j:1 + j + S],
                                                                  scalar=cw[:, ch, j:j + 1], in1=acc[:],
                                                                  op0=ALU.mult, op1=ALU.add),
                      r=['raw', 'cw', 'acc'], w=['acc'])
            kb.op('act', lambda e: e.activation(out=acc[:], in_=acc[:], func=AF.Silu), r=['acc'], w=['acc'])
            if ch < 8:
                kb.op('act', lambda e: e.activation(out=raw[:, 4:4 + S], in_=acc[:], func=AF.Square),
                      r=['acc'], w=['raw'])
                qs = (128 ** -0.5) if ch < 4 else 1.0
                for tc in range(8):
                    p = state['ps'] % 4
                    state['ps'] += 1
                    tb = tc % 2
                    kb.op('pe', lambda e, p=p, tc=tc: e.matmul(ps[p][:], lhsT=cst['ones_f'][:, :],
                                                               rhs=raw[:, 4 + tc * 512:4 + (tc + 1) * 512],
                                                               start=True, stop=True),
                          r=['raw', 'cst'], w=['pj_ps%d' % p])
                    kb.op('act', lambda e, p=p, tb=tb: e.activation(out=tmp[tb][:], in_=ps[p][:], func=AF.Sqrt,
                                                                    bias=1e-6, scale=1.0),
                          r=['pj_ps%d' % p], w=['tmp%d' % tb])
                    kb.op('dve', lambda e, tb=tb: e.reciprocal(out=tmp[tb][:], in_=tmp[tb][:]),
                          r=['tmp%d' % tb], w=['tmp%d' % tb])
                    kb.op('dve', lambda e, tb=tb, tc=tc: e.scalar_tensor_tensor(
                        out=acc[:, tc * 512:(tc + 1) * 512], in0=acc[:, tc * 512:(tc + 1) * 512], scalar=qs,
                        in1=tmp[tb][:], op0=ALU.mult, op1=ALU.mult), r=['tmp%d' % tb, 'acc'], w=['acc'])
            kb.dma('sp', SC['qkv'][ch], acc[:], r=['acc'], w=['s_qkv'])

        for gi in range(3):
            b = load_group([(0, gi * 512, 512)])
            for j in range(4):
                conv_chunk(b, j * 128, gi * 4 + j)

        b = load_group([(0, 1536, 512)])
        for t in range(NT):
            p = state['ps'] % 4
            state['ps'] += 1
            for kc in range(8):
                kb.op('pe', lambda e, kc=kc, p=p, t=t: e.matmul(ps[p][:], lhsT=hT[:, kc, t * 128:(t + 1) * 128],
                                                                rhs=wts[b][:, kc, 0:512], start=(kc == 0),
                                                                stop=(kc == 7)),
                      r=['wt%d' % b, 'hT'], w=['pj_ps%d' % p])
            zb = t % 2
            kb.op('act', lambda e, p=p, zb=zb: e.activation(out=zt[zb][:], in_=ps[p][:], func=AF.Silu),
                  r=['pj_ps%d' % p], w=['zt%d' % zb])
            kb.dma('sp', SC['sz'][t * 128:(t + 1) * 128, :], zt[zb][:], r=['zt%d' % zb], w=['s_sz'])

        b = load_group([(0, 2048, 8), (128, 2568, 256)])
        for t in range(NT):
            p = state['ps'] % 4
            state['ps'] += 1
            for kc in range(8):
                kb.op('pe', lambda e, kc=kc, p=p, t=t: e.matmul(ps[p][:, 0:8], lhsT=hT[:, kc, t * 128:(t + 1) * 128],
                                                                rhs=wts[b][:, kc, 0:8], start=(kc == 0),
                                                                stop=(kc == 7)),
                      r=['wt%d' % b, 'hT'], w=['pj_ps%d' % p])
            pk = 'pj_ps%d' % p
            P = ps[p]
            kb.op('dve', lambda e, P=P: e.tensor_tensor(out=sm[:, 0, :], in0=P[:, 0:4], in1=dtb[:], op=ALU.add),
                  r=[pk, 'dtb'], w=['sm0'])
            kb.op('dve', lambda e: e.scalar_tensor_tensor(out=sm[:, 1, :], in0=sm[:, 0, :], scalar=-1.0,
                                                          in1=sm[:, 0, :], op0=ALU.mult, op1=ALU.min),
                  r=['sm0'], w=['sm1'])
            kb.op('act', lambda e: e.activation(out=sm[:, 2, :], in_=sm[:, 1, :], func=AF.Exp, scale=1.0),
                  r=['sm1'], w=['sm2'])
            kb.op('act', lambda e: e.activation(out=sm[:, 3, :], in_=sm[:, 2, :], func=AF.Ln, bias=1.0, scale=1.0),
                  r=['sm2'], w=['sm3'])
            kb.op('dve', lambda e: e.scalar_tensor_tensor(out=sm[:, 4, :], in0=sm[:, 0, :], scalar=0.0,
                                                          in1=sm[:, 3, :], op0=ALU.max, op1=ALU.add),
                  r=['sm0', 'sm3'], w=['sm4'])
            kb.op('dve', lambda e, t=t: e.scalar_tensor_tensor(out=g_all[:, t, :], in0=sm[:, 4, :], scalar=-1.0,
                                                              in1=alog[:], op0=ALU.mult, op1=ALU.mult),
                  r=['sm4', 'alog'], w=['g_all'])
            kb.op('act', lambda e, P=P, t=t: e.activation(out=b_all[:, t, :], in_=P[:, 4:8], func=AF.Sigmoid),
                  r=[pk], w=['b_all'])
        kb.dma('sp', SC['g'].rearrange("(t p) h -> p t h", p=128), g_all[:], r=['g_all'], w=['s_g'])
        kb.dma('sp', SC['beta'].rearrange("(t p) h -> p t h", p=128), b_all[:], r=['b_all'], w=['s_beta'])
        for t in range(NT):
            p = state['ps'] % 4
            state['ps'] += 1
            for kc in range(8):
                kb.op('pe', lambda e, kc=kc, p=p, t=t: e.matmul(ps[p][:, 0:128], lhsT=hT[:, kc, t * 128:(t + 1) * 128],
                                                                rhs=wts[b][:, kc, 256:384], start=(kc == 0),
                                                                stop=(kc == 7)),
                      r=['wt%d' % b, 'hT'], w=['pj_ps%d' % p])
            kb.op('dve', lambda e, p=p, t=t: e.tensor_copy(out=vb_all[:, t, :], in_=ps[p][:, 0:128]),
                  r=['pj_ps%d' % p], w=['vb_all'])
        kb.dma('sp', SC['vb'].rearrange("(t p) d -> p t d", p=128), vb_all[:], r=['vb_all'], w=['s_vb'])

        def bf_chunk(b, off, dst, fn=None):
            g = state['gb'] % 2
            state['gb'] += 1

            def ev(tc, p, pk):
                if fn is not None:
                    kb.op('act', lambda e: e.activation(out=gb[g][:, tc * 512:(tc + 1) * 512], in_=p[:], func=fn),
                          r=[pk], w=['gb%d' % g])
                elif tc % 2 == 0:
                    kb.op('act', lambda e: e.copy(out=gb[g][:, tc * 512:(tc + 1) * 512], in_=p[:]),
                          r=[pk], w=['gb%d' % g])
                else:
                    kb.op('dve', lambda e: e.tensor_copy(out=gb[g][:, tc * 512:(tc + 1) * 512], in_=p[:]),
                          r=[pk], w=['gb%d' % g])
            fm_chunk(b, off, ev)
            kb.dma('sp', dst, gb[g][:], r=['gb%d' % g], w=['s_misc'])

        bf_chunk(b, 128, SC['kb'])
        pieces = []
        for j in range(4):
            pieces.append((j * 128, 2056 + j * 64, 64))
            pieces.append((j * 128 + 64, 2056 + (j + 4) * 64, 64))
        b = load_group(pieces)
        for j in range(4):
            bf_chunk(b, j * 128, SC['qb'][j])
        b = load_group([(0, 2824, 512)])
        for j in range(4):
            def ev(tc, p, pk):
                if tc % 2 == 0:
                    kb.op('act', lambda e: e.copy(out=acc[:, tc * 512:(tc + 1) * 512], in_=p[:]), r=[pk], w=['acc'])
                else:
                    kb.op('dve', lambda e: e.tensor_copy(out=acc[:, tc * 512:(tc + 1) * 512], in_=p[:]),
                          r=[pk], w=['acc'])
            fm_chunk(b, j * 128, ev)
            kb.dma('sp', SC['uc'][j], acc[:], r=['acc'], w=['s_uc'])
        for gi in range(6):
            b = load_group([(0, 3336 + gi * 512, 512)])
            for j in range(4):
                bf_chunk(b, j * 128, SC['gate'][gi * 4 + j], fn=AF.Sigmoid)
        kb.barrier()


def alloc_scratch(nc):
    SC = {}
    SC['qkv'] = nc.dram_tensor("s_qkv", [12, 128, S], F32).ap()
    SC['sz'] = nc.dram_tensor("s_sz", [S, 512], F32).ap()
    SC['g'] = nc.dram_tensor("s_g", [S, 4], F32).ap()
    SC['beta'] = nc.dram_tensor("s_beta", [S, 4], F32).ap()
    SC['qb'] = nc.dram_tensor("s_qb", [4, 128, S], BF16).ap()
    SC['kb'] = nc.dram_tensor("s_kb", [128, S], BF16).ap()
    SC['vb'] = nc.dram_tensor("s_vb", [S, 128], BF16).ap()
    SC['uc'] = nc.dram_tensor("s_uc", [4, 128, S], F32).ap()
    SC['gate'] = nc.dram_tensor("s_gate", [24, 128, S], BF16).ap()
    return SC


def cp(kb, eng, out, in_, r, w):
    if eng == 'act':
        return kb.op('act', lambda e: e.copy(out=out, in_=in_), r=r, w=w)
    return kb.op(eng, lambda e: e.tensor_copy(out=out, in_=in_), r=r, w=w)


class PsRot:
    def __init__(self, kb, nc, st, n, pfx, dt=F32, shape=(128, 512)):
        self.t = [pst(nc, st, "%s%d" % (pfx, i), list(shape), dt) for i in range(n)]
        self.k = ["%s%d" % (pfx, i) for i in range(n)]
        self.i = 0
        kb.excl.update(self.k)

    def next(self):
        i = self.i
        self.i = (i + 1) % len(self.t)
        return self.t[i], self.k[i]


def stage_gdn(kb, nc, l, A, SC, cst):
    ones, ident = cst['ones_f'], cst['ident_f']
    with ExitStack() as st:
        gm = sbt(nc, st, "g_gm", [128, 6, 128], F32)
        kb.dma('sp', gm[:], A['gm'][:, :, :], w=['cst'])
        g_all = sbt(nc, st, "g_gall", [128, 128], F32)
        b_all = sbt(nc, st, "g_ball", [128, 128], F32)
        gc = sbt(nc, st, "g_gc", [128, 128], F32)
        ngc = sbt(nc, st, "g_ngc", [128, 128], F32)
        egc = sbt(nc, st, "g_egc", [128, 128], F32)
        ekl = sbt(nc, st, "g_ekl", [128, 128], F32)
        gl0e = sbt(nc, st, "g_gl0e", [128, 128], F32)
        gl1e = sbt(nc, st, "g_gl1e", [128, 128], F32)
        negb = sbt(nc, st, "g_negb", [128, 128], F32)
        nw = sbt(nc, st, "g_nw", [128, 128], F32)
        PS = PsRot(kb, nc, st, 8, "g_ps")
        kb.dma('sp', g_all[:].rearrange("p (t h) -> p t h", h=4), SC['g'].rearrange("(t p) h -> p t h", p=128),
               r=['s_g'], w=['g_gall'])
        kb.dma('sp', b_all[:].rearrange("p (t h) -> p t h", h=4), SC['beta'].rearrange("(t p) h -> p t h", p=128),
               r=['s_beta'], w=['g_ball'])
        kb.dma('sp', nw[:], A['gnw_bc'][l], w=['g_nw'])
        p, pk = PS.next()
        kb.op('pe', lambda e: e.matmul(p[:, 0:128], lhsT=gm[:, 0, :], rhs=g_all[:], start=True, stop=True),
              r=['g_gall', 'cst'], w=[pk])
        cp(kb, 'dve', gc[:], p[:, 0:128], [pk], ['g_gc'])
        kb.op('act', lambda e: e.activation(out=egc[:], in_=gc[:], func=AF.Exp), r=['g_gc'], w=['g_egc'])
        kb.op('dve', lambda e: e.tensor_scalar(out=ngc[:], in0=gc[:], scalar1=-1.0, scalar2=None, op0=ALU.mult),
              r=['g_gc'], w=['g_ngc'])
        p2, pk2 = PS.next()
        kb.op('pe', lambda e: e.matmul(p2[:, 0:128], lhsT=gm[:, 1, :], rhs=g_all[:], start=True, stop=True),
              r=['g_gall', 'cst'], w=[pk2])
        kb.op('dve', lambda e: e.tensor_tensor(out=ekl[:], in0=p2[:, 0:128], in1=gc[:], op=ALU.subtract),
              r=[pk2, 'g_gc'], w=['g_ekl'])
        kb.op('act', lambda e: e.activation(out=ekl[:], in_=ekl[:], func=AF.Exp), r=['g_ekl'], w=['g_ekl'])
        for idx, dst, dk in ((2, gl0e, 'g_gl0e'), (3, gl1e, 'g_gl1e')):
            p3, pk3 = PS.next()
            kb.op('pe', lambda e, p3=p3, idx=idx: e.matmul(p3[:, 0:128], lhsT=gm[:, idx, :], rhs=g_all[:],
                                                           start=True, stop=True), r=['g_gall', 'cst'], w=[pk3])
            kb.op('act', lambda e, p3=p3, dst=dst: e.activation(out=dst[:], in_=p3[:, 0:128], func=AF.Exp),
                  r=[pk3], w=[dk])
        kb.op('dve', lambda e: e.tensor_scalar(out=negb[:], in0=b_all[:], scalar1=-1.0, scalar2=None, op0=ALU.mult),
              r=['g_ball'], w=['g_negb'])

        HT = 2048
        qkvb = [sbt(nc, st, "g_qkv%d" % i, [128, 3, HT], F32) for i in range(2)]
        szb = sbt(nc, st, "g_sz", [128, NT, 128], F32)
        yaT = sbt(nc, st, "g_yaT", [128, S], BF16)
        names = ['ke', 'kg', 'vtok', 'dg', 'tmpm', 'decT', 'EB', 'qg', 'attnT', 'PT0', 'P0', 'PT1', 'P1', 'GT0', 'GT1',
                 'bu', 'wT', 'vnA', 'vnB', 'Sa', 'Sb', 'otok', 'junk', 'ytok']
        B = {n: sbt(nc, st, "g_" + n, [128, 128], F32) for n in names}
        ss = sbt(nc, st, "g_ss", [128, 1], F32)
        kb.op('dve', lambda e: e.memset(B['vnA'][:], 0.0), w=['g_vnA'])
        kb.op('dve', lambda e: e.memset(B['vnB'][:], 0.0), w=['g_vnB'])
        ecnt = [0]

        def alt():
            ecnt[0] += 1
            return 'act' if ecnt[0] % 2 == 0 else 'dve'

        def mm(out_p, pk, lhsT, rhs, r, start=True, stop=True):
            return kb.op('pe', lambda e: e.matmul(out_p, lhsT=lhsT, rhs=rhs, start=start, stop=stop), r=r, w=[pk])

        for h in range(4):
            kb.dma('pool', szb[:], SC['sz'][:, h * 128:(h + 1) * 128].rearrange("(t p) d -> p t d", p=128),
                   r=['s_sz'], w=['g_sz'])
            kb.op('dve', lambda e: e.memset(B['Sa'][:], 0.0), w=['g_Sa'])
            Scur, Snxt = 'Sa', 'Sb'
            for half in range(2):
                qb_ = (h * 2 + half) % 2
                for i3 in range(3):
                    kb.dma('sp', qkvb[qb_][:, i3, :], SC['qkv'][i3 * 4 + h, :, half * HT:(half + 1) * HT],
                           r=['s_qkv'], w=['g_qkv%d' % qb_])
                qk = 'g_qkv%d' % qb_
                for tt in range(16):
                    t = half * 16 + tt
                    c = t * 4 + h
                    if 'gdn_tiles' in DBG and (h * 32 + t) >= DBG['gdn_tiles']:
                        continue
                    qT = qkvb[qb_][:, 0, tt * 128:(tt + 1) * 128]
                    kT = qkvb[qb_][:, 1, tt * 128:(tt + 1) * 128]
                    vT = qkvb[qb_][:, 2, tt * 128:(tt + 1) * 128]
                    sub = DBG.get('gdn_sub', 31)
                    pa, pka = PS.next()
                    if sub & 1:
                        kb.op('pe', lambda e: e.matmul(pa[:, 0:128], lhsT=kT, rhs=ident, start=True, stop=True),
                              r=[qk, 'cst'], w=[pka])
                    if sub & 2:
                        kb.op('act', lambda e: e.activation(out=B['ke'][:], in_=pa[:, 0:128], func=AF.Identity,
                                                            scale=egc[:, c:c + 1]),
                              r=[pka, 'g_egc'], w=['g_ke'])
                    if sub & 4:
                        kb.op('dve', lambda e: e.tensor_scalar(out=B['kg'][:], in0=pa[:, 0:128],
                                                               scalar1=ekl[:, c:c + 1], scalar2=None, op0=ALU.mult),
                              r=[pka, 'g_ekl'], w=['g_kg'])
                    pb, pkb = PS.next()
                    if sub & 8:
                        kb.op('pe', lambda e: e.matmul(pb[:, 0:128], lhsT=vT, rhs=ident, start=True, stop=True),
                              r=[qk, 'cst'], w=[pkb])
                    if sub & 16:
                        cp(kb, 'act', B['vtok'][:], pb[:, 0:128], [pkb], ['g_vtok'])
                    if DBG.get('gdn_step', 99) < 1:
                        continue
                    pc, pkc = PS.next()
                    mm(pc[:, 0:128], pkc, kT, kT, [qk])
                    pd, pkd = PS.next()
                    mm(pd[:, 0:128], pkd, kT, qT, [qk])
                    if DBG.get('gdn_step', 99) < 2:
                        continue
                    kb.op('dve', lambda e, c=c: e.tensor_scalar(out=B['dg'][:], in0=ident, scalar1=gc[:, c:c + 1],
                                                                scalar2=None, op0=ALU.mult),
                          r=['cst', 'g_gc'], w=['g_dg'])
                    pe_, pke = PS.next()
                    mm(pe_[:, 0:128], pke, ones, B['dg'][:], ['cst', 'g_dg'])
                    kb.op('dve', lambda e, pe_=pe_: e.tensor_tensor(out=B['tmpm'][:], in0=pe_[:, 0:128],
                                                                    in1=gm[:, 4, :], op=ALU.add),
                          r=[pke, 'cst'], w=['g_tmpm'])
                    if DBG.get('gdn_step', 99) < 3:
                        continue
                    kb.op('act', lambda e, c=c: e.activation(out=B['decT'][:], in_=B['tmpm'][:], func=AF.Exp,
                                                             bias=ngc[:, c:c + 1], scale=1.0),
                          r=['g_tmpm', 'g_ngc'], w=['g_decT'])
                    kb.op('act', lambda e, pe_=pe_: e.activation(out=B['EB'][:], in_=pe_[:, 0:128], func=AF.Exp),
                          r=[pke], w=['g_EB'])
                    kb.op('dve', lambda e, qT=qT: e.tensor_tensor(out=B['qg'][:], in0=qT, in1=B['EB'][:], op=ALU.mult),
                          r=[qk, 'g_EB'], w=['g_qg'])
                    kb.op('dve', lambda e, pd=pd: e.tensor_tensor(out=B['attnT'][:], in0=pd[:, 0:128],
                                                                  in1=B['decT'][:], op=ALU.mult),
                          r=[pkd, 'g_decT'], w=['g_attnT'])
                    kb.op('dve', lambda e: e.tensor_tensor(out=B['tmpm'][:], in0=B['decT'][:], in1=gm[:, 5, :],
                                                           op=ALU.mult), r=['g_decT', 'cst'], w=['g_tmpm'])
                    kb.op('dve', lambda e, pc=pc, c=c: e.scalar_tensor_tensor(
                        out=B['PT0'][:], in0=pc[:, 0:128], scalar=negb[:, c:c + 1], in1=B['tmpm'][:],
                        op0=ALU.mult, op1=ALU.mult), r=[pkc, 'g_negb', 'g_tmpm'], w=['g_PT0'])
                    if DBG.get('gdn_step', 99) < 4:
                        continue
                    pf, pkf = PS.next()
                    kb.op('pe', lambda e, pf=pf: e.matmul(pf[:, 0:128], lhsT=B['PT0'][:], rhs=ident, start=True, stop=True),
                          r=['g_PT0', 'cst'], w=[pkf])
                    cp(kb, 'act', B['P0'][:], pf[:, 0:128], [pkf], ['g_P0'])
                    kb.op('dve', lambda e: e.tensor_tensor(out=B['GT0'][:], in0=B['PT0'][:], in1=ident, op=ALU.add),
                          r=['g_PT0', 'cst'], w=['g_GT0'])
                    if DBG.get('gdn_step', 99) < 5:
                        continue
                    Pc, PTc, Gc = 'P0', 'PT0', 'GT0'
                    for lv in range(1, 6):
                        Pn = 'P1' if Pc == 'P0' else 'P0'
                        PTn = 'PT1' if PTc == 'PT0' else 'PT0'
                        Gn = 'GT1' if Gc == 'GT0' else 'GT0'
                        p1, pk1 = PS.next()
                        mm(p1[:, 0:128], pk1, B[PTc][:], B[Pc][:], ['g_' + PTc, 'g_' + Pc])
                        cp(kb, 'act', B[Pn][:], p1[:, 0:128], [pk1], ['g_' + Pn])
                        if lv < 5:
                            p2_, pk2_ = PS.next()
                            mm(p2_[:, 0:128], pk2_, B[Pc][:], B[PTc][:], ['g_' + PTc, 'g_' + Pc])
                            cp(kb, 'dve', B[PTn][:], p2_[:, 0:128], [pk2_], ['g_' + PTn])
                        p3_, pk3_ = PS.next()
                        mm(p3_[:, 0:128], pk3_, B[Pn][:], B[Gc][:], ['g_' + Pn, 'g_' + Gc])
                        kb.op('dve', lambda e, p3_=p3_, Gn=Gn, Gc=Gc: e.tensor_tensor(
                            out=B[Gn][:], in0=p3_[:, 0:128], in1=B[Gc][:], op=ALU.add),
                            r=[pk3_, 'g_' + Gc], w=['g_' + Gn])
                        Pc, PTc, Gc = Pn, PTn, Gn
                    if DBG.get('gdn_step', 99) < 6:
                        continue
                    G = B[Gc]
                    Gk = 'g_' + Gc
                    pu, pku = PS.next()
                    mm(pu[:, 0:128], pku, G[:], B['vtok'][:], [Gk, 'g_vtok'])
                    kb.op('act', lambda e, pu=pu, c=c: e.activation(out=B['bu'][:], in_=pu[:, 0:128], func=AF.Identity,
                                                                    scale=b_all[:, c:c + 1]),
                          r=[pku, 'g_ball'], w=['g_bu'])
                    pw, pkw = PS.next()
                    mm(pw[:, 0:128], pkw, B['ke'][:], G[:], [Gk, 'g_ke'])
                    cp(kb, 'dve', B['wT'][:], pw[:, 0:128], [pkw], ['g_wT'])
                    if DBG.get('gdn_step', 99) < 7:
                        continue
                    for ci, vn, gle, gk in ((0, 'vnA', gl0e, 'g_gl0e'), (1, 'vnB', gl1e, 'g_gl1e')):
                        lo, hi = ci * 64, ci * 64 + 64
                        Sc = B[Scur]
                        Sk = 'g_' + Scur
                        p4, pk4 = PS.next()
                        mm(p4[:, 0:128], pk4, B['wT'][:], Sc[:], ['g_wT', Sk])
                        kb.op('dve', lambda e, p4=p4, vn=vn, lo=lo, hi=hi, c=c: e.scalar_tensor_tensor(
                            out=B[vn][lo:hi, :], in0=p4[lo:hi, 0:128], scalar=negb[lo:hi, c:c + 1],
                            in1=B['bu'][lo:hi, :], op0=ALU.mult, op1=ALU.add),
                            r=[pk4, 'g_negb', 'g_bu'], w=['g_' + vn])
                        p5, pk5 = PS.next()
                        mm(p5[:, 0:128], pk5, B['qg'][:], Sc[:], ['g_qg', Sk], start=True, stop=False)
                        mm(p5[:, 0:128], pk5, B['attnT'][:], B[vn][:], ['g_attnT', 'g_' + vn], start=False, stop=True)
                        cp(kb, 'act', B['otok'][lo:hi, :], p5[lo:hi, 0:128], [pk5], ['g_otok'])
                        p6, pk6 = PS.next()
                        mm(p6[:, 0:128], pk6, B['kg'][:], B[vn][:], ['g_kg', 'g_' + vn])
                        kb.op('dve', lambda e, p6=p6, Sc=Sc, gle=gle, c=c, Snxt=Snxt: e.scalar_tensor_tensor(
                            out=B[Snxt][:], in0=Sc[:], scalar=gle[:, c:c + 1], in1=p6[:, 0:128],
                            op0=ALU.mult, op1=ALU.add), r=[pk6, Sk, gk], w=['g_' + Snxt])
                        Scur, Snxt = Snxt, Scur
                    if DBG.get('gdn_step', 99) < 8:
                        continue
                    kb.op('act', lambda e: e.activation(out=B['junk'][:], in_=B['otok'][:], func=AF.Square,
                                                        accum_out=ss[:]), r=['g_otok'], w=['g_junk', 'g_ss'])
                    kb.op('act', lambda e: e.activation(out=ss[:], in_=ss[:], func=AF.Sqrt, bias=1e-6,
                                                        scale=1.0 / 128), r=['g_ss'], w=['g_ss'])
                    kb.op('dve', lambda e: e.reciprocal(out=ss[:], in_=ss[:]), r=['g_ss'], w=['g_ss'])
                    kb.op('dve', lambda e: e.scalar_tensor_tensor(out=B['ytok'][:], in0=B['otok'][:], scalar=ss[:],
                                                                  in1=nw[:], op0=ALU.mult, op1=ALU.mult),
                          r=['g_otok', 'g_ss', 'g_nw'], w=['g_ytok'])
                    kb.op('dve', lambda e, t=t: e.tensor_tensor(out=B['ytok'][:], in0=B['ytok'][:], in1=szb[:, t, :],
                                                                op=ALU.mult), r=['g_ytok', 'g_sz'], w=['g_ytok'])
                    if DBG.get('gdn_step', 99) < 9:
                        continue
                    p7, pk7 = PS.next()
                    kb.op('pe', lambda e, p7=p7: e.matmul(p7[:, 0:128], lhsT=B['ytok'][:], rhs=ident, start=True, stop=True),
                          r=['g_ytok', 'cst'], w=[pk7])
                    cp(kb, 'act', yaT[:, t * 128:(t + 1) * 128], p7[:, 0:128], [pk7], ['g_yaT'])
                    if 'gdn_dump' in DBG and h == 0 and t == 0:
                        for bi, n_ in enumerate(names):
                            kb.dma('sp', DBG['gdn_dump'][bi], B[n_][:], r=['g_' + n_], w=['dump'])
            kb.dma('sp', SC['ya'][h], yaT[:], r=['g_yaT'], w=['s_ya'])
        kb.barrier()


def stage_swa(kb, nc, l, A, SC, cst):
    with ExitStack() as st:
        swab = sbt(nc, st, "a_swab", [128, 8, 256], F32)
        kb.dma('sp', swab[:], A['swab'][:, :, :], w=['cst'])
        qb = sbt(nc, st, "a_qb", [128, 4, S], BF16)
        kbt = sbt(nc, st, "a_kb", [128, S], BF16)
        vb = sbt(nc, st, "a_vb", [128, NT, 128], BF16)
        ybT = sbt(nc, st, "a_ybT", [64, 8, S], BF16)
        snk = sbt(nc, st, "a_snk", [128, 8], F32)
        sc = [sbt(nc, st, "a_sc%d" % i, [128, 256], F32) for i in range(2)]
        pb_ = [sbt(nc, st, "a_p%d" % i, [128, 256], BF16) for i in range(2)]
        pT = [sbt(nc, st, "a_pT%d" % i, [128, 2, 128], BF16) for i in range(2)]
        sm = [sbt(nc, st, "a_sm%d" % i, [128, 8], F32) for i in range(2)]
        PS = PsRot(kb, nc, st, 3, "a_ps")
        PT = PsRot(kb, nc, st, 2, "a_pt", dt=BF16, shape=(128, 2, 128))
        PO = PsRot(kb, nc, st, 2, "a_po")
        for j in range(4):
            kb.dma('sp', qb[:, j, :], SC['qb'][j], r=['s_misc'], w=['a_qb'])
        kb.dma('sp', kbt[:], SC['kb'], r=['s_misc'], w=['a_kb'])
        kb.dma('sp', vb[:], SC['vb'].rearrange("(t p) d -> p t d", p=128), r=['s_vb'], w=['a_vb'])
        kb.dma('sp', snk[:], A['sink_bc'][l], w=['a_snk'])
        u = 0
        for n in range(NT):
            for hq in range(8):
                kv = hq // 4
                j = hq % 4
                lo = kv * 64
                W = 128 if n == 0 else 256
                k0 = 0 if n == 0 else (n - 1) * 128
                b = u % 2
                u += 1
                p, pk = PS.next()
                kb.op('pe', lambda e: e.matmul(p[:, 0:W], lhsT=qb[lo:lo + 64, j, n * 128:(n + 1) * 128],
                                               rhs=kbt[lo:lo + 64, k0:k0 + W], start=True, stop=True),
                      r=['a_qb', 'a_kb'], w=[pk])
                kb.op('dve', lambda e: e.scalar_tensor_tensor(out=sc[b][:, 0:W], in0=p[:, 0:W], scalar=0.125,
                                                              in1=swab[:, hq, 256 - W:256], op0=ALU.mult,
                                                              op1=ALU.add), r=[pk, 'cst'], w=['a_sc%d' % b])
                s_ = sm[b]
                sk = 'a_sm%d' % b
                kb.op('dve', lambda e: e.reduce_max(out=s_[:, 0:1], in_=sc[b][:, 0:W], axis=AX.X),
                      r=['a_sc%d' % b], w=[sk])
                kb.op('dve', lambda e: e.tensor_tensor(out=s_[:, 1:2], in0=s_[:, 0:1], in1=snk[:, hq:hq + 1],
                                                       op=ALU.max), r=[sk, 'a_snk'], w=[sk])
                kb.op('dve', lambda e: e.tensor_scalar(out=s_[:, 2:3], in0=s_[:, 1:2], scalar1=-1.0, scalar2=None,
                                                       op0=ALU.mult), r=[sk], w=[sk])
                kb.op('act', lambda e: e.activation(out=sc[b][:, 0:W], in_=sc[b][:, 0:W], func=AF.Exp,
                                                    bias=s_[:, 2:3], scale=1.0, accum_out=s_[:, 3:4]),
                      r=['a_sc%d' % b, sk], w=['a_sc%d' % b, sk])
                kb.op('act', lambda e: e.activation(out=s_[:, 4:5], in_=snk[:, hq:hq + 1], func=AF.Exp,
                                                    bias=s_[:, 2:3], scale=1.0), r=[sk, 'a_snk'], w=[sk])
                kb.op('dve', lambda e: e.tensor_tensor(out=s_[:, 5:6], in0=s_[:, 3:4], in1=s_[:, 4:5], op=ALU.add),
                      r=[sk], w=[sk])
                kb.op('dve', lambda e: e.reciprocal(out=s_[:, 6:7], in_=s_[:, 5:6]), r=[sk], w=[sk])
                kb.op('dve', lambda e: e.tensor_scalar(out=pb_[b][:, 0:W], in0=sc[b][:, 0:W], scalar1=s_[:, 6:7],
                                                       scalar2=None, op0=ALU.mult),
                      r=['a_sc%d' % b, sk], w=['a_p%d' % b])
                nk = W // 128
                pt, ptk = PT.next()
                for q_ in range(nk):
                    kb.op('pe', lambda e, q_=q_: e.transpose(out=pt[:, q_, :], in_=pb_[b][:, q_ * 128:(q_ + 1) * 128],
                                                            identity=cst['ident_b'][:, :]),
                          r=['a_p%d' % b, 'cst'], w=[ptk])
                cp(kb, 'act', pT[b][:, 0:nk, :], pt[:, 0:nk, :], [ptk], ['a_pT%d' % b])
                po, pok = PO.next()
                for q_ in range(nk):
                    tk = n if n == 0 else n - 1 + q_
                    kb.op('pe', lambda e, q_=q_, tk=tk: e.matmul(po[0:64, 0:128], lhsT=vb[:, tk, kv * 64:(kv + 1) * 64],
                                                                rhs=pT[b][:, q_, :], start=(q_ == 0),
                                                                stop=(q_ == nk - 1)),
                          r=['a_vb', 'a_pT%d' % b], w=[pok])
                cp(kb, 'act' if u % 2 else 'dve', ybT[:, hq, n * 128:(n + 1) * 128], po[0:64, 0:128], [pok], ['a_ybT'])
        kb.dma('sp', SC['yb'], ybT[:], r=['a_ybT'], w=['s_yb'])
        kb.barrier()


def ln_affine_store(kb, nc, xin, xk, bufs, g_bc, b_bc, dst, sfx, dk):
    stt, mv, rstd, nb = bufs
    kb.op('dve', lambda e: e.bn_stats(out=stt[:, 0, :], in_=xin[:, 0:512]), r=[xk], w=['la_st' + sfx])
    kb.op('dve', lambda e: e.bn_stats(out=stt[:, 1, :], in_=xin[:, 512:1024]), r=[xk], w=['la_st' + sfx])
    kb.op('dve', lambda e: e.bn_aggr(out=mv[:], in_=stt[:].rearrange("p a b -> p (a b)")),
          r=['la_st' + sfx], w=['la_mv' + sfx])
    kb.op('act', lambda e: e.activation(out=rstd[:], in_=mv[:, 1:2], func=AF.Sqrt, bias=1e-5, scale=1.0),
          r=['la_mv' + sfx], w=['la_rstd' + sfx])
    kb.op('dve', lambda e: e.reciprocal(out=rstd[:], in_=rstd[:]), r=['la_rstd' + sfx], w=['la_rstd' + sfx])
    kb.op('dve', lambda e: e.scalar_tensor_tensor(out=nb[:], in0=mv[:, 0:1], scalar=-1.0, in1=rstd[:],
                                                  op0=ALU.mult, op1=ALU.mult),
          r=['la_mv' + sfx, 'la_rstd' + sfx], w=['la_nb' + sfx])
    kb.op('act', lambda e: e.activation(out=xin[:], in_=xin[:], func=AF.Identity, bias=nb[:], scale=rstd[:]),
          r=[xk, 'la_nb' + sfx, 'la_rstd' + sfx], w=[xk])
    kb.op('pool', lambda e: e.tensor_tensor(out=xin[:], in0=xin[:], in1=g_bc, op=ALU.mult), r=[xk, 'lngb'], w=[xk])
    kb.op('pool', lambda e: e.tensor_tensor(out=xin[:], in0=xin[:], in1=b_bc, op=ALU.add), r=[xk, 'lngb'], w=[xk])
    kb.dma('sp', dst, xin[:], r=[xk], w=[dk])


def stage_merge(kb, nc, l, A, SC, x_src, x_dst, mod_bc, cst):
    with ExitStack() as st:
        U = [sbt(nc, st, "c_u%d" % i, [128, 16 + S], F32) for i in range(3)]
        pw = sbt(nc, st, "c_pw", [128, 4, 128], F32)
        pscl = sbt(nc, st, "c_ps", [128, 4], F32)
        ic = sbt(nc, st, "c_ic", [128, 4, 16], F32)
        yc = [sbt(nc, st, "c_yc%d" % i, [128, S], BF16) for i in range(2)]
        PS = PsRot(kb, nc, st, 4, "c_pp")
        kb.dma('sp', pw[:], A['pool_w'][l].rearrange("g c d -> c g d"), w=['c_pw'])
        kb.dma('sp', pscl[:], A['pscale_l'][l], w=['c_ps'])
        kb.dma('sp', ic[:], A['invcnt'][:, :, :], w=['c_ic'])
        for i in range(3):
            kb.op('dve', lambda e, i=i: e.memset(U[i][:, 0:16], 0.0), w=['c_u%d' % i])
        for g, win in enumerate((2, 4, 8, 16)):
            kb.dma('sp', U[0][:, 16:], SC['uc'][g], r=['s_uc'], w=['c_u0'])
            cur = 0
            sh = 1
            while sh < win:
                nxt = 1 if cur != 1 else 2
                kb.op('dve' if sh % 4 == 1 else 'pool', lambda e, cur=cur, nxt=nxt, sh=sh: e.tensor_tensor(
                    out=U[nxt][:, 16:], in0=U[cur][:, 16:], in1=U[cur][:, 16 - sh:16 - sh + S], op=ALU.add),
                    r=['c_u%d' % cur], w=['c_u%d' % nxt])
                cur = nxt
                sh *= 2
            dn = 1 if cur != 1 else 2
            kb.op('dve', lambda e, cur=cur, dn=dn: e.scalar_tensor_tensor(
                out=U[dn][:, 16:], in0=U[cur][:, 16:], scalar=1.0 / win, in1=U[0][:, 16:], op0=ALU.mult,
                op1=ALU.subtract), r=['c_u%d' % cur, 'c_u0'], w=['c_u%d' % dn])
            kb.op('dve', lambda e, cur=cur, dn=dn: e.tensor_tensor(out=U[dn][:, 16:32], in0=U[cur][:, 16:32],
                                                                  in1=ic[:, g, :], op=ALU.mult),
                  r=['c_u%d' % cur, 'c_ic'], w=['c_u%d' % dn])
            kb.op('dve', lambda e, dn=dn: e.tensor_tensor(out=U[dn][:, 16:32], in0=U[dn][:, 16:32],
                                                          in1=U[0][:, 16:32], op=ALU.subtract),
                  r=['c_u%d' % dn, 'c_u0'], w=['c_u%d' % dn])
            yb_ = g % 2
            for tc in range(8):
                p, pk = PS.next()
                kb.op('pe', lambda e, tc=tc, dn=dn: e.matmul(p[:], lhsT=pw[:, g, :],
                                                             rhs=U[dn][:, 16 + tc * 512:16 + (tc + 1) * 512],
                                                             start=True, stop=True), r=['c_pw', 'c_u%d' % dn], w=[pk])
                kb.op('act', lambda e, tc=tc: e.activation(out=yc[yb_][:, tc * 512:(tc + 1) * 512], in_=p[:],
                                                           func=AF.Identity, scale=pscl[:, g:g + 1]),
                      r=[pk, 'c_ps'], w=['c_yc%d' % yb_])
            kb.dma('sp', SC['yc'][g], yc[yb_][:], r=['c_yc%d' % yb_], w=['s_yc'])
        kb.barrier()
    with ExitStack() as st:
        wp = sbt(nc, st, "e_wp", [128, 12, D], BF16)
        wpb = sbt(nc, st, "e_wpb", [64, 8, D], BF16)
        wo = sbt(nc, st, "e_wo", [128, 8, D], BF16)
        lng = sbt(nc, st, "e_lng", [128, D], F32)
        lnb = sbt(nc, st, "e_lnb", [128, D], F32)
        gt = [sbt(nc, st, "e_gt0", [128, 24, 512], BF16)] * 2
        ya = [sbt(nc, st, "e_ya%d" % i, [128, 4, 512], BF16) for i in range(2)]
        ycb = [sbt(nc, st, "e_yc%d" % i, [128, 4, 512], BF16) for i in range(2)]
        ybb = [sbt(nc, st, "e_yb%d" % i, [64, 8, 512], BF16) for i in range(2)]
        mg = [sbt(nc, st, "e_mg%d" % i, [128, 8, 512], BF16) for i in range(2)]
        t1 = [sbt(nc, st, "e_t1%d" % i, [128, 512], F32) for i in range(2)]
        t2 = [sbt(nc, st, "e_t2%d" % i, [128, 512], F32) for i in range(2)]
        t3 = [sbt(nc, st, "e_t3%d" % i, [128, 512], F32) for i in range(2)]
        xt = [sbt(nc, st, "e_xt%d" % i, [128, D], F32) for i in range(2)]
        yt = [sbt(nc, st, "e_yt%d" % i, [128, D], F32) for i in range(2)]
        lb = [(sbt(nc, st, "e_st%d" % i, [128, 2, 6], F32), sbt(nc, st, "e_mv%d" % i, [128, 2], F32),
               sbt(nc, st, "e_rs%d" % i, [128, 1], F32), sbt(nc, st, "e_nb%d" % i, [128, 1], F32)) for i in range(2)]
        PA = PsRot(kb, nc, st, 6, "e_pa")
        PY = PsRot(kb, nc, st, 2, "e_py")
        kb.dma('pool', wp[:, 0:4, :], A['w_pa'][l].rearrange("(kc p) n -> p kc n", p=128), w=['e_wp'])
        kb.dma('pool', wp[:, 4:8, :], A['w_pc'][l].rearrange("(kc p) n -> p kc n", p=128), w=['e_wp'])
        kb.dma('pool', wpb[:], A['w_pb'][l].rearrange("(h p) n -> p h n", p=64), w=['e_wpb'])
        kb.dma('pool', wo[:], A['w_o'][l].rearrange("(kc p) n -> p kc n", p=128), w=['e_wo'])
        kb.dma('sp', lng[:], A['ln1g_bc'][l], w=['lngb'])
        kb.dma('sp', lnb[:], A['ln1b_bc'][l], w=['lngb'])
        u = 0
        for tc in range(8):
            b = tc % 2
            ts = slice(tc * 512, (tc + 1) * 512)
            kb.dma('sp', gt[b][:], SC['gate'][:, :, ts].rearrange("c p t -> p c t"), r=['s_misc'], w=['e_gt0'])
            kb.dma('sp', ya[b][:], SC['ya'][:, :, ts].rearrange("c p t -> p c t"), r=['s_ya'], w=['e_ya%d' % b])
            kb.dma('sp', ycb[b][:], SC['yc'][:, :, ts].rearrange("c p t -> p c t"), r=['s_yc'], w=['e_yc%d' % b])
            kb.dma('sp', ybb[b][:], SC['yb'][:, :, ts], r=['s_yb'], w=['e_yb%d' % b])
            for m in range(8):
                ms = slice(m * 128, (m + 1) * 128)
                tb = u % 2
                u += 1
                pa, pka = PA.next()
                for kc in range(4):
                    kb.op('pe', lambda e, kc=kc: e.matmul(pa[:], lhsT=wp[:, kc, ms], rhs=ya[b][:, kc, :],
                                                          start=(kc == 0), stop=(kc == 3)),
                          r=['e_wp', 'e_ya%d' % b], w=[pka])
                kb.op('dve', lambda e: e.tensor_tensor(out=t1[tb][:], in0=pa[:], in1=gt[b][:, m, :], op=ALU.mult),
                      r=[pka, 'e_gt0'], w=['e_t1%d' % tb])
                pb2, pkb2 = PA.next()
                for hh in range(8):
                    kb.op('pe', lambda e, hh=hh: e.matmul(pb2[:], lhsT=wpb[:, hh, ms], rhs=ybb[b][:, hh, :],
                                                          start=(hh == 0), stop=(hh == 7)),
                          r=['e_wpb', 'e_yb%d' % b], w=[pkb2])
                kb.op('dve', lambda e: e.tensor_tensor(out=t2[tb][:], in0=pb2[:], in1=gt[b][:, 8 + m, :], op=ALU.mult),
                      r=[pkb2, 'e_gt0'], w=['e_t2%d' % tb])
                pc2, pkc2 = PA.next()
                for kc in range(4):
                    kb.op('pe', lambda e, kc=kc: e.matmul(pc2[:], lhsT=wp[:, 4 + kc, ms], rhs=ycb[b][:, kc, :],
                                                          start=(kc == 0), stop=(kc == 3)),
                          r=['e_wp', 'e_yc%d' % b], w=[pkc2])
                kb.op('dve', lambda e: e.tensor_tensor(out=t3[tb][:], in0=pc2[:], in1=gt[b][:, 16 + m, :], op=ALU.mult),
                      r=[pkc2, 'e_gt0'], w=['e_t3%d' % tb])
                kb.op('pool', lambda e: e.tensor_tensor(out=t1[tb][:], in0=t1[tb][:], in1=t2[tb][:], op=ALU.add),
                      r=['e_t1%d' % tb, 'e_t2%d' % tb], w=['e_t1%d' % tb])
                kb.op('pool', lambda e: e.tensor_tensor(out=mg[b][:, m, :], in0=t1[tb][:], in1=t3[tb][:], op=ALU.add),
                      r=['e_t1%d' % tb, 'e_t3%d' % tb], w=['e_mg%d' % b])
            for tt in range(4):
                t = tc * 4 + tt
                xb = t % 2
                kb.dma('sp', xt[xb][:], x_src[t * 128:(t + 1) * 128, :], r=['xsrc'], w=['e_xt%d' % xb])
                for half in range(2):
                    py, pyk = PY.next()
                    for kc in range(8):
                        kb.op('pe', lambda e, kc=kc: e.matmul(py[:], lhsT=mg[b][:, kc, tt * 128:(tt + 1) * 128],
                                                              rhs=wo[:, kc, half * 512:(half + 1) * 512],
                                                              start=(kc == 0), stop=(kc == 7)),
                              r=['e_mg%d' % b, 'e_wo'], w=[pyk])
                    kb.op('dve', lambda e, half=half: e.tensor_tensor(
                        out=yt[xb][:, half * 512:(half + 1) * 512], in0=py[:],
                        in1=mod_bc[:, 2 * D + half * 512:2 * D + (half + 1) * 512], op=ALU.mult),
                        r=[pyk, 'mod_bc'], w=['e_yt%d' % xb])
                kb.op('dve', lambda e: e.scalar_tensor_tensor(out=yt[xb][:], in0=xt[xb][:], scalar=ALPHA, in1=yt[xb][:],
                                                              op0=ALU.mult, op1=ALU.add),
                      r=['e_xt%d' % xb, 'e_yt%d' % xb], w=['e_yt%d' % xb])
                ln_affine_store(kb, nc, yt[xb], 'e_yt%d' % xb, lb[xb], lng[:], lnb[:],
                                x_dst[t * 128:(t + 1) * 128, :], 'e%d' % xb, 'xdst')
        kb.barrier()


def stage_moe(kb, nc, l, A, SC, x_src, x_dst, mod_bc, cst):
    ident = cst['ident_f']
    with ExitStack() as st:
        hT = sbt(nc, st, "hT2", [128, 8, S], BF16)
        gate_all = sbt(nc, st, "f_gate", [128, NT, NE], F32)
        with ExitStack() as s2:
            xts = [sbt(nc, s2, "f_xt%d" % i, [128, D], F32) for i in range(2)]
            hbs = [sbt(nc, s2, "f_hb%d" % i, [128, D], BF16) for i in range(2)]
            bufs = [(sbt(nc, s2, "f_st%d" % i, [128, 2, 6], F32), sbt(nc, s2, "f_mv%d" % i, [128, 2], F32),
                     sbt(nc, s2, "f_rs%d" % i, [128, 1], F32), sbt(nc, s2, "f_nb%d" % i, [128, 1], F32),
                     sbt(nc, s2, "f_xn%d" % i, [128, D], F32)) for i in range(2)]
            h32 = [sbt(nc, s2, "f_h32%d" % i, [128, 8, 128], F32) for i in range(2)]
            rw = sbt(nc, s2, "f_rw", [128, 8, NE], F32)
            rb = sbt(nc, s2, "f_rb", [128, NE], F32)
            lg = [sbt(nc, s2, "f_lg%d" % i, [128, NE], F32) for i in range(2)]
            mk = [sbt(nc, s2, "f_mk%d" % i, [128, NE], F32) for i in range(2)]
            m8 = [sbt(nc, s2, "f_m8%d" % i, [128, 12], F32) for i in range(2)]
            pT = [pst(nc, s2, "f_pT%d" % i, [128, 8, 128], BF16) for i in range(2)]
            pR = [pst(nc, s2, "f_pR%d" % i, [128, 2, 512], F32) for i in range(2)]
            pL = [pst(nc, s2, "f_pL%d" % i, [128, 512], F32) for i in range(2)]
            kb.dma('sp', rw[:], A['router_w'][l].rearrange("(kc p) n -> p kc n", p=128), w=['f_rw'])
            kb.dma('sp', rb[:], A['rb_bc'][l], w=['f_rb'])
            for t in range(NT):
                b = t % 2
                sx = 'f' + str(b)
                kb.dma('sp', xts[b][:], x_src[t * 128:(t + 1) * 128, :], r=['xsrc5'], w=['f_xt%d' % b])
                stt, mv, rstd, nb, xn = bufs[b]
                ln_mod_tile(kb, nc, xts[b], 'f_xt%d' % b, bufs[b], mod_bc[:, 4 * D:5 * D], mod_bc[:, 3 * D:4 * D],
                            xn[:], 'ln_xn' + sx, sx)
                cp(kb, 'act', hbs[b][:], xn[:], ['ln_xn' + sx], ['f_hb%d' % b])
                for kc in range(8):
                    kb.op('pe', lambda e, kc=kc: e.transpose(out=pT[b][:, kc, :], in_=hbs[b][:, kc * 128:(kc + 1) * 128],
                                                             identity=cst['ident_b'][:, :]),
                          r=['f_hb%d' % b, 'cst'], w=['f_pT%d' % b])
                cp(kb, 'act', hT[:, :, t * 128:(t + 1) * 128], pT[b][:], ['f_pT%d' % b], ['hT2'])
                for kc in range(8):
                    kb.op('pe', lambda e, kc=kc: e.matmul(pR[b][:, kc // 4, (kc % 4) * 128:(kc % 4 + 1) * 128],
                                                          lhsT=xn[:, kc * 128:(kc + 1) * 128], rhs=ident,
                                                          start=True, stop=True),
                          r=['ln_xn' + sx, 'cst'], w=['f_pR%d' % b])
                cp(kb, 'dve', h32[b][:].rearrange("p (a c) n -> p a (c n)", a=2), pR[b][:], ['f_pR%d' % b],
                   ['f_h32%d' % b])
                for kc in range(8):
                    kb.op('pe', lambda e, kc=kc: e.matmul(pL[b][:, 0:NE], lhsT=h32[b][:, kc, :], rhs=rw[:, kc, :],
                                                          start=(kc == 0), stop=(kc == 7)),
                          r=['f_h32%d' % b, 'f_rw'], w=['f_pL%d' % b])
                kb.op('dve', lambda e: e.tensor_tensor(out=lg[b][:], in0=pL[b][:, 0:NE], in1=rb[:], op=ALU.add),
                      r=['f_pL%d' % b, 'f_rb'], w=['f_lg%d' % b])
                kb.op('dve', lambda e: e.max(out=m8[b][:, 0:8], in_=lg[b][:]), r=['f_lg%d' % b], w=['f_m8%d' % b])
                kb.op('dve', lambda e: e.tensor_scalar(out=mk[b][:], in0=lg[b][:], scalar1=m8[b][:, 3:4], scalar2=None,
                                                       op0=ALU.is_ge), r=['f_lg%d' % b, 'f_m8%d' % b], w=['f_mk%d' % b])
                kb.op('dve', lambda e: e.tensor_scalar(out=m8[b][:, 8:9], in0=m8[b][:, 0:1], scalar1=-1.0, scalar2=None,
                                                       op0=ALU.mult), r=['f_m8%d' % b], w=['f_m8%d' % b])
                kb.op('act', lambda e: e.activation(out=lg[b][:], in_=lg[b][:], func=AF.Exp, bias=m8[b][:, 8:9],
                                                    scale=1.0), r=['f_lg%d' % b, 'f_m8%d' % b], w=['f_lg%d' % b])
                kb.op('dve', lambda e: e.tensor_tensor(out=lg[b][:], in0=lg[b][:], in1=mk[b][:], op=ALU.mult),
                      r=['f_lg%d' % b, 'f_mk%d' % b], w=['f_lg%d' % b])
                kb.op('dve', lambda e: e.reduce_sum(out=m8[b][:, 9:10], in_=lg[b][:], axis=AX.X),
                      r=['f_lg%d' % b], w=['f_m8%d' % b])
                kb.op('dve', lambda e: e.reciprocal(out=m8[b][:, 10:11], in_=m8[b][:, 9:10]),
                      r=['f_m8%d' % b], w=['f_m8%d' % b])
                kb.op('dve', lambda e: e.tensor_scalar(out=gate_all[:, t, :], in0=lg[b][:], scalar1=m8[b][:, 10:11],
                                                       scalar2=None, op0=ALU.mult),
                      r=['f_lg%d' % b, 'f_m8%d' % b], w=['f_gate'])
            kb.barrier()
        PT_ = 8
        acc = sbt(nc, st, "f_acc", [128, PT_, D], F32)
        w1t = [sbt(nc, st, "f_w1%d" % i, [128, 8, 512], BF16) for i in range(2)]
        w2t = sbt(nc, st, "f_w2", [128, 8, D], BF16)
        b1t = [sbt(nc, st, "f_b1%d" % i, [128, 16], F32) for i in range(2)]
        b2t = [sbt(nc, st, "f_b2%d" % i, [1, D], BF16) for i in range(2)]
        onesb = sbt(nc, st, "f_onesb", [1, 128], BF16)
        actT = sbt(nc, st, "f_actT", [128, 8, PT_ * 128], BF16)
        gl = [sbt(nc, st, "f_gl%d" % i, [128, 512], F32) for i in range(2)]
        sg = [sbt(nc, st, "f_sg%d" % i, [128, 512], F32) for i in range(2)]
        li = [sbt(nc, st, "f_li%d" % i, [128, 512], F32) for i in range(2)]
        xt = [sbt(nc, st, "f_x%d" % i, [128, D], F32) for i in range(2)]
        lng = sbt(nc, st, "f_lng", [128, D], F32)
        lnb = sbt(nc, st, "f_lnb", [128, D], F32)
        lb = [(sbt(nc, st, "f_lst%d" % i, [128, 2, 6], F32), sbt(nc, st, "f_lmv%d" % i, [128, 2], F32),
               sbt(nc, st, "f_lrs%d" % i, [128, 1], F32), sbt(nc, st, "f_lnb%d" % i, [128, 1], F32)) for i in range(2)]
        PG = PsRot(kb, nc, st, 4, "f_pg")
        PY = PsRot(kb, nc, st, 3, "f_py")
        kb.op('dve', lambda e: e.tensor_copy(out=onesb[:], in_=cst['ones_f'][0:1, :]), r=['cst'], w=['f_onesb'])
        kb.dma('sp', lng[:], A['ln2g_bc'][l], w=['lngb'])
        kb.dma('sp', lnb[:], A['ln2b_bc'][l], w=['lngb'])
        W1, W2 = A['exp_w1'], A['exp_w2']
        gcount = 0
        u = 0
        n_exp = DBG.get('n_exp', NE)
        for ps_ in range(NT // PT_):
            kb.op('pool', lambda e: e.memset(acc[:], 0.0), w=['f_acc'])
            for ex in range(n_exp):
                eb = ex % 2
                kb.dma('sp', b1t[eb][:], A['exp_b1l'][l, ex], w=['f_b1%d' % eb])
                kb.dma('pool', b2t[eb][:], A['exp_b2'][l, ex:ex + 1, :], w=['f_b2%d' % eb])
                for g in range(4):
                    wb = gcount % 2
                    gcount += 1
                    kb.dma('pool', w1t[wb][:, :, 0:256],
                           W1[l, ex, :, g * 256:(g + 1) * 256].rearrange("(kc p) n -> p kc n", p=128), w=['f_w1%d' % wb])
                    kb.dma('pool', w1t[wb][:, :, 256:512],
                           W1[l, ex, :, DFF + g * 256:DFF + (g + 1) * 256].rearrange("(kc p) n -> p kc n", p=128),
                           w=['f_w1%d' % wb])
                    for fl in range(2):
                        fc = g * 2 + fl
                        for tcl in range(PT_ // 4):
                            tb = u % 2
                            u += 1
                            tok = slice((ps_ * PT_ + tcl * 4) * 128, (ps_ * PT_ + tcl * 4 + 4) * 128)
                            pg, pgk = PG.next()
                            for kc in range(8):
                                kb.op('pe', lambda e, kc=kc: e.matmul(pg[:], lhsT=w1t[wb][:, kc, fl * 128:(fl + 1) * 128],
                                                                      rhs=hT[:, kc, tok], start=(kc == 0), stop=(kc == 7)),
                                      r=['f_w1%d' % wb, 'hT2'], w=[pgk])
                            pl, plk = PG.next()
                            for kc in range(8):
                                kb.op('pe', lambda e, kc=kc: e.matmul(
                                    pl[:], lhsT=w1t[wb][:, kc, 256 + fl * 128:256 + (fl + 1) * 128],
                                    rhs=hT[:, kc, tok], start=(kc == 0), stop=(kc == 7)),
                                    r=['f_w1%d' % wb, 'hT2'], w=[plk])
                            kb.op('dve', lambda e: e.tensor_scalar(out=gl[tb][:], in0=pg[:], scalar1=b1t[eb][:, fc:fc + 1],
                                                                   scalar2=7.0, op0=ALU.add, op1=ALU.min),
                                  r=[pgk, 'f_b1%d' % eb], w=['f_gl%d' % tb])
                            kb.op('act', lambda e: e.activation(out=sg[tb][:], in_=gl[tb][:], func=AF.Sigmoid,
                                                                scale=1.702), r=['f_gl%d' % tb], w=['f_sg%d' % tb])
                            kb.op('dve', lambda e: e.tensor_scalar(out=li[tb][:], in0=pl[:],
                                                                   scalar1=b1t[eb][:, 8 + fc:9 + fc], scalar2=7.0,
                                                                   op0=ALU.add, op1=ALU.min),
                                  r=[plk, 'f_b1%d' % eb], w=['f_li%d' % tb])
                            kb.op('dve', lambda e: e.tensor_scalar(out=li[tb][:], in0=li[tb][:], scalar1=-7.0,
                                                                   scalar2=1.0, op0=ALU.max, op1=ALU.add),
                                  r=['f_li%d' % tb], w=['f_li%d' % tb])
                            kb.op('pool', lambda e: e.tensor_tensor(out=gl[tb][:], in0=gl[tb][:], in1=sg[tb][:],
                                                                    op=ALU.mult),
                                  r=['f_gl%d' % tb, 'f_sg%d' % tb], w=['f_gl%d' % tb])
                            kb.op('pool', lambda e: e.tensor_tensor(out=actT[:, fc, tcl * 512:(tcl + 1) * 512],
                                                                    in0=gl[tb][:], in1=li[tb][:], op=ALU.mult),
                                  r=['f_gl%d' % tb, 'f_li%d' % tb], w=['f_actT'])
                kb.dma('pool', w2t[:], W2[l, ex].rearrange("(kc p) n -> p kc n", p=128), w=['f_w2'],
                       max_dma_last_dim=4096)
                for tt in range(PT_):
                    T = ps_ * PT_ + tt
                    for half in range(2):
                        py, pyk = PY.next()
                        for fc in range(8):
                            kb.op('pe', lambda e, fc=fc: e.matmul(py[:], lhsT=actT[:, fc, tt * 128:(tt + 1) * 128],
                                                                  rhs=w2t[:, fc, half * 512:(half + 1) * 512],
                                                                  start=(fc == 0), stop=False),
                                  r=['f_actT', 'f_w2'], w=[pyk])
                        kb.op('pe', lambda e: e.matmul(py[:], lhsT=onesb[0:1, :], rhs=b2t[eb][0:1, half * 512:(half + 1) * 512],
                                                       start=False, stop=True), r=['f_onesb', 'f_b2%d' % eb], w=[pyk])
                        kb.op('dve', lambda e: e.scalar_tensor_tensor(
                            out=acc[:, tt, half * 512:(half + 1) * 512], in0=py[:], scalar=gate_all[:, T, ex:ex + 1],
                            in1=acc[:, tt, half * 512:(half + 1) * 512], op0=ALU.mult, op1=ALU.add),
                            r=[pyk, 'f_gate', 'f_acc'], w=['f_acc'])
            for tt in range(PT_):
                T = ps_ * PT_ + tt
                xb = T % 2
                kb.dma('sp', xt[xb][:], x_src[T * 128:(T + 1) * 128, :], r=['xsrc5'], w=['f_x%d' % xb])
                kb.op('dve', lambda e: e.tensor_tensor(out=acc[:, tt, :], in0=acc[:, tt, :], in1=mod_bc[:, 5 * D:6 * D],
                                                       op=ALU.mult), r=['f_acc', 'mod_bc'], w=['f_acc'])
                kb.op('dve', lambda e: e.scalar_tensor_tensor(out=xt[xb][:], in0=xt[xb][:], scalar=ALPHA,
                                                              in1=acc[:, tt, :], op0=ALU.mult, op1=ALU.add),
                      r=['f_x%d' % xb, 'f_acc'], w=['f_x%d' % xb])
                ln_affine_store(kb, nc, xt[xb], 'f_x%d' % xb, lb[xb], lng[:], lnb[:],
                                x_dst[T * 128:(T + 1) * 128, :], 'f%d' % xb, 'xdst5')
        kb.barrier()


def input_shapes(L):
    return {
        'x': [S, D], 'c_col': [128, 8], 'ada_w': [L, D, 6 * D], 'ada_b': [L, 6 * D], 'w_in': [L, D, INW],
        'conv_wl': [L, 128, 12, 4], 'alog_bc': [L, 128, 4], 'dtb_bc': [L, 128, 4], 'gnw_bc': [L, 128, 128],
        'sink_bc': [L, 128, 8], 'pool_w': [L, 4, 128, 128], 'pscale_l': [L, 128, 4],
        'w_pa': [L, 512, D], 'w_pb': [L, 512, D], 'w_pc': [L, 512, D], 'w_o': [L, D, D],
        'ln1g_bc': [L, 128, D], 'ln1b_bc': [L, 128, D], 'ln2g_bc': [L, 128, D], 'ln2b_bc': [L, 128, D],
        'router_w': [L, D, NE], 'rb_bc': [L, 128, NE], 'exp_w1': [L, NE, D, 2 * DFF], 'exp_w2': [L, NE, DFF, D],
        'exp_b1l': [L, NE, 128, 16], 'exp_b2': [L, NE, D],
        'cst_f': [128, 2, 128], 'gm': [128, 6, 128], 'swab': [128, 8, 256], 'invcnt': [128, 4, 16], 'moec': [128, 160],
    }


def build_program(L):
    nc = bass.Bass("TRN2", target_bir_lowering=False)
    A = {k: nc.dram_tensor(k, s, F32, kind="ExternalInput").ap() for k, s in input_shapes(L).items()}
    out = nc.dram_tensor("out", [S, D], F32, kind="ExternalOutput").ap()
    SC = alloc_scratch(nc)
    SC['ya'] = nc.dram_tensor("s_ya", [4, 128, S], BF16).ap()
    SC['yb'] = nc.dram_tensor("s_yb", [64, 8, S], BF16).ap()
    SC['yc'] = nc.dram_tensor("s_yc", [4, 128, S], BF16).ap()
    x1 = nc.dram_tensor("s_x1", [S, D], F32).ap()
    SC['hg'] = nc.dram_tensor("s_hg", [NE * CAP, D], BF16).ap()
    SC['yg'] = nc.dram_tensor("s_yg", [NE * CAP, D], F32).ap()
    xs = [A['x']] + [nc.dram_tensor("s_xl%d" % i, [S, D], F32).ap() for i in range(L - 1)] + [out]
    with ExitStack() as es:
        kb = KB(nc, es)
        cf = sbt(nc, es, "cst_sb", [128, 2, 128], F32)
        ib = sbt(nc, es, "ident_b", [128, 128], BF16)
        mod_bc = sbt(nc, es, "mod_bc", [128, 6 * D], F32)
        kb.dma('sp', cf[:], A['cst_f'][:, :, :], w=['cst'])
        kb.op('dve', lambda e: e.tensor_copy(out=ib[:], in_=cf[:, 1, :]), r=['cst'], w=['cst'])
        cst = {'ones_f': cf[:, 0, :], 'ident_f': cf[:, 1, :], 'ident_b': ib, 'breg': nc.gpsimd.to_reg(NE * CAP - 1)}
        with ExitStack() as zs:
            zt = sbt(nc, zs, "zero_t", [128, 8 * D], BF16)
            kb.op('dve', lambda e: e.memset(zt[:], 0.0), w=['zero_t'])
            rows_per = 128 * 8
            for i in range(NE * CAP // rows_per):
                kb.dma('sp' if i % 2 == 0 else 'act', SC['hg'][i * rows_per:(i + 1) * rows_per, :].rearrange(
                    "(p r) d -> p (r d)", p=128), zt[:], r=['zero_t'], w=['s_hg_z%d' % (i % 8)])
            kb.barrier()
        for l in range(L):
            stage_mod(kb, nc, l, A, mod_bc, cst)
            stage_proj(kb, nc, l, A, SC, xs[l], mod_bc, cst)
            stage_gdn(kb, nc, l, A, SC, cst)
            stage_swa(kb, nc, l, A, SC, cst)
            stage_merge(kb, nc, l, A, SC, xs[l], x1, mod_bc, cst)
            stage_moe_sparse(kb, nc, l, A, SC, x1, xs[l + 1], mod_bc, cst)
        kb.barrier()
    return nc


def host_consts():
    i = np.arange(128)[:, None]
    j = np.arange(128)[None, :]
    same = (i // 64) == (j // 64)
    gm = np.stack([((i <= j) & same).astype(np.float32), same.astype(np.float32),
                   np.broadcast_to(i < 64, (128, 128)).astype(np.float32),
                   np.broadcast_to(i >= 64, (128, 128)).astype(np.float32),
                   np.where((j >= i) & same, 0.0, NEG).astype(np.float32),
                   (1.0 - np.eye(128)).astype(np.float32)], 1)
    q = np.arange(128)[:, None]
    k = np.arange(256)[None, :]
    dist = q + 128 - k
    valid = (dist >= 0) & (dist < 128)
    slopes = 2.0 ** (-8.0 * np.arange(1, 9, dtype=np.float32) / 8)
    swab = np.where(valid[:, None, :], -slopes[None, :, None] * dist[:, None, :].astype(np.float32), NEG)
    ic = np.zeros((4, 16), np.float32)
    for g, win in enumerate((2, 4, 8, 16)):
        ic[g] = 1.0 / np.minimum(np.arange(16) + 1, win)
    moec = np.concatenate([(i <= j).astype(np.float32),
                           np.broadcast_to((np.arange(NE) * CAP - 1).astype(np.float32)[None, :], (128, NE))], 1)
    return {'moec': np.ascontiguousarray(moec), 'cst_f': np.stack([np.ones((128, 128), np.float32), np.eye(128, dtype=np.float32)], 1),
            'gm': np.ascontiguousarray(gm), 'swab': np.ascontiguousarray(swab.astype(np.float32)),
            'invcnt': np.ascontiguousarray(np.broadcast_to(ic[None], (128, 4, 16)))}


def host_layer_inputs(p, ls):
    f = lambda a: np.ascontiguousarray(np.asarray(a, np.float32))
    n = len(range(*ls.indices(DEPTH)))
    bc = lambda a, w: f(np.broadcast_to(np.asarray(a)[ls][:, None, :], (n, 128, w)))
    W = {}
    for k in ['ada_w', 'ada_b', 'w_in', 'pool_w', 'w_pa', 'w_pb', 'w_pc', 'w_o', 'router_w', 'exp_w1', 'exp_w2', 'exp_b2']:
        W[k] = f(np.asarray(p[k])[ls])
    W['conv_wl'] = f(np.asarray(p['conv_w'])[ls].reshape(n, 4, 12, 128).transpose(0, 3, 2, 1))
    W['alog_bc'] = bc(p['a_log'], 4)
    W['dtb_bc'] = bc(p['dt_bias'], 4)
    W['gnw_bc'] = bc(p['gdn_norm_w'], 128)
    W['sink_bc'] = bc(p['sinks'], 8)
    W['pscale_l'] = f(np.asarray(p['pool_scale'])[ls].reshape(n, 4, 128).transpose(0, 2, 1))
    W['ln1g_bc'] = bc(p['ln1_g'], D)
    W['ln1b_bc'] = bc(p['ln1_b'], D)
    W['ln2g_bc'] = bc(p['ln2_g'], D)
    W['ln2b_bc'] = bc(p['ln2_b'], D)
    W['rb_bc'] = bc(p['router_b'], NE)
    W['exp_b1l'] = f(np.asarray(p['exp_b1'])[ls].reshape(n, NE, 16, 128).transpose(0, 1, 3, 2))
    return W


LAYERS_PER_LAUNCH = 4


def kernel(**inputs):
    x = np.asarray(inputs['x'], np.float32)
    c = np.asarray(inputs['c'], np.float32)
    nb = x.shape[0]
    consts = host_consts()
    Lp = LAYERS_PER_LAUNCH
    nc = build_program(Lp)
    cur = [np.ascontiguousarray(x[b]) for b in range(nb)]
    for l0 in range(0, DEPTH, Lp):
        W = host_layer_inputs(inputs, slice(l0, l0 + Lp))
        in_maps = []
        for b in range(nb):
            m = dict(W)
            m.update(consts)
            m['x'] = cur[b]
            m['c_col'] = np.ascontiguousarray(c[b].reshape(8, 128).T)
            in_maps.append(m)
        res = run_bass_kernel_spmd(nc, in_maps, core_ids=list(range(nb)))
        cur = [np.asarray(res.results[b]['out'], np.float32) for b in range(nb)]
    return np.stack(cur, 0).astype(np.float32)


def stage_moe_sparse(kb, nc, l, A, SC, x_src, x_dst, mod_bc, cst):
    ident = cst['ident_f']
    C = CAP
    NB = C // 128
    hg, yg = SC['hg'], SC['yg']
    with ExitStack() as st:
        dest_all = sbt(nc, st, "f_dest", [128, NT * 4], I32)
        gk_all = sbt(nc, st, "f_gk", [128, NT, 4], F32)
        with ExitStack() as s2:
            moec = sbt(nc, s2, "f_moec", [128, 160], F32)
            xts = [sbt(nc, s2, "f_xt%d" % i, [128, D], F32) for i in range(2)]
            hbs = [sbt(nc, s2, "f_hb%d" % i, [128, D], BF16) for i in range(2)]
            bufs = [(sbt(nc, s2, "f_st%d" % i, [128, 2, 6], F32), sbt(nc, s2, "f_mv%d" % i, [128, 2], F32),
                     sbt(nc, s2, "f_rs%d" % i, [128, 1], F32), sbt(nc, s2, "f_nb%d" % i, [128, 1], F32),
                     sbt(nc, s2, "f_xn%d" % i, [128, D], F32)) for i in range(2)]
            h32 = [sbt(nc, s2, "f_h32%d" % i, [128, 8, 128], F32) for i in range(2)]
            rw = sbt(nc, s2, "f_rw", [128, 8, NE], F32)
            rb = sbt(nc, s2, "f_rb", [128, NE], F32)
            lg = [sbt(nc, s2, "f_lg%d" % i, [128, NE], F32) for i in range(2)]
            mk = [sbt(nc, s2, "f_mk%d" % i, [128, NE], F32) for i in range(2)]
            gt_ = [sbt(nc, s2, "f_gt%d" % i, [128, NE], F32) for i in range(2)]
            ngp = [sbt(nc, s2, "f_ngp%d" % i, [128, NE], F32) for i in range(2)]
            oh = [sbt(nc, s2, "f_oh%d" % i, [128, NE], F32) for i in range(2)]
            jk = [sbt(nc, s2, "f_jk%d" % i, [128, NE], F32) for i in range(2)]
            m8 = [sbt(nc, s2, "f_m8%d" % i, [128, 12], F32) for i in range(2)]
            t8 = [sbt(nc, s2, "f_t8%d" % i, [128, 8], F32) for i in range(2)]
            msum = sbt(nc, s2, "f_msum", [128, NE], F32)
            pR = [pst(nc, s2, "f_pR%d" % i, [128, 2, 512], F32) for i in range(2)]
            pL = [pst(nc, s2, "f_pL%d" % i, [128, 512], F32) for i in range(2)]
            pC = [pst(nc, s2, "f_pC%d" % i, [128, 512], F32) for i in range(2)]
            kb.dma('sp', moec[:], A['moec'][:, :], w=['f_moec'])
            kb.dma('sp', rw[:], A['router_w'][l].rearrange("(kc p) n -> p kc n", p=128), w=['f_rw'])
            kb.dma('sp', rb[:], A['rb_bc'][l], w=['f_rb'])
            kb.op('dve', lambda e: e.memset(msum[:], 0.0), w=['f_msum'])
            triu = moec[:, 0:128]
            ecb = moec[:, 128:160]
            for t in range(NT):
                b = t % 2
                sx = 'f' + str(b)
                B_ = str(b)
                kb.dma('sp', xts[b][:], x_src[t * 128:(t + 1) * 128, :], r=['xsrc5'], w=['f_xt' + B_])
                stt, mv, rstd, nb, xn = bufs[b]
                ln_mod_tile(kb, nc, xts[b], 'f_xt' + B_, bufs[b], mod_bc[:, 4 * D:5 * D], mod_bc[:, 3 * D:4 * D],
                            xn[:], 'ln_xn' + sx, sx)
                cp(kb, 'act', hbs[b][:], xn[:], ['ln_xn' + sx], ['f_hb' + B_])
                for kc in range(8):
                    kb.op('pe', lambda e, kc=kc: e.matmul(pR[b][:, kc // 4, (kc % 4) * 128:(kc % 4 + 1) * 128],
                                                          lhsT=xn[:, kc * 128:(kc + 1) * 128], rhs=ident,
                                                          start=True, stop=True),
                          r=['ln_xn' + sx, 'cst'], w=['f_pR' + B_])
                cp(kb, 'dve', h32[b][:].rearrange("p (a c) n -> p a (c n)", a=2), pR[b][:], ['f_pR' + B_],
                   ['f_h32' + B_])
                for kc in range(8):
                    kb.op('pe', lambda e, kc=kc: e.matmul(pL[b][:, 0:NE], lhsT=h32[b][:, kc, :], rhs=rw[:, kc, :],
                                                          start=(kc == 0), stop=(kc == 7)),
                          r=['f_h32' + B_, 'f_rw'], w=['f_pL' + B_])
                kb.op('dve', lambda e: e.tensor_tensor(out=lg[b][:], in0=pL[b][:, 0:NE], in1=rb[:], op=ALU.add),
                      r=['f_pL' + B_, 'f_rb'], w=['f_lg' + B_])
                kb.op('dve', lambda e: e.max(out=m8[b][:, 0:8], in_=lg[b][:]), r=['f_lg' + B_], w=['f_m8' + B_])
                kb.op('dve', lambda e: e.tensor_scalar(out=mk[b][:], in0=lg[b][:], scalar1=m8[b][:, 3:4], scalar2=None,
                                                       op0=ALU.is_ge), r=['f_lg' + B_, 'f_m8' + B_], w=['f_mk' + B_])
                kb.op('dve', lambda e: e.tensor_scalar(out=m8[b][:, 8:9], in0=m8[b][:, 0:1], scalar1=-1.0, scalar2=None,
                                                       op0=ALU.mult), r=['f_m8' + B_], w=['f_m8' + B_])
                kb.op('act', lambda e: e.activation(out=lg[b][:], in_=lg[b][:], func=AF.Exp, bias=m8[b][:, 8:9],
                                                    scale=1.0), r=['f_lg' + B_, 'f_m8' + B_], w=['f_lg' + B_])
                kb.op('dve', lambda e: e.tensor_tensor(out=lg[b][:], in0=lg[b][:], in1=mk[b][:], op=ALU.mult),
                      r=['f_lg' + B_, 'f_mk' + B_], w=['f_lg' + B_])
                kb.op('dve', lambda e: e.reduce_sum(out=m8[b][:, 9:10], in_=lg[b][:], axis=AX.X),
                      r=['f_lg' + B_], w=['f_m8' + B_])
                kb.op('dve', lambda e: e.reciprocal(out=m8[b][:, 10:11], in_=m8[b][:, 9:10]),
                      r=['f_m8' + B_], w=['f_m8' + B_])
                kb.op('dve', lambda e: e.tensor_scalar(out=gt_[b][:], in0=lg[b][:], scalar1=m8[b][:, 10:11],
                                                       scalar2=None, op0=ALU.mult),
                      r=['f_lg' + B_, 'f_m8' + B_], w=['f_gt' + B_])
                kb.op('pe', lambda e: e.matmul(pC[b][:, 0:NE], lhsT=triu, rhs=mk[b][:], start=True, stop=False),
                      r=['f_moec', 'f_mk' + B_], w=['f_pC' + B_])
                kb.op('pe', lambda e: e.matmul(pC[b][:, 0:NE], lhsT=cst['ones_f'], rhs=msum[:], start=False, stop=True),
                      r=['cst', 'f_msum'], w=['f_pC' + B_])
                kb.op('dve', lambda e: e.tensor_tensor(out=msum[:], in0=msum[:], in1=mk[b][:], op=ALU.add),
                      r=['f_msum', 'f_mk' + B_], w=['f_msum'])
                kb.op('dve', lambda e: e.tensor_tensor(out=ngp[b][:], in0=pC[b][:, 0:NE], in1=ecb, op=ALU.add),
                      r=['f_pC' + B_, 'f_moec'], w=['f_ngp' + B_])
                kb.op('dve', lambda e: e.tensor_scalar(out=ngp[b][:], in0=ngp[b][:], scalar1=-1.0, scalar2=BIG,
                                                       op0=ALU.mult, op1=ALU.add), r=['f_ngp' + B_], w=['f_ngp' + B_])
                kb.op('dve', lambda e: e.tensor_tensor(out=ngp[b][:], in0=ngp[b][:], in1=mk[b][:], op=ALU.mult),
                      r=['f_ngp' + B_, 'f_mk' + B_], w=['f_ngp' + B_])
                kb.op('dve', lambda e: e.tensor_scalar(out=ngp[b][:], in0=ngp[b][:], scalar1=-BIG, scalar2=None,
                                                       op0=ALU.add), r=['f_ngp' + B_], w=['f_ngp' + B_])
                kb.op('dve', lambda e: e.max(out=t8[b][:], in_=ngp[b][:]), r=['f_ngp' + B_], w=['f_t8' + B_])
                dk = 'f_dest%d' % t
                kb.op('dve', lambda e: e.tensor_scalar(out=dest_all[:, t * 4:(t + 1) * 4], in0=t8[b][:, 0:4], scalar1=-1.0,
                                                       scalar2=None, op0=ALU.mult), r=['f_t8' + B_], w=[dk])
                for k in range(4):
                    kb.op('dve', lambda e, k=k: e.tensor_scalar(out=oh[b][:], in0=ngp[b][:], scalar1=t8[b][:, k:k + 1],
                                                               scalar2=None, op0=ALU.is_equal),
                          r=['f_ngp' + B_, 'f_t8' + B_], w=['f_oh' + B_])
                    kb.op('dve', lambda e, k=k: e.scalar_tensor_tensor(out=jk[b][:], in0=oh[b][:], scalar=1.0,
                                                                      in1=gt_[b][:], op0=ALU.mult, op1=ALU.mult,
                                                                      accum_out=gk_all[:, t, k:k + 1]),
                          r=['f_oh' + B_, 'f_gt' + B_], w=['f_jk' + B_, 'f_gk'])
                for k in range(4):
                    kb.ind('pool', ['f_hb' + B_, dk], ['s_hg'], out=hg[:, :],
                           out_offset=bass.IndirectOffsetOnAxis(ap=dest_all[:, t * 4 + k:t * 4 + k + 1], axis=0),
                           in_=hbs[b][:, :], in_offset=None, bounds_check=cst['breg'], oob_is_err=False)
            kb.barrier()
        with ExitStack() as s3:
            hgT = [sbt(nc, s3, "f_hgT%d" % i, [128, 8, C], BF16) for i in range(2)]
            hgb = [sbt(nc, s3, "f_hgb%d" % i, [128, D], BF16) for i in range(2)]
            actT = sbt(nc, s3, "f_actT", [128, 8, C], BF16)
            w1t = [sbt(nc, s3, "f_w1%d" % i, [128, 8, 512], BF16) for i in range(2)]
            w2t = [sbt(nc, s3, "f_w2%d" % i, [128, 8, D], BF16) for i in range(2)]
            b1t = [sbt(nc, s3, "f_b1%d" % i, [128, 16], F32) for i in range(2)]
            b2t = [sbt(nc, s3, "f_b2%d" % i, [1, D], BF16) for i in range(2)]
            onesb = sbt(nc, s3, "f_onesb", [1, 128], BF16)
            gl = [sbt(nc, s3, "f_gl%d" % i, [128, 512], F32) for i in range(2)]
            sg = [sbt(nc, s3, "f_sg%d" % i, [128, 512], F32) for i in range(2)]
            li = [sbt(nc, s3, "f_li%d" % i, [128, 512], F32) for i in range(2)]
            yrow = [sbt(nc, s3, "f_yr%d" % i, [128, D], F32) for i in range(2)]
            PTr = PsRot(kb, nc, s3, 2, "f_ptr", dt=BF16, shape=(128, 8, 128))
            PG = PsRot(kb, nc, s3, 4, "f_pg")
            PY = PsRot(kb, nc, s3, 2, "f_py")
            kb.op('dve', lambda e: e.tensor_copy(out=onesb[:], in_=cst['ones_f'][0:1, :]), r=['cst'], w=['f_onesb'])
            W1, W2 = A['exp_w1'], A['exp_w2']
            gcount = 0
            u = 0
            yc_ = 0
            for ex in range(NE):
                eb = ex % 2
                EB = str(eb)
                kb.dma('sp', b1t[eb][:], A['exp_b1l'][l, ex], w=['f_b1' + EB])
                kb.dma('pool', b2t[eb][:], A['exp_b2'][l, ex:ex + 1, :], w=['f_b2' + EB])
                kb.dma('pool', w2t[eb][:], W2[l, ex].rearrange("(kc p) n -> p kc n", p=128), w=['f_w2' + EB],
                       max_dma_last_dim=4096)
                for blk in range(NB):
                    hb_ = blk % 2
                    kb.dma('sp', hgb[hb_][:], hg[ex * C + blk * 128:ex * C + (blk + 1) * 128, :], r=['s_hg'],
                           w=['f_hgb%d' % hb_])
                    ptr, ptrk = PTr.next()
                    for kc in range(8):
                        kb.op('pe', lambda e, kc=kc: e.transpose(out=ptr[:, kc, :], in_=hgb[hb_][:, kc * 128:(kc + 1) * 128],
                                                                 identity=cst['ident_b'][:, :]),
                              r=['f_hgb%d' % hb_, 'cst'], w=[ptrk])
                    cp(kb, 'act' if blk % 2 == 0 else 'dve', hgT[eb][:, :, blk * 128:(blk + 1) * 128], ptr[:], [ptrk],
                       ['f_hgT' + EB])
                for g in range(4):
                    wb = gcount % 2
                    gcount += 1
                    kb.dma('pool', w1t[wb][:, :, 0:256],
                           W1[l, ex, :, g * 256:(g + 1) * 256].rearrange("(kc p) n -> p kc n", p=128), w=['f_w1%d' % wb])
                    kb.dma('pool', w1t[wb][:, :, 256:512],
                           W1[l, ex, :, DFF + g * 256:DFF + (g + 1) * 256].rearrange("(kc p) n -> p kc n", p=128),
                           w=['f_w1%d' % wb])
                    for fl in range(2):
                        fc = g * 2 + fl
                        for scn in range(C // 512):
                            tb = u % 2
                            u += 1
                            TB = str(tb)
                            sl = slice(scn * 512, (scn + 1) * 512)
                            pg, pgk = PG.next()
                            for kc in range(8):
                                kb.op('pe', lambda e, kc=kc: e.matmul(pg[:], lhsT=w1t[wb][:, kc, fl * 128:(fl + 1) * 128],
                                                                      rhs=hgT[eb][:, kc, sl], start=(kc == 0),
                                                                      stop=(kc == 7)),
                                      r=['f_w1%d' % wb, 'f_hgT' + EB], w=[pgk])
                            pl, plk = PG.next()
                            for kc in range(8):
                                kb.op('pe', lambda e, kc=kc: e.matmul(
                                    pl[:], lhsT=w1t[wb][:, kc, 256 + fl * 128:256 + (fl + 1) * 128],
                                    rhs=hgT[eb][:, kc, sl], start=(kc == 0), stop=(kc == 7)),
                                    r=['f_w1%d' % wb, 'f_hgT' + EB], w=[plk])
                            kb.op('dve', lambda e: e.tensor_scalar(out=gl[tb][:], in0=pg[:], scalar1=b1t[eb][:, fc:fc + 1],
                                                                   scalar2=7.0, op0=ALU.add, op1=ALU.min),
                                  r=[pgk, 'f_b1' + EB], w=['f_gl' + TB])
                            kb.op('act', lambda e: e.activation(out=sg[tb][:], in_=gl[tb][:], func=AF.Sigmoid,
                                                                scale=1.702), r=['f_gl' + TB], w=['f_sg' + TB])
                            kb.op('dve', lambda e: e.tensor_scalar(out=li[tb][:], in0=pl[:],
                                                                   scalar1=b1t[eb][:, 8 + fc:9 + fc], scalar2=7.0,
                                                                   op0=ALU.add, op1=ALU.min),
                                  r=[plk, 'f_b1' + EB], w=['f_li' + TB])
                            kb.op('dve', lambda e: e.tensor_scalar(out=li[tb][:], in0=li[tb][:], scalar1=-7.0,
                                                                   scalar2=1.0, op0=ALU.max, op1=ALU.add),
                                  r=['f_li' + TB], w=['f_li' + TB])
                            kb.op('pool', lambda e: e.tensor_tensor(out=gl[tb][:], in0=gl[tb][:], in1=sg[tb][:],
                                                                    op=ALU.mult),
                                  r=['f_gl' + TB, 'f_sg' + TB], w=['f_gl' + TB])
                            kb.op('pool', lambda e: e.tensor_tensor(out=actT[:, fc, sl], in0=gl[tb][:], in1=li[tb][:],
                                                                    op=ALU.mult),
                                  r=['f_gl' + TB, 'f_li' + TB], w=['f_actT'])
                for blk in range(NB):
                    yb_ = yc_ % 2
                    yc_ += 1
                    for half in range(2):
                        py, pyk = PY.next()
                        for fc in range(8):
                            kb.op('pe', lambda e, fc=fc: e.matmul(py[:], lhsT=actT[:, fc, blk * 128:(blk + 1) * 128],
                                                                  rhs=w2t[eb][:, fc, half * 512:(half + 1) * 512],
                                                                  start=(fc == 0), stop=False),
                                  r=['f_actT', 'f_w2' + EB], w=[pyk])
                        kb.op('pe', lambda e: e.matmul(py[:], lhsT=onesb[0:1, :],
                                                       rhs=b2t[eb][0:1, half * 512:(half + 1) * 512],
                                                       start=False, stop=True), r=['f_onesb', 'f_b2' + EB], w=[pyk])
                        cp(kb, 'act' if half == 0 else 'dve', yrow[yb_][:, half * 512:(half + 1) * 512], py[:], [pyk],
                           ['f_yr%d' % yb_])
                    kb.dma('sp', yg[ex * C + blk * 128:ex * C + (blk + 1) * 128, :], yrow[yb_][:], r=['f_yr%d' % yb_],
                           w=['s_yg'])
            kb.barrier()
        with ExitStack() as s4:
            rows = [sbt(nc, s4, "f_row%d" % i, [128, D], F32) for i in range(4)]
            accs = [sbt(nc, s4, "f_acc%d" % i, [128, D], F32) for i in range(2)]
            xt = [sbt(nc, s4, "f_x%d" % i, [128, D], F32) for i in range(2)]
            lng = sbt(nc, s4, "f_lng", [128, D], F32)
            lnb = sbt(nc, s4, "f_lnb", [128, D], F32)
            lb = [(sbt(nc, s4, "f_lst%d" % i, [128, 2, 6], F32), sbt(nc, s4, "f_lmv%d" % i, [128, 2], F32),
                   sbt(nc, s4, "f_lrs%d" % i, [128, 1], F32), sbt(nc, s4, "f_lnb%d" % i, [128, 1], F32))
                  for i in range(2)]
            kb.dma('sp', lng[:], A['ln2g_bc'][l], w=['lngb'])
            kb.dma('sp', lnb[:], A['ln2b_bc'][l], w=['lngb'])
            for T in range(NT):
                xb = T % 2
                XB = str(xb)
                kb.dma('sp', xt[xb][:], x_src[T * 128:(T + 1) * 128, :], r=['xsrc5'], w=['f_x' + XB])
                for k in range(4):
                    kb.ind('pool', ['s_yg', 'f_dest%d' % T], ['f_row%d' % k], out=rows[k][:, :], out_offset=None,
                           in_=yg[:, :], in_offset=bass.IndirectOffsetOnAxis(ap=dest_all[:, T * 4 + k:T * 4 + k + 1], axis=0),
                           bounds_check=cst['breg'], oob_is_err=False)
                kb.op('dve', lambda e: e.tensor_scalar(out=accs[xb][:], in0=rows[0][:], scalar1=gk_all[:, T, 0:1],
                                                       scalar2=None, op0=ALU.mult),
                      r=['f_row0', 'f_gk'], w=['f_acc' + XB])
                for k in range(1, 4):
                    kb.op('dve', lambda e, k=k: e.scalar_tensor_tensor(out=accs[xb][:], in0=rows[k][:],
                                                                      scalar=gk_all[:, T, k:k + 1], in1=accs[xb][:],
                                                                      op0=ALU.mult, op1=ALU.add),
                          r=['f_row%d' % k, 'f_gk', 'f_acc' + XB], w=['f_acc' + XB])
                kb.op('pool', lambda e: e.tensor_tensor(out=accs[xb][:], in0=accs[xb][:], in1=mod_bc[:, 5 * D:6 * D],
                                                        op=ALU.mult), r=['f_acc' + XB, 'mod_bc'], w=['f_acc' + XB])
                kb.op('dve', lambda e: e.scalar_tensor_tensor(out=xt[xb][:], in0=xt[xb][:], scalar=ALPHA,
                                                              in1=accs[xb][:], op0=ALU.mult, op1=ALU.add),
                      r=['f_x' + XB, 'f_acc' + XB], w=['f_x' + XB])
                ln_affine_store(kb, nc, xt[xb], 'f_x' + XB, lb[xb], lng[:], lnb[:],
                                x_dst[T * 128:(T + 1) * 128, :], 'f' + XB, 'xdst5')
            kb.barrier()
```

```python
import numpy as np
from contextlib import ExitStack
import concourse.bass as bass
import concourse.mybir as mybir
from concourse.bass_utils import run_bass_kernel_spmd
from concourse.alu_op_type import AluOpType as ALU

F32, BF16 = mybir.dt.float32, mybir.dt.bfloat16
I32 = mybir.dt.int32
CAP = 1536
BIG = 1.0e6
AF = mybir.ActivationFunctionType
AX = mybir.AxisListType

D = 1024
S = 4096
NT = S // 128
DEPTH = 4
INW = 6408
NE = 32
DFF = 1024
ALPHA = (2 * DEPTH) ** 0.25
NEG = -30000.0
DBG = {}


class KB:
    def __init__(self, nc, es, ndma=48):
        self.nc = nc
        self.eng = {'pe': nc.tensor, 'dve': nc.vector, 'act': nc.scalar, 'pool': nc.gpsimd, 'sp': nc.sync}
        self.sems = []
        self.psid = {}
        for e in self.eng:
            self.psid[e] = len(self.sems)
            self.sems.append(es.enter_context(nc.semaphore("ps_" + e)))
        self.cnt = {e: 0 for e in self.eng}
        self.dsid = []
        for i in range(ndma):
            self.dsid.append(len(self.sems))
            self.sems.append(es.enter_context(nc.semaphore("ds%d" % i)))
        self.dcum = [0] * ndma
        self.dnext = 0
        self.seen = {e: {} for e in self.eng}
        self.lastw = {}
        self.reads = {}
        self.nins = 0
        self.excl = set()

    def need(self, E, tok):
        sid, val, _ = tok
        if self.seen[E].get(sid, 0) < val:
            self.eng[E].wait_ge(self.sems[sid], val)
            self.seen[E][sid] = val
            if 'log' in DBG:
                DBG['log'].append("%s WAIT sem%d>=%d" % (E, sid, val))

    def _deps(self, E, r, w, isdma):
        for k in r:
            t = self.lastw.get(k)
            if t is not None:
                self.need(E, t)
            if k in self.excl:
                for t in self.reads.get(k, {}).values():
                    if t[2] != E:
                        self.need(E, t)
        for k in w:
            t = self.lastw.get(k)
            if t is not None and (isdma or t[2] != E):
                self.need(E, t)
            for t in self.reads.get(k, {}).values():
                if isdma or t[2] != E:
                    self.need(E, t)

    def _commit(self, tok, r, w):
        for k in r:
            self.reads.setdefault(k, {})[tok[0]] = tok
        for k in w:
            self.lastw[k] = tok
            self.reads[k] = {}

    def op(self, E, fn, r=(), w=()):
        self._deps(E, r, w, False)
        ins = fn(self.eng[E])
        ins.then_inc(self.sems[self.psid[E]], 1)
        self.cnt[E] += 1
        self.nins += 1
        tok = (self.psid[E], self.cnt[E], E)
        if 'log' in DBG:
            DBG['log'].append("%s OP#%d r=%s w=%s" % (E, self.cnt[E], list(r), list(w)))
        self._commit(tok, r, w)
        return tok

    def dma(self, Q, out, in_, r=(), w=(), **kw):
        self._deps(Q, r, w, True)
        i = self.dnext
        self.dnext = (i + 1) % len(self.dsid)
        if self.dcum[i] > 0:
            self.need(Q, (self.dsid[i], self.dcum[i], None))
        self.eng[Q].dma_start(out=out, in_=in_, **kw).then_inc(self.sems[self.dsid[i]], 16)
        self.dcum[i] += 16
        self.nins += 1
        tok = (self.dsid[i], self.dcum[i], None)
        if 'log' in DBG:
            DBG['log'].append("%s DMA sem%d->%d r=%s w=%s" % (Q, self.dsid[i], self.dcum[i], list(r), list(w)))
        self._commit(tok, r, w)
        return tok

    def ind(self, Q, r, w, **kw):
        self._deps(Q, r, w, True)
        i = self.dnext
        self.dnext = (i + 1) % len(self.dsid)
        if self.dcum[i] > 0:
            self.need(Q, (self.dsid[i], self.dcum[i], None))
        self.eng[Q].indirect_dma_start(**kw).then_inc(self.sems[self.dsid[i]], 16)
        self.dcum[i] += 16
        self.nins += 1
        tok = (self.dsid[i], self.dcum[i], None)
        self._commit(tok, r, w)
        return tok

    def barrier(self):
        for E in self.eng:
            for F in self.eng:
                if F != E and self.cnt[F] > 0:
                    self.need(E, (self.psid[F], self.cnt[F], F))
            for i, s in enumerate(self.dsid):
                if self.dcum[i] > 0:
                    self.need(E, (s, self.dcum[i], None))


_UID = [0]


def _uname(name):
    _UID[0] += 1
    return "%s_u%d" % (name, _UID[0])


def sbt(nc, st, name, shape, dt):
    return st.enter_context(nc.sbuf_tensor(_uname(name), shape, dt))


def pst(nc, st, name, shape, dt):
    return st.enter_context(nc.psum_tensor(_uname(name), shape, dt))


def stage_mod(kb, nc, l, A, mod_bc, cst):
    with ExitStack() as st:
        cc = sbt(nc, st, "m_cc", [128, 8], F32)
        cond = sbt(nc, st, "m_cond", [128, 8], F32)
        crep = sbt(nc, st, "m_crep", [128, 8, 128], F32)
        brow = sbt(nc, st, "m_brow", [1, 6144], F32)
        wa = [sbt(nc, st, "m_wa%d" % i, [128, 8, 512], F32) for i in range(2)]
        ps = [pst(nc, st, "m_ps%d" % i, [128, 512], F32) for i in range(2)]
        kb.dma('sp', cc[:], A['c_col'][:, :], w=['m_cc'])
        kb.dma('sp', brow[:], A['ada_b'][l:l + 1, :], w=['m_brow'])
        kb.op('act', lambda e: e.activation(out=cond[:], in_=cc[:], func=AF.Silu), r=['m_cc'], w=['m_cond'])
        for kc in range(8):
            kb.op('dve', lambda e, kc=kc: e.tensor_scalar(out=crep[:, kc, :], in0=cst['ones_f'][:, :],
                                                         scalar1=cond[:, kc:kc + 1], scalar2=None, op0=ALU.mult),
                  r=['m_cond', 'cst'], w=['m_crep'])
        for j in range(12):
            b = j % 2
            kb.dma('sp' if j % 2 == 0 else 'pool', wa[b][:],
                   A['ada_w'][l, :, j * 512:(j + 1) * 512].rearrange("(kc p) n -> p kc n", p=128),
                   w=['m_wa%d' % b])
            for kc in range(8):
                kb.op('pe', lambda e, kc=kc, b=b: e.matmul(ps[b][:], lhsT=crep[:, kc, :], rhs=wa[b][:, kc, :],
                                                           start=(kc == 0), stop=False),
                      r=['m_crep', 'm_wa%d' % b], w=['m_ps%d' % b])
            kb.op('pe', lambda e, b=b, j=j: e.matmul(ps[b][:], lhsT=cst['ones_f'][0:1, :],
                                                     rhs=brow[0:1, j * 512:(j + 1) * 512], start=False, stop=True),
                  r=['m_brow', 'cst'], w=['m_ps%d' % b])
            addone = 1.0 if (j // 2) in (1, 2, 4, 5) else 0.0
            kb.op('act', lambda e, b=b, j=j, addone=addone: e.activation(
                out=mod_bc[:, j * 512:(j + 1) * 512], in_=ps[b][:], func=AF.Identity, bias=addone, scale=1.0),
                r=['m_ps%d' % b], w=['mod_bc'])
        kb.barrier()


def ln_mod_tile(kb, nc, xt, xk, bufs, sc_ap, sh_ap, h_out, hk, sfx):
    stt, mv, rstd, nb, xn = bufs
    kb.op('dve', lambda e: e.bn_stats(out=stt[:, 0, :], in_=xt[:, 0:512]), r=[xk], w=['ln_st' + sfx])
    kb.op('dve', lambda e: e.bn_stats(out=stt[:, 1, :], in_=xt[:, 512:1024]), r=[xk], w=['ln_st' + sfx])
    kb.op('dve', lambda e: e.bn_aggr(out=mv[:], in_=stt[:].rearrange("p a b -> p (a b)")),
          r=['ln_st' + sfx], w=['ln_mv' + sfx])
    kb.op('act', lambda e: e.activation(out=rstd[:], in_=mv[:, 1:2], func=AF.Sqrt, bias=1e-5, scale=1.0),
          r=['ln_mv' + sfx], w=['ln_rstd' + sfx])
    kb.op('dve', lambda e: e.reciprocal(out=rstd[:], in_=rstd[:]), r=['ln_rstd' + sfx], w=['ln_rstd' + sfx])
    kb.op('dve', lambda e: e.scalar_tensor_tensor(out=nb[:], in0=mv[:, 0:1], scalar=-1.0, in1=rstd[:],
                                                  op0=ALU.mult, op1=ALU.mult),
          r=['ln_mv' + sfx, 'ln_rstd' + sfx], w=['ln_nb' + sfx])
    kb.op('act', lambda e: e.activation(out=xn[:], in_=xt[:], func=AF.Identity, bias=nb[:], scale=rstd[:]),
          r=[xk, 'ln_nb' + sfx, 'ln_rstd' + sfx], w=['ln_xn' + sfx])
    kb.op('dve', lambda e: e.tensor_tensor(out=xn[:], in0=xn[:], in1=sc_ap, op=ALU.mult),
          r=['ln_xn' + sfx, 'mod_bc'], w=['ln_xn' + sfx])
    kb.op('dve', lambda e: e.tensor_tensor(out=h_out, in0=xn[:], in1=sh_ap, op=ALU.add),
          r=['ln_xn' + sfx, 'mod_bc'], w=[hk])


def build_hT(kb, nc, st, x_src, mod_bc, sc_off, sh_off, hT, cst, pfx):
    xts = [sbt(nc, st, pfx + "xt%d" % i, [128, D], F32) for i in range(2)]
    hbs = [sbt(nc, st, pfx + "hb%d" % i, [128, D], BF16) for i in range(2)]
    bufs = []
    for i in range(2):
        bufs.append((sbt(nc, st, pfx + "st%d" % i, [128, 2, 6], F32), sbt(nc, st, pfx + "mv%d" % i, [128, 2], F32),
                     sbt(nc, st, pfx + "rs%d" % i, [128, 1], F32), sbt(nc, st, pfx + "nb%d" % i, [128, 1], F32),
                     sbt(nc, st, pfx + "xn%d" % i, [128, D], F32)))
    pT = [pst(nc, st, pfx + "pT%d" % i, [128, 8, 128], BF16) for i in range(2)]
    for t in range(NT):
        b = t % 2
        kb.dma('sp', xts[b][:], x_src[t * 128:(t + 1) * 128, :], r=[pfx + 'xsrc'], w=[pfx + 'xt%d' % b])
        ln_mod_tile(kb, nc, xts[b], pfx + 'xt%d' % b, bufs[b], mod_bc[:, sc_off:sc_off + D],
                    mod_bc[:, sh_off:sh_off + D], hbs[b][:], pfx + 'hb%d' % b, pfx + str(b))
        for kc in range(8):
            kb.op('pe', lambda e, kc=kc, b=b: e.transpose(out=pT[b][:, kc, :], in_=hbs[b][:, kc * 128:(kc + 1) * 128],
                                                          identity=cst['ident_b'][:, :]),
                  r=[pfx + 'hb%d' % b, 'cst'], w=[pfx + 'pT%d' % b])
        eng = 'act' if t % 2 == 0 else 'dve'
        if eng == 'act':
            kb.op('act', lambda e, b=b, t=t: e.copy(out=hT[:, :, t * 128:(t + 1) * 128], in_=pT[b][:]),
                  r=[pfx + 'pT%d' % b], w=['hT'])
        else:
            kb.op('dve', lambda e, b=b, t=t: e.tensor_copy(out=hT[:, :, t * 128:(t + 1) * 128], in_=pT[b][:]),
                  r=[pfx + 'pT%d' % b], w=['hT'])


def stage_proj(kb, nc, l, A, SC, x_src, mod_bc, cst):
    with ExitStack() as st:
        hT = sbt(nc, st, "hT", [128, 8, S], BF16)
        with ExitStack() as st2:
            build_hT(kb, nc, st2, x_src, mod_bc, 1 * D, 0, hT, cst, "p1_")
            kb.barrier()
        wts = [sbt(nc, st, "wt%d" % i, [128, 8, 512], BF16) for i in range(2)]
        raw = sbt(nc, st, "raw", [128, S + 4], F32)
        acc = sbt(nc, st, "acc", [128, S], F32)
        tmp = [sbt(nc, st, "tmp%d" % i, [128, 512], F32) for i in range(2)]
        gb = [sbt(nc, st, "gb%d" % i, [128, S], BF16) for i in range(2)]
        cw = sbt(nc, st, "cw", [128, 12, 4], F32)
        alog = sbt(nc, st, "alog", [128, 4], F32)
        dtb = sbt(nc, st, "dtb", [128, 4], F32)
        sm = sbt(nc, st, "sm", [128, 6, 4], F32)
        g_all = sbt(nc, st, "g_all", [128, NT, 4], F32)
        b_all = sbt(nc, st, "b_all", [128, NT, 4], F32)
        vb_all = sbt(nc, st, "vb_all", [128, NT, 128], BF16)
        zt = [sbt(nc, st, "zt%d" % i, [128, 512], F32) for i in range(2)]
        ps = [pst(nc, st, "pj_ps%d" % i, [128, 512], F32) for i in range(4)]
        kb.dma('sp', cw[:], A['conv_wl'][l], w=['cw'])
        kb.dma('sp', alog[:], A['alog_bc'][l], w=['alog'])
        kb.dma('sp', dtb[:], A['dtb_bc'][l], w=['dtb'])
        kb.op('act', lambda e: e.activation(out=alog[:], in_=alog[:], func=AF.Exp), r=['alog'], w=['alog'])
        kb.op('dve', lambda e: e.memset(raw[:, 0:4], 0.0), w=['raw'])
        W = A['w_in']
        state = {'g': 0, 'ps': 0, 'gb': 0}

        def load_group(pieces):
            b = state['g'] % 2
            state['g'] += 1
            for (off, c0, n) in pieces:
                kb.dma('pool', wts[b][:, :, off:off + n],
                       W[l, :, c0:c0 + n].rearrange("(kc p) n -> p kc n", p=128), w=['wt%d' % b])
            return b

        def fm_chunk(b, off, evac):
            for tc in range(8):
                p = state['ps'] % 4
                state['ps'] += 1
                for kc in range(8):
                    kb.op('pe', lambda e, kc=kc, p=p, tc=tc: e.matmul(
                        ps[p][:], lhsT=wts[b][:, kc, off:off + 128], rhs=hT[:, kc, tc * 512:(tc + 1) * 512],
                        start=(kc == 0), stop=(kc == 7)), r=['wt%d' % b, 'hT'], w=['pj_ps%d' % p])
                evac(tc, ps[p], 'pj_ps%d' % p)

        def conv_chunk(b, off, ch):
            def ev(tc, p, pk):
                eng = 'act' if tc % 2 == 0 else 'dve'
                if eng == 'act':
                    kb.op('act', lambda e: e.copy(out=raw[:, 4 + tc * 512: 4 + (tc + 1) * 512], in_=p[:]),
                          r=[pk], w=['raw'])
                else:
                    kb.op('dve', lambda e: e.tensor_copy(out=raw[:, 4 + tc * 512: 4 + (tc + 1) * 512], in_=p[:]),
                          r=[pk], w=['raw'])
            fm_chunk(b, off, ev)
            kb.op('dve', lambda e: e.tensor_scalar(out=acc[:], in0=raw[:, 1:1 + S], scalar1=cw[:, ch, 0:1],
                                                   scalar2=None, op0=ALU.mult), r=['raw', 'cw'], w=['acc'])
            for j in range(1, 4):
                kb.op('dve', lambda e, j=j: e.scalar_tensor_tensor(out=acc[:], in0=raw[:, 1 + j:1 + j + S],
                                                                  scalar=cw[:, ch, j:j + 1], in1=acc[:],
                                                                  op0=ALU.mult, op1=ALU.add),
                      r=['raw', 'cw', 'acc'], w=['acc'])
            kb.op('act', lambda e: e.activation(out=acc[:], in_=acc[:], func=AF.Silu), r=['acc'], w=['acc'])
            if ch < 8:
                kb.op('act', lambda e: e.activation(out=raw[:, 4:4 + S], in_=acc[:], func=AF.Square),
                      r=['acc'], w=['raw'])
                qs = (128 ** -0.5) if ch < 4 else 1.0
                for tc in range(8):
                    p = state['ps'] % 4
                    state['ps'] += 1
                    tb = tc % 2
                    kb.op('pe', lambda e, p=p, tc=tc: e.matmul(ps[p][:], lhsT=cst['ones_f'][:, :],
                                                               rhs=raw[:, 4 + tc * 512:4 + (tc + 1) * 512],
                                                               start=True, stop=True),
                          r=['raw', 'cst'], w=['pj_ps%d' % p])
                    kb.op('act', lambda e, p=p, tb=tb: e.activation(out=tmp[tb][:], in_=ps[p][:], func=AF.Sqrt,
                                                                    bias=1e-6, scale=1.0),
                          r=['pj_ps%d' % p], w=['tmp%d' % tb])
                    kb.op('dve', lambda e, tb=tb: e.reciprocal(out=tmp[tb][:], in_=tmp[tb][:]),
                          r=['tmp%d' % tb], w=['tmp%d' % tb])
                    kb.op('dve', lambda e, tb=tb, tc=tc: e.scalar_tensor_tensor(
                        out=acc[:, tc * 512:(tc + 1) * 512], in0=acc[:, tc * 512:(tc + 1) * 512], scalar=qs,
                        in1=tmp[tb][:], op0=ALU.mult, op1=ALU.mult), r=['tmp%d' % tb, 'acc'], w=['acc'])
            kb.dma('sp', SC['qkv'][ch], acc[:], r=['acc'], w=['s_qkv'])

        for gi in range(3):
            b = load_group([(0, gi * 512, 512)])
            for j in range(4):
                conv_chunk(b, j * 128, gi * 4 + j)

        b = load_group([(0, 1536, 512)])
        for t in range(NT):
            p = state['ps'] % 4
            state['ps'] += 1
            for kc in range(8):
                kb.op('pe', lambda e, kc=kc, p=p, t=t: e.matmul(ps[p][:], lhsT=hT[:, kc, t * 128:(t + 1) * 128],
                                                                rhs=wts[b][:, kc, 0:512], start=(kc == 0),
                                                                stop=(kc == 7)),
                      r=['wt%d' % b, 'hT'], w=['pj_ps%d' % p])
            zb = t % 2
            kb.op('act', lambda e, p=p, zb=zb: e.activation(out=zt[zb][:], in_=ps[p][:], func=AF.Silu),
                  r=['pj_ps%d' % p], w=['zt%d' % zb])
            kb.dma('sp', SC['sz'][t * 128:(t + 1) * 128, :], zt[zb][:], r=['zt%d' % zb], w=['s_sz'])

        b = load_group([(0, 2048, 8), (128, 2568, 256)])
        for t in range(NT):
            p = state['ps'] % 4
            state['ps'] += 1
            for kc in range(8):
                kb.op('pe', lambda e, kc=kc, p=p, t=t: e.matmul(ps[p][:, 0:8], lhsT=hT[:, kc, t * 128:(t + 1) * 128],
                                                                rhs=wts[b][:, kc, 0:8], start=(kc == 0),
                                                                stop=(kc == 7)),
                      r=['wt%d' % b, 'hT'], w=['pj_ps%d' % p])
            pk = 'pj_ps%d' % p
            P = ps[p]
            kb.op('dve', lambda e, P=P: e.tensor_tensor(out=sm[:, 0, :], in0=P[:, 0:4], in1=dtb[:], op=ALU.add),
                  r=[pk, 'dtb'], w=['sm0'])
            kb.op('dve', lambda e: e.scalar_tensor_tensor(out=sm[:, 1, :], in0=sm[:, 0, :], scalar=-1.0,
                                                          in1=sm[:, 0, :], op0=ALU.mult, op1=ALU.min),
                  r=['sm0'], w=['sm1'])
            kb.op('act', lambda e: e.activation(out=sm[:, 2, :], in_=sm[:, 1, :], func=AF.Exp, scale=1.0),
                  r=['sm1'], w=['sm2'])
            kb.op('act', lambda e: e.activation(out=sm[:, 3, :], in_=sm[:, 2, :], func=AF.Ln, bias=1.0, scale=1.0),
                  r=['sm2'], w=['sm3'])
            kb.op('dve', lambda e: e.scalar_tensor_tensor(out=sm[:, 4, :], in0=sm[:, 0, :], scalar=0.0,
                                                          in1=sm[:, 3, :], op0=ALU.max, op1=ALU.add),
                  r=['sm0', 'sm3'], w=['sm4'])
            kb.op('dve', lambda e, t=t: e.scalar_tensor_tensor(out=g_all[:, t, :], in0=sm[:, 4, :], scalar=-1.0,
                                                              in1=alog[:], op0=ALU.mult, op1=ALU.mult),
                  r=['sm4', 'alog'], w=['g_all'])
            kb.op('act', lambda e, P=P, t=t: e.activation(out=b_all[:, t, :], in_=P[:, 4:8], func=AF.Sigmoid),
                  r=[pk], w=['b_all'])
        kb.dma('sp', SC['g'].rearrange("(t p) h -> p t h", p=128), g_all[:], r=['g_all'], w=['s_g'])
        kb.dma('sp', SC['beta'].rearrange("(t p) h -> p t h", p=128), b_all[:], r=['b_all'], w=['s_beta'])
        for t in range(NT):
            p = state['ps'] % 4
            state['ps'] += 1
            for kc in range(8):
                kb.op('pe', lambda e, kc=kc, p=p, t=t: e.matmul(ps[p][:, 0:128], lhsT=hT[:, kc, t * 128:(t + 1) * 128],
                                                                rhs=wts[b][:, kc, 256:384], start=(kc == 0),
                                                                stop=(kc == 7)),
                      r=['wt%d' % b, 'hT'], w=['pj_ps%d' % p])
            kb.op('dve', lambda e, p=p, t=t: e.tensor_copy(out=vb_all[:, t, :], in_=ps[p][:, 0:128]),
                  r=['pj_ps%d' % p], w=['vb_all'])
        kb.dma('sp', SC['vb'].rearrange("(t p) d -> p t d", p=128), vb_all[:], r=['vb_all'], w=['s_vb'])

        def bf_chunk(b, off, dst, fn=None):
            g = state['gb'] % 2
            state['gb'] += 1

            def ev(tc, p, pk):
                if fn is not None:
                    kb.op('act', lambda e: e.activation(out=gb[g][:, tc * 512:(tc + 1) * 512], in_=p[:], func=fn),
                          r=[pk], w=['gb%d' % g])
                elif tc % 2 == 0:
                    kb.op('act', lambda e: e.copy(out=gb[g][:, tc * 512:(tc + 1) * 512], in_=p[:]),
                          r=[pk], w=['gb%d' % g])
                else:
                    kb.op('dve', lambda e: e.tensor_copy(out=gb[g][:, tc * 512:(tc + 1) * 512], in_=p[:]),
                          r=[pk], w=['gb%d' % g])
            fm_chunk(b, off, ev)
            kb.dma('sp', dst, gb[g][:], r=['gb%d' % g], w=['s_misc'])

        bf_chunk(b, 128, SC['kb'])
        pieces = []
        for j in range(4):
            pieces.append((j * 128, 2056 + j * 64, 64))
            pieces.append((j * 128 + 64, 2056 + (j + 4) * 64, 64))
        b = load_group(pieces)
        for j in range(4):
            bf_chunk(b, j * 128, SC['qb'][j])
        b = load_group([(0, 2824, 512)])
        for j in range(4):
            def ev(tc, p, pk):
                if tc % 2 == 0:
                    kb.op('act', lambda e: e.copy(out=acc[:, tc * 512:(tc + 1) * 512], in_=p[:]), r=[pk], w=['acc'])
                else:
                    kb.op('dve', lambda e: e.tensor_copy(out=acc[:, tc * 512:(tc + 1) * 512], in_=p[:]),
                          r=[pk], w=['acc'])
            fm_chunk(b, j * 128, ev)
            kb.dma('sp', SC['uc'][j], acc[:], r=['acc'], w=['s_uc'])
        for gi in range(6):
            b = load_group([(0, 3336 + gi * 512, 512)])
            for j in range(4):
                bf_chunk(b, j * 128, SC['gate'][gi * 4 + j], fn=AF.Sigmoid)
        kb.barrier()


def alloc_scratch(nc):
    SC = {}
    SC['qkv'] = nc.dram_tensor("s_qkv", [12, 128, S], F32).ap()
    SC['sz'] = nc.dram_tensor("s_sz", [S, 512], F32).ap()
    SC['g'] = nc.dram_tensor("s_g", [S, 4], F32).ap()
    SC['beta'] = nc.dram_tensor("s_beta", [S, 4], F32).ap()
    SC['qb'] = nc.dram_tensor("s_qb", [4, 128, S], BF16).ap()
    SC['kb'] = nc.dram_tensor("s_kb", [128, S], BF16).ap()
    SC['vb'] = nc.dram_tensor("s_vb", [S, 128], BF16).ap()
    SC['uc'] = nc.dram_tensor("s_uc", [4, 128, S], F32).ap()
    SC['gate'] = nc.dram_tensor("s_gate", [24, 128, S], BF16).ap()
    return SC


def cp(kb, eng, out, in_, r, w):
    if eng == 'act':
        return kb.op('act', lambda e: e.copy(out=out, in_=in_), r=r, w=w)
    return kb.op(eng, lambda e: e.tensor_copy(out=out, in_=in_), r=r, w=w)


class PsRot:
    def __init__(self, kb, nc, st, n, pfx, dt=F32, shape=(128, 512)):
        self.t = [pst(nc, st, "%s%d" % (pfx, i), list(shape), dt) for i in range(n)]
        self.k = ["%s%d" % (pfx, i) for i in range(n)]
        self.i = 0
        kb.excl.update(self.k)

    def next(self):
        i = self.i
        self.i = (i + 1) % len(self.t)
        return self.t[i], self.k[i]


def stage_gdn(kb, nc, l, A, SC, cst):
    ones, ident = cst['ones_f'], cst['ident_f']
    with ExitStack() as st:
        gm = sbt(nc, st, "g_gm", [128, 6, 128], F32)
        kb.dma('sp', gm[:], A['gm'][:, :, :], w=['cst'])
        g_all = sbt(nc, st, "g_gall", [128, 128], F32)
        b_all = sbt(nc, st, "g_ball", [128, 128], F32)
        gc = sbt(nc, st, "g_gc", [128, 128], F32)
        ngc = sbt(nc, st, "g_ngc", [128, 128], F32)
        egc = sbt(nc, st, "g_egc", [128, 128], F32)
        ekl = sbt(nc, st, "g_ekl", [128, 128], F32)
        gl0e = sbt(nc, st, "g_gl0e", [128, 128], F32)
        gl1e = sbt(nc, st, "g_gl1e", [128, 128], F32)
        negb = sbt(nc, st, "g_negb", [128, 128], F32)
        nw = sbt(nc, st, "g_nw", [128, 128], F32)
        PS = PsRot(kb, nc, st, 8, "g_ps")
        kb.dma('sp', g_all[:].rearrange("p (t h) -> p t h", h=4), SC['g'].rearrange("(t p) h -> p t h", p=128),
               r=['s_g'], w=['g_gall'])
        kb.dma('sp', b_all[:].rearrange("p (t h) -> p t h", h=4), SC['beta'].rearrange("(t p) h -> p t h", p=128),
               r=['s_beta'], w=['g_ball'])
        kb.dma('sp', nw[:], A['gnw_bc'][l], w=['g_nw'])
        p, pk = PS.next()
        kb.op('pe', lambda e: e.matmul(p[:, 0:128], lhsT=gm[:, 0, :], rhs=g_all[:], start=True, stop=True),
              r=['g_gall', 'cst'], w=[pk])
        cp(kb, 'dve', gc[:], p[:, 0:128], [pk], ['g_gc'])
        kb.op('act', lambda e: e.activation(out=egc[:], in_=gc[:], func=AF.Exp), r=['g_gc'], w=['g_egc'])
        kb.op('dve', lambda e: e.tensor_scalar(out=ngc[:], in0=gc[:], scalar1=-1.0, scalar2=None, op0=ALU.mult),
              r=['g_gc'], w=['g_ngc'])
        p2, pk2 = PS.next()
        kb.op('pe', lambda e: e.matmul(p2[:, 0:128], lhsT=gm[:, 1, :], rhs=g_all[:], start=True, stop=True),
              r=['g_gall', 'cst'], w=[pk2])
        kb.op('dve', lambda e: e.tensor_tensor(out=ekl[:], in0=p2[:, 0:128], in1=gc[:], op=ALU.subtract),
              r=[pk2, 'g_gc'], w=['g_ekl'])
        kb.op('act', lambda e: e.activation(out=ekl[:], in_=ekl[:], func=AF.Exp), r=['g_ekl'], w=['g_ekl'])
        for idx, dst, dk in ((2, gl0e, 'g_gl0e'), (3, gl1e, 'g_gl1e')):
            p3, pk3 = PS.next()
            kb.op('pe', lambda e, p3=p3, idx=idx: e.matmul(p3[:, 0:128], lhsT=gm[:, idx, :], rhs=g_all[:],
                                                           start=True, stop=True), r=['g_gall', 'cst'], w=[pk3])
            kb.op('act', lambda e, p3=p3, dst=dst: e.activation(out=dst[:], in_=p3[:, 0:128], func=AF.Exp),
                  r=[pk3], w=[dk])
        kb.op('dve', lambda e: e.tensor_scalar(out=negb[:], in0=b_all[:], scalar1=-1.0, scalar2=None, op0=ALU.mult),
              r=['g_ball'], w=['g_negb'])

        HT = 1024
        ecnt = [0]

        def mm(out_p, pk, lhsT, rhs, r, start=True, stop=True):
            return kb.op('pe', lambda e: e.matmul(out_p, lhsT=lhsT, rhs=rhs, start=start, stop=stop), r=r, w=[pk])

        names = ['ke', 'kg', 'vtok', 'dg', 'tmpm', 'decT', 'EB', 'qg', 'attnT', 'PT0', 'P0', 'PT1', 'P1', 'GT0', 'GT1',
                 'bu', 'wT', 'vnA', 'vnB', 'Sa', 'Sb', 'otok', 'junk', 'ytok']

        def head_gen(h, ch):
            GP = 'g%d_' % ch
            qkvb = [sbt(nc, st, "g_qkv%d" % i, [128, 3, HT], F32) for i in range(2)]
            szb = sbt(nc, st, "g_sz", [128, NT, 128], F32)
            yaT = sbt(nc, st, "g_yaT", [128, S], BF16)
            B = {n: sbt(nc, st, "g_" + n, [128, 128], F32) for n in names}
            ss = sbt(nc, st, "g_ss", [128, 1], F32)
            for h in (h, h + 2):
                kb.op('dve', lambda e: e.memset(B['vnA'][:], 0.0), w=[GP + 'vnA'])
                kb.op('dve', lambda e: e.memset(B['vnB'][:], 0.0), w=[GP + 'vnB'])
                kb.dma('pool', szb[:], SC['sz'][:, h * 128:(h + 1) * 128].rearrange("(t p) d -> p t d", p=128),
                       r=['s_sz'], w=[GP + 'sz'])
                kb.op('dve', lambda e: e.memset(B['Sa'][:], 0.0), w=[GP + 'Sa'])
                Scur, Snxt = 'Sa', 'Sb'
                for half in range(S // HT):
                    qb_ = half % 2
                    for i3 in range(3):
                        kb.dma('sp', qkvb[qb_][:, i3, :], SC['qkv'][i3 * 4 + h, :, half * HT:(half + 1) * HT],
                               r=['s_qkv'], w=[GP + 'qkv%d' % qb_])
                    qk = GP + 'qkv%d' % qb_
                    for tt in range(HT // 128):
                        t = half * (HT // 128) + tt
                        c = t * 4 + h
                        if 'gdn_tiles' in DBG and (h * 32 + t) >= DBG['gdn_tiles']:
                            continue
                        qT = qkvb[qb_][:, 0, tt * 128:(tt + 1) * 128]
                        kT = qkvb[qb_][:, 1, tt * 128:(tt + 1) * 128]
                        vT = qkvb[qb_][:, 2, tt * 128:(tt + 1) * 128]
                        sub = DBG.get('gdn_sub', 31)
                        pa, pka = PS.next()
                        if sub & 1:
                            kb.op('pe', lambda e: e.matmul(pa[:, 0:128], lhsT=kT, rhs=ident, start=True, stop=True),
                                  r=[qk, 'cst'], w=[pka])
                            yield
                        if sub & 2:
                            kb.op('act', lambda e: e.activation(out=B['ke'][:], in_=pa[:, 0:128], func=AF.Identity,
                                                                scale=egc[:, c:c + 1]),
                                  r=[pka, 'g_egc'], w=[GP + 'ke'])
                            yield
                        if sub & 4:
                            kb.op('dve', lambda e: e.tensor_scalar(out=B['kg'][:], in0=pa[:, 0:128],
                                                                   scalar1=ekl[:, c:c + 1], scalar2=None, op0=ALU.mult),
                                  r=[pka, 'g_ekl'], w=[GP + 'kg'])
                            yield
                        pb, pkb = PS.next()
                        if sub & 8:
                            kb.op('pe', lambda e: e.matmul(pb[:, 0:128], lhsT=vT, rhs=ident, start=True, stop=True),
                                  r=[qk, 'cst'], w=[pkb])
                            yield
                        if sub & 16:
                            cp(kb, 'act', B['vtok'][:], pb[:, 0:128], [pkb], [GP + 'vtok'])
                            yield
                        if DBG.get('gdn_step', 99) < 1:
                            continue
                        pc, pkc = PS.next()
                        mm(pc[:, 0:128], pkc, kT, kT, [qk])
                        yield
                        pd, pkd = PS.next()
                        mm(pd[:, 0:128], pkd, kT, qT, [qk])
                        yield
                        if DBG.get('gdn_step', 99) < 2:
                            continue
                        kb.op('dve', lambda e, c=c: e.tensor_scalar(out=B['dg'][:], in0=ident, scalar1=gc[:, c:c + 1],
                                                                    scalar2=None, op0=ALU.mult),
                              r=['cst', 'g_gc'], w=[GP + 'dg'])
                        yield
                        pe_, pke = PS.next()
                        mm(pe_[:, 0:128], pke, ones, B['dg'][:], ['cst', GP + 'dg'])
                        yield
                        kb.op('dve', lambda e, pe_=pe_: e.tensor_tensor(out=B['tmpm'][:], in0=pe_[:, 0:128],
                                                                        in1=gm[:, 4, :], op=ALU.add),
                              r=[pke, 'cst'], w=[GP + 'tmpm'])
                        yield
                        if DBG.get('gdn_step', 99) < 3:
                            continue
                        kb.op('act', lambda e, c=c: e.activation(out=B['decT'][:], in_=B['tmpm'][:], func=AF.Exp,
                                                                 bias=ngc[:, c:c + 1], scale=1.0),
                              r=[GP + 'tmpm', 'g_ngc'], w=[GP + 'decT'])
                        yield
                        kb.op('act', lambda e, pe_=pe_: e.activation(out=B['EB'][:], in_=pe_[:, 0:128], func=AF.Exp),
                              r=[pke], w=[GP + 'EB'])
                        yield
                        kb.op('dve', lambda e, qT=qT: e.tensor_tensor(out=B['qg'][:], in0=qT, in1=B['EB'][:], op=ALU.mult),
                              r=[qk, GP + 'EB'], w=[GP + 'qg'])
                        yield
                        kb.op('dve', lambda e, pd=pd: e.tensor_tensor(out=B['attnT'][:], in0=pd[:, 0:128],
                                                                      in1=B['decT'][:], op=ALU.mult),
                              r=[pkd, GP + 'decT'], w=[GP + 'attnT'])
                        yield
                        kb.op('dve', lambda e: e.tensor_tensor(out=B['tmpm'][:], in0=B['decT'][:], in1=gm[:, 5, :],
                                                               op=ALU.mult), r=[GP + 'decT', 'cst'], w=[GP + 'tmpm'])
                        yield
                        kb.op('dve', lambda e, pc=pc, c=c: e.scalar_tensor_tensor(
                            out=B['PT0'][:], in0=pc[:, 0:128], scalar=negb[:, c:c + 1], in1=B['tmpm'][:],
                            op0=ALU.mult, op1=ALU.mult), r=[pkc, 'g_negb', GP + 'tmpm'], w=[GP + 'PT0'])
                        yield
                        if DBG.get('gdn_step', 99) < 4:
                            continue
                        pf, pkf = PS.next()
                        kb.op('pe', lambda e, pf=pf: e.matmul(pf[:, 0:128], lhsT=B['PT0'][:], rhs=ident, start=True, stop=True),
                              r=[GP + 'PT0', 'cst'], w=[pkf])
                        yield
                        cp(kb, 'act', B['P0'][:], pf[:, 0:128], [pkf], [GP + 'P0'])
                        yield
                        kb.op('dve', lambda e: e.tensor_tensor(out=B['GT0'][:], in0=B['PT0'][:], in1=ident, op=ALU.add),
                              r=[GP + 'PT0', 'cst'], w=[GP + 'GT0'])
                        yield
                        if DBG.get('gdn_step', 99) < 5:
                            continue
                        Pc, PTc, Gc = 'P0', 'PT0', 'GT0'
                        for lv in range(1, 6):
                            Pn = 'P1' if Pc == 'P0' else 'P0'
                            PTn = 'PT1' if PTc == 'PT0' else 'PT0'
                            Gn = 'GT1' if Gc == 'GT0' else 'GT0'
                            p1, pk1 = PS.next()
                            mm(p1[:, 0:128], pk1, B[PTc][:], B[Pc][:], [GP + PTc, GP + Pc])
                            yield
                            cp(kb, 'act', B[Pn][:], p1[:, 0:128], [pk1], [GP + Pn])
                            yield
                            if lv < 5:
                                p2_, pk2_ = PS.next()
                                mm(p2_[:, 0:128], pk2_, B[Pc][:], B[PTc][:], [GP + PTc, GP + Pc])
                                yield
                                cp(kb, 'dve', B[PTn][:], p2_[:, 0:128], [pk2_], [GP + PTn])
                                yield
                            p3_, pk3_ = PS.next()
                            mm(p3_[:, 0:128], pk3_, B[Pn][:], B[Gc][:], [GP + Pn, GP + Gc])
                            yield
                            kb.op('dve', lambda e, p3_=p3_, Gn=Gn, Gc=Gc: e.tensor_tensor(
                                out=B[Gn][:], in0=p3_[:, 0:128], in1=B[Gc][:], op=ALU.add),
                                r=[pk3_, GP + Gc], w=[GP + Gn])
                            yield
                            Pc, PTc, Gc = Pn, PTn, Gn
                        if DBG.get('gdn_step', 99) < 6:
                            continue
                        G = B[Gc]
                        Gk = GP + Gc
                        pu, pku = PS.next()
                        mm(pu[:, 0:128], pku, G[:], B['vtok'][:], [Gk, GP + 'vtok'])
                        yield
                        kb.op('act', lambda e, pu=pu, c=c: e.activation(out=B['bu'][:], in_=pu[:, 0:128], func=AF.Identity,
                                                                        scale=b_all[:, c:c + 1]),
                              r=[pku, 'g_ball'], w=[GP + 'bu'])
                        yield
                        pw, pkw = PS.next()
                        mm(pw[:, 0:128], pkw, B['ke'][:], G[:], [Gk, GP + 'ke'])
                        yield
                        cp(kb, 'dve', B['wT'][:], pw[:, 0:128], [pkw], [GP + 'wT'])
                        yield
                        if DBG.get('gdn_step', 99) < 7:
                            continue
                        for ci, vn, gle, gk in ((0, 'vnA', gl0e, 'g_gl0e'), (1, 'vnB', gl1e, 'g_gl1e')):
                            lo, hi = ci * 64, ci * 64 + 64
                            Sc = B[Scur]
                            Sk = GP + Scur
                            p4, pk4 = PS.next()
                            mm(p4[:, 0:128], pk4, B['wT'][:], Sc[:], [GP + 'wT', Sk])
                            yield
                            kb.op('dve', lambda e, p4=p4, vn=vn, lo=lo, hi=hi, c=c: e.scalar_tensor_tensor(
                                out=B[vn][lo:hi, :], in0=p4[lo:hi, 0:128], scalar=negb[lo:hi, c:c + 1],
                                in1=B['bu'][lo:hi, :], op0=ALU.mult, op1=ALU.add),
                                r=[pk4, 'g_negb', GP + 'bu'], w=[GP + vn])
                            yield
                            p5, pk5 = PS.next()
                            mm(p5[:, 0:128], pk5, B['qg'][:], Sc[:], [GP + 'qg', Sk], start=True, stop=False)
                            yield
                            mm(p5[:, 0:128], pk5, B['attnT'][:], B[vn][:], [GP + 'attnT', GP + vn], start=False, stop=True)
                            yield
                            cp(kb, 'act', B['otok'][lo:hi, :], p5[lo:hi, 0:128], [pk5], [GP + 'otok'])
                            yield
                            p6, pk6 = PS.next()
                            mm(p6[:, 0:128], pk6, B['kg'][:], B[vn][:], [GP + 'kg', GP + vn])
                            yield
                            kb.op('dve', lambda e, p6=p6, Sc=Sc, gle=gle, c=c, Snxt=Snxt: e.scalar_tensor_tensor(
                                out=B[Snxt][:], in0=Sc[:], scalar=gle[:, c:c + 1], in1=p6[:, 0:128],
                                op0=ALU.mult, op1=ALU.add), r=[pk6, Sk, gk], w=[GP + Snxt])
                            yield
                            Scur, Snxt = Snxt, Scur
                        if DBG.get('gdn_step', 99) < 8:
                            continue
                        kb.op('act', lambda e: e.activation(out=B['junk'][:], in_=B['otok'][:], func=AF.Square,
                                                            accum_out=ss[:]), r=[GP + 'otok'], w=[GP + 'junk', GP + 'ss'])
                        yield
                        kb.op('act', lambda e: e.activation(out=ss[:], in_=ss[:], func=AF.Sqrt, bias=1e-6,
                                                            scale=1.0 / 128), r=[GP + 'ss'], w=[GP + 'ss'])
                        yield
                        kb.op('dve', lambda e: e.reciprocal(out=ss[:], in_=ss[:]), r=[GP + 'ss'], w=[GP + 'ss'])
                        yield
                        kb.op('dve', lambda e: e.scalar_tensor_tensor(out=B['ytok'][:], in0=B['otok'][:], scalar=ss[:],
                                                                      in1=nw[:], op0=ALU.mult, op1=ALU.mult),
                              r=[GP + 'otok', GP + 'ss', 'g_nw'], w=[GP + 'ytok'])
                        yield
                        kb.op('dve', lambda e, t=t: e.tensor_tensor(out=B['ytok'][:], in0=B['ytok'][:], in1=szb[:, t, :],
                                                                    op=ALU.mult), r=[GP + 'ytok', GP + 'sz'], w=[GP + 'ytok'])
                        yield
                        if DBG.get('gdn_step', 99) < 9:
                            continue
                        p7, pk7 = PS.next()
                        kb.op('pe', lambda e, p7=p7: e.matmul(p7[:, 0:128], lhsT=B['ytok'][:], rhs=ident, start=True, stop=True),
                              r=[GP + 'ytok', 'cst'], w=[pk7])
                        yield
                        cp(kb, 'act', yaT[:, t * 128:(t + 1) * 128], p7[:, 0:128], [pk7], [GP + 'yaT'])
                        yield
                        if 'gdn_dump' in DBG and h == 0 and t == 0:
                            for bi, n_ in enumerate(names):
                                kb.dma('sp', DBG['gdn_dump'][bi], B[n_][:], r=[GP + n_], w=['dump'])
                kb.dma('sp', SC['ya'][h], yaT[:], r=[GP + 'yaT'], w=['s_ya'])


        gens = [head_gen(0, 0), head_gen(1, 1)]
        while gens:
            for g_ in list(gens):
                try:
                    next(g_)
                except StopIteration:
                    gens.remove(g_)
        kb.barrier()


def stage_swa(kb, nc, l, A, SC, cst):
    with ExitStack() as st:
        swab = sbt(nc, st, "a_swab", [128, 8, 256], F32)
        kb.dma('sp', swab[:], A['swab'][:, :, :], w=['cst'])
        qb = sbt(nc, st, "a_qb", [128, 4, S], BF16)
        kbt = sbt(nc, st, "a_kb", [128, S], BF16)
        vb = sbt(nc, st, "a_vb", [128, NT, 128], BF16)
        ybT = sbt(nc, st, "a_ybT", [64, 8, S], BF16)
        snk = sbt(nc, st, "a_snk", [128, 8], F32)
        sc = [sbt(nc, st, "a_sc%d" % i, [128, 256], F32) for i in range(2)]
        pb_ = [sbt(nc, st, "a_p%d" % i, [128, 256], BF16) for i in range(2)]
        pT = [sbt(nc, st, "a_pT%d" % i, [128, 2, 128], BF16) for i in range(2)]
        sm = [sbt(nc, st, "a_sm%d" % i, [128, 8], F32) for i in range(2)]
        PS = PsRot(kb, nc, st, 3, "a_ps")
        PT = PsRot(kb, nc, st, 2, "a_pt", dt=BF16, shape=(128, 2, 128))
        PO = PsRot(kb, nc, st, 2, "a_po")
        for j in range(4):
            kb.dma('sp', qb[:, j, :], SC['qb'][j], r=['s_misc'], w=['a_qb'])
        kb.dma('sp', kbt[:], SC['kb'], r=['s_misc'], w=['a_kb'])
        kb.dma('sp', vb[:], SC['vb'].rearrange("(t p) d -> p t d", p=128), r=['s_vb'], w=['a_vb'])
        kb.dma('sp', snk[:], A['sink_bc'][l], w=['a_snk'])
        u = 0
        for n in range(NT):
            for hq in range(8):
                kv = hq // 4
                j = hq % 4
                lo = kv * 64
                W = 128 if n == 0 else 256
                k0 = 0 if n == 0 else (n - 1) * 128
                b = u % 2
                u += 1
                p, pk = PS.next()
                kb.op('pe', lambda e: e.matmul(p[:, 0:W], lhsT=qb[lo:lo + 64, j, n * 128:(n + 1) * 128],
                                               rhs=kbt[lo:lo + 64, k0:k0 + W], start=True, stop=True),
                      r=['a_qb', 'a_kb'], w=[pk])
                kb.op('dve', lambda e: e.scalar_tensor_tensor(out=sc[b][:, 0:W], in0=p[:, 0:W], scalar=0.125,
                                                              in1=swab[:, hq, 256 - W:256], op0=ALU.mult,
                                                              op1=ALU.add), r=[pk, 'cst'], w=['a_sc%d' % b])
                s_ = sm[b]
                sk = 'a_sm%d' % b
                kb.op('dve', lambda e: e.reduce_max(out=s_[:, 0:1], in_=sc[b][:, 0:W], axis=AX.X),
                      r=['a_sc%d' % b], w=[sk])
                kb.op('dve', lambda e: e.tensor_tensor(out=s_[:, 1:2], in0=s_[:, 0:1], in1=snk[:, hq:hq + 1],
                                                       op=ALU.max), r=[sk, 'a_snk'], w=[sk])
                kb.op('dve', lambda e: e.tensor_scalar(out=s_[:, 2:3], in0=s_[:, 1:2], scalar1=-1.0, scalar2=None,
                                                       op0=ALU.mult), r=[sk], w=[sk])
                kb.op('act', lambda e: e.activation(out=sc[b][:, 0:W], in_=sc[b][:, 0:W], func=AF.Exp,
                                                    bias=s_[:, 2:3], scale=1.0, accum_out=s_[:, 3:4]),
                      r=['a_sc%d' % b, sk], w=['a_sc%d' % b, sk])
                kb.op('act', lambda e: e.activation(out=s_[:, 4:5], in_=snk[:, hq:hq + 1], func=AF.Exp,
                                                    bias=s_[:, 2:3], scale=1.0), r=[sk, 'a_snk'], w=[sk])
                kb.op('dve', lambda e: e.tensor_tensor(out=s_[:, 5:6], in0=s_[:, 3:4], in1=s_[:, 4:5], op=ALU.add),
                      r=[sk], w=[sk])
                kb.op('dve', lambda e: e.reciprocal(out=s_[:, 6:7], in_=s_[:, 5:6]), r=[sk], w=[sk])
                kb.op('dve', lambda e: e.tensor_scalar(out=pb_[b][:, 0:W], in0=sc[b][:, 0:W], scalar1=s_[:, 6:7],
                                                       scalar2=None, op0=ALU.mult),
                      r=['a_sc%d' % b, sk], w=['a_p%d' % b])
                nk = W // 128
                pt, ptk = PT.next()
                for q_ in range(nk):
                    kb.op('pe', lambda e, q_=q_: e.transpose(out=pt[:, q_, :], in_=pb_[b][:, q_ * 128:(q_ + 1) * 128],
                                                            identity=cst['ident_b'][:, :]),
                          r=['a_p%d' % b, 'cst'], w=[ptk])
                cp(kb, 'act', pT[b][:, 0:nk, :], pt[:, 0:nk, :], [ptk], ['a_pT%d' % b])
                po, pok = PO.next()
                for q_ in range(nk):
                    tk = n if n == 0 else n - 1 + q_
                    kb.op('pe', lambda e, q_=q_, tk=tk: e.matmul(po[0:64, 0:128], lhsT=vb[:, tk, kv * 64:(kv + 1) * 64],
                                                                rhs=pT[b][:, q_, :], start=(q_ == 0),
                                                                stop=(q_ == nk - 1)),
                          r=['a_vb', 'a_pT%d' % b], w=[pok])
                cp(kb, 'act' if u % 2 else 'dve', ybT[:, hq, n * 128:(n + 1) * 128], po[0:64, 0:128], [pok], ['a_ybT'])
        kb.dma('sp', SC['yb'], ybT[:], r=['a_ybT'], w=['s_yb'])
        kb.barrier()


def ln_affine_store(kb, nc, xin, xk, bufs, g_bc, b_bc, dst, sfx, dk):
    stt, mv, rstd, nb = bufs
    kb.op('dve', lambda e: e.bn_stats(out=stt[:, 0, :], in_=xin[:, 0:512]), r=[xk], w=['la_st' + sfx])
    kb.op('dve', lambda e: e.bn_stats(out=stt[:, 1, :], in_=xin[:, 512:1024]), r=[xk], w=['la_st' + sfx])
    kb.op('dve', lambda e: e.bn_aggr(out=mv[:], in_=stt[:].rearrange("p a b -> p (a b)")),
          r=['la_st' + sfx], w=['la_mv' + sfx])
    kb.op('act', lambda e: e.activation(out=rstd[:], in_=mv[:, 1:2], func=AF.Sqrt, bias=1e-5, scale=1.0),
          r=['la_mv' + sfx], w=['la_rstd' + sfx])
    kb.op('dve', lambda e: e.reciprocal(out=rstd[:], in_=rstd[:]), r=['la_rstd' + sfx], w=['la_rstd' + sfx])
    kb.op('dve', lambda e: e.scalar_tensor_tensor(out=nb[:], in0=mv[:, 0:1], scalar=-1.0, in1=rstd[:],
                                                  op0=ALU.mult, op1=ALU.mult),
          r=['la_mv' + sfx, 'la_rstd' + sfx], w=['la_nb' + sfx])
    kb.op('act', lambda e: e.activation(out=xin[:], in_=xin[:], func=AF.Identity, bias=nb[:], scale=rstd[:]),
          r=[xk, 'la_nb' + sfx, 'la_rstd' + sfx], w=[xk])
    kb.op('pool', lambda e: e.tensor_tensor(out=xin[:], in0=xin[:], in1=g_bc, op=ALU.mult), r=[xk, 'lngb'], w=[xk])
    kb.op('pool', lambda e: e.tensor_tensor(out=xin[:], in0=xin[:], in1=b_bc, op=ALU.add), r=[xk, 'lngb'], w=[xk])
    kb.dma('sp', dst, xin[:], r=[xk], w=[dk])


def stage_merge(kb, nc, l, A, SC, x_src, x_dst, mod_bc, cst):
    with ExitStack() as st:
        U = [sbt(nc, st, "c_u%d" % i, [128, 16 + S], F32) for i in range(3)]
        pw = sbt(nc, st, "c_pw", [128, 4, 128], F32)
        pscl = sbt(nc, st, "c_ps", [128, 4], F32)
        ic = sbt(nc, st, "c_ic", [128, 4, 16], F32)
        yc = [sbt(nc, st, "c_yc%d" % i, [128, S], BF16) for i in range(2)]
        PS = PsRot(kb, nc, st, 4, "c_pp")
        kb.dma('sp', pw[:], A['pool_w'][l].rearrange("g c d -> c g d"), w=['c_pw'])
        kb.dma('sp', pscl[:], A['pscale_l'][l], w=['c_ps'])
        kb.dma('sp', ic[:], A['invcnt'][:, :, :], w=['c_ic'])
        for i in range(3):
            kb.op('dve', lambda e, i=i: e.memset(U[i][:, 0:16], 0.0), w=['c_u%d' % i])
        for g, win in enumerate((2, 4, 8, 16)):
            kb.dma('sp', U[0][:, 16:], SC['uc'][g], r=['s_uc'], w=['c_u0'])
            cur = 0
            sh = 1
            while sh < win:
                nxt = 1 if cur != 1 else 2
                kb.op('dve' if sh % 4 == 1 else 'pool', lambda e, cur=cur, nxt=nxt, sh=sh: e.tensor_tensor(
                    out=U[nxt][:, 16:], in0=U[cur][:, 16:], in1=U[cur][:, 16 - sh:16 - sh + S], op=ALU.add),
                    r=['c_u%d' % cur], w=['c_u%d' % nxt])
                cur = nxt
                sh *= 2
            dn = 1 if cur != 1 else 2
            kb.op('dve', lambda e, cur=cur, dn=dn: e.scalar_tensor_tensor(
                out=U[dn][:, 16:], in0=U[cur][:, 16:], scalar=1.0 / win, in1=U[0][:, 16:], op0=ALU.mult,
                op1=ALU.subtract), r=['c_u%d' % cur, 'c_u0'], w=['c_u%d' % dn])
            kb.op('dve', lambda e, cur=cur, dn=dn: e.tensor_tensor(out=U[dn][:, 16:32], in0=U[cur][:, 16:32],
                                                                  in1=ic[:, g, :], op=ALU.mult),
                  r=['c_u%d' % cur, 'c_ic'], w=['c_u%d' % dn])
            kb.op('dve', lambda e, dn=dn: e.tensor_tensor(out=U[dn][:, 16:32], in0=U[dn][:, 16:32],
                                                          in1=U[0][:, 16:32], op=ALU.subtract),
                  r=['c_u%d' % dn, 'c_u0'], w=['c_u%d' % dn])
            yb_ = g % 2
            for tc in range(8):
                p, pk = PS.next()
                kb.op('pe', lambda e, tc=tc, dn=dn: e.matmul(p[:], lhsT=pw[:, g, :],
                                                             rhs=U[dn][:, 16 + tc * 512:16 + (tc + 1) * 512],
                                                             start=True, stop=True), r=['c_pw', 'c_u%d' % dn], w=[pk])
                kb.op('act', lambda e, tc=tc: e.activation(out=yc[yb_][:, tc * 512:(tc + 1) * 512], in_=p[:],
                                                           func=AF.Identity, scale=pscl[:, g:g + 1]),
                      r=[pk, 'c_ps'], w=['c_yc%d' % yb_])
            kb.dma('sp', SC['yc'][g], yc[yb_][:], r=['c_yc%d' % yb_], w=['s_yc'])
        kb.barrier()
    with ExitStack() as st:
        wp = sbt(nc, st, "e_wp", [128, 12, D], BF16)
        wpb = sbt(nc, st, "e_wpb", [64, 8, D], BF16)
        wo = sbt(nc, st, "e_wo", [128, 8, D], BF16)
        lng = sbt(nc, st, "e_lng", [128, D], F32)
        lnb = sbt(nc, st, "e_lnb", [128, D], F32)
        gt = [sbt(nc, st, "e_gt0", [128, 24, 512], BF16)] * 2
        ya = [sbt(nc, st, "e_ya%d" % i, [128, 4, 512], BF16) for i in range(2)]
        ycb = [sbt(nc, st, "e_yc%d" % i, [128, 4, 512], BF16) for i in range(2)]
        ybb = [sbt(nc, st, "e_yb%d" % i, [64, 8, 512], BF16) for i in range(2)]
        mg = [sbt(nc, st, "e_mg%d" % i, [128, 8, 512], BF16) for i in range(2)]
        t1 = [sbt(nc, st, "e_t1%d" % i, [128, 512], F32) for i in range(2)]
        t2 = [sbt(nc, st, "e_t2%d" % i, [128, 512], F32) for i in range(2)]
        t3 = [sbt(nc, st, "e_t3%d" % i, [128, 512], F32) for i in range(2)]
        xt = [sbt(nc, st, "e_xt%d" % i, [128, D], F32) for i in range(2)]
        yt = [sbt(nc, st, "e_yt%d" % i, [128, D], F32) for i in range(2)]
        lb = [(sbt(nc, st, "e_st%d" % i, [128, 2, 6], F32), sbt(nc, st, "e_mv%d" % i, [128, 2], F32),
               sbt(nc, st, "e_rs%d" % i, [128, 1], F32), sbt(nc, st, "e_nb%d" % i, [128, 1], F32)) for i in range(2)]
        PA = PsRot(kb, nc, st, 6, "e_pa")
        PY = PsRot(kb, nc, st, 2, "e_py")
        kb.dma('pool', wp[:, 0:4, :], A['w_pa'][l].rearrange("(kc p) n -> p kc n", p=128), w=['e_wp'])
        kb.dma('pool', wp[:, 4:8, :], A['w_pc'][l].rearrange("(kc p) n -> p kc n", p=128), w=['e_wp'])
        kb.dma('pool', wpb[:], A['w_pb'][l].rearrange("(h p) n -> p h n", p=64), w=['e_wpb'])
        kb.dma('pool', wo[:], A['w_o'][l].rearrange("(kc p) n -> p kc n", p=128), w=['e_wo'])
        kb.dma('sp', lng[:], A['ln1g_bc'][l], w=['lngb'])
        kb.dma('sp', lnb[:], A['ln1b_bc'][l], w=['lngb'])
        u = 0
        for tc in range(8):
            b = tc % 2
            ts = slice(tc * 512, (tc + 1) * 512)
            kb.dma('sp', gt[b][:], SC['gate'][:, :, ts].rearrange("c p t -> p c t"), r=['s_misc'], w=['e_gt0'])
            kb.dma('sp', ya[b][:], SC['ya'][:, :, ts].rearrange("c p t -> p c t"), r=['s_ya'], w=['e_ya%d' % b])
            kb.dma('sp', ycb[b][:], SC['yc'][:, :, ts].rearrange("c p t -> p c t"), r=['s_yc'], w=['e_yc%d' % b])
            kb.dma('sp', ybb[b][:], SC['yb'][:, :, ts], r=['s_yb'], w=['e_yb%d' % b])
            for m in range(8):
                ms = slice(m * 128, (m + 1) * 128)
                tb = u % 2
                u += 1
                pa, pka = PA.next()
                for kc in range(4):
                    kb.op('pe', lambda e, kc=kc: e.matmul(pa[:], lhsT=wp[:, kc, ms], rhs=ya[b][:, kc, :],
                                                          start=(kc == 0), stop=(kc == 3)),
                          r=['e_wp', 'e_ya%d' % b], w=[pka])
                kb.op('dve', lambda e: e.tensor_tensor(out=t1[tb][:], in0=pa[:], in1=gt[b][:, m, :], op=ALU.mult),
                      r=[pka, 'e_gt0'], w=['e_t1%d' % tb])
                pb2, pkb2 = PA.next()
                for hh in range(8):
                    kb.op('pe', lambda e, hh=hh: e.matmul(pb2[:], lhsT=wpb[:, hh, ms], rhs=ybb[b][:, hh, :],
                                                          start=(hh == 0), stop=(hh == 7)),
                          r=['e_wpb', 'e_yb%d' % b], w=[pkb2])
                kb.op('dve', lambda e: e.tensor_tensor(out=t2[tb][:], in0=pb2[:], in1=gt[b][:, 8 + m, :], op=ALU.mult),
                      r=[pkb2, 'e_gt0'], w=['e_t2%d' % tb])
                pc2, pkc2 = PA.next()
                for kc in range(4):
                    kb.op('pe', lambda e, kc=kc: e.matmul(pc2[:], lhsT=wp[:, 4 + kc, ms], rhs=ycb[b][:, kc, :],
                                                          start=(kc == 0), stop=(kc == 3)),
                          r=['e_wp', 'e_yc%d' % b], w=[pkc2])
                kb.op('dve', lambda e: e.tensor_tensor(out=t3[tb][:], in0=pc2[:], in1=gt[b][:, 16 + m, :], op=ALU.mult),
                      r=[pkc2, 'e_gt0'], w=['e_t3%d' % tb])
                kb.op('pool', lambda e: e.tensor_tensor(out=t1[tb][:], in0=t1[tb][:], in1=t2[tb][:], op=ALU.add),
                      r=['e_t1%d' % tb, 'e_t2%d' % tb], w=['e_t1%d' % tb])
                kb.op('pool', lambda e: e.tensor_tensor(out=mg[b][:, m, :], in0=t1[tb][:], in1=t3[tb][:], op=ALU.add),
                      r=['e_t1%d' % tb, 'e_t3%d' % tb], w=['e_mg%d' % b])
            for tt in range(4):
                t = tc * 4 + tt
                xb = t % 2
                kb.dma('sp', xt[xb][:], x_src[t * 128:(t + 1) * 128, :], r=['xsrc'], w=['e_xt%d' % xb])
                for half in range(2):
                    py, pyk = PY.next()
                    for kc in range(8):
                        kb.op('pe', lambda e, kc=kc: e.matmul(py[:], lhsT=mg[b][:, kc, tt * 128:(tt + 1) * 128],
                                                              rhs=wo[:, kc, half * 512:(half + 1) * 512],
                                                              start=(kc == 0), stop=(kc == 7)),
                              r=['e_mg%d' % b, 'e_wo'], w=[pyk])
                    kb.op('dve', lambda e, half=half: e.tensor_tensor(
                        out=yt[xb][:, half * 512:(half + 1) * 512], in0=py[:],
                        in1=mod_bc[:, 2 * D + half * 512:2 * D + (half + 1) * 512], op=ALU.mult),
                        r=[pyk, 'mod_bc'], w=['e_yt%d' % xb])
                kb.op('dve', lambda e: e.scalar_tensor_tensor(out=yt[xb][:], in0=xt[xb][:], scalar=ALPHA, in1=yt[xb][:],
                                                              op0=ALU.mult, op1=ALU.add),
                      r=['e_xt%d' % xb, 'e_yt%d' % xb], w=['e_yt%d' % xb])
                ln_affine_store(kb, nc, yt[xb], 'e_yt%d' % xb, lb[xb], lng[:], lnb[:],
                                x_dst[t * 128:(t + 1) * 128, :], 'e%d' % xb, 'xdst')
        kb.barrier()


def stage_moe(kb, nc, l, A, SC, x_src, x_dst, mod_bc, cst):
    ident = cst['ident_f']
    with ExitStack() as st:
        hT = sbt(nc, st, "hT2", [128, 8, S], BF16)
        gate_all = sbt(nc, st, "f_gate", [128, NT, NE], F32)
        with ExitStack() as s2:
            xts = [sbt(nc, s2, "f_xt%d" % i, [128, D], F32) for i in range(2)]
            hbs = [sbt(nc, s2, "f_hb%d" % i, [128, D], BF16) for i in range(2)]
            bufs = [(sbt(nc, s2, "f_st%d" % i, [128, 2, 6], F32), sbt(nc, s2, "f_mv%d" % i, [128, 2], F32),
                     sbt(nc, s2, "f_rs%d" % i, [128, 1], F32), sbt(nc, s2, "f_nb%d" % i, [128, 1], F32),
                     sbt(nc, s2, "f_xn%d" % i, [128, D], F32)) for i in range(2)]
            h32 = [sbt(nc, s2, "f_h32%d" % i, [128, 8, 128], F32) for i in range(2)]
            rw = sbt(nc, s2, "f_rw", [128, 8, NE], F32)
            rb = sbt(nc, s2, "f_rb", [128, NE], F32)
            lg = [sbt(nc, s2, "f_lg%d" % i, [128, NE], F32) for i in range(2)]
            mk = [sbt(nc, s2, "f_mk%d" % i, [128, NE], F32) for i in range(2)]
            m8 = [sbt(nc, s2, "f_m8%d" % i, [128, 12], F32) for i in range(2)]
            pT = [pst(nc, s2, "f_pT%d" % i, [128, 8, 128], BF16) for i in range(2)]
            pR = [pst(nc, s2, "f_pR%d" % i, [128, 2, 512], F32) for i in range(2)]
            pL = [pst(nc, s2, "f_pL%d" % i, [128, 512], F32) for i in range(2)]
            kb.dma('sp', rw[:], A['router_w'][l].rearrange("(kc p) n -> p kc n", p=128), w=['f_rw'])
            kb.dma('sp', rb[:], A['rb_bc'][l], w=['f_rb'])
            for t in range(NT):
                b = t % 2
                sx = 'f' + str(b)
                kb.dma('sp', xts[b][:], x_src[t * 128:(t + 1) * 128, :], r=['xsrc5'], w=['f_xt%d' % b])
                stt, mv, rstd, nb, xn = bufs[b]
                ln_mod_tile(kb, nc, xts[b], 'f_xt%d' % b, bufs[b], mod_bc[:, 4 * D:5 * D], mod_bc[:, 3 * D:4 * D],
                            xn[:], 'ln_xn' + sx, sx)
                cp(kb, 'act', hbs[b][:], xn[:], ['ln_xn' + sx], ['f_hb%d' % b])
                for kc in range(8):
                    kb.op('pe', lambda e, kc=kc: e.transpose(out=pT[b][:, kc, :], in_=hbs[b][:, kc * 128:(kc + 1) * 128],
                                                             identity=cst['ident_b'][:, :]),
                          r=['f_hb%d' % b, 'cst'], w=['f_pT%d' % b])
                cp(kb, 'act', hT[:, :, t * 128:(t + 1) * 128], pT[b][:], ['f_pT%d' % b], ['hT2'])
                for kc in range(8):
                    kb.op('pe', lambda e, kc=kc: e.matmul(pR[b][:, kc // 4, (kc % 4) * 128:(kc % 4 + 1) * 128],
                                                          lhsT=xn[:, kc * 128:(kc + 1) * 128], rhs=ident,
                                                          start=True, stop=True),
                          r=['ln_xn' + sx, 'cst'], w=['f_pR%d' % b])
                cp(kb, 'dve', h32[b][:].rearrange("p (a c) n -> p a (c n)", a=2), pR[b][:], ['f_pR%d' % b],
                   ['f_h32%d' % b])
                for kc in range(8):
                    kb.op('pe', lambda e, kc=kc: e.matmul(pL[b][:, 0:NE], lhsT=h32[b][:, kc, :], rhs=rw[:, kc, :],
                                                          start=(kc == 0), stop=(kc == 7)),
                          r=['f_h32%d' % b, 'f_rw'], w=['f_pL%d' % b])
                kb.op('dve', lambda e: e.tensor_tensor(out=lg[b][:], in0=pL[b][:, 0:NE], in1=rb[:], op=ALU.add),
                      r=['f_pL%d' % b, 'f_rb'], w=['f_lg%d' % b])
                kb.op('dve', lambda e: e.max(out=m8[b][:, 0:8], in_=lg[b][:]), r=['f_lg%d' % b], w=['f_m8%d' % b])
                kb.op('dve', lambda e: e.tensor_scalar(out=mk[b][:], in0=lg[b][:], scalar1=m8[b][:, 3:4], scalar2=None,
                                                       op0=ALU.is_ge), r=['f_lg%d' % b, 'f_m8%d' % b], w=['f_mk%d' % b])
                kb.op('dve', lambda e: e.tensor_scalar(out=m8[b][:, 8:9], in0=m8[b][:, 0:1], scalar1=-1.0, scalar2=None,
                                                       op0=ALU.mult), r=['f_m8%d' % b], w=['f_m8%d' % b])
                kb.op('act', lambda e: e.activation(out=lg[b][:], in_=lg[b][:], func=AF.Exp, bias=m8[b][:, 8:9],
                                                    scale=1.0), r=['f_lg%d' % b, 'f_m8%d' % b], w=['f_lg%d' % b])
                kb.op('dve', lambda e: e.tensor_tensor(out=lg[b][:], in0=lg[b][:], in1=mk[b][:], op=ALU.mult),
                      r=['f_lg%d' % b, 'f_mk%d' % b], w=['f_lg%d' % b])
                kb.op('dve', lambda e: e.reduce_sum(out=m8[b][:, 9:10], in_=lg[b][:], axis=AX.X),
                      r=['f_lg%d' % b], w=['f_m8%d' % b])
                kb.op('dve', lambda e: e.reciprocal(out=m8[b][:, 10:11], in_=m8[b][:, 9:10]),
                      r=['f_m8%d' % b], w=['f_m8%d' % b])
                kb.op('dve', lambda e: e.tensor_scalar(out=gate_all[:, t, :], in0=lg[b][:], scalar1=m8[b][:, 10:11],
                                                       scalar2=None, op0=ALU.mult),
                      r=['f_lg%d' % b, 'f_m8%d' % b], w=['f_gate'])
            kb.barrier()
        PT_ = 8
        acc = sbt(nc, st, "f_acc", [128, PT_, D], F32)
        w1t = [sbt(nc, st, "f_w1%d" % i, [128, 8, 512], BF16) for i in range(2)]
        w2t = sbt(nc, st, "f_w2", [128, 8, D], BF16)
        b1t = [sbt(nc, st, "f_b1%d" % i, [128, 16], F32) for i in range(2)]
        b2t = [sbt(nc, st, "f_b2%d" % i, [1, D], BF16) for i in range(2)]
        onesb = sbt(nc, st, "f_onesb", [1, 128], BF16)
        actT = sbt(nc, st, "f_actT", [128, 8, PT_ * 128], BF16)
        gl = [sbt(nc, st, "f_gl%d" % i, [128, 512], F32) for i in range(2)]
        sg = [sbt(nc, st, "f_sg%d" % i, [128, 512], F32) for i in range(2)]
        li = [sbt(nc, st, "f_li%d" % i, [128, 512], F32) for i in range(2)]
        xt = [sbt(nc, st, "f_x%d" % i, [128, D], F32) for i in range(2)]
        lng = sbt(nc, st, "f_lng", [128, D], F32)
        lnb = sbt(nc, st, "f_lnb", [128, D], F32)
        lb = [(sbt(nc, st, "f_lst%d" % i, [128, 2, 6], F32), sbt(nc, st, "f_lmv%d" % i, [128, 2], F32),
               sbt(nc, st, "f_lrs%d" % i, [128, 1], F32), sbt(nc, st, "f_lnb%d" % i, [128, 1], F32)) for i in range(2)]
        PG = PsRot(kb, nc, st, 4, "f_pg")
        PY = PsRot(kb, nc, st, 3, "f_py")
        kb.op('dve', lambda e: e.tensor_copy(out=onesb[:], in_=cst['ones_f'][0:1, :]), r=['cst'], w=['f_onesb'])
        kb.dma('sp', lng[:], A['ln2g_bc'][l], w=['lngb'])
        kb.dma('sp', lnb[:], A['ln2b_bc'][l], w=['lngb'])
        W1, W2 = A['exp_w1'], A['exp_w2']
        gcount = 0
        u = 0
        n_exp = DBG.get('n_exp', NE)
        for ps_ in range(NT // PT_):
            kb.op('pool', lambda e: e.memset(acc[:], 0.0), w=['f_acc'])
            for ex in range(n_exp):
                eb = ex % 2
                kb.dma('sp', b1t[eb][:], A['exp_b1l'][l, ex], w=['f_b1%d' % eb])
                kb.dma('pool', b2t[eb][:], A['exp_b2'][l, ex:ex + 1, :], w=['f_b2%d' % eb])
                for g in range(4):
                    wb = gcount % 2
                    gcount += 1
                    kb.dma('pool', w1t[wb][:, :, 0:256],
                           W1[l, ex, :, g * 256:(g + 1) * 256].rearrange("(kc p) n -> p kc n", p=128), w=['f_w1%d' % wb])
                    kb.dma('pool', w1t[wb][:, :, 256:512],
                           W1[l, ex, :, DFF + g * 256:DFF + (g + 1) * 256].rearrange("(kc p) n -> p kc n", p=128),
                           w=['f_w1%d' % wb])
                    for fl in range(2):
                        fc = g * 2 + fl
                        for tcl in range(PT_ // 4):
                            tb = u % 2
                            u += 1
                            tok = slice((ps_ * PT_ + tcl * 4) * 128, (ps_ * PT_ + tcl * 4 + 4) * 128)
                            pg, pgk = PG.next()
                            for kc in range(8):
                                kb.op('pe', lambda e, kc=kc: e.matmul(pg[:], lhsT=w1t[wb][:, kc, fl * 128:(fl + 1) * 128],
                                                                      rhs=hT[:, kc, tok], start=(kc == 0), stop=(kc == 7)),
                                      r=['f_w1%d' % wb, 'hT2'], w=[pgk])
                            pl, plk = PG.next()
                            for kc in range(8):
                                kb.op('pe', lambda e, kc=kc: e.matmul(
                                    pl[:], lhsT=w1t[wb][:, kc, 256 + fl * 128:256 + (fl + 1) * 128],
                                    rhs=hT[:, kc, tok], start=(kc == 0), stop=(kc == 7)),
                                    r=['f_w1%d' % wb, 'hT2'], w=[plk])
                            kb.op('dve', lambda e: e.tensor_scalar(out=gl[tb][:], in0=pg[:], scalar1=b1t[eb][:, fc:fc + 1],
                                                                   scalar2=7.0, op0=ALU.add, op1=ALU.min),
                                  r=[pgk, 'f_b1%d' % eb], w=['f_gl%d' % tb])
                            kb.op('act', lambda e: e.activation(out=sg[tb][:], in_=gl[tb][:], func=AF.Sigmoid,
                                                                scale=1.702), r=['f_gl%d' % tb], w=['f_sg%d' % tb])
                            kb.op('dve', lambda e: e.tensor_scalar(out=li[tb][:], in0=pl[:],
                                                                   scalar1=b1t[eb][:, 8 + fc:9 + fc], scalar2=7.0,
                                                                   op0=ALU.add, op1=ALU.min),
                                  r=[plk, 'f_b1%d' % eb], w=['f_li%d' % tb])
                            kb.op('dve', lambda e: e.tensor_scalar(out=li[tb][:], in0=li[tb][:], scalar1=-7.0,
                                                                   scalar2=1.0, op0=ALU.max, op1=ALU.add),
                                  r=['f_li%d' % tb], w=['f_li%d' % tb])
                            kb.op('pool', lambda e: e.tensor_tensor(out=gl[tb][:], in0=gl[tb][:], in1=sg[tb][:],
                                                                    op=ALU.mult),
                                  r=['f_gl%d' % tb, 'f_sg%d' % tb], w=['f_gl%d' % tb])
                            kb.op('pool', lambda e: e.tensor_tensor(out=actT[:, fc, tcl * 512:(tcl + 1) * 512],
                                                                    in0=gl[tb][:], in1=li[tb][:], op=ALU.mult),
                                  r=['f_gl%d' % tb, 'f_li%d' % tb], w=['f_actT'])
                kb.dma('pool', w2t[:], W2[l, ex].rearrange("(kc p) n -> p kc n", p=128), w=['f_w2'],
                       max_dma_last_dim=4096)
                for tt in range(PT_):
                    T = ps_ * PT_ + tt
                    for half in range(2):
                        py, pyk = PY.next()
                        for fc in range(8):
                            kb.op('pe', lambda e, fc=fc: e.matmul(py[:], lhsT=actT[:, fc, tt * 128:(tt + 1) * 128],
                                                                  rhs=w2t[:, fc, half * 512:(half + 1) * 512],
                                                                  start=(fc == 0), stop=False),
                                  r=['f_actT', 'f_w2'], w=[pyk])
                        kb.op('pe', lambda e: e.matmul(py[:], lhsT=onesb[0:1, :], rhs=b2t[eb][0:1, half * 512:(half + 1) * 512],
                                                       start=False, stop=True), r=['f_onesb', 'f_b2%d' % eb], w=[pyk])
                        kb.op('dve', lambda e: e.scalar_tensor_tensor(
                            out=acc[:, tt, half * 512:(half + 1) * 512], in0=py[:], scalar=gate_all[:, T, ex:ex + 1],
                            in1=acc[:, tt, half * 512:(half + 1) * 512], op0=ALU.mult, op1=ALU.add),
                            r=[pyk, 'f_gate', 'f_acc'], w=['f_acc'])
            for tt in range(PT_):
                T = ps_ * PT_ + tt
                xb = T % 2
                kb.dma('sp', xt[xb][:], x_src[T * 128:(T + 1) * 128, :], r=['xsrc5'], w=['f_x%d' % xb])
                kb.op('dve', lambda e: e.tensor_tensor(out=acc[:, tt, :], in0=acc[:, tt, :], in1=mod_bc[:, 5 * D:6 * D],
                                                       op=ALU.mult), r=['f_acc', 'mod_bc'], w=['f_acc'])
                kb.op('dve', lambda e: e.scalar_tensor_tensor(out=xt[xb][:], in0=xt[xb][:], scalar=ALPHA,
                                                              in1=acc[:, tt, :], op0=ALU.mult, op1=ALU.add),
                      r=['f_x%d' % xb, 'f_acc'], w=['f_x%d' % xb])
                ln_affine_store(kb, nc, xt[xb], 'f_x%d' % xb, lb[xb], lng[:], lnb[:],
                                x_dst[T * 128:(T + 1) * 128, :], 'f%d' % xb, 'xdst5')
        kb.barrier()


def input_shapes(L):
    return {
        'x': [S, D], 'c_col': [128, 8], 'ada_w': [L, D, 6 * D], 'ada_b': [L, 6 * D], 'w_in': [L, D, INW],
        'conv_wl': [L, 128, 12, 4], 'alog_bc': [L, 128, 4], 'dtb_bc': [L, 128, 4], 'gnw_bc': [L, 128, 128],
        'sink_bc': [L, 128, 8], 'pool_w': [L, 4, 128, 128], 'pscale_l': [L, 128, 4],
        'w_pa': [L, 512, D], 'w_pb': [L, 512, D], 'w_pc': [L, 512, D], 'w_o': [L, D, D],
        'ln1g_bc': [L, 128, D], 'ln1b_bc': [L, 128, D], 'ln2g_bc': [L, 128, D], 'ln2b_bc': [L, 128, D],
        'router_w': [L, D, NE], 'rb_bc': [L, 128, NE], 'exp_w1': [L, NE, D, 2 * DFF], 'exp_w2': [L, NE, DFF, D],
        'exp_b1l': [L, NE, 128, 16], 'exp_b2': [L, NE, D],
        'cst_f': [128, 2, 128], 'gm': [128, 6, 128], 'swab': [128, 8, 256], 'invcnt': [128, 4, 16], 'moec': [128, 160],
    }


def build_program(L):
    nc = bass.Bass("TRN2", target_bir_lowering=False)
    A = {k: nc.dram_tensor(k, s, F32, kind="ExternalInput").ap() for k, s in input_shapes(L).items()}
    out = nc.dram_tensor("out", [S, D], F32, kind="ExternalOutput").ap()
    SC = alloc_scratch(nc)
    SC['ya'] = nc.dram_tensor("s_ya", [4, 128, S], BF16).ap()
    SC['yb'] = nc.dram_tensor("s_yb", [64, 8, S], BF16).ap()
    SC['yc'] = nc.dram_tensor("s_yc", [4, 128, S], BF16).ap()
    x1 = nc.dram_tensor("s_x1", [S, D], F32).ap()
    SC['hg'] = nc.dram_tensor("s_hg", [NE * CAP, D], BF16).ap()
    SC['yg'] = nc.dram_tensor("s_yg", [NE * CAP, D], F32).ap()
    xs = [A['x']] + [nc.dram_tensor("s_xl%d" % i, [S, D], F32).ap() for i in range(L - 1)] + [out]
    with ExitStack() as es:
        kb = KB(nc, es)
        cf = sbt(nc, es, "cst_sb", [128, 2, 128], F32)
        ib = sbt(nc, es, "ident_b", [128, 128], BF16)
        mod_bc = sbt(nc, es, "mod_bc", [128, 6 * D], F32)
        kb.dma('sp', cf[:], A['cst_f'][:, :, :], w=['cst'])
        kb.op('dve', lambda e: e.tensor_copy(out=ib[:], in_=cf[:, 1, :]), r=['cst'], w=['cst'])
        cst = {'ones_f': cf[:, 0, :], 'ident_f': cf[:, 1, :], 'ident_b': ib, 'breg': nc.gpsimd.to_reg(NE * CAP - 1)}
        with ExitStack() as zs:
            zt = sbt(nc, zs, "zero_t", [128, 8 * D], BF16)
            kb.op('dve', lambda e: e.memset(zt[:], 0.0), w=['zero_t'])
            rows_per = 128 * 8
            for i in range(NE * CAP // rows_per):
                kb.dma('sp' if i % 2 == 0 else 'act', SC['hg'][i * rows_per:(i + 1) * rows_per, :].rearrange(
                    "(p r) d -> p (r d)", p=128), zt[:], r=['zero_t'], w=['s_hg_z%d' % (i % 8)])
            kb.barrier()
        for l in range(L):
            stage_mod(kb, nc, l, A, mod_bc, cst)
            stage_proj(kb, nc, l, A, SC, xs[l], mod_bc, cst)
            stage_gdn(kb, nc, l, A, SC, cst)
            stage_swa(kb, nc, l, A, SC, cst)
            stage_merge(kb, nc, l, A, SC, xs[l], x1, mod_bc, cst)
            stage_moe_sparse(kb, nc, l, A, SC, x1, xs[l + 1], mod_bc, cst)
        kb.barrier()
    return nc


def host_consts():
    i = np.arange(128)[:, None]
    j = np.arange(128)[None, :]
    same = (i // 64) == (j // 64)
    gm = np.stack([((i <= j) & same).astype(np.float32), same.astype(np.float32),
                   np.broadcast_to(i < 64, (128, 128)).astype(np.float32),
                   np.broadcast_to(i >= 64, (128, 128)).astype(np.float32),
                   np.where((j >= i) & same, 0.0, NEG).astype(np.float32),
                   (1.0 - np.eye(128)).astype(np.float32)], 1)
    q = np.arange(128)[:, None]
    k = np.arange(256)[None, :]
    dist = q + 128 - k
    valid = (dist >= 0) & (dist < 128)
    slopes = 2.0 ** (-8.0 * np.arange(1, 9, dtype=np.float32) / 8)
    swab = np.where(valid[:, None, :], -slopes[None, :, None] * dist[:, None, :].astype(np.float32), NEG)
    ic = np.zeros((4, 16), np.float32)
    for g, win in enumerate((2, 4, 8, 16)):
        ic[g] = 1.0 / np.minimum(np.arange(16) + 1, win)
    moec = np.concatenate([(i <= j).astype(np.float32),
                           np.broadcast_to((np.arange(NE) * CAP - 1).astype(np.float32)[None, :], (128, NE))], 1)
    return {'moec': np.ascontiguousarray(moec), 'cst_f': np.stack([np.ones((128, 128), np.float32), np.eye(128, dtype=np.float32)], 1),
            'gm': np.ascontiguousarray(gm), 'swab': np.ascontiguousarray(swab.astype(np.float32)),
            'invcnt': np.ascontiguousarray(np.broadcast_to(ic[None], (128, 4, 16)))}


def host_layer_inputs(p, ls):
    f = lambda a: np.ascontiguousarray(np.asarray(a, np.float32))
    n = len(range(*ls.indices(DEPTH)))
    bc = lambda a, w: f(np.broadcast_to(np.asarray(a)[ls][:, None, :], (n, 128, w)))
    W = {}
    for k in ['ada_w', 'ada_b', 'w_in', 'pool_w', 'w_pa', 'w_pb', 'w_pc', 'w_o', 'router_w', 'exp_w1', 'exp_w2', 'exp_b2']:
        W[k] = f(np.asarray(p[k])[ls])
    W['conv_wl'] = f(np.asarray(p['conv_w'])[ls].reshape(n, 4, 12, 128).transpose(0, 3, 2, 1))
    W['alog_bc'] = bc(p['a_log'], 4)
    W['dtb_bc'] = bc(p['dt_bias'], 4)
    W['gnw_bc'] = bc(p['gdn_norm_w'], 128)
    W['sink_bc'] = bc(p['sinks'], 8)
    W['pscale_l'] = f(np.asarray(p['pool_scale'])[ls].reshape(n, 4, 128).transpose(0, 2, 1))
    W['ln1g_bc'] = bc(p['ln1_g'], D)
    W['ln1b_bc'] = bc(p['ln1_b'], D)
    W['ln2g_bc'] = bc(p['ln2_g'], D)
    W['ln2b_bc'] = bc(p['ln2_b'], D)
    W['rb_bc'] = bc(p['router_b'], NE)
    W['exp_b1l'] = f(np.asarray(p['exp_b1'])[ls].reshape(n, NE, 16, 128).transpose(0, 1, 3, 2))
    return W


LAYERS_PER_LAUNCH = 4


def kernel(**inputs):
    x = np.asarray(inputs['x'], np.float32)
    c = np.asarray(inputs['c'], np.float32)
    nb = x.shape[0]
    consts = host_consts()
    Lp = LAYERS_PER_LAUNCH
    nc = build_program(Lp)
    cur = [np.ascontiguousarray(x[b]) for b in range(nb)]
    for l0 in range(0, DEPTH, Lp):
        W = host_layer_inputs(inputs, slice(l0, l0 + Lp))
        in_maps = []
        for b in range(nb):
            m = dict(W)
            m.update(consts)
            m['x'] = cur[b]
            m['c_col'] = np.ascontiguousarray(c[b].reshape(8, 128).T)
            in_maps.append(m)
        res = run_bass_kernel_spmd(nc, in_maps, core_ids=list(range(nb)))
        cur = [np.asarray(res.results[b]['out'], np.float32) for b in range(nb)]
    return np.stack(cur, 0).astype(np.float32)


def stage_moe_sparse(kb, nc, l, A, SC, x_src, x_dst, mod_bc, cst):
    ident = cst['ident_f']
    C = CAP
    NB = C // 128
    hg, yg = SC['hg'], SC['yg']
    with ExitStack() as st:
        dest_all = sbt(nc, st, "f_dest", [128, NT * 4], I32)
        gk_all = sbt(nc, st, "f_gk", [128, NT, 4], F32)
        with ExitStack() as s2:
            moec = sbt(nc, s2, "f_moec", [128, 160], F32)
            xts = [sbt(nc, s2, "f_xt%d" % i, [128, D], F32) for i in range(2)]
            hbs = [sbt(nc, s2, "f_hb%d" % i, [128, D], BF16) for i in range(2)]
            bufs = [(sbt(nc, s2, "f_st%d" % i, [128, 2, 6], F32), sbt(nc, s2, "f_mv%d" % i, [128, 2], F32),
                     sbt(nc, s2, "f_rs%d" % i, [128, 1], F32), sbt(nc, s2, "f_nb%d" % i, [128, 1], F32),
                     sbt(nc, s2, "f_xn%d" % i, [128, D], F32)) for i in range(2)]
            h32 = [sbt(nc, s2, "f_h32%d" % i, [128, 8, 128], F32) for i in range(2)]
            rw = sbt(nc, s2, "f_rw", [128, 8, NE], F32)
            rb = sbt(nc, s2, "f_rb", [128, NE], F32)
            lg = [sbt(nc, s2, "f_lg%d" % i, [128, NE], F32) for i in range(2)]
            mk = [sbt(nc, s2, "f_mk%d" % i, [128, NE], F32) for i in range(2)]
            gt_ = [sbt(nc, s2, "f_gt%d" % i, [128, NE], F32) for i in range(2)]
            ngp = [sbt(nc, s2, "f_ngp%d" % i, [128, NE], F32) for i in range(2)]
            oh = [sbt(nc, s2, "f_oh%d" % i, [128, NE], F32) for i in range(2)]
            jk = [sbt(nc, s2, "f_jk%d" % i, [128, NE], F32) for i in range(2)]
            m8 = [sbt(nc, s2, "f_m8%d" % i, [128, 12], F32) for i in range(2)]
            t8 = [sbt(nc, s2, "f_t8%d" % i, [128, 8], F32) for i in range(2)]
            msum = sbt(nc, s2, "f_msum", [128, NE], F32)
            pR = [pst(nc, s2, "f_pR%d" % i, [128, 2, 512], F32) for i in range(2)]
            pL = [pst(nc, s2, "f_pL%d" % i, [128, 512], F32) for i in range(2)]
            pC = [pst(nc, s2, "f_pC%d" % i, [128, 512], F32) for i in range(2)]
            kb.dma('sp', moec[:], A['moec'][:, :], w=['f_moec'])
            kb.dma('sp', rw[:], A['router_w'][l].rearrange("(kc p) n -> p kc n", p=128), w=['f_rw'])
            kb.dma('sp', rb[:], A['rb_bc'][l], w=['f_rb'])
            kb.op('dve', lambda e: e.memset(msum[:], 0.0), w=['f_msum'])
            triu = moec[:, 0:128]
            ecb = moec[:, 128:160]
            for t in range(NT):
                b = t % 2
                sx = 'f' + str(b)
                B_ = str(b)
                kb.dma('sp', xts[b][:], x_src[t * 128:(t + 1) * 128, :], r=['xsrc5'], w=['f_xt' + B_])
                stt, mv, rstd, nb, xn = bufs[b]
                ln_mod_tile(kb, nc, xts[b], 'f_xt' + B_, bufs[b], mod_bc[:, 4 * D:5 * D], mod_bc[:, 3 * D:4 * D],
                            xn[:], 'ln_xn' + sx, sx)
                cp(kb, 'act', hbs[b][:], xn[:], ['ln_xn' + sx], ['f_hb' + B_])
                for kc in range(8):
                    kb.op('pe', lambda e, kc=kc: e.matmul(pR[b][:, kc // 4, (kc % 4) * 128:(kc % 4 + 1) * 128],
                                                          lhsT=xn[:, kc * 128:(kc + 1) * 128], rhs=ident,
                                                          start=True, stop=True),
                          r=['ln_xn' + sx, 'cst'], w=['f_pR' + B_])
                cp(kb, 'dve', h32[b][:].rearrange("p (a c) n -> p a (c n)", a=2), pR[b][:], ['f_pR' + B_],
                   ['f_h32' + B_])
                for kc in range(8):
                    kb.op('pe', lambda e, kc=kc: e.matmul(pL[b][:, 0:NE], lhsT=h32[b][:, kc, :], rhs=rw[:, kc, :],
                                                          start=(kc == 0), stop=(kc == 7)),
                          r=['f_h32' + B_, 'f_rw'], w=['f_pL' + B_])
                kb.op('dve', lambda e: e.tensor_tensor(out=lg[b][:], in0=pL[b][:, 0:NE], in1=rb[:], op=ALU.add),
                      r=['f_pL' + B_, 'f_rb'], w=['f_lg' + B_])
                kb.op('dve', lambda e: e.max(out=m8[b][:, 0:8], in_=lg[b][:]), r=['f_lg' + B_], w=['f_m8' + B_])
                kb.op('dve', lambda e: e.tensor_scalar(out=mk[b][:], in0=lg[b][:], scalar1=m8[b][:, 3:4], scalar2=None,
                                                       op0=ALU.is_ge), r=['f_lg' + B_, 'f_m8' + B_], w=['f_mk' + B_])
                kb.op('dve', lambda e: e.tensor_scalar(out=m8[b][:, 8:9], in0=m8[b][:, 0:1], scalar1=-1.0, scalar2=None,
                                                       op0=ALU.mult), r=['f_m8' + B_], w=['f_m8' + B_])
                kb.op('act', lambda e: e.activation(out=lg[b][:], in_=lg[b][:], func=AF.Exp, bias=m8[b][:, 8:9],
                                                    scale=1.0), r=['f_lg' + B_, 'f_m8' + B_], w=['f_lg' + B_])
                kb.op('dve', lambda e: e.tensor_tensor(out=lg[b][:], in0=lg[b][:], in1=mk[b][:], op=ALU.mult),
                      r=['f_lg' + B_, 'f_mk' + B_], w=['f_lg' + B_])
                kb.op('dve', lambda e: e.reduce_sum(out=m8[b][:, 9:10], in_=lg[b][:], axis=AX.X),
                      r=['f_lg' + B_], w=['f_m8' + B_])
                kb.op('dve', lambda e: e.reciprocal(out=m8[b][:, 10:11], in_=m8[b][:, 9:10]),
                      r=['f_m8' + B_], w=['f_m8' + B_])
                kb.op('dve', lambda e: e.tensor_scalar(out=gt_[b][:], in0=lg[b][:], scalar1=m8[b][:, 10:11],
                                                       scalar2=None, op0=ALU.mult),
                      r=['f_lg' + B_, 'f_m8' + B_], w=['f_gt' + B_])
                kb.op('pe', lambda e: e.matmul(pC[b][:, 0:NE], lhsT=triu, rhs=mk[b][:], start=True, stop=False),
                      r=['f_moec', 'f_mk' + B_], w=['f_pC' + B_])
                kb.op('pe', lambda e: e.matmul(pC[b][:, 0:NE], lhsT=cst['ones_f'], rhs=msum[:], start=False, stop=True),
                      r=['cst', 'f_msum'], w=['f_pC' + B_])
                kb.op('dve', lambda e: e.tensor_tensor(out=msum[:], in0=msum[:], in1=mk[b][:], op=ALU.add),
                      r=['f_msum', 'f_mk' + B_], w=['f_msum'])
                kb.op('dve', lambda e: e.tensor_tensor(out=ngp[b][:], in0=pC[b][:, 0:NE], in1=ecb, op=ALU.add),
                      r=['f_pC' + B_, 'f_moec'], w=['f_ngp' + B_])
                kb.op('dve', lambda e: e.tensor_scalar(out=ngp[b][:], in0=ngp[b][:], scalar1=-1.0, scalar2=BIG,
                                                       op0=ALU.mult, op1=ALU.add), r=['f_ngp' + B_], w=['f_ngp' + B_])
                kb.op('dve', lambda e: e.tensor_tensor(out=ngp[b][:], in0=ngp[b][:], in1=mk[b][:], op=ALU.mult),
                      r=['f_ngp' + B_, 'f_mk' + B_], w=['f_ngp' + B_])
                kb.op('dve', lambda e: e.tensor_scalar(out=ngp[b][:], in0=ngp[b][:], scalar1=-BIG, scalar2=None,
                                                       op0=ALU.add), r=['f_ngp' + B_], w=['f_ngp' + B_])
                kb.op('dve', lambda e: e.max(out=t8[b][:], in_=ngp[b][:]), r=['f_ngp' + B_], w=['f_t8' + B_])
                dk = 'f_dest%d' % t
                kb.op('dve', lambda e: e.tensor_scalar(out=dest_all[:, t * 4:(t + 1) * 4], in0=t8[b][:, 0:4], scalar1=-1.0,
                                                       scalar2=None, op0=ALU.mult), r=['f_t8' + B_], w=[dk])
                for k in range(4):
                    kb.op('dve', lambda e, k=k: e.tensor_scalar(out=oh[b][:], in0=ngp[b][:], scalar1=t8[b][:, k:k + 1],
                                                               scalar2=None, op0=ALU.is_equal),
                          r=['f_ngp' + B_, 'f_t8' + B_], w=['f_oh' + B_])
                    kb.op('dve', lambda e, k=k: e.scalar_tensor_tensor(out=jk[b][:], in0=oh[b][:], scalar=1.0,
                                                                      in1=gt_[b][:], op0=ALU.mult, op1=ALU.mult,
                                                                      accum_out=gk_all[:, t, k:k + 1]),
                          r=['f_oh' + B_, 'f_gt' + B_], w=['f_jk' + B_, 'f_gk'])
                for k in range(4):
                    kb.ind('pool', ['f_hb' + B_, dk], ['s_hg'], out=hg[:, :],
                           out_offset=bass.IndirectOffsetOnAxis(ap=dest_all[:, t * 4 + k:t * 4 + k + 1], axis=0),
                           in_=hbs[b][:, :], in_offset=None, bounds_check=cst['breg'], oob_is_err=False)
            kb.barrier()
        with ExitStack() as s3:
            hgT = [sbt(nc, s3, "f_hgT%d" % i, [128, 8, C], BF16) for i in range(2)]
            hgb = [sbt(nc, s3, "f_hgb%d" % i, [128, D], BF16) for i in range(2)]
            actT = sbt(nc, s3, "f_actT", [128, 8, C], BF16)
            w1t = [sbt(nc, s3, "f_w1%d" % i, [128, 8, 512], BF16) for i in range(2)]
            w2t = [sbt(nc, s3, "f_w2%d" % i, [128, 8, D], BF16) for i in range(2)]
            b1t = [sbt(nc, s3, "f_b1%d" % i, [128, 16], F32) for i in range(2)]
            b2t = [sbt(nc, s3, "f_b2%d" % i, [1, D], BF16) for i in range(2)]
            onesb = sbt(nc, s3, "f_onesb", [1, 128], BF16)
            gl = [sbt(nc, s3, "f_gl%d" % i, [128, 512], F32) for i in range(2)]
            sg = [sbt(nc, s3, "f_sg%d" % i, [128, 512], F32) for i in range(2)]
            li = [sbt(nc, s3, "f_li%d" % i, [128, 512], F32) for i in range(2)]
            yrow = [sbt(nc, s3, "f_yr%d" % i, [128, D], F32) for i in range(2)]
            PTr = PsRot(kb, nc, s3, 2, "f_ptr", dt=BF16, shape=(128, 8, 128))
            PG = PsRot(kb, nc, s3, 4, "f_pg")
            PY = PsRot(kb, nc, s3, 2, "f_py")
            kb.op('dve', lambda e: e.tensor_copy(out=onesb[:], in_=cst['ones_f'][0:1, :]), r=['cst'], w=['f_onesb'])
            W1, W2 = A['exp_w1'], A['exp_w2']
            gcount = 0
            u = 0
            yc_ = 0
            for ex in range(NE):
                eb = ex % 2
                EB = str(eb)
                kb.dma('sp', b1t[eb][:], A['exp_b1l'][l, ex], w=['f_b1' + EB])
                kb.dma('pool', b2t[eb][:], A['exp_b2'][l, ex:ex + 1, :], w=['f_b2' + EB])
                kb.dma('pool', w2t[eb][:], W2[l, ex].rearrange("(kc p) n -> p kc n", p=128), w=['f_w2' + EB],
                       max_dma_last_dim=4096)
                for blk in range(NB):
                    hb_ = blk % 2
                    kb.dma('sp', hgb[hb_][:], hg[ex * C + blk * 128:ex * C + (blk + 1) * 128, :], r=['s_hg'],
                           w=['f_hgb%d' % hb_])
                    ptr, ptrk = PTr.next()
                    for kc in range(8):
                        kb.op('pe', lambda e, kc=kc: e.transpose(out=ptr[:, kc, :], in_=hgb[hb_][:, kc * 128:(kc + 1) * 128],
                                                                 identity=cst['ident_b'][:, :]),
                              r=['f_hgb%d' % hb_, 'cst'], w=[ptrk])
                    cp(kb, 'act' if blk % 2 == 0 else 'dve', hgT[eb][:, :, blk * 128:(blk + 1) * 128], ptr[:], [ptrk],
                       ['f_hgT' + EB])
                for g in range(4):
                    wb = gcount % 2
                    gcount += 1
                    kb.dma('pool', w1t[wb][:, :, 0:256],
                           W1[l, ex, :, g * 256:(g + 1) * 256].rearrange("(kc p) n -> p kc n", p=128), w=['f_w1%d' % wb])
                    kb.dma('pool', w1t[wb][:, :, 256:512],
                           W1[l, ex, :, DFF + g * 256:DFF + (g + 1) * 256].rearrange("(kc p) n -> p kc n", p=128),
                           w=['f_w1%d' % wb])
                    for fl in range(2):
                        fc = g * 2 + fl
                        for scn in range(C // 512):
                            tb = u % 2
                            u += 1
                            TB = str(tb)
                            sl = slice(scn * 512, (scn + 1) * 512)
                            pg, pgk = PG.next()
                            for kc in range(8):
                                kb.op('pe', lambda e, kc=kc: e.matmul(pg[:], lhsT=w1t[wb][:, kc, fl * 128:(fl + 1) * 128],
                                                                      rhs=hgT[eb][:, kc, sl], start=(kc == 0),
                                                                      stop=(kc == 7)),
                                      r=['f_w1%d' % wb, 'f_hgT' + EB], w=[pgk])
                            pl, plk = PG.next()
                            for kc in range(8):
                                kb.op('pe', lambda e, kc=kc: e.matmul(
                                    pl[:], lhsT=w1t[wb][:, kc, 256 + fl * 128:256 + (fl + 1) * 128],
                                    rhs=hgT[eb][:, kc, sl], start=(kc == 0), stop=(kc == 7)),
                                    r=['f_w1%d' % wb, 'f_hgT' + EB], w=[plk])
                            kb.op('dve', lambda e: e.tensor_scalar(out=gl[tb][:], in0=pg[:], scalar1=b1t[eb][:, fc:fc + 1],
                                                                   scalar2=7.0, op0=ALU.add, op1=ALU.min),
                                  r=[pgk, 'f_b1' + EB], w=['f_gl' + TB])
                            kb.op('act', lambda e: e.activation(out=sg[tb][:], in_=gl[tb][:], func=AF.Sigmoid,
                                                                scale=1.702), r=['f_gl' + TB], w=['f_sg' + TB])
                            kb.op('dve', lambda e: e.tensor_scalar(out=li[tb][:], in0=pl[:],
                                                                   scalar1=b1t[eb][:, 8 + fc:9 + fc], scalar2=7.0,
                                                                   op0=ALU.add, op1=ALU.min),
                                  r=[plk, 'f_b1' + EB], w=['f_li' + TB])
                            kb.op('dve', lambda e: e.tensor_scalar(out=li[tb][:], in0=li[tb][:], scalar1=-7.0,
                                                                   scalar2=1.0, op0=ALU.max, op1=ALU.add),
                                  r=['f_li' + TB], w=['f_li' + TB])
                            kb.op('pool', lambda e: e.tensor_tensor(out=gl[tb][:], in0=gl[tb][:], in1=sg[tb][:],
                                                                    op=ALU.mult),
                                  r=['f_gl' + TB, 'f_sg' + TB], w=['f_gl' + TB])
                            kb.op('pool', lambda e: e.tensor_tensor(out=actT[:, fc, sl], in0=gl[tb][:], in1=li[tb][:],
                                                                    op=ALU.mult),
                                  r=['f_gl' + TB, 'f_li' + TB], w=['f_actT'])
                for blk in range(NB):
                    yb_ = yc_ % 2
                    yc_ += 1
                    for half in range(2):
                        py, pyk = PY.next()
                        for fc in range(8):
                            kb.op('pe', lambda e, fc=fc: e.matmul(py[:], lhsT=actT[:, fc, blk * 128:(blk + 1) * 128],
                                                                  rhs=w2t[eb][:, fc, half * 512:(half + 1) * 512],
                                                                  start=(fc == 0), stop=False),
                                  r=['f_actT', 'f_w2' + EB], w=[pyk])
                        kb.op('pe', lambda e: e.matmul(py[:], lhsT=onesb[0:1, :],
                                                       rhs=b2t[eb][0:1, half * 512:(half + 1) * 512],
                                                       start=False, stop=True), r=['f_onesb', 'f_b2' + EB], w=[pyk])
                        cp(kb, 'act' if half == 0 else 'dve', yrow[yb_][:, half * 512:(half + 1) * 512], py[:], [pyk],
                           ['f_yr%d' % yb_])
                    kb.dma('sp', yg[ex * C + blk * 128:ex * C + (blk + 1) * 128, :], yrow[yb_][:], r=['f_yr%d' % yb_],
                           w=['s_yg'])
            kb.barrier()
        with ExitStack() as s4:
            rows = [sbt(nc, s4, "f_row%d" % i, [128, D], F32) for i in range(4)]
            accs = [sbt(nc, s4, "f_acc%d" % i, [128, D], F32) for i in range(2)]
            xt = [sbt(nc, s4, "f_x%d" % i, [128, D], F32) for i in range(2)]
            lng = sbt(nc, s4, "f_lng", [128, D], F32)
            lnb = sbt(nc, s4, "f_lnb", [128, D], F32)
            lb = [(sbt(nc, s4, "f_lst%d" % i, [128, 2, 6], F32), sbt(nc, s4, "f_lmv%d" % i, [128, 2], F32),
                   sbt(nc, s4, "f_lrs%d" % i, [128, 1], F32), sbt(nc, s4, "f_lnb%d" % i, [128, 1], F32))
                  for i in range(2)]
            kb.dma('sp', lng[:], A['ln2g_bc'][l], w=['lngb'])
            kb.dma('sp', lnb[:], A['ln2b_bc'][l], w=['lngb'])
            for T in range(NT):
                xb = T % 2
                XB = str(xb)
                kb.dma('sp', xt[xb][:], x_src[T * 128:(T + 1) * 128, :], r=['xsrc5'], w=['f_x' + XB])
                for k in range(4):
                    kb.ind('pool', ['s_yg', 'f_dest%d' % T], ['f_row%d' % k], out=rows[k][:, :], out_offset=None,
                           in_=yg[:, :], in_offset=bass.IndirectOffsetOnAxis(ap=dest_all[:, T * 4 + k:T * 4 + k + 1], axis=0),
                           bounds_check=cst['breg'], oob_is_err=False)
                kb.op('dve', lambda e: e.tensor_scalar(out=accs[xb][:], in0=rows[0][:], scalar1=gk_all[:, T, 0:1],
                                                       scalar2=None, op0=ALU.mult),
                      r=['f_row0', 'f_gk'], w=['f_acc' + XB])
                for k in range(1, 4):
                    kb.op('dve', lambda e, k=k: e.scalar_tensor_tensor(out=accs[xb][:], in0=rows[k][:],
                                                                      scalar=gk_all[:, T, k:k + 1], in1=accs[xb][:],
                                                                      op0=ALU.mult, op1=ALU.add),
                          r=['f_row%d' % k, 'f_gk', 'f_acc' + XB], w=['f_acc' + XB])
                kb.op('pool', lambda e: e.tensor_tensor(out=accs[xb][:], in0=accs[xb][:], in1=mod_bc[:, 5 * D:6 * D],
                                                        op=ALU.mult), r=['f_acc' + XB, 'mod_bc'], w=['f_acc' + XB])
                kb.op('dve', lambda e: e.scalar_tensor_tensor(out=xt[xb][:], in0=xt[xb][:], scalar=ALPHA,
                                                              in1=accs[xb][:], op0=ALU.mult, op1=ALU.add),
                      r=['f_x' + XB, 'f_acc' + XB], w=['f_x' + XB])
                ln_affine_store(kb, nc, xt[xb], 'f_x' + XB, lb[xb], lng[:], lnb[:],
                                x_dst[T * 128:(T + 1) * 128, :], 'f' + XB, 'xdst5')
            kb.barrier()
```

```python
import numpy as np
from contextlib import ExitStack
import concourse.bass as bass
import concourse.mybir as mybir
from concourse.bass_utils import run_bass_kernel_spmd
from concourse.alu_op_type import AluOpType as ALU

F32, BF16 = mybir.dt.float32, mybir.dt.bfloat16
I32 = mybir.dt.int32
CAP = 1536
BIG = 1.0e6
AF = mybir.ActivationFunctionType
AX = mybir.AxisListType

D = 1024
S = 4096
NT = S // 128
DEPTH = 4
INW = 6408
NE = 32
DFF = 1024
ALPHA = (2 * DEPTH) ** 0.25
NEG = -30000.0
DBG = {}


class KB:
    def __init__(self, nc, es, ndma=48):
        self.nc = nc
        self.eng = {'pe': nc.tensor, 'dve': nc.vector, 'act': nc.scalar, 'pool': nc.gpsimd, 'sp': nc.sync}
        self.sems = []
        self.psid = {}
        for e in self.eng:
            self.psid[e] = len(self.sems)
            self.sems.append(es.enter_context(nc.semaphore("ps_" + e)))
        self.cnt = {e: 0 for e in self.eng}
        self.dsid = []
        for i in range(ndma):
            self.dsid.append(len(self.sems))
            self.sems.append(es.enter_context(nc.semaphore("ds%d" % i)))
        self.dcum = [0] * ndma
        self.dnext = 0
        self.seen = {e: {} for e in self.eng}
        self.lastw = {}
        self.reads = {}
        self.nins = 0
        self.excl = set()

    def need(self, E, tok):
        sid, val, _ = tok
        if self.seen[E].get(sid, 0) < val:
            self.eng[E].wait_ge(self.sems[sid], val)
            self.seen[E][sid] = val
            if 'log' in DBG:
                DBG['log'].append("%s WAIT sem%d>=%d" % (E, sid, val))

    def _deps(self, E, r, w, isdma):
        for k in r:
            t = self.lastw.get(k)
            if t is not None:
                self.need(E, t)
            if k in self.excl:
                for t in self.reads.get(k, {}).values():
                    if t[2] != E:
                        self.need(E, t)
        for k in w:
            t = self.lastw.get(k)
            if t is not None and (isdma or t[2] != E):
                self.need(E, t)
            for t in self.reads.get(k, {}).values():
                if isdma or t[2] != E:
                    self.need(E, t)

    def _commit(self, tok, r, w):
        for k in r:
            self.reads.setdefault(k, {})[tok[0]] = tok
        for k in w:
            self.lastw[k] = tok
            self.reads[k] = {}

    def op(self, E, fn, r=(), w=()):
        self._deps(E, r, w, False)
        ins = fn(self.eng[E])
        ins.then_inc(self.sems[self.psid[E]], 1)
        self.cnt[E] += 1
        self.nins += 1
        tok = (self.psid[E], self.cnt[E], E)
        if 'log' in DBG:
            DBG['log'].append("%s OP#%d r=%s w=%s" % (E, self.cnt[E], list(r), list(w)))
        self._commit(tok, r, w)
        return tok

    def dma(self, Q, out, in_, r=(), w=(), **kw):
        self._deps(Q, r, w, True)
        i = self.dnext
        self.dnext = (i + 1) % len(self.dsid)
        if self.dcum[i] > 0:
            self.need(Q, (self.dsid[i], self.dcum[i], None))
        self.eng[Q].dma_start(out=out, in_=in_, **kw).then_inc(self.sems[self.dsid[i]], 16)
        self.dcum[i] += 16
        self.nins += 1
        tok = (self.dsid[i], self.dcum[i], None)
        if 'log' in DBG:
            DBG['log'].append("%s DMA sem%d->%d r=%s w=%s" % (Q, self.dsid[i], self.dcum[i], list(r), list(w)))
        self._commit(tok, r, w)
        return tok

    def ind(self, Q, r, w, **kw):
        self._deps(Q, r, w, True)
        i = self.dnext
        self.dnext = (i + 1) % len(self.dsid)
        if self.dcum[i] > 0:
            self.need(Q, (self.dsid[i], self.dcum[i], None))
        self.eng[Q].indirect_dma_start(**kw).then_inc(self.sems[self.dsid[i]], 16)
        self.dcum[i] += 16
        self.nins += 1
        tok = (self.dsid[i], self.dcum[i], None)
        self._commit(tok, r, w)
        return tok

    def barrier(self):
        for E in self.eng:
            for F in self.eng:
                if F != E and self.cnt[F] > 0:
                    self.need(E, (self.psid[F], self.cnt[F], F))
            for i, s in enumerate(self.dsid):
                if self.dcum[i] > 0:
                    self.need(E, (s, self.dcum[i], None))


_UID = [0]


def _uname(name):
    _UID[0] += 1
    return "%s_u%d" % (name, _UID[0])


def sbt(nc, st, name, shape, dt):
    return st.enter_context(nc.sbuf_tensor(_uname(name), shape, dt))


def pst(nc, st, name, shape, dt):
    return st.enter_context(nc.psum_tensor(_uname(name), shape, dt))


def stage_mod(kb, nc, l, A, mod_bc, cst):
    with ExitStack() as st:
        cc = sbt(nc, st, "m_cc", [128, 8], F32)
        cond = sbt(nc, st, "m_cond", [128, 8], F32)
        crep = sbt(nc, st, "m_crep", [128, 8, 128], F32)
        brow = sbt(nc, st, "m_brow", [1, 6144], F32)
        wa = [sbt(nc, st, "m_wa%d" % i, [128, 8, 512], F32) for i in range(2)]
        ps = [pst(nc, st, "m_ps%d" % i, [128, 512], F32) for i in range(2)]
        kb.dma('sp', cc[:], A['c_col'][:, :], w=['m_cc'])
        kb.dma('sp', brow[:], A['ada_b'][l:l + 1, :], w=['m_brow'])
        kb.op('act', lambda e: e.activation(out=cond[:], in_=cc[:], func=AF.Silu), r=['m_cc'], w=['m_cond'])
        for kc in range(8):
            kb.op('dve', lambda e, kc=kc: e.tensor_scalar(out=crep[:, kc, :], in0=cst['ones_f'][:, :],
                                                         scalar1=cond[:, kc:kc + 1], scalar2=None, op0=ALU.mult),
                  r=['m_cond', 'cst'], w=['m_crep'])
        for j in range(12):
            b = j % 2
            kb.dma('sp' if j % 2 == 0 else 'pool', wa[b][:],
                   A['ada_w'][l, :, j * 512:(j + 1) * 512].rearrange("(kc p) n -> p kc n", p=128),
                   w=['m_wa%d' % b])
            for kc in range(8):
                kb.op('pe', lambda e, kc=kc, b=b: e.matmul(ps[b][:], lhsT=crep[:, kc, :], rhs=wa[b][:, kc, :],
                                                           start=(kc == 0), stop=False),
                      r=['m_crep', 'm_wa%d' % b], w=['m_ps%d' % b])
            kb.op('pe', lambda e, b=b, j=j: e.matmul(ps[b][:], lhsT=cst['ones_f'][0:1, :],
                                                     rhs=brow[0:1, j * 512:(j + 1) * 512], start=False, stop=True),
                  r=['m_brow', 'cst'], w=['m_ps%d' % b])
            addone = 1.0 if (j // 2) in (1, 2, 4, 5) else 0.0
            kb.op('act', lambda e, b=b, j=j, addone=addone: e.activation(
                out=mod_bc[:, j * 512:(j + 1) * 512], in_=ps[b][:], func=AF.Identity, bias=addone, scale=1.0),
                r=['m_ps%d' % b], w=['mod_bc'])
        kb.barrier()


def ln_mod_tile(kb, nc, xt, xk, bufs, sc_ap, sh_ap, h_out, hk, sfx):
    stt, mv, rstd, nb, xn = bufs
    kb.op('dve', lambda e: e.bn_stats(out=stt[:, 0, :], in_=xt[:, 0:512]), r=[xk], w=['ln_st' + sfx])
    kb.op('dve', lambda e: e.bn_stats(out=stt[:, 1, :], in_=xt[:, 512:1024]), r=[xk], w=['ln_st' + sfx])
    kb.op('dve', lambda e: e.bn_aggr(out=mv[:], in_=stt[:].rearrange("p a b -> p (a b)")),
          r=['ln_st' + sfx], w=['ln_mv' + sfx])
    kb.op('act', lambda e: e.activation(out=rstd[:], in_=mv[:, 1:2], func=AF.Sqrt, bias=1e-5, scale=1.0),
          r=['ln_mv' + sfx], w=['ln_rstd' + sfx])
    kb.op('dve', lambda e: e.reciprocal(out=rstd[:], in_=rstd[:]), r=['ln_rstd' + sfx], w=['ln_rstd' + sfx])
    kb.op('dve', lambda e: e.scalar_tensor_tensor(out=nb[:], in0=mv[:, 0:1], scalar=-1.0, in1=rstd[:],
                                                  op0=ALU.mult, op1=ALU.mult),
          r=['ln_mv' + sfx, 'ln_rstd' + sfx], w=['ln_nb' + sfx])
    kb.op('act', lambda e: e.activation(out=xn[:], in_=xt[:], func=AF.Identity, bias=nb[:], scale=rstd[:]),
          r=[xk, 'ln_nb' + sfx, 'ln_rstd' + sfx], w=['ln_xn' + sfx])
    kb.op('dve', lambda e: e.tensor_tensor(out=xn[:], in0=xn[:], in1=sc_ap, op=ALU.mult),
          r=['ln_xn' + sfx, 'mod_bc'], w=['ln_xn' + sfx])
    kb.op('dve', lambda e: e.tensor_tensor(out=h_out, in0=xn[:], in1=sh_ap, op=ALU.add),
          r=['ln_xn' + sfx, 'mod_bc'], w=[hk])


def build_hT(kb, nc, st, x_src, mod_bc, sc_off, sh_off, hT, cst, pfx):
    xts = [sbt(nc, st, pfx + "xt%d" % i, [128, D], F32) for i in range(2)]
    hbs = [sbt(nc, st, pfx + "hb%d" % i, [128, D], BF16) for i in range(2)]
    bufs = []
    for i in range(2):
        bufs.append((sbt(nc, st, pfx + "st%d" % i, [128, 2, 6], F32), sbt(nc, st, pfx + "mv%d" % i, [128, 2], F32),
                     sbt(nc, st, pfx + "rs%d" % i, [128, 1], F32), sbt(nc, st, pfx + "nb%d" % i, [128, 1], F32),
                     sbt(nc, st, pfx + "xn%d" % i, [128, D], F32)))
    pT = [pst(nc, st, pfx + "pT%d" % i, [128, 8, 128], BF16) for i in range(2)]
    for t in range(NT):
        b = t % 2
        kb.dma('sp', xts[b][:], x_src[t * 128:(t + 1) * 128, :], r=[pfx + 'xsrc'], w=[pfx + 'xt%d' % b])
        ln_mod_tile(kb, nc, xts[b], pfx + 'xt%d' % b, bufs[b], mod_bc[:, sc_off:sc_off + D],
                    mod_bc[:, sh_off:sh_off + D], hbs[b][:], pfx + 'hb%d' % b, pfx + str(b))
        for kc in range(8):
            kb.op('pe', lambda e, kc=kc, b=b: e.transpose(out=pT[b][:, kc, :], in_=hbs[b][:, kc * 128:(kc + 1) * 128],
                                                          identity=cst['ident_b'][:, :]),
                  r=[pfx + 'hb%d' % b, 'cst'], w=[pfx + 'pT%d' % b])
        eng = 'act' if t % 2 == 0 else 'dve'
        if eng == 'act':
            kb.op('act', lambda e, b=b, t=t: e.copy(out=hT[:, :, t * 128:(t + 1) * 128], in_=pT[b][:]),
                  r=[pfx + 'pT%d' % b], w=['hT'])
        else:
            kb.op('dve', lambda e, b=b, t=t: e.tensor_copy(out=hT[:, :, t * 128:(t + 1) * 128], in_=pT[b][:]),
                  r=[pfx + 'pT%d' % b], w=['hT'])


def stage_proj(kb, nc, l, A, SC, x_src, mod_bc, cst):
    with ExitStack() as st:
        hT = sbt(nc, st, "hT", [128, 8, S], BF16)
        with ExitStack() as st2:
            build_hT(kb, nc, st2, x_src, mod_bc, 1 * D, 0, hT, cst, "p1_")
            kb.barrier()
        wts = [sbt(nc, st, "wt%d" % i, [128, 8, 512], BF16) for i in range(2)]
        raw = sbt(nc, st, "raw", [128, S + 4], F32)
        acc = sbt(nc, st, "acc", [128, S], F32)
        tmp = [sbt(nc, st, "tmp%d" % i, [128, 512], F32) for i in range(2)]
        gb = [sbt(nc, st, "gb%d" % i, [128, S], BF16) for i in range(2)]
        cw = sbt(nc, st, "cw", [128, 12, 4], F32)
        alog = sbt(nc, st, "alog", [128, 4], F32)
        dtb = sbt(nc, st, "dtb", [128, 4], F32)
        sm = sbt(nc, st, "sm", [128, 6, 4], F32)
        g_all = sbt(nc, st, "g_all", [128, NT, 4], F32)
        b_all = sbt(nc, st, "b_all", [128, NT, 4], F32)
        vb_all = sbt(nc, st, "vb_all", [128, NT, 128], BF16)
        zt = [sbt(nc, st, "zt%d" % i, [128, 512], F32) for i in range(2)]
        ps = [pst(nc, st, "pj_ps%d" % i, [128, 512], F32) for i in range(4)]
        kb.dma('sp', cw[:], A['conv_wl'][l], w=['cw'])
        kb.dma('sp', alog[:], A['alog_bc'][l], w=['alog'])
        kb.dma('sp', dtb[:], A['dtb_bc'][l], w=['dtb'])
        kb.op('act', lambda e: e.activation(out=alog[:], in_=alog[:], func=AF.Exp), r=['alog'], w=['alog'])
        kb.op('dve', lambda e: e.memset(raw[:, 0:4], 0.0), w=['raw'])
        W = A['w_in']
        state = {'g': 0, 'ps': 0, 'gb': 0}

        def load_group(pieces):
            b = state['g'] % 2
            state['g'] += 1
            for (off, c0, n) in pieces:
                kb.dma('pool', wts[b][:, :, off:off + n],
                       W[l, :, c0:c0 + n].rearrange("(kc p) n -> p kc n", p=128), w=['wt%d' % b])
            return b

        def fm_chunk(b, off, evac):
            for tc in range(8):
                p = state['ps'] % 4
                state['ps'] += 1
                for kc in range(8):
                    kb.op('pe', lambda e, kc=kc, p=p, tc=tc: e.matmul(
                        ps[p][:], lhsT=wts[b][:, kc, off:off + 128], rhs=hT[:, kc, tc * 512:(tc + 1) * 512],
                        start=(kc == 0), stop=(kc == 7)), r=['wt%d' % b, 'hT'], w=['pj_ps%d' % p])
                evac(tc, ps[p], 'pj_ps%d' % p)

        def conv_chunk(b, off, ch):
            def ev(tc, p, pk):
                eng = 'act' if tc % 2 == 0 else 'dve'
                if eng == 'act':
                    kb.op('act', lambda e: e.copy(out=raw[:, 4 + tc * 512: 4 + (tc + 1) * 512], in_=p[:]),
                          r=[pk], w=['raw'])
                else:
                    kb.op('dve', lambda e: e.tensor_copy(out=raw[:, 4 + tc * 512: 4 + (tc + 1) * 512], in_=p[:]),
                          r=[pk], w=['raw'])
            fm_chunk(b, off, ev)
            kb.op('dve', lambda e: e.tensor_scalar(out=acc[:], in0=raw[:, 1:1 + S], scalar1=cw[:, ch, 0:1],
                                                   scalar2=None, op0=ALU.mult), r=['raw', 'cw'], w=['acc'])
            for j in range(1, 4):
                kb.op('dve', lambda e, j=j: e.scalar_tensor_tensor(out=acc[:], in0=raw[:, 1 + j:1 + j + S],
                                                                  scalar=cw[:, ch, j:j + 1], in1=acc[:],
                                                                  op0=ALU.mult, op1=ALU.add),
                      r=['raw', 'cw', 'acc'], w=['acc'])
            kb.op('act', lambda e: e.activation(out=acc[:], in_=acc[:], func=AF.Silu), r=['acc'], w=['acc'])
            if ch < 8:
                kb.op('act', lambda e: e.activation(out=raw[:, 4:4 + S], in_=acc[:], func=AF.Square),
                      r=['acc'], w=['raw'])
                qs = (128 ** -0.5) if ch < 4 else 1.0
                for tc in range(8):
                    p = state['ps'] % 4
                    state['ps'] += 1
                    tb = tc % 2
                    kb.op('pe', lambda e, p=p, tc=tc: e.matmul(ps[p][:], lhsT=cst['ones_f'][:, :],
                                                               rhs=raw[:, 4 + tc * 512:4 + (tc + 1) * 512],
                                                               start=True, stop=True),
                          r=['raw', 'cst'], w=['pj_ps%d' % p])
                    kb.op('act', lambda e, p=p, tb=tb: e.activation(out=tmp[tb][:], in_=ps[p][:], func=AF.Sqrt,
                                                                    bias=1e-6, scale=1.0),
                          r=['pj_ps%d' % p], w=['tmp%d' % tb])
                    kb.op('dve', lambda e, tb=tb: e.reciprocal(out=tmp[tb][:], in_=tmp[tb][:]),
                          r=['tmp%d' % tb], w=['tmp%d' % tb])
                    kb.op('dve', lambda e, tb=tb, tc=tc: e.scalar_tensor_tensor(
                        out=acc[:, tc * 512:(tc + 1) * 512], in0=acc[:, tc * 512:(tc + 1) * 512], scalar=qs,
                        in1=tmp[tb][:], op0=ALU.mult, op1=ALU.mult), r=['tmp%d' % tb, 'acc'], w=['acc'])
            kb.dma('sp', SC['qkv'][ch], acc[:], r=['acc'], w=['s_qkv'])

        for gi in range(3):
            b = load_group([(0, gi * 512, 512)])
            for j in range(4):
                conv_chunk(b, j * 128, gi * 4 + j)

        b = load_group([(0, 1536, 512)])
        for t in range(NT):
            p = state['ps'] % 4
            state['ps'] += 1
            for kc in range(8):
                kb.op('pe', lambda e, kc=kc, p=p, t=t: e.matmul(ps[p][:], lhsT=hT[:, kc, t * 128:(t + 1) * 128],
                                                                rhs=wts[b][:, kc, 0:512], start=(kc == 0),
                                                                stop=(kc == 7)),
                      r=['wt%d' % b, 'hT'], w=['pj_ps%d' % p])
            zb = t % 2
            kb.op('act', lambda e, p=p, zb=zb: e.activation(out=zt[zb][:], in_=ps[p][:], func=AF.Silu),
                  r=['pj_ps%d' % p], w=['zt%d' % zb])
            kb.dma('sp', SC['sz'][t * 128:(t + 1) * 128, :], zt[zb][:], r=['zt%d' % zb], w=['s_sz'])

        b = load_group([(0, 2048, 8), (128, 2568, 256)])
        for t in range(NT):
            p = state['ps'] % 4
            state['ps'] += 1
            for kc in range(8):
                kb.op('pe', lambda e, kc=kc, p=p, t=t: e.matmul(ps[p][:, 0:8], lhsT=hT[:, kc, t * 128:(t + 1) * 128],
                                                                rhs=wts[b][:, kc, 0:8], start=(kc == 0),
                                                                stop=(kc == 7)),
                      r=['wt%d' % b, 'hT'], w=['pj_ps%d' % p])
            pk = 'pj_ps%d' % p
            P = ps[p]
            kb.op('dve', lambda e, P=P: e.tensor_tensor(out=sm[:, 0, :], in0=P[:, 0:4], in1=dtb[:], op=ALU.add),
                  r=[pk, 'dtb'], w=['sm0'])
            kb.op('dve', lambda e: e.scalar_tensor_tensor(out=sm[:, 1, :], in0=sm[:, 0, :], scalar=-1.0,
                                                          in1=sm[:, 0, :], op0=ALU.mult, op1=ALU.min),
                  r=['sm0'], w=['sm1'])
            kb.op('act', lambda e: e.activation(out=sm[:, 2, :], in_=sm[:, 1, :], func=AF.Exp, scale=1.0),
                  r=['sm1'], w=['sm2'])
            kb.op('act', lambda e: e.activation(out=sm[:, 3, :], in_=sm[:, 2, :], func=AF.Ln, bias=1.0, scale=1.0),
                  r=['sm2'], w=['sm3'])
            kb.op('dve', lambda e: e.scalar_tensor_tensor(out=sm[:, 4, :], in0=sm[:, 0, :], scalar=0.0,
                                                          in1=sm[:, 3, :], op0=ALU.max, op1=ALU.add),
                  r=['sm0', 'sm3'], w=['sm4'])
            kb.op('dve', lambda e, t=t: e.scalar_tensor_tensor(out=g_all[:, t, :], in0=sm[:, 4, :], scalar=-1.0,
                                                              in1=alog[:], op0=ALU.mult, op1=ALU.mult),
                  r=['sm4', 'alog'], w=['g_all'])
            kb.op('act', lambda e, P=P, t=t: e.activation(out=b_all[:, t, :], in_=P[:, 4:8], func=AF.Sigmoid),
                  r=[pk], w=['b_all'])
        kb.dma('sp', SC['g'].rearrange("(t p) h -> p t h", p=128), g_all[:], r=['g_all'], w=['s_g'])
        kb.dma('sp', SC['beta'].rearrange("(t p) h -> p t h", p=128), b_all[:], r=['b_all'], w=['s_beta'])
        for t in range(NT):
            p = state['ps'] % 4
            state['ps'] += 1
            for kc in range(8):
                kb.op('pe', lambda e, kc=kc, p=p, t=t: e.matmul(ps[p][:, 0:128], lhsT=hT[:, kc, t * 128:(t + 1) * 128],
                                                                rhs=wts[b][:, kc, 256:384], start=(kc == 0),
                                                                stop=(kc == 7)),
                      r=['wt%d' % b, 'hT'], w=['pj_ps%d' % p])
            kb.op('dve', lambda e, p=p, t=t: e.tensor_copy(out=vb_all[:, t, :], in_=ps[p][:, 0:128]),
                  r=['pj_ps%d' % p], w=['vb_all'])
        kb.dma('sp', SC['vb'].rearrange("(t p) d -> p t d", p=128), vb_all[:], r=['vb_all'], w=['s_vb'])

        def bf_chunk(b, off, dst, fn=None):
            g = state['gb'] % 2
            state['gb'] += 1

            def ev(tc, p, pk):
                if fn is not None:
                    kb.op('act', lambda e: e.activation(out=gb[g][:, tc * 512:(tc + 1) * 512], in_=p[:], func=fn),
                          r=[pk], w=['gb%d' % g])
                elif tc % 2 == 0:
                    kb.op('act', lambda e: e.copy(out=gb[g][:, tc * 512:(tc + 1) * 512], in_=p[:]),
                          r=[pk], w=['gb%d' % g])
                else:
                    kb.op('dve', lambda e: e.tensor_copy(out=gb[g][:, tc * 512:(tc + 1) * 512], in_=p[:]),
                          r=[pk], w=['gb%d' % g])
            fm_chunk(b, off, ev)
            kb.dma('sp', dst, gb[g][:], r=['gb%d' % g], w=['s_misc'])

        bf_chunk(b, 128, SC['kb'])
        pieces = []
        for j in range(4):
            pieces.append((j * 128, 2056 + j * 64, 64))
            pieces.append((j * 128 + 64, 2056 + (j + 4) * 64, 64))
        b = load_group(pieces)
        for j in range(4):
            bf_chunk(b, j * 128, SC['qb'][j])
        b = load_group([(0, 2824, 512)])
        for j in range(4):
            def ev(tc, p, pk):
                if tc % 2 == 0:
                    kb.op('act', lambda e: e.copy(out=acc[:, tc * 512:(tc + 1) * 512], in_=p[:]), r=[pk], w=['acc'])
                else:
                    kb.op('dve', lambda e: e.tensor_copy(out=acc[:, tc * 512:(tc + 1) * 512], in_=p[:]),
                          r=[pk], w=['acc'])
            fm_chunk(b, j * 128, ev)
            kb.dma('sp', SC['uc'][j], acc[:], r=['acc'], w=['s_uc'])
        for gi in range(6):
            b = load_group([(0, 3336 + gi * 512, 512)])
            for j in range(4):
                bf_chunk(b, j * 128, SC['gate'][gi * 4 + j], fn=AF.Sigmoid)
        kb.barrier()


def alloc_scratch(nc):
    SC = {}
    SC['qkv'] = nc.dram_tensor("s_qkv", [12, 128, S], F32).ap()
    SC['sz'] = nc.dram_tensor("s_sz", [S, 512], F32).ap()
    SC['g'] = nc.dram_tensor("s_g", [S, 4], F32).ap()
    SC['beta'] = nc.dram_tensor("s_beta", [S, 4], F32).ap()
    SC['qb'] = nc.dram_tensor("s_qb", [4, 128, S], BF16).ap()
    SC['kb'] = nc.dram_tensor("s_kb", [128, S], BF16).ap()
    SC['vb'] = nc.dram_tensor("s_vb", [S, 128], BF16).ap()
    SC['uc'] = nc.dram_tensor("s_uc", [4, 128, S], F32).ap()
    SC['gate'] = nc.dram_tensor("s_gate", [24, 128, S], BF16).ap()
    return SC


def cp(kb, eng, out, in_, r, w):
    if eng == 'act':
        return kb.op('act', lambda e: e.copy(out=out, in_=in_), r=r, w=w)
    return kb.op(eng, lambda e: e.tensor_copy(out=out, in_=in_), r=r, w=w)


class PsRot:
    def __init__(self, kb, nc, st, n, pfx, dt=F32, shape=(128, 512)):
        self.t = [pst(nc, st, "%s%d" % (pfx, i), list(shape), dt) for i in range(n)]
        self.k = ["%s%d" % (pfx, i) for i in range(n)]
        self.i = 0
        kb.excl.update(self.k)

    def next(self):
        i = self.i
        self.i = (i + 1) % len(self.t)
        return self.t[i], self.k[i]


def stage_gdn(kb, nc, l, A, SC, cst):
    ones, ident = cst['ones_f'], cst['ident_f']
    with ExitStack() as st:
        gm = sbt(nc, st, "g_gm", [128, 6, 128], F32)
        kb.dma('sp', gm[:], A['gm'][:, :, :], w=['cst'])
        g_all = sbt(nc, st, "g_gall", [128, 128], F32)
        b_all = sbt(nc, st, "g_ball", [128, 128], F32)
        gc = sbt(nc, st, "g_gc", [128, 128], F32)
        ngc = sbt(nc, st, "g_ngc", [128, 128], F32)
        egc = sbt(nc, st, "g_egc", [128, 128], F32)
        ekl = sbt(nc, st, "g_ekl", [128, 128], F32)
        gl0e = sbt(nc, st, "g_gl0e", [128, 128], F32)
        gl1e = sbt(nc, st, "g_gl1e", [128, 128], F32)
        negb = sbt(nc, st, "g_negb", [128, 128], F32)
        nw = sbt(nc, st, "g_nw", [128, 128], F32)
        pst0 = ExitStack()
        PS = PsRot(kb, nc, pst0, 4, "g_ps")
        kb.dma('sp', g_all[:].rearrange("p (t h) -> p t h", h=4), SC['g'].rearrange("(t p) h -> p t h", p=128),
               r=['s_g'], w=['g_gall'])
        kb.dma('sp', b_all[:].rearrange("p (t h) -> p t h", h=4), SC['beta'].rearrange("(t p) h -> p t h", p=128),
               r=['s_beta'], w=['g_ball'])
        kb.dma('sp', nw[:], A['gnw_bc'][l], w=['g_nw'])
        p, pk = PS.next()
        kb.op('pe', lambda e: e.matmul(p[:, 0:128], lhsT=gm[:, 0, :], rhs=g_all[:], start=True, stop=True),
              r=['g_gall', 'cst'], w=[pk])
        cp(kb, 'dve', gc[:], p[:, 0:128], [pk], ['g_gc'])
        kb.op('act', lambda e: e.activation(out=egc[:], in_=gc[:], func=AF.Exp), r=['g_gc'], w=['g_egc'])
        kb.op('dve', lambda e: e.tensor_scalar(out=ngc[:], in0=gc[:], scalar1=-1.0, scalar2=None, op0=ALU.mult),
              r=['g_gc'], w=['g_ngc'])
        p2, pk2 = PS.next()
        kb.op('pe', lambda e: e.matmul(p2[:, 0:128], lhsT=gm[:, 1, :], rhs=g_all[:], start=True, stop=True),
              r=['g_gall', 'cst'], w=[pk2])
        kb.op('dve', lambda e: e.tensor_tensor(out=ekl[:], in0=p2[:, 0:128], in1=gc[:], op=ALU.subtract),
              r=[pk2, 'g_gc'], w=['g_ekl'])
        kb.op('act', lambda e: e.activation(out=ekl[:], in_=ekl[:], func=AF.Exp), r=['g_ekl'], w=['g_ekl'])
        for idx, dst, dk in ((2, gl0e, 'g_gl0e'), (3, gl1e, 'g_gl1e')):
            p3, pk3 = PS.next()
            kb.op('pe', lambda e, p3=p3, idx=idx: e.matmul(p3[:, 0:128], lhsT=gm[:, idx, :], rhs=g_all[:],
                                                           start=True, stop=True), r=['g_gall', 'cst'], w=[pk3])
            kb.op('act', lambda e, p3=p3, dst=dst: e.activation(out=dst[:], in_=p3[:, 0:128], func=AF.Exp),
                  r=[pk3], w=[dk])
        kb.op('dve', lambda e: e.tensor_scalar(out=negb[:], in0=b_all[:], scalar1=-1.0, scalar2=None, op0=ALU.mult),
              r=['g_ball'], w=['g_negb'])

        kb.barrier()
        pst0.close()
        HT = 1024
        ecnt = [0]

        def mm(out_p, pk, lhsT, rhs, r, start=True, stop=True):
            return kb.op('pe', lambda e: e.matmul(out_p, lhsT=lhsT, rhs=rhs, start=start, stop=stop), r=r, w=[pk])

        names = ['ke', 'kg', 'vtok', 'dg', 'tmpm', 'decT', 'EB', 'qg', 'attnT', 'PT0', 'P0', 'PT1', 'P1', 'GT0', 'GT1',
                 'bu', 'wT', 'vnA', 'vnB', 'Sa', 'Sb', 'otok', 'junk', 'ytok']

        def head_gen(h, ch):
            GP = 'g%d_' % ch
            qkvb = [sbt(nc, st, "g_qkv%d" % i, [128, 3, HT], F32) for i in range(2)]
            szb = sbt(nc, st, "g_sz", [128, NT, 128], F32)
            yaT = sbt(nc, st, "g_yaT", [128, S], BF16)
            B = {n: sbt(nc, st, "g_" + n, [128, 128], F32) for n in names}
            ss = sbt(nc, st, "g_ss", [128, 1], F32)
            PS = PsRot(kb, nc, st, 4, "g%d_ps" % ch)
            for h in (h, h + 2):
                kb.op('dve', lambda e: e.memset(B['vnA'][:], 0.0), w=[GP + 'vnA'])
                kb.op('dve', lambda e: e.memset(B['vnB'][:], 0.0), w=[GP + 'vnB'])
                kb.dma('pool', szb[:], SC['sz'][:, h * 128:(h + 1) * 128].rearrange("(t p) d -> p t d", p=128),
                       r=['s_sz'], w=[GP + 'sz'])
                kb.op('dve', lambda e: e.memset(B['Sa'][:], 0.0), w=[GP + 'Sa'])
                Scur, Snxt = 'Sa', 'Sb'
                for half in range(S // HT):
                    qb_ = half % 2
                    for i3 in range(3):
                        kb.dma('sp', qkvb[qb_][:, i3, :], SC['qkv'][i3 * 4 + h, :, half * HT:(half + 1) * HT],
                               r=['s_qkv'], w=[GP + 'qkv%d' % qb_])
                    qk = GP + 'qkv%d' % qb_
                    for tt in range(HT // 128):
                        t = half * (HT // 128) + tt
                        c = t * 4 + h
                        if 'gdn_tiles' in DBG and (h * 32 + t) >= DBG['gdn_tiles']:
                            continue
                        qT = qkvb[qb_][:, 0, tt * 128:(tt + 1) * 128]
                        kT = qkvb[qb_][:, 1, tt * 128:(tt + 1) * 128]
                        vT = qkvb[qb_][:, 2, tt * 128:(tt + 1) * 128]
                        sub = DBG.get('gdn_sub', 31)
                        pa, pka = PS.next()
                        if sub & 1:
                            kb.op('pe', lambda e: e.matmul(pa[:, 0:128], lhsT=kT, rhs=ident, start=True, stop=True),
                                  r=[qk, 'cst'], w=[pka])
                            yield
                        if sub & 2:
                            kb.op('act', lambda e: e.activation(out=B['ke'][:], in_=pa[:, 0:128], func=AF.Identity,
                                                                scale=egc[:, c:c + 1]),
                                  r=[pka, 'g_egc'], w=[GP + 'ke'])
                            yield
                        if sub & 4:
                            kb.op('dve', lambda e: e.tensor_scalar(out=B['kg'][:], in0=pa[:, 0:128],
                                                                   scalar1=ekl[:, c:c + 1], scalar2=None, op0=ALU.mult),
                                  r=[pka, 'g_ekl'], w=[GP + 'kg'])
                            yield
                        pb, pkb = PS.next()
                        if sub & 8:
                            kb.op('pe', lambda e: e.matmul(pb[:, 0:128], lhsT=vT, rhs=ident, start=True, stop=True),
                                  r=[qk, 'cst'], w=[pkb])
                            yield
                        if sub & 16:
                            cp(kb, 'act', B['vtok'][:], pb[:, 0:128], [pkb], [GP + 'vtok'])
                            yield
                        if DBG.get('gdn_step', 99) < 1:
                            continue
                        pc, pkc = PS.next()
                        mm(pc[:, 0:128], pkc, kT, kT, [qk])
                        yield
                        pd, pkd = PS.next()
                        mm(pd[:, 0:128], pkd, kT, qT, [qk])
                        yield
                        if DBG.get('gdn_step', 99) < 2:
                            continue
                        kb.op('dve', lambda e, c=c: e.tensor_scalar(out=B['dg'][:], in0=ident, scalar1=gc[:, c:c + 1],
                                                                    scalar2=None, op0=ALU.mult),
                              r=['cst', 'g_gc'], w=[GP + 'dg'])
                        yield
                        pe_, pke = PS.next()
                        mm(pe_[:, 0:128], pke, ones, B['dg'][:], ['cst', GP + 'dg'])
                        yield
                        kb.op('dve', lambda e, pe_=pe_: e.tensor_tensor(out=B['tmpm'][:], in0=pe_[:, 0:128],
                                                                        in1=gm[:, 4, :], op=ALU.add),
                              r=[pke, 'cst'], w=[GP + 'tmpm'])
                        yield
                        if DBG.get('gdn_step', 99) < 3:
                            continue
                        kb.op('act', lambda e, c=c: e.activation(out=B['decT'][:], in_=B['tmpm'][:], func=AF.Exp,
                                                                 bias=ngc[:, c:c + 1], scale=1.0),
                              r=[GP + 'tmpm', 'g_ngc'], w=[GP + 'decT'])
                        yield
                        kb.op('act', lambda e, pe_=pe_: e.activation(out=B['EB'][:], in_=pe_[:, 0:128], func=AF.Exp),
                              r=[pke], w=[GP + 'EB'])
                        yield
                        kb.op('dve', lambda e, qT=qT: e.tensor_tensor(out=B['qg'][:], in0=qT, in1=B['EB'][:], op=ALU.mult),
                              r=[qk, GP + 'EB'], w=[GP + 'qg'])
                        yield
                        kb.op('dve', lambda e, pd=pd: e.tensor_tensor(out=B['attnT'][:], in0=pd[:, 0:128],
                                                                      in1=B['decT'][:], op=ALU.mult),
                              r=[pkd, GP + 'decT'], w=[GP + 'attnT'])
                        yield
                        kb.op('dve', lambda e: e.tensor_tensor(out=B['tmpm'][:], in0=B['decT'][:], in1=gm[:, 5, :],
                                                               op=ALU.mult), r=[GP + 'decT', 'cst'], w=[GP + 'tmpm'])
                        yield
                        kb.op('dve', lambda e, pc=pc, c=c: e.scalar_tensor_tensor(
                            out=B['PT0'][:], in0=pc[:, 0:128], scalar=negb[:, c:c + 1], in1=B['tmpm'][:],
                            op0=ALU.mult, op1=ALU.mult), r=[pkc, 'g_negb', GP + 'tmpm'], w=[GP + 'PT0'])
                        yield
                        if DBG.get('gdn_step', 99) < 4:
                            continue
                        pf, pkf = PS.next()
                        kb.op('pe', lambda e, pf=pf: e.matmul(pf[:, 0:128], lhsT=B['PT0'][:], rhs=ident, start=True, stop=True),
                              r=[GP + 'PT0', 'cst'], w=[pkf])
                        yield
                        cp(kb, 'act', B['P0'][:], pf[:, 0:128], [pkf], [GP + 'P0'])
                        yield
                        kb.op('dve', lambda e: e.tensor_tensor(out=B['GT0'][:], in0=B['PT0'][:], in1=ident, op=ALU.add),
                              r=[GP + 'PT0', 'cst'], w=[GP + 'GT0'])
                        yield
                        if DBG.get('gdn_step', 99) < 5:
                            continue
                        Pc, PTc, Gc = 'P0', 'PT0', 'GT0'
                        for lv in range(1, 6):
                            Pn = 'P1' if Pc == 'P0' else 'P0'
                            PTn = 'PT1' if PTc == 'PT0' else 'PT0'
                            Gn = 'GT1' if Gc == 'GT0' else 'GT0'
                            p1, pk1 = PS.next()
                            mm(p1[:, 0:128], pk1, B[PTc][:], B[Pc][:], [GP + PTc, GP + Pc])
                            yield
                            cp(kb, 'act', B[Pn][:], p1[:, 0:128], [pk1], [GP + Pn])
                            yield
                            if lv < 5:
                                p2_, pk2_ = PS.next()
                                mm(p2_[:, 0:128], pk2_, B[Pc][:], B[PTc][:], [GP + PTc, GP + Pc])
                                yield
                                cp(kb, 'dve', B[PTn][:], p2_[:, 0:128], [pk2_], [GP + PTn])
                                yield
                            p3_, pk3_ = PS.next()
                            mm(p3_[:, 0:128], pk3_, B[Pn][:], B[Gc][:], [GP + Pn, GP + Gc])
                            yield
                            kb.op('dve', lambda e, p3_=p3_, Gn=Gn, Gc=Gc: e.tensor_tensor(
                                out=B[Gn][:], in0=p3_[:, 0:128], in1=B[Gc][:], op=ALU.add),
                                r=[pk3_, GP + Gc], w=[GP + Gn])
                            yield
                            Pc, PTc, Gc = Pn, PTn, Gn
                        if DBG.get('gdn_step', 99) < 6:
                            continue
                        G = B[Gc]
                        Gk = GP + Gc
                        pu, pku = PS.next()
                        mm(pu[:, 0:128], pku, G[:], B['vtok'][:], [Gk, GP + 'vtok'])
                        yield
                        kb.op('act', lambda e, pu=pu, c=c: e.activation(out=B['bu'][:], in_=pu[:, 0:128], func=AF.Identity,
                                                                        scale=b_all[:, c:c + 1]),
                              r=[pku, 'g_ball'], w=[GP + 'bu'])
                        yield
                        pw, pkw = PS.next()
                        mm(pw[:, 0:128], pkw, B['ke'][:], G[:], [Gk, GP + 'ke'])
                        yield
                        cp(kb, 'dve', B['wT'][:], pw[:, 0:128], [pkw], [GP + 'wT'])
                        yield
                        if DBG.get('gdn_step', 99) < 7:
                            continue
                        for ci, vn, gle, gk in ((0, 'vnA', gl0e, 'g_gl0e'), (1, 'vnB', gl1e, 'g_gl1e')):
                            lo, hi = ci * 64, ci * 64 + 64
                            Sc = B[Scur]
                            Sk = GP + Scur
                            p4, pk4 = PS.next()
                            mm(p4[:, 0:128], pk4, B['wT'][:], Sc[:], [GP + 'wT', Sk])
                            yield
                            kb.op('dve', lambda e, p4=p4, vn=vn, lo=lo, hi=hi, c=c: e.scalar_tensor_tensor(
                                out=B[vn][lo:hi, :], in0=p4[lo:hi, 0:128], scalar=negb[lo:hi, c:c + 1],
                                in1=B['bu'][lo:hi, :], op0=ALU.mult, op1=ALU.add),
                                r=[pk4, 'g_negb', GP + 'bu'], w=[GP + vn])
                            yield
                            p5, pk5 = PS.next()
                            mm(p5[:, 0:128], pk5, B['qg'][:], Sc[:], [GP + 'qg', Sk], start=True, stop=False)
                            yield
                            mm(p5[:, 0:128], pk5, B['attnT'][:], B[vn][:], [GP + 'attnT', GP + vn], start=False, stop=True)
                            yield
                            cp(kb, 'act', B['otok'][lo:hi, :], p5[lo:hi, 0:128], [pk5], [GP + 'otok'])
                            yield
                            p6, pk6 = PS.next()
                            mm(p6[:, 0:128], pk6, B['kg'][:], B[vn][:], [GP + 'kg', GP + vn])
                            yield
                            kb.op('dve', lambda e, p6=p6, Sc=Sc, gle=gle, c=c, Snxt=Snxt: e.scalar_tensor_tensor(
                                out=B[Snxt][:], in0=Sc[:], scalar=gle[:, c:c + 1], in1=p6[:, 0:128],
                                op0=ALU.mult, op1=ALU.add), r=[pk6, Sk, gk], w=[GP + Snxt])
                            yield
                            Scur, Snxt = Snxt, Scur
                        if DBG.get('gdn_step', 99) < 8:
                            continue
                        kb.op('act', lambda e: e.activation(out=B['junk'][:], in_=B['otok'][:], func=AF.Square,
                                                            accum_out=ss[:]), r=[GP + 'otok'], w=[GP + 'junk', GP + 'ss'])
                        yield
                        kb.op('act', lambda e: e.activation(out=ss[:], in_=ss[:], func=AF.Sqrt, bias=1e-6,
                                                            scale=1.0 / 128), r=[GP + 'ss'], w=[GP + 'ss'])
                        yield
                        kb.op('dve', lambda e: e.reciprocal(out=ss[:], in_=ss[:]), r=[GP + 'ss'], w=[GP + 'ss'])
                        yield
                        kb.op('dve', lambda e: e.scalar_tensor_tensor(out=B['ytok'][:], in0=B['otok'][:], scalar=ss[:],
                                                                      in1=nw[:], op0=ALU.mult, op1=ALU.mult),
                              r=[GP + 'otok', GP + 'ss', 'g_nw'], w=[GP + 'ytok'])
                        yield
                        kb.op('dve', lambda e, t=t: e.tensor_tensor(out=B['ytok'][:], in0=B['ytok'][:], in1=szb[:, t, :],
                                                                    op=ALU.mult), r=[GP + 'ytok', GP + 'sz'], w=[GP + 'ytok'])
                        yield
                        if DBG.get('gdn_step', 99) < 9:
                            continue
                        p7, pk7 = PS.next()
                        kb.op('pe', lambda e, p7=p7: e.matmul(p7[:, 0:128], lhsT=B['ytok'][:], rhs=ident, start=True, stop=True),
                              r=[GP + 'ytok', 'cst'], w=[pk7])
                        yield
                        cp(kb, 'act', yaT[:, t * 128:(t + 1) * 128], p7[:, 0:128], [pk7], [GP + 'yaT'])
                        yield
                        if 'gdn_dump' in DBG and h == 0 and t == 0:
                            for bi, n_ in enumerate(names):
                                kb.dma('sp', DBG['gdn_dump'][bi], B[n_][:], r=[GP + n_], w=['dump'])
                kb.dma('sp', SC['ya'][h], yaT[:], r=[GP + 'yaT'], w=['s_ya'])


        gens = [head_gen(0, 0), head_gen(1, 1)]
        while gens:
            for g_ in list(gens):
                try:
                    next(g_)
                except StopIteration:
                    gens.remove(g_)
        kb.barrier()


def stage_swa(kb, nc, l, A, SC, cst):
    with ExitStack() as st:
        swab = sbt(nc, st, "a_swab", [128, 8, 256], F32)
        kb.dma('sp', swab[:], A['swab'][:, :, :], w=['cst'])
        qb = sbt(nc, st, "a_qb", [128, 4, S], BF16)
        kbt = sbt(nc, st, "a_kb", [128, S], BF16)
        vb = sbt(nc, st, "a_vb", [128, NT, 128], BF16)
        ybT = sbt(nc, st, "a_ybT", [64, 8, S], BF16)
        snk = sbt(nc, st, "a_snk", [128, 8], F32)
        for j in range(4):
            kb.dma('sp', qb[:, j, :], SC['qb'][j], r=['s_misc'], w=['a_qb'])
        kb.dma('sp', kbt[:], SC['kb'], r=['s_misc'], w=['a_kb'])
        kb.dma('sp', vb[:], SC['vb'].rearrange("(t p) d -> p t d", p=128), r=['s_vb'], w=['a_vb'])
        kb.dma('sp', snk[:], A['sink_bc'][l], w=['a_snk'])
        def swa_gen(heads, ch):
            CP = 'a%d_' % ch
            sc = [sbt(nc, st, "a_sc%d" % i, [128, 256], F32) for i in range(2)]
            pb_ = [sbt(nc, st, "a_p%d" % i, [128, 256], BF16) for i in range(2)]
            pT = [sbt(nc, st, "a_pT%d" % i, [128, 2, 128], BF16) for i in range(2)]
            sm = [sbt(nc, st, "a_sm%d" % i, [128, 8], F32) for i in range(2)]
            PS = PsRot(kb, nc, st, 2, "a%d_ps" % ch)
            PT = PsRot(kb, nc, st, 1, "a%d_pt" % ch, dt=BF16, shape=(128, 2, 128))
            PO = PsRot(kb, nc, st, 1, "a%d_po" % ch)
            u = 0
            for n in range(NT):
                for hq in heads:
                    kv = hq // 4
                    j = hq % 4
                    lo = kv * 64
                    W = 128 if n == 0 else 256
                    k0 = 0 if n == 0 else (n - 1) * 128
                    b = u % 2
                    u += 1
                    p, pk = PS.next()
                    kb.op('pe', lambda e: e.matmul(p[:, 0:W], lhsT=qb[lo:lo + 64, j, n * 128:(n + 1) * 128],
                                                   rhs=kbt[lo:lo + 64, k0:k0 + W], start=True, stop=True),
                          r=['a_qb', 'a_kb'], w=[pk])
                    yield
                    kb.op('dve', lambda e: e.scalar_tensor_tensor(out=sc[b][:, 0:W], in0=p[:, 0:W], scalar=0.125,
                                                                  in1=swab[:, hq, 256 - W:256], op0=ALU.mult,
                                                                  op1=ALU.add), r=[pk, 'cst'], w=[CP + 'sc%d' % b])
                    yield
                    s_ = sm[b]
                    sk = CP + 'sm%d' % b
                    kb.op('dve', lambda e: e.reduce_max(out=s_[:, 0:1], in_=sc[b][:, 0:W], axis=AX.X),
                          r=[CP + 'sc%d' % b], w=[sk])
                    yield
                    kb.op('dve', lambda e: e.tensor_tensor(out=s_[:, 1:2], in0=s_[:, 0:1], in1=snk[:, hq:hq + 1],
                                                           op=ALU.max), r=[sk, 'a_snk'], w=[sk])
                    yield
                    kb.op('dve', lambda e: e.tensor_scalar(out=s_[:, 2:3], in0=s_[:, 1:2], scalar1=-1.0, scalar2=None,
                                                           op0=ALU.mult), r=[sk], w=[sk])
                    yield
                    kb.op('act', lambda e: e.activation(out=sc[b][:, 0:W], in_=sc[b][:, 0:W], func=AF.Exp,
                                                        bias=s_[:, 2:3], scale=1.0, accum_out=s_[:, 3:4]),
                          r=[CP + 'sc%d' % b, sk], w=[CP + 'sc%d' % b, sk])
                    yield
                    kb.op('act', lambda e: e.activation(out=s_[:, 4:5], in_=snk[:, hq:hq + 1], func=AF.Exp,
                                                        bias=s_[:, 2:3], scale=1.0), r=[sk, 'a_snk'], w=[sk])
                    yield
                    kb.op('dve', lambda e: e.tensor_tensor(out=s_[:, 5:6], in0=s_[:, 3:4], in1=s_[:, 4:5], op=ALU.add),
                          r=[sk], w=[sk])
                    yield
                    kb.op('dve', lambda e: e.reciprocal(out=s_[:, 6:7], in_=s_[:, 5:6]), r=[sk], w=[sk])
                    yield
                    kb.op('dve', lambda e: e.tensor_scalar(out=pb_[b][:, 0:W], in0=sc[b][:, 0:W], scalar1=s_[:, 6:7],
                                                           scalar2=None, op0=ALU.mult),
                          r=[CP + 'sc%d' % b, sk], w=[CP + 'p%d' % b])
                    yield
                    nk = W // 128
                    pt, ptk = PT.next()
                    for q_ in range(nk):
                        kb.op('pe', lambda e, q_=q_: e.transpose(out=pt[:, q_, :], in_=pb_[b][:, q_ * 128:(q_ + 1) * 128],
                                                                identity=cst['ident_b'][:, :]),
                              r=[CP + 'p%d' % b, 'cst'], w=[ptk])
                        yield
                    cp(kb, 'act', pT[b][:, 0:nk, :], pt[:, 0:nk, :], [ptk], [CP + 'pT%d' % b])
                    yield
                    po, pok = PO.next()
                    for q_ in range(nk):
                        tk = n if n == 0 else n - 1 + q_
                        kb.op('pe', lambda e, q_=q_, tk=tk: e.matmul(po[0:64, 0:128], lhsT=vb[:, tk, kv * 64:(kv + 1) * 64],
                                                                    rhs=pT[b][:, q_, :], start=(q_ == 0),
                                                                    stop=(q_ == nk - 1)),
                              r=['a_vb', CP + 'pT%d' % b], w=[pok])
                        yield
                    cp(kb, 'act' if u % 2 else 'dve', ybT[:, hq, n * 128:(n + 1) * 128], po[0:64, 0:128], [pok], ['a_ybT%d' % hq])
                    yield


        gens = [swa_gen((0, 1, 2, 3), 0), swa_gen((4, 5, 6, 7), 1)]
        while gens:
            for g_ in list(gens):
                try:
                    next(g_)
                except StopIteration:
                    gens.remove(g_)
        kb.dma('sp', SC['yb'], ybT[:], r=['a_ybT%d' % i for i in range(8)], w=['s_yb'])
        kb.barrier()


def ln_affine_store(kb, nc, xin, xk, bufs, g_bc, b_bc, dst, sfx, dk):
    stt, mv, rstd, nb = bufs
    kb.op('dve', lambda e: e.bn_stats(out=stt[:, 0, :], in_=xin[:, 0:512]), r=[xk], w=['la_st' + sfx])
    kb.op('dve', lambda e: e.bn_stats(out=stt[:, 1, :], in_=xin[:, 512:1024]), r=[xk], w=['la_st' + sfx])
    kb.op('dve', lambda e: e.bn_aggr(out=mv[:], in_=stt[:].rearrange("p a b -> p (a b)")),
          r=['la_st' + sfx], w=['la_mv' + sfx])
    kb.op('act', lambda e: e.activation(out=rstd[:], in_=mv[:, 1:2], func=AF.Sqrt, bias=1e-5, scale=1.0),
          r=['la_mv' + sfx], w=['la_rstd' + sfx])
    kb.op('dve', lambda e: e.reciprocal(out=rstd[:], in_=rstd[:]), r=['la_rstd' + sfx], w=['la_rstd' + sfx])
    kb.op('dve', lambda e: e.scalar_tensor_tensor(out=nb[:], in0=mv[:, 0:1], scalar=-1.0, in1=rstd[:],
                                                  op0=ALU.mult, op1=ALU.mult),
          r=['la_mv' + sfx, 'la_rstd' + sfx], w=['la_nb' + sfx])
    kb.op('act', lambda e: e.activation(out=xin[:], in_=xin[:], func=AF.Identity, bias=nb[:], scale=rstd[:]),
          r=[xk, 'la_nb' + sfx, 'la_rstd' + sfx], w=[xk])
    kb.op('pool', lambda e: e.tensor_tensor(out=xin[:], in0=xin[:], in1=g_bc, op=ALU.mult), r=[xk, 'lngb'], w=[xk])
    kb.op('pool', lambda e: e.tensor_tensor(out=xin[:], in0=xin[:], in1=b_bc, op=ALU.add), r=[xk, 'lngb'], w=[xk])
    kb.dma('sp', dst, xin[:], r=[xk], w=[dk])


def stage_merge(kb, nc, l, A, SC, x_src, x_dst, mod_bc, cst):
    with ExitStack() as st:
        U = [sbt(nc, st, "c_u%d" % i, [128, 16 + S], F32) for i in range(3)]
        pw = sbt(nc, st, "c_pw", [128, 4, 128], F32)
        pscl = sbt(nc, st, "c_ps", [128, 4], F32)
        ic = sbt(nc, st, "c_ic", [128, 4, 16], F32)
        yc = [sbt(nc, st, "c_yc%d" % i, [128, S], BF16) for i in range(2)]
        PS = PsRot(kb, nc, st, 4, "c_pp")
        kb.dma('sp', pw[:], A['pool_w'][l].rearrange("g c d -> c g d"), w=['c_pw'])
        kb.dma('sp', pscl[:], A['pscale_l'][l], w=['c_ps'])
        kb.dma('sp', ic[:], A['invcnt'][:, :, :], w=['c_ic'])
        for i in range(3):
            kb.op('dve', lambda e, i=i: e.memset(U[i][:, 0:16], 0.0), w=['c_u%d' % i])
        for g, win in enumerate((2, 4, 8, 16)):
            kb.dma('sp', U[0][:, 16:], SC['uc'][g], r=['s_uc'], w=['c_u0'])
            cur = 0
            sh = 1
            while sh < win:
                nxt = 1 if cur != 1 else 2
                kb.op('dve' if sh % 4 == 1 else 'pool', lambda e, cur=cur, nxt=nxt, sh=sh: e.tensor_tensor(
                    out=U[nxt][:, 16:], in0=U[cur][:, 16:], in1=U[cur][:, 16 - sh:16 - sh + S], op=ALU.add),
                    r=['c_u%d' % cur], w=['c_u%d' % nxt])
                cur = nxt
                sh *= 2
            dn = 1 if cur != 1 else 2
            kb.op('dve', lambda e, cur=cur, dn=dn: e.scalar_tensor_tensor(
                out=U[dn][:, 16:], in0=U[cur][:, 16:], scalar=1.0 / win, in1=U[0][:, 16:], op0=ALU.mult,
                op1=ALU.subtract), r=['c_u%d' % cur, 'c_u0'], w=['c_u%d' % dn])
            kb.op('dve', lambda e, cur=cur, dn=dn: e.tensor_tensor(out=U[dn][:, 16:32], in0=U[cur][:, 16:32],
                                                                  in1=ic[:, g, :], op=ALU.mult),
                  r=['c_u%d' % cur, 'c_ic'], w=['c_u%d' % dn])
            kb.op('dve', lambda e, dn=dn: e.tensor_tensor(out=U[dn][:, 16:32], in0=U[dn][:, 16:32],
                                                          in1=U[0][:, 16:32], op=ALU.subtract),
                  r=['c_u%d' % dn, 'c_u0'], w=['c_u%d' % dn])
            yb_ = g % 2
            for tc in range(8):
                p, pk = PS.next()
                kb.op('pe', lambda e, tc=tc, dn=dn: e.matmul(p[:], lhsT=pw[:, g, :],
                                                             rhs=U[dn][:, 16 + tc * 512:16 + (tc + 1) * 512],
                                                             start=True, stop=True), r=['c_pw', 'c_u%d' % dn], w=[pk])
                kb.op('act', lambda e, tc=tc: e.activation(out=yc[yb_][:, tc * 512:(tc + 1) * 512], in_=p[:],
                                                           func=AF.Identity, scale=pscl[:, g:g + 1]),
                      r=[pk, 'c_ps'], w=['c_yc%d' % yb_])
            kb.dma('sp', SC['yc'][g], yc[yb_][:], r=['c_yc%d' % yb_], w=['s_yc'])
        kb.barrier()
    with ExitStack() as st:
        wp = sbt(nc, st, "e_wp", [128, 12, D], BF16)
        wpb = sbt(nc, st, "e_wpb", [64, 8, D], BF16)
        wo = sbt(nc, st, "e_wo", [128, 8, D], BF16)
        lng = sbt(nc, st, "e_lng", [128, D], F32)
        lnb = sbt(nc, st, "e_lnb", [128, D], F32)
        gt = [sbt(nc, st, "e_gt0", [128, 24, 512], BF16)] * 2
        ya = [sbt(nc, st, "e_ya%d" % i, [128, 4, 512], BF16) for i in range(2)]
        ycb = [sbt(nc, st, "e_yc%d" % i, [128, 4, 512], BF16) for i in range(2)]
        ybb = [sbt(nc, st, "e_yb%d" % i, [64, 8, 512], BF16) for i in range(2)]
        mg = [sbt(nc, st, "e_mg%d" % i, [128, 8, 512], BF16) for i in range(2)]
        t1 = [sbt(nc, st, "e_t1%d" % i, [128, 512], F32) for i in range(2)]
        t2 = [sbt(nc, st, "e_t2%d" % i, [128, 512], F32) for i in range(2)]
        t3 = [sbt(nc, st, "e_t3%d" % i, [128, 512], F32) for i in range(2)]
        xt = [sbt(nc, st, "e_xt%d" % i, [128, D], F32) for i in range(2)]
        yt = [sbt(nc, st, "e_yt%d" % i, [128, D], F32) for i in range(2)]
        lb = [(sbt(nc, st, "e_st%d" % i, [128, 2, 6], F32), sbt(nc, st, "e_mv%d" % i, [128, 2], F32),
               sbt(nc, st, "e_rs%d" % i, [128, 1], F32), sbt(nc, st, "e_nb%d" % i, [128, 1], F32)) for i in range(2)]
        PA = PsRot(kb, nc, st, 6, "e_pa")
        PY = PsRot(kb, nc, st, 2, "e_py")
        kb.dma('pool', wp[:, 0:4, :], A['w_pa'][l].rearrange("(kc p) n -> p kc n", p=128), w=['e_wp'])
        kb.dma('pool', wp[:, 4:8, :], A['w_pc'][l].rearrange("(kc p) n -> p kc n", p=128), w=['e_wp'])
        kb.dma('pool', wpb[:], A['w_pb'][l].rearrange("(h p) n -> p h n", p=64), w=['e_wpb'])
        kb.dma('pool', wo[:], A['w_o'][l].rearrange("(kc p) n -> p kc n", p=128), w=['e_wo'])
        kb.dma('sp', lng[:], A['ln1g_bc'][l], w=['lngb'])
        kb.dma('sp', lnb[:], A['ln1b_bc'][l], w=['lngb'])
        u = 0
        for tc in range(8):
            b = tc % 2
            ts = slice(tc * 512, (tc + 1) * 512)
            kb.dma('sp', gt[b][:], SC['gate'][:, :, ts].rearrange("c p t -> p c t"), r=['s_misc'], w=['e_gt0'])
            kb.dma('sp', ya[b][:], SC['ya'][:, :, ts].rearrange("c p t -> p c t"), r=['s_ya'], w=['e_ya%d' % b])
            kb.dma('sp', ycb[b][:], SC['yc'][:, :, ts].rearrange("c p t -> p c t"), r=['s_yc'], w=['e_yc%d' % b])
            kb.dma('sp', ybb[b][:], SC['yb'][:, :, ts], r=['s_yb'], w=['e_yb%d' % b])
            for m in range(8):
                ms = slice(m * 128, (m + 1) * 128)
                tb = u % 2
                u += 1
                pa, pka = PA.next()
                for kc in range(4):
                    kb.op('pe', lambda e, kc=kc: e.matmul(pa[:], lhsT=wp[:, kc, ms], rhs=ya[b][:, kc, :],
                                                          start=(kc == 0), stop=(kc == 3)),
                          r=['e_wp', 'e_ya%d' % b], w=[pka])
                kb.op('dve', lambda e: e.tensor_tensor(out=t1[tb][:], in0=pa[:], in1=gt[b][:, m, :], op=ALU.mult),
                      r=[pka, 'e_gt0'], w=['e_t1%d' % tb])
                pb2, pkb2 = PA.next()
                for hh in range(8):
                    kb.op('pe', lambda e, hh=hh: e.matmul(pb2[:], lhsT=wpb[:, hh, ms], rhs=ybb[b][:, hh, :],
                                                          start=(hh == 0), stop=(hh == 7)),
                          r=['e_wpb', 'e_yb%d' % b], w=[pkb2])
                kb.op('dve', lambda e: e.tensor_tensor(out=t2[tb][:], in0=pb2[:], in1=gt[b][:, 8 + m, :], op=ALU.mult),
                      r=[pkb2, 'e_gt0'], w=['e_t2%d' % tb])
                pc2, pkc2 = PA.next()
                for kc in range(4):
                    kb.op('pe', lambda e, kc=kc: e.matmul(pc2[:], lhsT=wp[:, 4 + kc, ms], rhs=ycb[b][:, kc, :],
                                                          start=(kc == 0), stop=(kc == 3)),
                          r=['e_wp', 'e_yc%d' % b], w=[pkc2])
                kb.op('dve', lambda e: e.tensor_tensor(out=t3[tb][:], in0=pc2[:], in1=gt[b][:, 16 + m, :], op=ALU.mult),
                      r=[pkc2, 'e_gt0'], w=['e_t3%d' % tb])
                kb.op('pool', lambda e: e.tensor_tensor(out=t1[tb][:], in0=t1[tb][:], in1=t2[tb][:], op=ALU.add),
                      r=['e_t1%d' % tb, 'e_t2%d' % tb], w=['e_t1%d' % tb])
                kb.op('pool', lambda e: e.tensor_tensor(out=mg[b][:, m, :], in0=t1[tb][:], in1=t3[tb][:], op=ALU.add),
                      r=['e_t1%d' % tb, 'e_t3%d' % tb], w=['e_mg%d' % b])
            for tt in range(4):
                t = tc * 4 + tt
                xb = t % 2
                kb.dma('sp', xt[xb][:], x_src[t * 128:(t + 1) * 128, :], r=['xsrc'], w=['e_xt%d' % xb])
                for half in range(2):
                    py, pyk = PY.next()
                    for kc in range(8):
                        kb.op('pe', lambda e, kc=kc: e.matmul(py[:], lhsT=mg[b][:, kc, tt * 128:(tt + 1) * 128],
                                                              rhs=wo[:, kc, half * 512:(half + 1) * 512],
                                                              start=(kc == 0), stop=(kc == 7)),
                              r=['e_mg%d' % b, 'e_wo'], w=[pyk])
                    kb.op('dve', lambda e, half=half: e.tensor_tensor(
                        out=yt[xb][:, half * 512:(half + 1) * 512], in0=py[:],
                        in1=mod_bc[:, 2 * D + half * 512:2 * D + (half + 1) * 512], op=ALU.mult),
                        r=[pyk, 'mod_bc'], w=['e_yt%d' % xb])
                kb.op('dve', lambda e: e.scalar_tensor_tensor(out=yt[xb][:], in0=xt[xb][:], scalar=ALPHA, in1=yt[xb][:],
                                                              op0=ALU.mult, op1=ALU.add),
                      r=['e_xt%d' % xb, 'e_yt%d' % xb], w=['e_yt%d' % xb])
                ln_affine_store(kb, nc, yt[xb], 'e_yt%d' % xb, lb[xb], lng[:], lnb[:],
                                x_dst[t * 128:(t + 1) * 128, :], 'e%d' % xb, 'xdst')
        kb.barrier()


def stage_moe(kb, nc, l, A, SC, x_src, x_dst, mod_bc, cst):
    ident = cst['ident_f']
    with ExitStack() as st:
        hT = sbt(nc, st, "hT2", [128, 8, S], BF16)
        gate_all = sbt(nc, st, "f_gate", [128, NT, NE], F32)
        with ExitStack() as s2:
            xts = [sbt(nc, s2, "f_xt%d" % i, [128, D], F32) for i in range(2)]
            hbs = [sbt(nc, s2, "f_hb%d" % i, [128, D], BF16) for i in range(2)]
            bufs = [(sbt(nc, s2, "f_st%d" % i, [128, 2, 6], F32), sbt(nc, s2, "f_mv%d" % i, [128, 2], F32),
                     sbt(nc, s2, "f_rs%d" % i, [128, 1], F32), sbt(nc, s2, "f_nb%d" % i, [128, 1], F32),
                     sbt(nc, s2, "f_xn%d" % i, [128, D], F32)) for i in range(2)]
            h32 = [sbt(nc, s2, "f_h32%d" % i, [128, 8, 128], F32) for i in range(2)]
            rw = sbt(nc, s2, "f_rw", [128, 8, NE], F32)
            rb = sbt(nc, s2, "f_rb", [128, NE], F32)
            lg = [sbt(nc, s2, "f_lg%d" % i, [128, NE], F32) for i in range(2)]
            mk = [sbt(nc, s2, "f_mk%d" % i, [128, NE], F32) for i in range(2)]
            m8 = [sbt(nc, s2, "f_m8%d" % i, [128, 12], F32) for i in range(2)]
            pT = [pst(nc, s2, "f_pT%d" % i, [128, 8, 128], BF16) for i in range(2)]
            pR = [pst(nc, s2, "f_pR%d" % i, [128, 2, 512], F32) for i in range(2)]
            pL = [pst(nc, s2, "f_pL%d" % i, [128, 512], F32) for i in range(2)]
            kb.dma('sp', rw[:], A['router_w'][l].rearrange("(kc p) n -> p kc n", p=128), w=['f_rw'])
            kb.dma('sp', rb[:], A['rb_bc'][l], w=['f_rb'])
            for t in range(NT):
                b = t % 2
                sx = 'f' + str(b)
                kb.dma('sp', xts[b][:], x_src[t * 128:(t + 1) * 128, :], r=['xsrc5'], w=['f_xt%d' % b])
                stt, mv, rstd, nb, xn = bufs[b]
                ln_mod_tile(kb, nc, xts[b], 'f_xt%d' % b, bufs[b], mod_bc[:, 4 * D:5 * D], mod_bc[:, 3 * D:4 * D],
                            xn[:], 'ln_xn' + sx, sx)
                cp(kb, 'act', hbs[b][:], xn[:], ['ln_xn' + sx], ['f_hb%d' % b])
                for kc in range(8):
                    kb.op('pe', lambda e, kc=kc: e.transpose(out=pT[b][:, kc, :], in_=hbs[b][:, kc * 128:(kc + 1) * 128],
                                                             identity=cst['ident_b'][:, :]),
                          r=['f_hb%d' % b, 'cst'], w=['f_pT%d' % b])
                cp(kb, 'act', hT[:, :, t * 128:(t + 1) * 128], pT[b][:], ['f_pT%d' % b], ['hT2'])
                for kc in range(8):
                    kb.op('pe', lambda e, kc=kc: e.matmul(pR[b][:, kc // 4, (kc % 4) * 128:(kc % 4 + 1) * 128],
                                                          lhsT=xn[:, kc * 128:(kc + 1) * 128], rhs=ident,
                                                          start=True, stop=True),
                          r=['ln_xn' + sx, 'cst'], w=['f_pR%d' % b])
                cp(kb, 'dve', h32[b][:].rearrange("p (a c) n -> p a (c n)", a=2), pR[b][:], ['f_pR%d' % b],
                   ['f_h32%d' % b])
                for kc in range(8):
                    kb.op('pe', lambda e, kc=kc: e.matmul(pL[b][:, 0:NE], lhsT=h32[b][:, kc, :], rhs=rw[:, kc, :],
                                                          start=(kc == 0), stop=(kc == 7)),
                          r=['f_h32%d' % b, 'f_rw'], w=['f_pL%d' % b])
                kb.op('dve', lambda e: e.tensor_tensor(out=lg[b][:], in0=pL[b][:, 0:NE], in1=rb[:], op=ALU.add),
                      r=['f_pL%d' % b, 'f_rb'], w=['f_lg%d' % b])
                kb.op('dve', lambda e: e.max(out=m8[b][:, 0:8], in_=lg[b][:]), r=['f_lg%d' % b], w=['f_m8%d' % b])
                kb.op('dve', lambda e: e.tensor_scalar(out=mk[b][:], in0=lg[b][:], scalar1=m8[b][:, 3:4], scalar2=None,
                                                       op0=ALU.is_ge), r=['f_lg%d' % b, 'f_m8%d' % b], w=['f_mk%d' % b])
                kb.op('dve', lambda e: e.tensor_scalar(out=m8[b][:, 8:9], in0=m8[b][:, 0:1], scalar1=-1.0, scalar2=None,
                                                       op0=ALU.mult), r=['f_m8%d' % b], w=['f_m8%d' % b])
                kb.op('act', lambda e: e.activation(out=lg[b][:], in_=lg[b][:], func=AF.Exp, bias=m8[b][:, 8:9],
                                                    scale=1.0), r=['f_lg%d' % b, 'f_m8%d' % b], w=['f_lg%d' % b])
                kb.op('dve', lambda e: e.tensor_tensor(out=lg[b][:], in0=lg[b][:], in1=mk[b][:], op=ALU.mult),
                      r=['f_lg%d' % b, 'f_mk%d' % b], w=['f_lg%d' % b])
                kb.op('dve', lambda e: e.reduce_sum(out=m8[b][:, 9:10], in_=lg[b][:], axis=AX.X),
                      r=['f_lg%d' % b], w=['f_m8%d' % b])
                kb.op('dve', lambda e: e.reciprocal(out=m8[b][:, 10:11], in_=m8[b][:, 9:10]),
                      r=['f_m8%d' % b], w=['f_m8%d' % b])
                kb.op('dve', lambda e: e.tensor_scalar(out=gate_all[:, t, :], in0=lg[b][:], scalar1=m8[b][:, 10:11],
                                                       scalar2=None, op0=ALU.mult),
                      r=['f_lg%d' % b, 'f_m8%d' % b], w=['f_gate'])
            kb.barrier()
        PT_ = 8
        acc = sbt(nc, st, "f_acc", [128, PT_, D], F32)
        w1t = [sbt(nc, st, "f_w1%d" % i, [128, 8, 512], BF16) for i in range(2)]
        w2t = sbt(nc, st, "f_w2", [128, 8, D], BF16)
        b1t = [sbt(nc, st, "f_b1%d" % i, [128, 16], F32) for i in range(2)]
        b2t = [sbt(nc, st, "f_b2%d" % i, [1, D], BF16) for i in range(2)]
        onesb = sbt(nc, st, "f_onesb", [1, 128], BF16)
        actT = sbt(nc, st, "f_actT", [128, 8, PT_ * 128], BF16)
        gl = [sbt(nc, st, "f_gl%d" % i, [128, 512], F32) for i in range(2)]
        sg = [sbt(nc, st, "f_sg%d" % i, [128, 512], F32) for i in range(2)]
        li = [sbt(nc, st, "f_li%d" % i, [128, 512], F32) for i in range(2)]
        xt = [sbt(nc, st, "f_x%d" % i, [128, D], F32) for i in range(2)]
        lng = sbt(nc, st, "f_lng", [128, D], F32)
        lnb = sbt(nc, st, "f_lnb", [128, D], F32)
        lb = [(sbt(nc, st, "f_lst%d" % i, [128, 2, 6], F32), sbt(nc, st, "f_lmv%d" % i, [128, 2], F32),
               sbt(nc, st, "f_lrs%d" % i, [128, 1], F32), sbt(nc, st, "f_lnb%d" % i, [128, 1], F32)) for i in range(2)]
        PG = PsRot(kb, nc, st, 4, "f_pg")
        PY = PsRot(kb, nc, st, 3, "f_py")
        kb.op('dve', lambda e: e.tensor_copy(out=onesb[:], in_=cst['ones_f'][0:1, :]), r=['cst'], w=['f_onesb'])
        kb.dma('sp', lng[:], A['ln2g_bc'][l], w=['lngb'])
        kb.dma('sp', lnb[:], A['ln2b_bc'][l], w=['lngb'])
        W1, W2 = A['exp_w1'], A['exp_w2']
        gcount = 0
        u = 0
        n_exp = DBG.get('n_exp', NE)
        for ps_ in range(NT // PT_):
            kb.op('pool', lambda e: e.memset(acc[:], 0.0), w=['f_acc'])
            for ex in range(n_exp):
                eb = ex % 2
                kb.dma('sp', b1t[eb][:], A['exp_b1l'][l, ex], w=['f_b1%d' % eb])
                kb.dma('pool', b2t[eb][:], A['exp_b2'][l, ex:ex + 1, :], w=['f_b2%d' % eb])
                for g in range(4):
                    wb = gcount % 2
                    gcount += 1
                    kb.dma('pool', w1t[wb][:, :, 0:256],
                           W1[l, ex, :, g * 256:(g + 1) * 256].rearrange("(kc p) n -> p kc n", p=128), w=['f_w1%d' % wb])
                    kb.dma('pool', w1t[wb][:, :, 256:512],
                           W1[l, ex, :, DFF + g * 256:DFF + (g + 1) * 256].rearrange("(kc p) n -> p kc n", p=128),
                           w=['f_w1%d' % wb])
                    for fl in range(2):
                        fc = g * 2 + fl
                        for tcl in range(PT_ // 4):
                            tb = u % 2
                            u += 1
                            tok = slice((ps_ * PT_ + tcl * 4) * 128, (ps_ * PT_ + tcl * 4 + 4) * 128)
                            pg, pgk = PG.next()
                            for kc in range(8):
                                kb.op('pe', lambda e, kc=kc: e.matmul(pg[:], lhsT=w1t[wb][:, kc, fl * 128:(fl + 1) * 128],
                                                                      rhs=hT[:, kc, tok], start=(kc == 0), stop=(kc == 7)),
                                      r=['f_w1%d' % wb, 'hT2'], w=[pgk])
                            pl, plk = PG.next()
                            for kc in range(8):
                                kb.op('pe', lambda e, kc=kc: e.matmul(
                                    pl[:], lhsT=w1t[wb][:, kc, 256 + fl * 128:256 + (fl + 1) * 128],
                                    rhs=hT[:, kc, tok], start=(kc == 0), stop=(kc == 7)),
                                    r=['f_w1%d' % wb, 'hT2'], w=[plk])
                            kb.op('dve', lambda e: e.tensor_scalar(out=gl[tb][:], in0=pg[:], scalar1=b1t[eb][:, fc:fc + 1],
                                                                   scalar2=7.0, op0=ALU.add, op1=ALU.min),
                                  r=[pgk, 'f_b1%d' % eb], w=['f_gl%d' % tb])
                            kb.op('act', lambda e: e.activation(out=sg[tb][:], in_=gl[tb][:], func=AF.Sigmoid,
                                                                scale=1.702), r=['f_gl%d' % tb], w=['f_sg%d' % tb])
                            kb.op('dve', lambda e: e.tensor_scalar(out=li[tb][:], in0=pl[:],
                                                                   scalar1=b1t[eb][:, 8 + fc:9 + fc], scalar2=7.0,
                                                                   op0=ALU.add, op1=ALU.min),
                                  r=[plk, 'f_b1%d' % eb], w=['f_li%d' % tb])
                            kb.op('dve', lambda e: e.tensor_scalar(out=li[tb][:], in0=li[tb][:], scalar1=-7.0,
                                                                   scalar2=1.0, op0=ALU.max, op1=ALU.add),
                                  r=['f_li%d' % tb], w=['f_li%d' % tb])
                            kb.op('pool', lambda e: e.tensor_tensor(out=gl[tb][:], in0=gl[tb][:], in1=sg[tb][:],
                                                                    op=ALU.mult),
                                  r=['f_gl%d' % tb, 'f_sg%d' % tb], w=['f_gl%d' % tb])
                            kb.op('pool', lambda e: e.tensor_tensor(out=actT[:, fc, tcl * 512:(tcl + 1) * 512],
                                                                    in0=gl[tb][:], in1=li[tb][:], op=ALU.mult),
                                  r=['f_gl%d' % tb, 'f_li%d' % tb], w=['f_actT'])
                kb.dma('pool', w2t[:], W2[l, ex].rearrange("(kc p) n -> p kc n", p=128), w=['f_w2'],
                       max_dma_last_dim=4096)
                for tt in range(PT_):
                    T = ps_ * PT_ + tt
                    for half in range(2):
                        py, pyk = PY.next()
                        for fc in range(8):
                            kb.op('pe', lambda e, fc=fc: e.matmul(py[:], lhsT=actT[:, fc, tt * 128:(tt + 1) * 128],
                                                                  rhs=w2t[:, fc, half * 512:(half + 1) * 512],
                                                                  start=(fc == 0), stop=False),
                                  r=['f_actT', 'f_w2'], w=[pyk])
                        kb.op('pe', lambda e: e.matmul(py[:], lhsT=onesb[0:1, :], rhs=b2t[eb][0:1, half * 512:(half + 1) * 512],
                                                       start=False, stop=True), r=['f_onesb', 'f_b2%d' % eb], w=[pyk])
                        kb.op('dve', lambda e: e.scalar_tensor_tensor(
                            out=acc[:, tt, half * 512:(half + 1) * 512], in0=py[:], scalar=gate_all[:, T, ex:ex + 1],
                            in1=acc[:, tt, half * 512:(half + 1) * 512], op0=ALU.mult, op1=ALU.add),
                            r=[pyk, 'f_gate', 'f_acc'], w=['f_acc'])
            for tt in range(PT_):
                T = ps_ * PT_ + tt
                xb = T % 2
                kb.dma('sp', xt[xb][:], x_src[T * 128:(T + 1) * 128, :], r=['xsrc5'], w=['f_x%d' % xb])
                kb.op('dve', lambda e: e.tensor_tensor(out=acc[:, tt, :], in0=acc[:, tt, :], in1=mod_bc[:, 5 * D:6 * D],
                                                       op=ALU.mult), r=['f_acc', 'mod_bc'], w=['f_acc'])
                kb.op('dve', lambda e: e.scalar_tensor_tensor(out=xt[xb][:], in0=xt[xb][:], scalar=ALPHA,
                                                              in1=acc[:, tt, :], op0=ALU.mult, op1=ALU.add),
                      r=['f_x%d' % xb, 'f_acc'], w=['f_x%d' % xb])
                ln_affine_store(kb, nc, xt[xb], 'f_x%d' % xb, lb[xb], lng[:], lnb[:],
                                x_dst[T * 128:(T + 1) * 128, :], 'f%d' % xb, 'xdst5')
        kb.barrier()


def input_shapes(L):
    return {
        'x': [S, D], 'c_col': [128, 8], 'ada_w': [L, D, 6 * D], 'ada_b': [L, 6 * D], 'w_in': [L, D, INW],
        'conv_wl': [L, 128, 12, 4], 'alog_bc': [L, 128, 4], 'dtb_bc': [L, 128, 4], 'gnw_bc': [L, 128, 128],
        'sink_bc': [L, 128, 8], 'pool_w': [L, 4, 128, 128], 'pscale_l': [L, 128, 4],
        'w_pa': [L, 512, D], 'w_pb': [L, 512, D], 'w_pc': [L, 512, D], 'w_o': [L, D, D],
        'ln1g_bc': [L, 128, D], 'ln1b_bc': [L, 128, D], 'ln2g_bc': [L, 128, D], 'ln2b_bc': [L, 128, D],
        'router_w': [L, D, NE], 'rb_bc': [L, 128, NE], 'exp_w1': [L, NE, D, 2 * DFF], 'exp_w2': [L, NE, DFF, D],
        'exp_b1l': [L, NE, 128, 16], 'exp_b2': [L, NE, D],
        'cst_f': [128, 2, 128], 'gm': [128, 6, 128], 'swab': [128, 8, 256], 'invcnt': [128, 4, 16], 'moec': [128, 160],
    }


def build_program(L):
    nc = bass.Bass("TRN2", target_bir_lowering=False)
    A = {k: nc.dram_tensor(k, s, F32, kind="ExternalInput").ap() for k, s in input_shapes(L).items()}
    out = nc.dram_tensor("out", [S, D], F32, kind="ExternalOutput").ap()
    SC = alloc_scratch(nc)
    SC['ya'] = nc.dram_tensor("s_ya", [4, 128, S], BF16).ap()
    SC['yb'] = nc.dram_tensor("s_yb", [64, 8, S], BF16).ap()
    SC['yc'] = nc.dram_tensor("s_yc", [4, 128, S], BF16).ap()
    x1 = nc.dram_tensor("s_x1", [S, D], F32).ap()
    SC['hg'] = nc.dram_tensor("s_hg", [NE * CAP, D], BF16).ap()
    SC['yg'] = nc.dram_tensor("s_yg", [NE * CAP, D], F32).ap()
    xs = [A['x']] + [nc.dram_tensor("s_xl%d" % i, [S, D], F32).ap() for i in range(L - 1)] + [out]
    with ExitStack() as es:
        kb = KB(nc, es)
        cf = sbt(nc, es, "cst_sb", [128, 2, 128], F32)
        ib = sbt(nc, es, "ident_b", [128, 128], BF16)
        mod_bc = sbt(nc, es, "mod_bc", [128, 6 * D], F32)
        kb.dma('sp', cf[:], A['cst_f'][:, :, :], w=['cst'])
        kb.op('dve', lambda e: e.tensor_copy(out=ib[:], in_=cf[:, 1, :]), r=['cst'], w=['cst'])
        cst = {'ones_f': cf[:, 0, :], 'ident_f': cf[:, 1, :], 'ident_b': ib, 'breg': nc.gpsimd.to_reg(NE * CAP - 1)}
        with ExitStack() as zs:
            zt = sbt(nc, zs, "zero_t", [128, 8 * D], BF16)
            kb.op('dve', lambda e: e.memset(zt[:], 0.0), w=['zero_t'])
            rows_per = 128 * 8
            for i in range(NE * CAP // rows_per):
                kb.dma('sp' if i % 2 == 0 else 'act', SC['hg'][i * rows_per:(i + 1) * rows_per, :].rearrange(
                    "(p r) d -> p (r d)", p=128), zt[:], r=['zero_t'], w=['s_hg_z%d' % (i % 8)])
            kb.barrier()
        for l in range(L):
            stage_mod(kb, nc, l, A, mod_bc, cst)
            stage_proj(kb, nc, l, A, SC, xs[l], mod_bc, cst)
            stage_gdn(kb, nc, l, A, SC, cst)
            stage_swa(kb, nc, l, A, SC, cst)
            stage_merge(kb, nc, l, A, SC, xs[l], x1, mod_bc, cst)
            stage_moe_sparse(kb, nc, l, A, SC, x1, xs[l + 1], mod_bc, cst)
        kb.barrier()
    return nc


def host_consts():
    i = np.arange(128)[:, None]
    j = np.arange(128)[None, :]
    same = (i // 64) == (j // 64)
    gm = np.stack([((i <= j) & same).astype(np.float32), same.astype(np.float32),
                   np.broadcast_to(i < 64, (128, 128)).astype(np.float32),
                   np.broadcast_to(i >= 64, (128, 128)).astype(np.float32),
                   np.where((j >= i) & same, 0.0, NEG).astype(np.float32),
                   (1.0 - np.eye(128)).astype(np.float32)], 1)
    q = np.arange(128)[:, None]
    k = np.arange(256)[None, :]
    dist = q + 128 - k
    valid = (dist >= 0) & (dist < 128)
    slopes = 2.0 ** (-8.0 * np.arange(1, 9, dtype=np.float32) / 8)
    swab = np.where(valid[:, None, :], -slopes[None, :, None] * dist[:, None, :].astype(np.float32), NEG)
    ic = np.zeros((4, 16), np.float32)
    for g, win in enumerate((2, 4, 8, 16)):
        ic[g] = 1.0 / np.minimum(np.arange(16) + 1, win)
    moec = np.concatenate([(i <= j).astype(np.float32),
                           np.broadcast_to((np.arange(NE) * CAP - 1).astype(np.float32)[None, :], (128, NE))], 1)
    return {'moec': np.ascontiguousarray(moec), 'cst_f': np.stack([np.ones((128, 128), np.float32), np.eye(128, dtype=np.float32)], 1),
            'gm': np.ascontiguousarray(gm), 'swab': np.ascontiguousarray(swab.astype(np.float32)),
            'invcnt': np.ascontiguousarray(np.broadcast_to(ic[None], (128, 4, 16)))}


def host_layer_inputs(p, ls):
    f = lambda a: np.ascontiguousarray(np.asarray(a, np.float32))
    n = len(range(*ls.indices(DEPTH)))
    bc = lambda a, w: f(np.broadcast_to(np.asarray(a)[ls][:, None, :], (n, 128, w)))
    W = {}
    for k in ['ada_w', 'ada_b', 'w_in', 'pool_w', 'w_pa', 'w_pb', 'w_pc', 'w_o', 'router_w', 'exp_w1', 'exp_w2', 'exp_b2']:
        W[k] = f(np.asarray(p[k])[ls])
    W['conv_wl'] = f(np.asarray(p['conv_w'])[ls].reshape(n, 4, 12, 128).transpose(0, 3, 2, 1))
    W['alog_bc'] = bc(p['a_log'], 4)
    W['dtb_bc'] = bc(p['dt_bias'], 4)
    W['gnw_bc'] = bc(p['gdn_norm_w'], 128)
    W['sink_bc'] = bc(p['sinks'], 8)
    W['pscale_l'] = f(np.asarray(p['pool_scale'])[ls].reshape(n, 4, 128).transpose(0, 2, 1))
    W['ln1g_bc'] = bc(p['ln1_g'], D)
    W['ln1b_bc'] = bc(p['ln1_b'], D)
    W['ln2g_bc'] = bc(p['ln2_g'], D)
    W['ln2b_bc'] = bc(p['ln2_b'], D)
    W['rb_bc'] = bc(p['router_b'], NE)
    W['exp_b1l'] = f(np.asarray(p['exp_b1'])[ls].reshape(n, NE, 16, 128).transpose(0, 1, 3, 2))
    return W


LAYERS_PER_LAUNCH = 4


def kernel(**inputs):
    x = np.asarray(inputs['x'], np.float32)
    c = np.asarray(inputs['c'], np.float32)
    nb = x.shape[0]
    consts = host_consts()
    Lp = LAYERS_PER_LAUNCH
    nc = build_program(Lp)
    cur = [np.ascontiguousarray(x[b]) for b in range(nb)]
    for l0 in range(0, DEPTH, Lp):
        W = host_layer_inputs(inputs, slice(l0, l0 + Lp))
        in_maps = []
        for b in range(nb):
            m = dict(W)
            m.update(consts)
            m['x'] = cur[b]
            m['c_col'] = np.ascontiguousarray(c[b].reshape(8, 128).T)
            in_maps.append(m)
        res = run_bass_kernel_spmd(nc, in_maps, core_ids=list(range(nb)))
        cur = [np.asarray(res.results[b]['out'], np.float32) for b in range(nb)]
    return np.stack(cur, 0).astype(np.float32)


def stage_moe_sparse(kb, nc, l, A, SC, x_src, x_dst, mod_bc, cst):
    ident = cst['ident_f']
    C = CAP
    NB = C // 128
    hg, yg = SC['hg'], SC['yg']
    with ExitStack() as st:
        dest_all = sbt(nc, st, "f_dest", [128, NT * 4], I32)
        gk_all = sbt(nc, st, "f_gk", [128, NT, 4], F32)
        with ExitStack() as s2:
            moec = sbt(nc, s2, "f_moec", [128, 160], F32)
            xts = [sbt(nc, s2, "f_xt%d" % i, [128, D], F32) for i in range(2)]
            hbs = [sbt(nc, s2, "f_hb%d" % i, [128, D], BF16) for i in range(2)]
            bufs = [(sbt(nc, s2, "f_st%d" % i, [128, 2, 6], F32), sbt(nc, s2, "f_mv%d" % i, [128, 2], F32),
                     sbt(nc, s2, "f_rs%d" % i, [128, 1], F32), sbt(nc, s2, "f_nb%d" % i, [128, 1], F32),
                     sbt(nc, s2, "f_xn%d" % i, [128, D], F32)) for i in range(2)]
            h32 = [sbt(nc, s2, "f_h32%d" % i, [128, 8, 128], F32) for i in range(2)]
            rw = sbt(nc, s2, "f_rw", [128, 8, NE], F32)
            rb = sbt(nc, s2, "f_rb", [128, NE], F32)
            lg = [sbt(nc, s2, "f_lg%d" % i, [128, NE], F32) for i in range(2)]
            mk = [sbt(nc, s2, "f_mk%d" % i, [128, NE], F32) for i in range(2)]
            gt_ = [sbt(nc, s2, "f_gt%d" % i, [128, NE], F32) for i in range(2)]
            ngp = [sbt(nc, s2, "f_ngp%d" % i, [128, NE], F32) for i in range(2)]
            oh = [sbt(nc, s2, "f_oh%d" % i, [128, NE], F32) for i in range(2)]
            jk = [sbt(nc, s2, "f_jk%d" % i, [128, NE], F32) for i in range(2)]
            m8 = [sbt(nc, s2, "f_m8%d" % i, [128, 12], F32) for i in range(2)]
            t8 = [sbt(nc, s2, "f_t8%d" % i, [128, 8], F32) for i in range(2)]
            msum = sbt(nc, s2, "f_msum", [128, NE], F32)
            pR = [pst(nc, s2, "f_pR%d" % i, [128, 2, 512], F32) for i in range(2)]
            pL = [pst(nc, s2, "f_pL%d" % i, [128, 512], F32) for i in range(2)]
            pC = [pst(nc, s2, "f_pC%d" % i, [128, 512], F32) for i in range(2)]
            kb.dma('sp', moec[:], A['moec'][:, :], w=['f_moec'])
            kb.dma('sp', rw[:], A['router_w'][l].rearrange("(kc p) n -> p kc n", p=128), w=['f_rw'])
            kb.dma('sp', rb[:], A['rb_bc'][l], w=['f_rb'])
            kb.op('dve', lambda e: e.memset(msum[:], 0.0), w=['f_msum'])
            triu = moec[:, 0:128]
            ecb = moec[:, 128:160]
            for t in range(NT):
                b = t % 2
                sx = 'f' + str(b)
                B_ = str(b)
                kb.dma('sp', xts[b][:], x_src[t * 128:(t + 1) * 128, :], r=['xsrc5'], w=['f_xt' + B_])
                stt, mv, rstd, nb, xn = bufs[b]
                ln_mod_tile(kb, nc, xts[b], 'f_xt' + B_, bufs[b], mod_bc[:, 4 * D:5 * D], mod_bc[:, 3 * D:4 * D],
                            xn[:], 'ln_xn' + sx, sx)
                cp(kb, 'act', hbs[b][:], xn[:], ['ln_xn' + sx], ['f_hb' + B_])
                for kc in range(8):
                    kb.op('pe', lambda e, kc=kc: e.matmul(pR[b][:, kc // 4, (kc % 4) * 128:(kc % 4 + 1) * 128],
                                                          lhsT=xn[:, kc * 128:(kc + 1) * 128], rhs=ident,
                                                          start=True, stop=True),
                          r=['ln_xn' + sx, 'cst'], w=['f_pR' + B_])
                cp(kb, 'dve', h32[b][:].rearrange("p (a c) n -> p a (c n)", a=2), pR[b][:], ['f_pR' + B_],
                   ['f_h32' + B_])
                for kc in range(8):
                    kb.op('pe', lambda e, kc=kc: e.matmul(pL[b][:, 0:NE], lhsT=h32[b][:, kc, :], rhs=rw[:, kc, :],
                                                          start=(kc == 0), stop=(kc == 7)),
                          r=['f_h32' + B_, 'f_rw'], w=['f_pL' + B_])
                kb.op('dve', lambda e: e.tensor_tensor(out=lg[b][:], in0=pL[b][:, 0:NE], in1=rb[:], op=ALU.add),
                      r=['f_pL' + B_, 'f_rb'], w=['f_lg' + B_])
                kb.op('dve', lambda e: e.max(out=m8[b][:, 0:8], in_=lg[b][:]), r=['f_lg' + B_], w=['f_m8' + B_])
                kb.op('dve', lambda e: e.tensor_scalar(out=mk[b][:], in0=lg[b][:], scalar1=m8[b][:, 3:4], scalar2=None,
                                                       op0=ALU.is_ge), r=['f_lg' + B_, 'f_m8' + B_], w=['f_mk' + B_])
                kb.op('dve', lambda e: e.tensor_scalar(out=m8[b][:, 8:9], in0=m8[b][:, 0:1], scalar1=-1.0, scalar2=None,
                                                       op0=ALU.mult), r=['f_m8' + B_], w=['f_m8' + B_])
                kb.op('act', lambda e: e.activation(out=lg[b][:], in_=lg[b][:], func=AF.Exp, bias=m8[b][:, 8:9],
                                                    scale=1.0), r=['f_lg' + B_, 'f_m8' + B_], w=['f_lg' + B_])
                kb.op('dve', lambda e: e.tensor_tensor(out=lg[b][:], in0=lg[b][:], in1=mk[b][:], op=ALU.mult),
                      r=['f_lg' + B_, 'f_mk' + B_], w=['f_lg' + B_])
                kb.op('dve', lambda e: e.reduce_sum(out=m8[b][:, 9:10], in_=lg[b][:], axis=AX.X),
                      r=['f_lg' + B_], w=['f_m8' + B_])
                kb.op('dve', lambda e: e.reciprocal(out=m8[b][:, 10:11], in_=m8[b][:, 9:10]),
                      r=['f_m8' + B_], w=['f_m8' + B_])
                kb.op('dve', lambda e: e.tensor_scalar(out=gt_[b][:], in0=lg[b][:], scalar1=m8[b][:, 10:11],
                                                       scalar2=None, op0=ALU.mult),
                      r=['f_lg' + B_, 'f_m8' + B_], w=['f_gt' + B_])
                kb.op('pe', lambda e: e.matmul(pC[b][:, 0:NE], lhsT=triu, rhs=mk[b][:], start=True, stop=False),
                      r=['f_moec', 'f_mk' + B_], w=['f_pC' + B_])
                kb.op('pe', lambda e: e.matmul(pC[b][:, 0:NE], lhsT=cst['ones_f'], rhs=msum[:], start=False, stop=True),
                      r=['cst', 'f_msum'], w=['f_pC' + B_])
                kb.op('dve', lambda e: e.tensor_tensor(out=msum[:], in0=msum[:], in1=mk[b][:], op=ALU.add),
                      r=['f_msum', 'f_mk' + B_], w=['f_msum'])
                kb.op('dve', lambda e: e.tensor_tensor(out=ngp[b][:], in0=pC[b][:, 0:NE], in1=ecb, op=ALU.add),
                      r=['f_pC' + B_, 'f_moec'], w=['f_ngp' + B_])
                kb.op('dve', lambda e: e.tensor_scalar(out=ngp[b][:], in0=ngp[b][:], scalar1=-1.0, scalar2=BIG,
                                                       op0=ALU.mult, op1=ALU.add), r=['f_ngp' + B_], w=['f_ngp' + B_])
                kb.op('dve', lambda e: e.tensor_tensor(out=ngp[b][:], in0=ngp[b][:], in1=mk[b][:], op=ALU.mult),
                      r=['f_ngp' + B_, 'f_mk' + B_], w=['f_ngp' + B_])
                kb.op('dve', lambda e: e.tensor_scalar(out=ngp[b][:], in0=ngp[b][:], scalar1=-BIG, scalar2=None,
                                                       op0=ALU.add), r=['f_ngp' + B_], w=['f_ngp' + B_])
                kb.op('dve', lambda e: e.max(out=t8[b][:], in_=ngp[b][:]), r=['f_ngp' + B_], w=['f_t8' + B_])
                dk = 'f_dest%d' % t
                kb.op('dve', lambda e: e.tensor_scalar(out=dest_all[:, t * 4:(t + 1) * 4], in0=t8[b][:, 0:4], scalar1=-1.0,
                                                       scalar2=None, op0=ALU.mult), r=['f_t8' + B_], w=[dk])
                for k in range(4):
                    kb.op('dve', lambda e, k=k: e.tensor_scalar(out=oh[b][:], in0=ngp[b][:], scalar1=t8[b][:, k:k + 1],
                                                               scalar2=None, op0=ALU.is_equal),
                          r=['f_ngp' + B_, 'f_t8' + B_], w=['f_oh' + B_])
                    kb.op('dve', lambda e, k=k: e.scalar_tensor_tensor(out=jk[b][:], in0=oh[b][:], scalar=1.0,
                                                                      in1=gt_[b][:], op0=ALU.mult, op1=ALU.mult,
                                                                      accum_out=gk_all[:, t, k:k + 1]),
                          r=['f_oh' + B_, 'f_gt' + B_], w=['f_jk' + B_, 'f_gk'])
                for k in range(4):
                    kb.ind('pool', ['f_hb' + B_, dk], ['s_hg'], out=hg[:, :],
                           out_offset=bass.IndirectOffsetOnAxis(ap=dest_all[:, t * 4 + k:t * 4 + k + 1], axis=0),
                           in_=hbs[b][:, :], in_offset=None, bounds_check=cst['breg'], oob_is_err=False)
            kb.barrier()
        with ExitStack() as s3:
            hgT = [sbt(nc, s3, "f_hgT%d" % i, [128, 8, C], BF16) for i in range(2)]
            hgb = [sbt(nc, s3, "f_hgb%d" % i, [128, D], BF16) for i in range(2)]
            actT = sbt(nc, s3, "f_actT", [128, 8, C], BF16)
            w1t = [sbt(nc, s3, "f_w1%d" % i, [128, 8, 512], BF16) for i in range(2)]
            w2t = [sbt(nc, s3, "f_w2%d" % i, [128, 8, D], BF16) for i in range(2)]
            b1t = [sbt(nc, s3, "f_b1%d" % i, [128, 16], F32) for i in range(2)]
            b2t = [sbt(nc, s3, "f_b2%d" % i, [1, D], BF16) for i in range(2)]
            onesb = sbt(nc, s3, "f_onesb", [1, 128], BF16)
            gl = [sbt(nc, s3, "f_gl%d" % i, [128, 512], F32) for i in range(2)]
            sg = [sbt(nc, s3, "f_sg%d" % i, [128, 512], F32) for i in range(2)]
            li = [sbt(nc, s3, "f_li%d" % i, [128, 512], F32) for i in range(2)]
            yrow = [sbt(nc, s3, "f_yr%d" % i, [128, D], F32) for i in range(2)]
            PTr = PsRot(kb, nc, s3, 2, "f_ptr", dt=BF16, shape=(128, 8, 128))
            PG = PsRot(kb, nc, s3, 4, "f_pg")
            PY = PsRot(kb, nc, s3, 2, "f_py")
            kb.op('dve', lambda e: e.tensor_copy(out=onesb[:], in_=cst['ones_f'][0:1, :]), r=['cst'], w=['f_onesb'])
            W1, W2 = A['exp_w1'], A['exp_w2']
            gcount = 0
            u = 0
            yc_ = 0
            for ex in range(NE):
                eb = ex % 2
                EB = str(eb)
                kb.dma('sp', b1t[eb][:], A['exp_b1l'][l, ex], w=['f_b1' + EB])
                kb.dma('pool', b2t[eb][:], A['exp_b2'][l, ex:ex + 1, :], w=['f_b2' + EB])
                kb.dma('pool', w2t[eb][:], W2[l, ex].rearrange("(kc p) n -> p kc n", p=128), w=['f_w2' + EB],
                       max_dma_last_dim=4096)
                for blk in range(NB):
                    hb_ = blk % 2
                    kb.dma('sp', hgb[hb_][:], hg[ex * C + blk * 128:ex * C + (blk + 1) * 128, :], r=['s_hg'],
                           w=['f_hgb%d' % hb_])
                    ptr, ptrk = PTr.next()
                    for kc in range(8):
                        kb.op('pe', lambda e, kc=kc: e.transpose(out=ptr[:, kc, :], in_=hgb[hb_][:, kc * 128:(kc + 1) * 128],
                                                                 identity=cst['ident_b'][:, :]),
                              r=['f_hgb%d' % hb_, 'cst'], w=[ptrk])
                    cp(kb, 'act' if blk % 2 == 0 else 'dve', hgT[eb][:, :, blk * 128:(blk + 1) * 128], ptr[:], [ptrk],
                       ['f_hgT' + EB])
                for g in range(4):
                    wb = gcount % 2
                    gcount += 1
                    kb.dma('pool', w1t[wb][:, :, 0:256],
                           W1[l, ex, :, g * 256:(g + 1) * 256].rearrange("(kc p) n -> p kc n", p=128), w=['f_w1%d' % wb])
                    kb.dma('pool', w1t[wb][:, :, 256:512],
                           W1[l, ex, :, DFF + g * 256:DFF + (g + 1) * 256].rearrange("(kc p) n -> p kc n", p=128),
                           w=['f_w1%d' % wb])
                    for fl in range(2):
                        fc = g * 2 + fl
                        for scn in range(C // 512):
                            tb = u % 2
                            u += 1
                            TB = str(tb)
                            sl = slice(scn * 512, (scn + 1) * 512)
                            pg, pgk = PG.next()
                            for kc in range(8):
                                kb.op('pe', lambda e, kc=kc: e.matmul(pg[:], lhsT=w1t[wb][:, kc, fl * 128:(fl + 1) * 128],
                                                                      rhs=hgT[eb][:, kc, sl], start=(kc == 0),
                                                                      stop=(kc == 7)),
                                      r=['f_w1%d' % wb, 'f_hgT' + EB], w=[pgk])
                            pl, plk = PG.next()
                            for kc in range(8):
                                kb.op('pe', lambda e, kc=kc: e.matmul(
                                    pl[:], lhsT=w1t[wb][:, kc, 256 + fl * 128:256 + (fl + 1) * 128],
                                    rhs=hgT[eb][:, kc, sl], start=(kc == 0), stop=(kc == 7)),
                                    r=['f_w1%d' % wb, 'f_hgT' + EB], w=[plk])
                            kb.op('dve', lambda e: e.tensor_scalar(out=gl[tb][:], in0=pg[:], scalar1=b1t[eb][:, fc:fc + 1],
                                                                   scalar2=7.0, op0=ALU.add, op1=ALU.min),
                                  r=[pgk, 'f_b1' + EB], w=['f_gl' + TB])
                            kb.op('act', lambda e: e.activation(out=sg[tb][:], in_=gl[tb][:], func=AF.Sigmoid,
                                                                scale=1.702), r=['f_gl' + TB], w=['f_sg' + TB])
                            kb.op('dve', lambda e: e.tensor_scalar(out=li[tb][:], in0=pl[:],
                                                                   scalar1=b1t[eb][:, 8 + fc:9 + fc], scalar2=7.0,
                                                                   op0=ALU.add, op1=ALU.min),
                                  r=[plk, 'f_b1' + EB], w=['f_li' + TB])
                            kb.op('dve', lambda e: e.tensor_scalar(out=li[tb][:], in0=li[tb][:], scalar1=-7.0,
                                                                   scalar2=1.0, op0=ALU.max, op1=ALU.add),
                                  r=['f_li' + TB], w=['f_li' + TB])
                            kb.op('pool', lambda e: e.tensor_tensor(out=gl[tb][:], in0=gl[tb][:], in1=sg[tb][:],
                                                                    op=ALU.mult),
                                  r=['f_gl' + TB, 'f_sg' + TB], w=['f_gl' + TB])
                            kb.op('pool', lambda e: e.tensor_tensor(out=actT[:, fc, sl], in0=gl[tb][:], in1=li[tb][:],
                                                                    op=ALU.mult),
                                  r=['f_gl' + TB, 'f_li' + TB], w=['f_actT'])
                for blk in range(NB):
                    yb_ = yc_ % 2
                    yc_ += 1
                    for half in range(2):
                        py, pyk = PY.next()
                        for fc in range(8):
                            kb.op('pe', lambda e, fc=fc: e.matmul(py[:], lhsT=actT[:, fc, blk * 128:(blk + 1) * 128],
                                                                  rhs=w2t[eb][:, fc, half * 512:(half + 1) * 512],
                                                                  start=(fc == 0), stop=False),
                                  r=['f_actT', 'f_w2' + EB], w=[pyk])
                        kb.op('pe', lambda e: e.matmul(py[:], lhsT=onesb[0:1, :],
                                                       rhs=b2t[eb][0:1, half * 512:(half + 1) * 512],
                                                       start=False, stop=True), r=['f_onesb', 'f_b2' + EB], w=[pyk])
                        cp(kb, 'act' if half == 0 else 'dve', yrow[yb_][:, half * 512:(half + 1) * 512], py[:], [pyk],
                           ['f_yr%d' % yb_])
                    kb.dma('sp', yg[ex * C + blk * 128:ex * C + (blk + 1) * 128, :], yrow[yb_][:], r=['f_yr%d' % yb_],
                           w=['s_yg'])
            kb.barrier()
        with ExitStack() as s4:
            rows = [sbt(nc, s4, "f_row%d" % i, [128, D], F32) for i in range(4)]
            accs = [sbt(nc, s4, "f_acc%d" % i, [128, D], F32) for i in range(2)]
            xt = [sbt(nc, s4, "f_x%d" % i, [128, D], F32) for i in range(2)]
            lng = sbt(nc, s4, "f_lng", [128, D], F32)
            lnb = sbt(nc, s4, "f_lnb", [128, D], F32)
            lb = [(sbt(nc, s4, "f_lst%d" % i, [128, 2, 6], F32), sbt(nc, s4, "f_lmv%d" % i, [128, 2], F32),
                   sbt(nc, s4, "f_lrs%d" % i, [128, 1], F32), sbt(nc, s4, "f_lnb%d" % i, [128, 1], F32))
                  for i in range(2)]
            kb.dma('sp', lng[:], A['ln2g_bc'][l], w=['lngb'])
            kb.dma('sp', lnb[:], A['ln2b_bc'][l], w=['lngb'])
            for T in range(NT):
                xb = T % 2
                XB = str(xb)
                kb.dma('sp', xt[xb][:], x_src[T * 128:(T + 1) * 128, :], r=['xsrc5'], w=['f_x' + XB])
                for k in range(4):
                    kb.ind('pool', ['s_yg', 'f_dest%d' % T], ['f_row%d' % k], out=rows[k][:, :], out_offset=None,
                           in_=yg[:, :], in_offset=bass.IndirectOffsetOnAxis(ap=dest_all[:, T * 4 + k:T * 4 + k + 1], axis=0),
                           bounds_check=cst['breg'], oob_is_err=False)
                kb.op('dve', lambda e: e.tensor_scalar(out=accs[xb][:], in0=rows[0][:], scalar1=gk_all[:, T, 0:1],
                                                       scalar2=None, op0=ALU.mult),
                      r=['f_row0', 'f_gk'], w=['f_acc' + XB])
                for k in range(1, 4):
                    kb.op('dve', lambda e, k=k: e.scalar_tensor_tensor(out=accs[xb][:], in0=rows[k][:],
                                                                      scalar=gk_all[:, T, k:k + 1], in1=accs[xb][:],
                                                                      op0=ALU.mult, op1=ALU.add),
                          r=['f_row%d' % k, 'f_gk', 'f_acc' + XB], w=['f_acc' + XB])
                kb.op('pool', lambda e: e.tensor_tensor(out=accs[xb][:], in0=accs[xb][:], in1=mod_bc[:, 5 * D:6 * D],
                                                        op=ALU.mult), r=['f_acc' + XB, 'mod_bc'], w=['f_acc' + XB])
                kb.op('dve', lambda e: e.scalar_tensor_tensor(out=xt[xb][:], in0=xt[xb][:], scalar=ALPHA,
                                                              in1=accs[xb][:], op0=ALU.mult, op1=ALU.add),
                      r=['f_x' + XB, 'f_acc' + XB], w=['f_x' + XB])
                ln_affine_store(kb, nc, xt[xb], 'f_x' + XB, lb[xb], lng[:], lnb[:],
                                x_dst[T * 128:(T + 1) * 128, :], 'f' + XB, 'xdst5')
            kb.barrier()
```

```python
import numpy as np
from contextlib import ExitStack
import concourse.bass as bass
import concourse.mybir as mybir
from concourse.bass_utils import run_bass_kernel_spmd
from concourse.alu_op_type import AluOpType as ALU

F32, BF16 = mybir.dt.float32, mybir.dt.bfloat16
I32 = mybir.dt.int32
CAP = 1536
BIG = 1.0e6
AF = mybir.ActivationFunctionType
AX = mybir.AxisListType

D = 1024
S = 4096
NT = S // 128
DEPTH = 4
INW = 6408
NE = 32
DFF = 1024
ALPHA = (2 * DEPTH) ** 0.25
NEG = -30000.0
DBG = {}


class KB:
    def __init__(self, nc, es, ndma=48):
        self.nc = nc
        self.eng = {'pe': nc.tensor, 'dve': nc.vector, 'act': nc.scalar, 'pool': nc.gpsimd, 'sp': nc.sync}
        self.sems = []
        self.psid = {}
        for e in self.eng:
            self.psid[e] = len(self.sems)
            self.sems.append(es.enter_context(nc.semaphore("ps_" + e)))
        self.cnt = {e: 0 for e in self.eng}
        self.dsid = []
        for i in range(ndma):
            self.dsid.append(len(self.sems))
            self.sems.append(es.enter_context(nc.semaphore("ds%d" % i)))
        self.dcum = [0] * ndma
        self.dnext = 0
        self.seen = {e: {} for e in self.eng}
        self.lastw = {}
        self.reads = {}
        self.nins = 0
        self.excl = set()

    def need(self, E, tok):
        sid, val, _ = tok
        if self.seen[E].get(sid, 0) < val:
            self.eng[E].wait_ge(self.sems[sid], val)
            self.seen[E][sid] = val
            if 'log' in DBG:
                DBG['log'].append("%s WAIT sem%d>=%d" % (E, sid, val))

    def _deps(self, E, r, w, isdma):
        for k in r:
            t = self.lastw.get(k)
            if t is not None:
                self.need(E, t)
            if k in self.excl:
                for t in self.reads.get(k, {}).values():
                    if t[2] != E:
                        self.need(E, t)
        for k in w:
            t = self.lastw.get(k)
            if t is not None and (isdma or t[2] != E):
                self.need(E, t)
            for t in self.reads.get(k, {}).values():
                if isdma or t[2] != E:
                    self.need(E, t)

    def _commit(self, tok, r, w):
        for k in r:
            self.reads.setdefault(k, {})[tok[0]] = tok
        for k in w:
            self.lastw[k] = tok
            self.reads[k] = {}

    def op(self, E, fn, r=(), w=()):
        self._deps(E, r, w, False)
        ins = fn(self.eng[E])
        ins.then_inc(self.sems[self.psid[E]], 1)
        self.cnt[E] += 1
        self.nins += 1
        tok = (self.psid[E], self.cnt[E], E)
        if 'log' in DBG:
            DBG['log'].append("%s OP#%d r=%s w=%s" % (E, self.cnt[E], list(r), list(w)))
        self._commit(tok, r, w)
        return tok

    def dma(self, Q, out, in_, r=(), w=(), **kw):
        self._deps(Q, r, w, True)
        i = self.dnext
        self.dnext = (i + 1) % len(self.dsid)
        if self.dcum[i] > 0:
            self.need(Q, (self.dsid[i], self.dcum[i], None))
        self.eng[Q].dma_start(out=out, in_=in_, **kw).then_inc(self.sems[self.dsid[i]], 16)
        self.dcum[i] += 16
        self.nins += 1
        tok = (self.dsid[i], self.dcum[i], None)
        if 'log' in DBG:
            DBG['log'].append("%s DMA sem%d->%d r=%s w=%s" % (Q, self.dsid[i], self.dcum[i], list(r), list(w)))
        self._commit(tok, r, w)
        return tok

    def ind(self, Q, r, w, **kw):
        self._deps(Q, r, w, True)
        i = self.dnext
        self.dnext = (i + 1) % len(self.dsid)
        if self.dcum[i] > 0:
            self.need(Q, (self.dsid[i], self.dcum[i], None))
        self.eng[Q].indirect_dma_start(**kw).then_inc(self.sems[self.dsid[i]], 16)
        self.dcum[i] += 16
        self.nins += 1
        tok = (self.dsid[i], self.dcum[i], None)
        self._commit(tok, r, w)
        return tok

    def barrier(self):
        for E in self.eng:
            for F in self.eng:
                if F != E and self.cnt[F] > 0:
                    self.need(E, (self.psid[F], self.cnt[F], F))
            for i, s in enumerate(self.dsid):
                if self.dcum[i] > 0:
                    self.need(E, (s, self.dcum[i], None))


_UID = [0]


def _uname(name):
    _UID[0] += 1
    return "%s_u%d" % (name, _UID[0])


def sbt(nc, st, name, shape, dt):
    return st.enter_context(nc.sbuf_tensor(_uname(name), shape, dt))


def pst(nc, st, name, shape, dt):
    return st.enter_context(nc.psum_tensor(_uname(name), shape, dt))


def stage_mod(kb, nc, l, A, mod_bc, cst):
    with ExitStack() as st:
        cc = sbt(nc, st, "m_cc", [128, 8], F32)
        cond = sbt(nc, st, "m_cond", [128, 8], F32)
        crep = sbt(nc, st, "m_crep", [128, 8, 128], F32)
        brow = sbt(nc, st, "m_brow", [1, 6144], F32)
        wa = [sbt(nc, st, "m_wa%d" % i, [128, 8, 512], F32) for i in range(2)]
        ps = [pst(nc, st, "m_ps%d" % i, [128, 512], F32) for i in range(2)]
        kb.dma('sp', cc[:], A['c_col'][:, :], w=['m_cc'])
        kb.dma('sp', brow[:], A['ada_b'][l:l + 1, :], w=['m_brow'])
        kb.op('act', lambda e: e.activation(out=cond[:], in_=cc[:], func=AF.Silu), r=['m_cc'], w=['m_cond'])
        for kc in range(8):
            kb.op('dve', lambda e, kc=kc: e.tensor_scalar(out=crep[:, kc, :], in0=cst['ones_f'][:, :],
                                                         scalar1=cond[:, kc:kc + 1], scalar2=None, op0=ALU.mult),
                  r=['m_cond', 'cst'], w=['m_crep'])
        for j in range(12):
            b = j % 2
            kb.dma('sp' if j % 2 == 0 else 'pool', wa[b][:],
                   A['ada_w'][l, :, j * 512:(j + 1) * 512].rearrange("(kc p) n -> p kc n", p=128),
                   w=['m_wa%d' % b])
            for kc in range(8):
                kb.op('pe', lambda e, kc=kc, b=b: e.matmul(ps[b][:], lhsT=crep[:, kc, :], rhs=wa[b][:, kc, :],
                                                           start=(kc == 0), stop=False),
                      r=['m_crep', 'm_wa%d' % b], w=['m_ps%d' % b])
            kb.op('pe', lambda e, b=b, j=j: e.matmul(ps[b][:], lhsT=cst['ones_f'][0:1, :],
                                                     rhs=brow[0:1, j * 512:(j + 1) * 512], start=False, stop=True),
                  r=['m_brow', 'cst'], w=['m_ps%d' % b])
            addone = 1.0 if (j // 2) in (1, 2, 4, 5) else 0.0
            kb.op('act', lambda e, b=b, j=j, addone=addone: e.activation(
                out=mod_bc[:, j * 512:(j + 1) * 512], in_=ps[b][:], func=AF.Identity, bias=addone, scale=1.0),
                r=['m_ps%d' % b], w=['mod_bc'])
        kb.barrier()


def ln_mod_tile(kb, nc, xt, xk, bufs, sc_ap, sh_ap, h_out, hk, sfx):
    stt, mv, rstd, nb, xn = bufs
    kb.op('dve', lambda e: e.bn_stats(out=stt[:, 0, :], in_=xt[:, 0:512]), r=[xk], w=['ln_st' + sfx])
    kb.op('dve', lambda e: e.bn_stats(out=stt[:, 1, :], in_=xt[:, 512:1024]), r=[xk], w=['ln_st' + sfx])
    kb.op('dve', lambda e: e.bn_aggr(out=mv[:], in_=stt[:].rearrange("p a b -> p (a b)")),
          r=['ln_st' + sfx], w=['ln_mv' + sfx])
    kb.op('act', lambda e: e.activation(out=rstd[:], in_=mv[:, 1:2], func=AF.Sqrt, bias=1e-5, scale=1.0),
          r=['ln_mv' + sfx], w=['ln_rstd' + sfx])
    kb.op('dve', lambda e: e.reciprocal(out=rstd[:], in_=rstd[:]), r=['ln_rstd' + sfx], w=['ln_rstd' + sfx])
    kb.op('dve', lambda e: e.scalar_tensor_tensor(out=nb[:], in0=mv[:, 0:1], scalar=-1.0, in1=rstd[:],
                                                  op0=ALU.mult, op1=ALU.mult),
          r=['ln_mv' + sfx, 'ln_rstd' + sfx], w=['ln_nb' + sfx])
    kb.op('act', lambda e: e.activation(out=xn[:], in_=xt[:], func=AF.Identity, bias=nb[:], scale=rstd[:]),
          r=[xk, 'ln_nb' + sfx, 'ln_rstd' + sfx], w=['ln_xn' + sfx])
    kb.op('dve', lambda e: e.tensor_tensor(out=xn[:], in0=xn[:], in1=sc_ap, op=ALU.mult),
          r=['ln_xn' + sfx, 'mod_bc'], w=['ln_xn' + sfx])
    kb.op('dve', lambda e: e.tensor_tensor(out=h_out, in0=xn[:], in1=sh_ap, op=ALU.add),
          r=['ln_xn' + sfx, 'mod_bc'], w=[hk])


def build_hT(kb, nc, st, x_src, mod_bc, sc_off, sh_off, hT, cst, pfx):
    xts = [sbt(nc, st, pfx + "xt%d" % i, [128, D], F32) for i in range(2)]
    hbs = [sbt(nc, st, pfx + "hb%d" % i, [128, D], BF16) for i in range(2)]
    bufs = []
    for i in range(2):
        bufs.append((sbt(nc, st, pfx + "st%d" % i, [128, 2, 6], F32), sbt(nc, st, pfx + "mv%d" % i, [128, 2], F32),
                     sbt(nc, st, pfx + "rs%d" % i, [128, 1], F32), sbt(nc, st, pfx + "nb%d" % i, [128, 1], F32),
                     sbt(nc, st, pfx + "xn%d" % i, [128, D], F32)))
    pT = [pst(nc, st, pfx + "pT%d" % i, [128, 8, 128], BF16) for i in range(2)]
    for t in range(NT):
        b = t % 2
        kb.dma('sp', xts[b][:], x_src[t * 128:(t + 1) * 128, :], r=[pfx + 'xsrc'], w=[pfx + 'xt%d' % b])
        ln_mod_tile(kb, nc, xts[b], pfx + 'xt%d' % b, bufs[b], mod_bc[:, sc_off:sc_off + D],
                    mod_bc[:, sh_off:sh_off + D], hbs[b][:], pfx + 'hb%d' % b, pfx + str(b))
        for kc in range(8):
            kb.op('pe', lambda e, kc=kc, b=b: e.transpose(out=pT[b][:, kc, :], in_=hbs[b][:, kc * 128:(kc + 1) * 128],
                                                          identity=cst['ident_b'][:, :]),
                  r=[pfx + 'hb%d' % b, 'cst'], w=[pfx + 'pT%d' % b])
        eng = 'act' if t % 2 == 0 else 'dve'
        if eng == 'act':
            kb.op('act', lambda e, b=b, t=t: e.copy(out=hT[:, :, t * 128:(t + 1) * 128], in_=pT[b][:]),
                  r=[pfx + 'pT%d' % b], w=['hT'])
        else:
            kb.op('dve', lambda e, b=b, t=t: e.tensor_copy(out=hT[:, :, t * 128:(t + 1) * 128], in_=pT[b][:]),
                  r=[pfx + 'pT%d' % b], w=['hT'])


def stage_proj(kb, nc, l, A, SC, x_src, mod_bc, cst):
    with ExitStack() as st:
        hT = sbt(nc, st, "hT", [128, 8, S], BF16)
        with ExitStack() as st2:
            build_hT(kb, nc, st2, x_src, mod_bc, 1 * D, 0, hT, cst, "p1_")
            kb.barrier()
        wts = [sbt(nc, st, "wt%d" % i, [128, 8, 512], BF16) for i in range(2)]
        raw = sbt(nc, st, "raw", [128, S + 4], F32)
        acc = sbt(nc, st, "acc", [128, S], F32)
        tmp = [sbt(nc, st, "tmp%d" % i, [128, 512], F32) for i in range(2)]
        gb = [sbt(nc, st, "gb%d" % i, [128, S], BF16) for i in range(2)]
        cw = sbt(nc, st, "cw", [128, 12, 4], F32)
        alog = sbt(nc, st, "alog", [128, 4], F32)
        dtb = sbt(nc, st, "dtb", [128, 4], F32)
        sm = sbt(nc, st, "sm", [128, 6, 4], F32)
        g_all = sbt(nc, st, "g_all", [128, NT, 4], F32)
        b_all = sbt(nc, st, "b_all", [128, NT, 4], F32)
        vb_all = sbt(nc, st, "vb_all", [128, NT, 128], BF16)
        zt = [sbt(nc, st, "zt%d" % i, [128, 512], F32) for i in range(2)]
        ps = [pst(nc, st, "pj_ps%d" % i, [128, 512], F32) for i in range(4)]
        kb.dma('sp', cw[:], A['conv_wl'][l], w=['cw'])
        kb.dma('sp', alog[:], A['alog_bc'][l], w=['alog'])
        kb.dma('sp', dtb[:], A['dtb_bc'][l], w=['dtb'])
        kb.op('act', lambda e: e.activation(out=alog[:], in_=alog[:], func=AF.Exp), r=['alog'], w=['alog'])
        kb.op('dve', lambda e: e.memset(raw[:, 0:4], 0.0), w=['raw'])
        W = A['w_in']
        state = {'g': 0, 'ps': 0, 'gb': 0}

        def load_group(pieces):
            b = state['g'] % 2
            state['g'] += 1
            for (off, c0, n) in pieces:
                kb.dma('pool', wts[b][:, :, off:off + n],
                       W[l, :, c0:c0 + n].rearrange("(kc p) n -> p kc n", p=128), w=['wt%d' % b])
            return b

        def fm_chunk(b, off, evac):
            for tc in range(8):
                p = state['ps'] % 4
                state['ps'] += 1
                for kc in range(8):
                    kb.op('pe', lambda e, kc=kc, p=p, tc=tc: e.matmul(
                        ps[p][:], lhsT=wts[b][:, kc, off:off + 128], rhs=hT[:, kc, tc * 512:(tc + 1) * 512],
                        start=(kc == 0), stop=(kc == 7)), r=['wt%d' % b, 'hT'], w=['pj_ps%d' % p])
                evac(tc, ps[p], 'pj_ps%d' % p)

        def conv_chunk(b, off, ch):
            def ev(tc, p, pk):
                eng = 'act' if tc % 2 == 0 else 'dve'
                if eng == 'act':
                    kb.op('act', lambda e: e.copy(out=raw[:, 4 + tc * 512: 4 + (tc + 1) * 512], in_=p[:]),
                          r=[pk], w=['raw'])
                else:
                    kb.op('dve', lambda e: e.tensor_copy(out=raw[:, 4 + tc * 512: 4 + (tc + 1) * 512], in_=p[:]),
                          r=[pk], w=['raw'])
            fm_chunk(b, off, ev)
            kb.op('dve', lambda e: e.tensor_scalar(out=acc[:], in0=raw[:, 1:1 + S], scalar1=cw[:, ch, 0:1],
                                                   scalar2=None, op0=ALU.mult), r=['raw', 'cw'], w=['acc'])
            for j in range(1, 4):
                kb.op('dve', lambda e, j=j: e.scalar_tensor_tensor(out=acc[:], in0=raw[:, 1 + j:1 + j + S],
                                                                  scalar=cw[:, ch, j:j + 1], in1=acc[:],
                                                                  op0=ALU.mult, op1=ALU.add),
                      r=['raw', 'cw', 'acc'], w=['acc'])
            kb.op('act', lambda e: e.activation(out=acc[:], in_=acc[:], func=AF.Silu), r=['acc'], w=['acc'])
            if ch < 8:
                kb.op('act', lambda e: e.activation(out=raw[:, 4:4 + S], in_=acc[:], func=AF.Square),
                      r=['acc'], w=['raw'])
                qs = (128 ** -0.5) if ch < 4 else 1.0
                for tc in range(8):
                    p = state['ps'] % 4
                    state['ps'] += 1
                    tb = tc % 2
                    kb.op('pe', lambda e, p=p, tc=tc: e.matmul(ps[p][:], lhsT=cst['ones_f'][:, :],
                                                               rhs=raw[:, 4 + tc * 512:4 + (tc + 1) * 512],
                                                               start=True, stop=True),
                          r=['raw', 'cst'], w=['pj_ps%d' % p])
                    kb.op('act', lambda e, p=p, tb=tb: e.activation(out=tmp[tb][:], in_=ps[p][:], func=AF.Sqrt,
                                                                    bias=1e-6, scale=1.0),
                          r=['pj_ps%d' % p], w=['tmp%d' % tb])
                    kb.op('dve', lambda e, tb=tb: e.reciprocal(out=tmp[tb][:], in_=tmp[tb][:]),
                          r=['tmp%d' % tb], w=['tmp%d' % tb])
                    kb.op('dve', lambda e, tb=tb, tc=tc: e.scalar_tensor_tensor(
                        out=acc[:, tc * 512:(tc + 1) * 512], in0=acc[:, tc * 512:(tc + 1) * 512], scalar=qs,
                        in1=tmp[tb][:], op0=ALU.mult, op1=ALU.mult), r=['tmp%d' % tb, 'acc'], w=['acc'])
            kb.dma('sp', SC['qkv'][ch], acc[:], r=['acc'], w=['s_qkv'])

        for gi in range(3):
            b = load_group([(0, gi * 512, 512)])
            for j in range(4):
                conv_chunk(b, j * 128, gi * 4 + j)

        b = load_group([(0, 1536, 512)])
        for t in range(NT):
            p = state['ps'] % 4
            state['ps'] += 1
            for kc in range(8):
                kb.op('pe', lambda e, kc=kc, p=p, t=t: e.matmul(ps[p][:], lhsT=hT[:, kc, t * 128:(t + 1) * 128],
                                                                rhs=wts[b][:, kc, 0:512], start=(kc == 0),
                                                                stop=(kc == 7)),
                      r=['wt%d' % b, 'hT'], w=['pj_ps%d' % p])
            zb = t % 2
            kb.op('act', lambda e, p=p, zb=zb: e.activation(out=zt[zb][:], in_=ps[p][:], func=AF.Silu),
                  r=['pj_ps%d' % p], w=['zt%d' % zb])
            kb.dma('sp', SC['sz'][t * 128:(t + 1) * 128, :], zt[zb][:], r=['zt%d' % zb], w=['s_sz'])

        b = load_group([(0, 2048, 8), (128, 2568, 256)])
        for t in range(NT):
            p = state['ps'] % 4
            state['ps'] += 1
            for kc in range(8):
                kb.op('pe', lambda e, kc=kc, p=p, t=t: e.matmul(ps[p][:, 0:8], lhsT=hT[:, kc, t * 128:(t + 1) * 128],
                                                                rhs=wts[b][:, kc, 0:8], start=(kc == 0),
                                                                stop=(kc == 7)),
                      r=['wt%d' % b, 'hT'], w=['pj_ps%d' % p])
            pk = 'pj_ps%d' % p
            P = ps[p]
            kb.op('dve', lambda e, P=P: e.tensor_tensor(out=sm[:, 0, :], in0=P[:, 0:4], in1=dtb[:], op=ALU.add),
                  r=[pk, 'dtb'], w=['sm0'])
            kb.op('dve', lambda e: e.scalar_tensor_tensor(out=sm[:, 1, :], in0=sm[:, 0, :], scalar=-1.0,
                                                          in1=sm[:, 0, :], op0=ALU.mult, op1=ALU.min),
                  r=['sm0'], w=['sm1'])
            kb.op('act', lambda e: e.activation(out=sm[:, 2, :], in_=sm[:, 1, :], func=AF.Exp, scale=1.0),
                  r=['sm1'], w=['sm2'])
            kb.op('act', lambda e: e.activation(out=sm[:, 3, :], in_=sm[:, 2, :], func=AF.Ln, bias=1.0, scale=1.0),
                  r=['sm2'], w=['sm3'])
            kb.op('dve', lambda e: e.scalar_tensor_tensor(out=sm[:, 4, :], in0=sm[:, 0, :], scalar=0.0,
                                                          in1=sm[:, 3, :], op0=ALU.max, op1=ALU.add),
                  r=['sm0', 'sm3'], w=['sm4'])
            kb.op('dve', lambda e, t=t: e.scalar_tensor_tensor(out=g_all[:, t, :], in0=sm[:, 4, :], scalar=-1.0,
                                                              in1=alog[:], op0=ALU.mult, op1=ALU.mult),
                  r=['sm4', 'alog'], w=['g_all'])
            kb.op('act', lambda e, P=P, t=t: e.activation(out=b_all[:, t, :], in_=P[:, 4:8], func=AF.Sigmoid),
                  r=[pk], w=['b_all'])
        kb.dma('sp', SC['g'].rearrange("(t p) h -> p t h", p=128), g_all[:], r=['g_all'], w=['s_g'])
        kb.dma('sp', SC['beta'].rearrange("(t p) h -> p t h", p=128), b_all[:], r=['b_all'], w=['s_beta'])
        for t in range(NT):
            p = state['ps'] % 4
            state['ps'] += 1
            for kc in range(8):
                kb.op('pe', lambda e, kc=kc, p=p, t=t: e.matmul(ps[p][:, 0:128], lhsT=hT[:, kc, t * 128:(t + 1) * 128],
                                                                rhs=wts[b][:, kc, 256:384], start=(kc == 0),
                                                                stop=(kc == 7)),
                      r=['wt%d' % b, 'hT'], w=['pj_ps%d' % p])
            kb.op('dve', lambda e, p=p, t=t: e.tensor_copy(out=vb_all[:, t, :], in_=ps[p][:, 0:128]),
                  r=['pj_ps%d' % p], w=['vb_all'])
        kb.dma('sp', SC['vb'].rearrange("(t p) d -> p t d", p=128), vb_all[:], r=['vb_all'], w=['s_vb'])

        def bf_chunk(b, off, dst, fn=None):
            g = state['gb'] % 2
            state['gb'] += 1

            def ev(tc, p, pk):
                if fn is not None:
                    kb.op('act', lambda e: e.activation(out=gb[g][:, tc * 512:(tc + 1) * 512], in_=p[:], func=fn),
                          r=[pk], w=['gb%d' % g])
                elif tc % 2 == 0:
                    kb.op('act', lambda e: e.copy(out=gb[g][:, tc * 512:(tc + 1) * 512], in_=p[:]),
                          r=[pk], w=['gb%d' % g])
                else:
                    kb.op('dve', lambda e: e.tensor_copy(out=gb[g][:, tc * 512:(tc + 1) * 512], in_=p[:]),
                          r=[pk], w=['gb%d' % g])
            fm_chunk(b, off, ev)
            kb.dma('sp', dst, gb[g][:], r=['gb%d' % g], w=['s_misc'])

        bf_chunk(b, 128, SC['kb'])
        pieces = []
        for j in range(4):
            pieces.append((j * 128, 2056 + j * 64, 64))
            pieces.append((j * 128 + 64, 2056 + (j + 4) * 64, 64))
        b = load_group(pieces)
        for j in range(4):
            bf_chunk(b, j * 128, SC['qb'][j])
        b = load_group([(0, 2824, 512)])
        for j in range(4):
            def ev(tc, p, pk):
                if tc % 2 == 0:
                    kb.op('act', lambda e: e.copy(out=acc[:, tc * 512:(tc + 1) * 512], in_=p[:]), r=[pk], w=['acc'])
                else:
                    kb.op('dve', lambda e: e.tensor_copy(out=acc[:, tc * 512:(tc + 1) * 512], in_=p[:]),
                          r=[pk], w=['acc'])
            fm_chunk(b, j * 128, ev)
            kb.dma('sp', SC['uc'][j], acc[:], r=['acc'], w=['s_uc'])
        for gi in range(6):
            b = load_group([(0, 3336 + gi * 512, 512)])
            for j in range(4):
                bf_chunk(b, j * 128, SC['gate'][gi * 4 + j], fn=AF.Sigmoid)
        kb.barrier()


def alloc_scratch(nc):
    SC = {}
    SC['qkv'] = nc.dram_tensor("s_qkv", [12, 128, S], F32).ap()
    SC['sz'] = nc.dram_tensor("s_sz", [S, 512], F32).ap()
    SC['g'] = nc.dram_tensor("s_g", [S, 4], F32).ap()
    SC['beta'] = nc.dram_tensor("s_beta", [S, 4], F32).ap()
    SC['qb'] = nc.dram_tensor("s_qb", [4, 128, S], BF16).ap()
    SC['kb'] = nc.dram_tensor("s_kb", [128, S], BF16).ap()
    SC['vb'] = nc.dram_tensor("s_vb", [S, 128], BF16).ap()
    SC['uc'] = nc.dram_tensor("s_uc", [4, 128, S], F32).ap()
    SC['gate'] = nc.dram_tensor("s_gate", [24, 128, S], BF16).ap()
    return SC


def cp(kb, eng, out, in_, r, w):
    if eng == 'act':
        return kb.op('act', lambda e: e.copy(out=out, in_=in_), r=r, w=w)
    return kb.op(eng, lambda e: e.tensor_copy(out=out, in_=in_), r=r, w=w)


class PsRot:
    def __init__(self, kb, nc, st, n, pfx, dt=F32, shape=(128, 512)):
        self.t = [pst(nc, st, "%s%d" % (pfx, i), list(shape), dt) for i in range(n)]
        self.k = ["%s%d" % (pfx, i) for i in range(n)]
        self.i = 0
        kb.excl.update(self.k)

    def next(self):
        i = self.i
        self.i = (i + 1) % len(self.t)
        return self.t[i], self.k[i]


def stage_gdn(kb, nc, l, A, SC, cst):
    ones, ident = cst['ones_f'], cst['ident_f']
    with ExitStack() as st:
        gm = sbt(nc, st, "g_gm", [128, 6, 128], F32)
        kb.dma('sp', gm[:], A['gm'][:, :, :], w=['cst'])
        g_all = sbt(nc, st, "g_gall", [128, 128], F32)
        b_all = sbt(nc, st, "g_ball", [128, 128], F32)
        gc = sbt(nc, st, "g_gc", [128, 128], F32)
        ngc = sbt(nc, st, "g_ngc", [128, 128], F32)
        egc = sbt(nc, st, "g_egc", [128, 128], F32)
        ekl = sbt(nc, st, "g_ekl", [128, 128], F32)
        gl0e = sbt(nc, st, "g_gl0e", [128, 128], F32)
        gl1e = sbt(nc, st, "g_gl1e", [128, 128], F32)
        negb = sbt(nc, st, "g_negb", [128, 128], F32)
        nw = sbt(nc, st, "g_nw", [128, 128], F32)
        pst0 = ExitStack()
        PS = PsRot(kb, nc, pst0, 4, "g_ps")
        kb.dma('sp', g_all[:].rearrange("p (t h) -> p t h", h=4), SC['g'].rearrange("(t p) h -> p t h", p=128),
               r=['s_g'], w=['g_gall'])
        kb.dma('sp', b_all[:].rearrange("p (t h) -> p t h", h=4), SC['beta'].rearrange("(t p) h -> p t h", p=128),
               r=['s_beta'], w=['g_ball'])
        kb.dma('sp', nw[:], A['gnw_bc'][l], w=['g_nw'])
        p, pk = PS.next()
        kb.op('pe', lambda e: e.matmul(p[:, 0:128], lhsT=gm[:, 0, :], rhs=g_all[:], start=True, stop=True),
              r=['g_gall', 'cst'], w=[pk])
        cp(kb, 'dve', gc[:], p[:, 0:128], [pk], ['g_gc'])
        kb.op('act', lambda e: e.activation(out=egc[:], in_=gc[:], func=AF.Exp), r=['g_gc'], w=['g_egc'])
        kb.op('dve', lambda e: e.tensor_scalar(out=ngc[:], in0=gc[:], scalar1=-1.0, scalar2=None, op0=ALU.mult),
              r=['g_gc'], w=['g_ngc'])
        p2, pk2 = PS.next()
        kb.op('pe', lambda e: e.matmul(p2[:, 0:128], lhsT=gm[:, 1, :], rhs=g_all[:], start=True, stop=True),
              r=['g_gall', 'cst'], w=[pk2])
        kb.op('dve', lambda e: e.tensor_tensor(out=ekl[:], in0=p2[:, 0:128], in1=gc[:], op=ALU.subtract),
              r=[pk2, 'g_gc'], w=['g_ekl'])
        kb.op('act', lambda e: e.activation(out=ekl[:], in_=ekl[:], func=AF.Exp), r=['g_ekl'], w=['g_ekl'])
        for idx, dst, dk in ((2, gl0e, 'g_gl0e'), (3, gl1e, 'g_gl1e')):
            p3, pk3 = PS.next()
            kb.op('pe', lambda e, p3=p3, idx=idx: e.matmul(p3[:, 0:128], lhsT=gm[:, idx, :], rhs=g_all[:],
                                                           start=True, stop=True), r=['g_gall', 'cst'], w=[pk3])
            kb.op('act', lambda e, p3=p3, dst=dst: e.activation(out=dst[:], in_=p3[:, 0:128], func=AF.Exp),
                  r=[pk3], w=[dk])
        kb.op('dve', lambda e: e.tensor_scalar(out=negb[:], in0=b_all[:], scalar1=-1.0, scalar2=None, op0=ALU.mult),
              r=['g_ball'], w=['g_negb'])

        kb.barrier()
        pst0.close()
        HT = 1024
        ecnt = [0]

        def mm(out_p, pk, lhsT, rhs, r, start=True, stop=True):
            return kb.op('pe', lambda e: e.matmul(out_p, lhsT=lhsT, rhs=rhs, start=start, stop=stop), r=r, w=[pk])

        names = ['ke', 'kg', 'vtok', 'dg', 'tmpm', 'decT', 'EB', 'qg', 'attnT', 'PT0', 'P0', 'PT1', 'P1', 'GT0', 'GT1',
                 'bu', 'wT', 'vnA', 'vnB', 'Sa', 'Sb', 'otok', 'junk', 'ytok']

        def head_gen(h, ch):
            GP = 'g%d_' % ch
            qkvb = [sbt(nc, st, "g_qkv%d" % i, [128, 3, HT], F32) for i in range(2)]
            szb = sbt(nc, st, "g_sz", [128, NT, 128], F32)
            yaT = sbt(nc, st, "g_yaT", [128, S], BF16)
            B = {n: sbt(nc, st, "g_" + n, [128, 128], F32) for n in names}
            ss = sbt(nc, st, "g_ss", [128, 1], F32)
            PS = PsRot(kb, nc, st, 4, "g%d_ps" % ch)
            for h in (h, h + 2):
                kb.op('dve', lambda e: e.memset(B['vnA'][:], 0.0), w=[GP + 'vnA'])
                kb.op('dve', lambda e: e.memset(B['vnB'][:], 0.0), w=[GP + 'vnB'])
                kb.dma('pool', szb[:], SC['sz'][:, h * 128:(h + 1) * 128].rearrange("(t p) d -> p t d", p=128),
                       r=['s_sz'], w=[GP + 'sz'])
                kb.op('dve', lambda e: e.memset(B['Sa'][:], 0.0), w=[GP + 'Sa'])
                Scur, Snxt = 'Sa', 'Sb'
                for half in range(S // HT):
                    qb_ = half % 2
                    for i3 in range(3):
                        kb.dma('sp', qkvb[qb_][:, i3, :], SC['qkv'][i3 * 4 + h, :, half * HT:(half + 1) * HT],
                               r=['s_qkv'], w=[GP + 'qkv%d' % qb_])
                    qk = GP + 'qkv%d' % qb_
                    for tt in range(HT // 128):
                        t = half * (HT // 128) + tt
                        c = t * 4 + h
                        if 'gdn_tiles' in DBG and (h * 32 + t) >= DBG['gdn_tiles']:
                            continue
                        qT = qkvb[qb_][:, 0, tt * 128:(tt + 1) * 128]
                        kT = qkvb[qb_][:, 1, tt * 128:(tt + 1) * 128]
                        vT = qkvb[qb_][:, 2, tt * 128:(tt + 1) * 128]
                        sub = DBG.get('gdn_sub', 31)
                        pa, pka = PS.next()
                        if sub & 1:
                            kb.op('pe', lambda e: e.matmul(pa[:, 0:128], lhsT=kT, rhs=ident, start=True, stop=True),
                                  r=[qk, 'cst'], w=[pka])
                            yield
                        if sub & 2:
                            kb.op('act', lambda e: e.activation(out=B['ke'][:], in_=pa[:, 0:128], func=AF.Identity,
                                                                scale=egc[:, c:c + 1]),
                                  r=[pka, 'g_egc'], w=[GP + 'ke'])
                            yield
                        if sub & 4:
                            kb.op('dve', lambda e: e.tensor_scalar(out=B['kg'][:], in0=pa[:, 0:128],
                                                                   scalar1=ekl[:, c:c + 1], scalar2=None, op0=ALU.mult),
                                  r=[pka, 'g_ekl'], w=[GP + 'kg'])
                            yield
                        pb, pkb = PS.next()
                        if sub & 8:
                            kb.op('pe', lambda e: e.matmul(pb[:, 0:128], lhsT=vT, rhs=ident, start=True, stop=True),
                                  r=[qk, 'cst'], w=[pkb])
                            yield
                        if sub & 16:
                            cp(kb, 'act', B['vtok'][:], pb[:, 0:128], [pkb], [GP + 'vtok'])
                            yield
                        if DBG.get('gdn_step', 99) < 1:
                            continue
                        pc, pkc = PS.next()
                        mm(pc[:, 0:128], pkc, kT, kT, [qk])
                        yield
                        pd, pkd = PS.next()
                        mm(pd[:, 0:128], pkd, kT, qT, [qk])
                        yield
                        if DBG.get('gdn_step', 99) < 2:
                            continue
                        kb.op('dve', lambda e, c=c: e.tensor_scalar(out=B['dg'][:], in0=ident, scalar1=gc[:, c:c + 1],
                                                                    scalar2=None, op0=ALU.mult),
                              r=['cst', 'g_gc'], w=[GP + 'dg'])
                        yield
                        pe_, pke = PS.next()
                        mm(pe_[:, 0:128], pke, ones, B['dg'][:], ['cst', GP + 'dg'])
                        yield
                        kb.op('dve', lambda e, pe_=pe_: e.tensor_tensor(out=B['tmpm'][:], in0=pe_[:, 0:128],
                                                                        in1=gm[:, 4, :], op=ALU.add),
                              r=[pke, 'cst'], w=[GP + 'tmpm'])
                        yield
                        if DBG.get('gdn_step', 99) < 3:
                            continue
                        kb.op('act', lambda e, c=c: e.activation(out=B['decT'][:], in_=B['tmpm'][:], func=AF.Exp,
                                                                 bias=ngc[:, c:c + 1], scale=1.0),
                              r=[GP + 'tmpm', 'g_ngc'], w=[GP + 'decT'])
                        yield
                        kb.op('act', lambda e, pe_=pe_: e.activation(out=B['EB'][:], in_=pe_[:, 0:128], func=AF.Exp),
                              r=[pke], w=[GP + 'EB'])
                        yield
                        kb.op('dve', lambda e, qT=qT: e.tensor_tensor(out=B['qg'][:], in0=qT, in1=B['EB'][:], op=ALU.mult),
                              r=[qk, GP + 'EB'], w=[GP + 'qg'])
                        yield
                        kb.op('dve', lambda e, pd=pd: e.tensor_tensor(out=B['attnT'][:], in0=pd[:, 0:128],
                                                                      in1=B['decT'][:], op=ALU.mult),
                              r=[pkd, GP + 'decT'], w=[GP + 'attnT'])
                        yield
                        kb.op('dve', lambda e: e.tensor_tensor(out=B['tmpm'][:], in0=B['decT'][:], in1=gm[:, 5, :],
                                                               op=ALU.mult), r=[GP + 'decT', 'cst'], w=[GP + 'tmpm'])
                        yield
                        kb.op('dve', lambda e, pc=pc, c=c: e.scalar_tensor_tensor(
                            out=B['PT0'][:], in0=pc[:, 0:128], scalar=negb[:, c:c + 1], in1=B['tmpm'][:],
                            op0=ALU.mult, op1=ALU.mult), r=[pkc, 'g_negb', GP + 'tmpm'], w=[GP + 'PT0'])
                        yield
                        if DBG.get('gdn_step', 99) < 4:
                            continue
                        pf, pkf = PS.next()
                        kb.op('pe', lambda e, pf=pf: e.matmul(pf[:, 0:128], lhsT=B['PT0'][:], rhs=ident, start=True, stop=True),
                              r=[GP + 'PT0', 'cst'], w=[pkf])
                        yield
                        cp(kb, 'act', B['P0'][:], pf[:, 0:128], [pkf], [GP + 'P0'])
                        yield
                        kb.op('dve', lambda e: e.tensor_tensor(out=B['GT0'][:], in0=B['PT0'][:], in1=ident, op=ALU.add),
                              r=[GP + 'PT0', 'cst'], w=[GP + 'GT0'])
                        yield
                        if DBG.get('gdn_step', 99) < 5:
                            continue
                        Pc, PTc, Gc = 'P0', 'PT0', 'GT0'
                        for lv in range(1, 6):
                            Pn = 'P1' if Pc == 'P0' else 'P0'
                            PTn = 'PT1' if PTc == 'PT0' else 'PT0'
                            Gn = 'GT1' if Gc == 'GT0' else 'GT0'
                            p1, pk1 = PS.next()
                            mm(p1[:, 0:128], pk1, B[PTc][:], B[Pc][:], [GP + PTc, GP + Pc])
                            yield
                            cp(kb, 'act', B[Pn][:], p1[:, 0:128], [pk1], [GP + Pn])
                            yield
                            if lv < 5:
                                p2_, pk2_ = PS.next()
                                mm(p2_[:, 0:128], pk2_, B[Pc][:], B[PTc][:], [GP + PTc, GP + Pc])
                                yield
                                cp(kb, 'dve', B[PTn][:], p2_[:, 0:128], [pk2_], [GP + PTn])
                                yield
                            p3_, pk3_ = PS.next()
                            mm(p3_[:, 0:128], pk3_, B[Pn][:], B[Gc][:], [GP + Pn, GP + Gc])
                            yield
                            kb.op('dve', lambda e, p3_=p3_, Gn=Gn, Gc=Gc: e.tensor_tensor(
                                out=B[Gn][:], in0=p3_[:, 0:128], in1=B[Gc][:], op=ALU.add),
                                r=[pk3_, GP + Gc], w=[GP + Gn])
                            yield
                            Pc, PTc, Gc = Pn, PTn, Gn
                        if DBG.get('gdn_step', 99) < 6:
                            continue
                        G = B[Gc]
                        Gk = GP + Gc
                        pu, pku = PS.next()
                        mm(pu[:, 0:128], pku, G[:], B['vtok'][:], [Gk, GP + 'vtok'])
                        yield
                        kb.op('act', lambda e, pu=pu, c=c: e.activation(out=B['bu'][:], in_=pu[:, 0:128], func=AF.Identity,
                                                                        scale=b_all[:, c:c + 1]),
                              r=[pku, 'g_ball'], w=[GP + 'bu'])
                        yield
                        pw, pkw = PS.next()
                        mm(pw[:, 0:128], pkw, B['ke'][:], G[:], [Gk, GP + 'ke'])
                        yield
                        cp(kb, 'dve', B['wT'][:], pw[:, 0:128], [pkw], [GP + 'wT'])
                        yield
                        if DBG.get('gdn_step', 99) < 7:
                            continue
                        for ci, vn, gle, gk in ((0, 'vnA', gl0e, 'g_gl0e'), (1, 'vnB', gl1e, 'g_gl1e')):
                            lo, hi = ci * 64, ci * 64 + 64
                            Sc = B[Scur]
                            Sk = GP + Scur
                            p4, pk4 = PS.next()
                            mm(p4[:, 0:128], pk4, B['wT'][:], Sc[:], [GP + 'wT', Sk])
                            yield
                            kb.op('dve', lambda e, p4=p4, vn=vn, lo=lo, hi=hi, c=c: e.scalar_tensor_tensor(
                                out=B[vn][lo:hi, :], in0=p4[lo:hi, 0:128], scalar=negb[lo:hi, c:c + 1],
                                in1=B['bu'][lo:hi, :], op0=ALU.mult, op1=ALU.add),
                                r=[pk4, 'g_negb', GP + 'bu'], w=[GP + vn])
                            yield
                            p5, pk5 = PS.next()
                            mm(p5[:, 0:128], pk5, B['qg'][:], Sc[:], [GP + 'qg', Sk], start=True, stop=False)
                            yield
                            mm(p5[:, 0:128], pk5, B['attnT'][:], B[vn][:], [GP + 'attnT', GP + vn], start=False, stop=True)
                            yield
                            cp(kb, 'act', B['otok'][lo:hi, :], p5[lo:hi, 0:128], [pk5], [GP + 'otok'])
                            yield
                            p6, pk6 = PS.next()
                            mm(p6[:, 0:128], pk6, B['kg'][:], B[vn][:], [GP + 'kg', GP + vn])
                            yield
                            kb.op('dve', lambda e, p6=p6, Sc=Sc, gle=gle, c=c, Snxt=Snxt: e.scalar_tensor_tensor(
                                out=B[Snxt][:], in0=Sc[:], scalar=gle[:, c:c + 1], in1=p6[:, 0:128],
                                op0=ALU.mult, op1=ALU.add), r=[pk6, Sk, gk], w=[GP + Snxt])
                            yield
                            Scur, Snxt = Snxt, Scur
                        if DBG.get('gdn_step', 99) < 8:
                            continue
                        kb.op('act', lambda e: e.activation(out=B['junk'][:], in_=B['otok'][:], func=AF.Square,
                                                            accum_out=ss[:]), r=[GP + 'otok'], w=[GP + 'junk', GP + 'ss'])
                        yield
                        kb.op('act', lambda e: e.activation(out=ss[:], in_=ss[:], func=AF.Sqrt, bias=1e-6,
                                                            scale=1.0 / 128), r=[GP + 'ss'], w=[GP + 'ss'])
                        yield
                        kb.op('dve', lambda e: e.reciprocal(out=ss[:], in_=ss[:]), r=[GP + 'ss'], w=[GP + 'ss'])
                        yield
                        kb.op('dve', lambda e: e.scalar_tensor_tensor(out=B['ytok'][:], in0=B['otok'][:], scalar=ss[:],
                                                                      in1=nw[:], op0=ALU.mult, op1=ALU.mult),
                              r=[GP + 'otok', GP + 'ss', 'g_nw'], w=[GP + 'ytok'])
                        yield
                        kb.op('dve', lambda e, t=t: e.tensor_tensor(out=B['ytok'][:], in0=B['ytok'][:], in1=szb[:, t, :],
                                                                    op=ALU.mult), r=[GP + 'ytok', GP + 'sz'], w=[GP + 'ytok'])
                        yield
                        if DBG.get('gdn_step', 99) < 9:
                            continue
                        p7, pk7 = PS.next()
                        kb.op('pe', lambda e, p7=p7: e.matmul(p7[:, 0:128], lhsT=B['ytok'][:], rhs=ident, start=True, stop=True),
                              r=[GP + 'ytok', 'cst'], w=[pk7])
                        yield
                        cp(kb, 'act', yaT[:, t * 128:(t + 1) * 128], p7[:, 0:128], [pk7], [GP + 'yaT'])
                        yield
                        if 'gdn_dump' in DBG and h == 0 and t == 0:
                            for bi, n_ in enumerate(names):
                                kb.dma('sp', DBG['gdn_dump'][bi], B[n_][:], r=[GP + n_], w=['dump'])
                kb.dma('sp', SC['ya'][h], yaT[:], r=[GP + 'yaT'], w=['s_ya'])


        gens = [head_gen(0, 0), head_gen(1, 1)]
        while gens:
            for g_ in list(gens):
                try:
                    next(g_)
                except StopIteration:
                    gens.remove(g_)
        kb.barrier()


def stage_swa(kb, nc, l, A, SC, cst):
    with ExitStack() as st:
        swab = sbt(nc, st, "a_swab", [128, 8, 256], F32)
        kb.dma('sp', swab[:], A['swab'][:, :, :], w=['cst'])
        qb = sbt(nc, st, "a_qb", [128, 4, S], BF16)
        kbt = sbt(nc, st, "a_kb", [128, S], BF16)
        vb = sbt(nc, st, "a_vb", [128, NT, 128], BF16)
        ybT = sbt(nc, st, "a_ybT", [64, 8, S], BF16)
        snk = sbt(nc, st, "a_snk", [128, 8], F32)
        for j in range(4):
            kb.dma('sp', qb[:, j, :], SC['qb'][j], r=['s_misc'], w=['a_qb'])
        kb.dma('sp', kbt[:], SC['kb'], r=['s_misc'], w=['a_kb'])
        kb.dma('sp', vb[:], SC['vb'].rearrange("(t p) d -> p t d", p=128), r=['s_vb'], w=['a_vb'])
        kb.dma('sp', snk[:], A['sink_bc'][l], w=['a_snk'])
        def swa_gen(heads, ch):
            CP = 'a%d_' % ch
            sc = [sbt(nc, st, "a_sc%d" % i, [128, 256], F32) for i in range(2)]
            pb_ = [sbt(nc, st, "a_p%d" % i, [128, 256], BF16) for i in range(2)]
            pT = [sbt(nc, st, "a_pT%d" % i, [128, 2, 128], BF16) for i in range(2)]
            sm = [sbt(nc, st, "a_sm%d" % i, [128, 8], F32) for i in range(2)]
            PS = PsRot(kb, nc, st, 2, "a%d_ps" % ch)
            PT = PsRot(kb, nc, st, 1, "a%d_pt" % ch, dt=BF16, shape=(128, 2, 128))
            PO = PsRot(kb, nc, st, 1, "a%d_po" % ch)
            u = 0
            for n in range(NT):
                for hq in heads:
                    kv = hq // 4
                    j = hq % 4
                    lo = kv * 64
                    W = 128 if n == 0 else 256
                    k0 = 0 if n == 0 else (n - 1) * 128
                    b = u % 2
                    u += 1
                    p, pk = PS.next()
                    kb.op('pe', lambda e: e.matmul(p[:, 0:W], lhsT=qb[lo:lo + 64, j, n * 128:(n + 1) * 128],
                                                   rhs=kbt[lo:lo + 64, k0:k0 + W], start=True, stop=True),
                          r=['a_qb', 'a_kb'], w=[pk])
                    yield
                    kb.op('dve', lambda e: e.scalar_tensor_tensor(out=sc[b][:, 0:W], in0=p[:, 0:W], scalar=0.125,
                                                                  in1=swab[:, hq, 256 - W:256], op0=ALU.mult,
                                                                  op1=ALU.add), r=[pk, 'cst'], w=[CP + 'sc%d' % b])
                    yield
                    s_ = sm[b]
                    sk = CP + 'sm%d' % b
                    kb.op('dve', lambda e: e.reduce_max(out=s_[:, 0:1], in_=sc[b][:, 0:W], axis=AX.X),
                          r=[CP + 'sc%d' % b], w=[sk])
                    yield
                    kb.op('dve', lambda e: e.tensor_tensor(out=s_[:, 1:2], in0=s_[:, 0:1], in1=snk[:, hq:hq + 1],
                                                           op=ALU.max), r=[sk, 'a_snk'], w=[sk])
                    yield
                    kb.op('dve', lambda e: e.tensor_scalar(out=s_[:, 2:3], in0=s_[:, 1:2], scalar1=-1.0, scalar2=None,
                                                           op0=ALU.mult), r=[sk], w=[sk])
                    yield
                    kb.op('act', lambda e: e.activation(out=sc[b][:, 0:W], in_=sc[b][:, 0:W], func=AF.Exp,
                                                        bias=s_[:, 2:3], scale=1.0, accum_out=s_[:, 3:4]),
                          r=[CP + 'sc%d' % b, sk], w=[CP + 'sc%d' % b, sk])
                    yield
                    kb.op('act', lambda e: e.activation(out=s_[:, 4:5], in_=snk[:, hq:hq + 1], func=AF.Exp,
                                                        bias=s_[:, 2:3], scale=1.0), r=[sk, 'a_snk'], w=[sk])
                    yield
                    kb.op('dve', lambda e: e.tensor_tensor(out=s_[:, 5:6], in0=s_[:, 3:4], in1=s_[:, 4:5], op=ALU.add),
                          r=[sk], w=[sk])
                    yield
                    kb.op('dve', lambda e: e.reciprocal(out=s_[:, 6:7], in_=s_[:, 5:6]), r=[sk], w=[sk])
                    yield
                    kb.op('dve', lambda e: e.tensor_scalar(out=pb_[b][:, 0:W], in0=sc[b][:, 0:W], scalar1=s_[:, 6:7],
                                                           scalar2=None, op0=ALU.mult),
                          r=[CP + 'sc%d' % b, sk], w=[CP + 'p%d' % b])
                    yield
                    nk = W // 128
                    pt, ptk = PT.next()
                    for q_ in range(nk):
                        kb.op('pe', lambda e, q_=q_: e.transpose(out=pt[:, q_, :], in_=pb_[b][:, q_ * 128:(q_ + 1) * 128],
                                                                identity=cst['ident_b'][:, :]),
                              r=[CP + 'p%d' % b, 'cst'], w=[ptk])
                        yield
                    cp(kb, 'act', pT[b][:, 0:nk, :], pt[:, 0:nk, :], [ptk], [CP + 'pT%d' % b])
                    yield
                    po, pok = PO.next()
                    for q_ in range(nk):
                        tk = n if n == 0 else n - 1 + q_
                        kb.op('pe', lambda e, q_=q_, tk=tk: e.matmul(po[0:64, 0:128], lhsT=vb[:, tk, kv * 64:(kv + 1) * 64],
                                                                    rhs=pT[b][:, q_, :], start=(q_ == 0),
                                                                    stop=(q_ == nk - 1)),
                              r=['a_vb', CP + 'pT%d' % b], w=[pok])
                        yield
                    cp(kb, 'act' if u % 2 else 'dve', ybT[:, hq, n * 128:(n + 1) * 128], po[0:64, 0:128], [pok], ['a_ybT%d' % hq])
                    yield


        gens = [swa_gen((0, 1, 2, 3), 0), swa_gen((4, 5, 6, 7), 1)]
        while gens:
            for g_ in list(gens):
                try:
                    next(g_)
                except StopIteration:
                    gens.remove(g_)
        kb.dma('sp', SC['yb'], ybT[:], r=['a_ybT%d' % i for i in range(8)], w=['s_yb'])
        kb.barrier()


def ln_affine_store(kb, nc, xin, xk, bufs, g_bc, b_bc, dst, sfx, dk):
    stt, mv, rstd, nb = bufs
    kb.op('dve', lambda e: e.bn_stats(out=stt[:, 0, :], in_=xin[:, 0:512]), r=[xk], w=['la_st' + sfx])
    kb.op('dve', lambda e: e.bn_stats(out=stt[:, 1, :], in_=xin[:, 512:1024]), r=[xk], w=['la_st' + sfx])
    kb.op('dve', lambda e: e.bn_aggr(out=mv[:], in_=stt[:].rearrange("p a b -> p (a b)")),
          r=['la_st' + sfx], w=['la_mv' + sfx])
    kb.op('act', lambda e: e.activation(out=rstd[:], in_=mv[:, 1:2], func=AF.Sqrt, bias=1e-5, scale=1.0),
          r=['la_mv' + sfx], w=['la_rstd' + sfx])
    kb.op('dve', lambda e: e.reciprocal(out=rstd[:], in_=rstd[:]), r=['la_rstd' + sfx], w=['la_rstd' + sfx])
    kb.op('dve', lambda e: e.scalar_tensor_tensor(out=nb[:], in0=mv[:, 0:1], scalar=-1.0, in1=rstd[:],
                                                  op0=ALU.mult, op1=ALU.mult),
          r=['la_mv' + sfx, 'la_rstd' + sfx], w=['la_nb' + sfx])
    kb.op('act', lambda e: e.activation(out=xin[:], in_=xin[:], func=AF.Identity, bias=nb[:], scale=rstd[:]),
          r=[xk, 'la_nb' + sfx, 'la_rstd' + sfx], w=[xk])
    kb.op('pool', lambda e: e.tensor_tensor(out=xin[:], in0=xin[:], in1=g_bc, op=ALU.mult), r=[xk, 'lngb'], w=[xk])
    kb.op('pool', lambda e: e.tensor_tensor(out=xin[:], in0=xin[:], in1=b_bc, op=ALU.add), r=[xk, 'lngb'], w=[xk])
    kb.dma('sp', dst, xin[:], r=[xk], w=[dk])


def stage_merge(kb, nc, l, A, SC, x_src, x_dst, mod_bc, cst):
    with ExitStack() as st:
        U = [sbt(nc, st, "c_u%d" % i, [128, 16 + S], F32) for i in range(3)]
        pw = sbt(nc, st, "c_pw", [128, 4, 128], F32)
        pscl = sbt(nc, st, "c_ps", [128, 4], F32)
        ic = sbt(nc, st, "c_ic", [128, 4, 16], F32)
        yc = [sbt(nc, st, "c_yc%d" % i, [128, S], BF16) for i in range(2)]
        PS = PsRot(kb, nc, st, 4, "c_pp")
        kb.dma('sp', pw[:], A['pool_w'][l].rearrange("g c d -> c g d"), w=['c_pw'])
        kb.dma('sp', pscl[:], A['pscale_l'][l], w=['c_ps'])
        kb.dma('sp', ic[:], A['invcnt'][:, :, :], w=['c_ic'])
        for i in range(3):
            kb.op('dve', lambda e, i=i: e.memset(U[i][:, 0:16], 0.0), w=['c_u%d' % i])
        for g, win in enumerate((2, 4, 8, 16)):
            kb.dma('sp', U[0][:, 16:], SC['uc'][g], r=['s_uc'], w=['c_u0'])
            cur = 0
            sh = 1
            while sh < win:
                nxt = 1 if cur != 1 else 2
                kb.op('dve' if sh % 4 == 1 else 'pool', lambda e, cur=cur, nxt=nxt, sh=sh: e.tensor_tensor(
                    out=U[nxt][:, 16:], in0=U[cur][:, 16:], in1=U[cur][:, 16 - sh:16 - sh + S], op=ALU.add),
                    r=['c_u%d' % cur], w=['c_u%d' % nxt])
                cur = nxt
                sh *= 2
            dn = 1 if cur != 1 else 2
            kb.op('dve', lambda e, cur=cur, dn=dn: e.scalar_tensor_tensor(
                out=U[dn][:, 16:], in0=U[cur][:, 16:], scalar=1.0 / win, in1=U[0][:, 16:], op0=ALU.mult,
                op1=ALU.subtract), r=['c_u%d' % cur, 'c_u0'], w=['c_u%d' % dn])
            kb.op('dve', lambda e, cur=cur, dn=dn: e.tensor_tensor(out=U[dn][:, 16:32], in0=U[cur][:, 16:32],
                                                                  in1=ic[:, g, :], op=ALU.mult),
                  r=['c_u%d' % cur, 'c_ic'], w=['c_u%d' % dn])
            kb.op('dve', lambda e, dn=dn: e.tensor_tensor(out=U[dn][:, 16:32], in0=U[dn][:, 16:32],
                                                          in1=U[0][:, 16:32], op=ALU.subtract),
                  r=['c_u%d' % dn, 'c_u0'], w=['c_u%d' % dn])
            yb_ = g % 2
            for tc in range(8):
                p, pk = PS.next()
                kb.op('pe', lambda e, tc=tc, dn=dn: e.matmul(p[:], lhsT=pw[:, g, :],
                                                             rhs=U[dn][:, 16 + tc * 512:16 + (tc + 1) * 512],
                                                             start=True, stop=True), r=['c_pw', 'c_u%d' % dn], w=[pk])
                kb.op('act', lambda e, tc=tc: e.activation(out=yc[yb_][:, tc * 512:(tc + 1) * 512], in_=p[:],
                                                           func=AF.Identity, scale=pscl[:, g:g + 1]),
                      r=[pk, 'c_ps'], w=['c_yc%d' % yb_])
            kb.dma('sp', SC['yc'][g], yc[yb_][:], r=['c_yc%d' % yb_], w=['s_yc'])
        kb.barrier()
    with ExitStack() as st:
        wp = sbt(nc, st, "e_wp", [128, 12, D], BF16)
        wpb = sbt(nc, st, "e_wpb", [64, 8, D], BF16)
        wo = sbt(nc, st, "e_wo", [128, 8, D], BF16)
        lng = sbt(nc, st, "e_lng", [128, D], F32)
        lnb = sbt(nc, st, "e_lnb", [128, D], F32)
        gt = [sbt(nc, st, "e_gt0", [128, 24, 512], BF16)] * 2
        ya = [sbt(nc, st, "e_ya%d" % i, [128, 4, 512], BF16) for i in range(2)]
        ycb = [sbt(nc, st, "e_yc%d" % i, [128, 4, 512], BF16) for i in range(2)]
        ybb = [sbt(nc, st, "e_yb%d" % i, [64, 8, 512], BF16) for i in range(2)]
        mg = [sbt(nc, st, "e_mg%d" % i, [128, 8, 512], BF16) for i in range(2)]
        t1 = [sbt(nc, st, "e_t1%d" % i, [128, 512], F32) for i in range(2)]
        t2 = [sbt(nc, st, "e_t2%d" % i, [128, 512], F32) for i in range(2)]
        t3 = [sbt(nc, st, "e_t3%d" % i, [128, 512], F32) for i in range(2)]
        xt = [sbt(nc, st, "e_xt%d" % i, [128, D], F32) for i in range(2)]
        yt = [sbt(nc, st, "e_yt%d" % i, [128, D], F32) for i in range(2)]
        lb = [(sbt(nc, st, "e_st%d" % i, [128, 2, 6], F32), sbt(nc, st, "e_mv%d" % i, [128, 2], F32),
               sbt(nc, st, "e_rs%d" % i, [128, 1], F32), sbt(nc, st, "e_nb%d" % i, [128, 1], F32)) for i in range(2)]
        PA = PsRot(kb, nc, st, 6, "e_pa")
        PY = PsRot(kb, nc, st, 2, "e_py")
        kb.dma('pool', wp[:, 0:4, :], A['w_pa'][l].rearrange("(kc p) n -> p kc n", p=128), w=['e_wp'])
        kb.dma('pool', wp[:, 4:8, :], A['w_pc'][l].rearrange("(kc p) n -> p kc n", p=128), w=['e_wp'])
        kb.dma('pool', wpb[:], A['w_pb'][l].rearrange("(h p) n -> p h n", p=64), w=['e_wpb'])
        kb.dma('pool', wo[:], A['w_o'][l].rearrange("(kc p) n -> p kc n", p=128), w=['e_wo'])
        kb.dma('sp', lng[:], A['ln1g_bc'][l], w=['lngb'])
        kb.dma('sp', lnb[:], A['ln1b_bc'][l], w=['lngb'])
        u = 0
        for tc in range(8):
            b = tc % 2
            ts = slice(tc * 512, (tc + 1) * 512)
            kb.dma('sp', gt[b][:], SC['gate'][:, :, ts].rearrange("c p t -> p c t"), r=['s_misc'], w=['e_gt0'])
            kb.dma('sp', ya[b][:], SC['ya'][:, :, ts].rearrange("c p t -> p c t"), r=['s_ya'], w=['e_ya%d' % b])
            kb.dma('sp', ycb[b][:], SC['yc'][:, :, ts].rearrange("c p t -> p c t"), r=['s_yc'], w=['e_yc%d' % b])
            kb.dma('sp', ybb[b][:], SC['yb'][:, :, ts], r=['s_yb'], w=['e_yb%d' % b])
            for m in range(8):
                ms = slice(m * 128, (m + 1) * 128)
                tb = u % 2
                u += 1
                pa, pka = PA.next()
                for kc in range(4):
                    kb.op('pe', lambda e, kc=kc: e.matmul(pa[:], lhsT=wp[:, kc, ms], rhs=ya[b][:, kc, :],
                                                          start=(kc == 0), stop=(kc == 3)),
                          r=['e_wp', 'e_ya%d' % b], w=[pka])
                kb.op('dve', lambda e: e.tensor_tensor(out=t1[tb][:], in0=pa[:], in1=gt[b][:, m, :], op=ALU.mult),
                      r=[pka, 'e_gt0'], w=['e_t1%d' % tb])
                pb2, pkb2 = PA.next()
                for hh in range(8):
                    kb.op('pe', lambda e, hh=hh: e.matmul(pb2[:], lhsT=wpb[:, hh, ms], rhs=ybb[b][:, hh, :],
                                                          start=(hh == 0), stop=(hh == 7)),
                          r=['e_wpb', 'e_yb%d' % b], w=[pkb2])
                kb.op('dve', lambda e: e.tensor_tensor(out=t2[tb][:], in0=pb2[:], in1=gt[b][:, 8 + m, :], op=ALU.mult),
                      r=[pkb2, 'e_gt0'], w=['e_t2%d' % tb])
                pc2, pkc2 = PA.next()
                for kc in range(4):
                    kb.op('pe', lambda e, kc=kc: e.matmul(pc2[:], lhsT=wp[:, 4 + kc, ms], rhs=ycb[b][:, kc, :],
                                                          start=(kc == 0), stop=(kc == 3)),
                          r=['e_wp', 'e_yc%d' % b], w=[pkc2])
                kb.op('dve', lambda e: e.tensor_tensor(out=t3[tb][:], in0=pc2[:], in1=gt[b][:, 16 + m, :], op=ALU.mult),
                      r=[pkc2, 'e_gt0'], w=['e_t3%d' % tb])
                kb.op('pool', lambda e: e.tensor_tensor(out=t1[tb][:], in0=t1[tb][:], in1=t2[tb][:], op=ALU.add),
                      r=['e_t1%d' % tb, 'e_t2%d' % tb], w=['e_t1%d' % tb])
                kb.op('pool', lambda e: e.tensor_tensor(out=mg[b][:, m, :], in0=t1[tb][:], in1=t3[tb][:], op=ALU.add),
                      r=['e_t1%d' % tb, 'e_t3%d' % tb], w=['e_mg%d' % b])
            for tt in range(4):
                t = tc * 4 + tt
                xb = t % 2
                kb.dma('sp', xt[xb][:], x_src[t * 128:(t + 1) * 128, :], r=['xsrc'], w=['e_xt%d' % xb])
                for half in range(2):
                    py, pyk = PY.next()
                    for kc in range(8):
                        kb.op('pe', lambda e, kc=kc: e.matmul(py[:], lhsT=mg[b][:, kc, tt * 128:(tt + 1) * 128],
                                                              rhs=wo[:, kc, half * 512:(half + 1) * 512],
                                                              start=(kc == 0), stop=(kc == 7)),
                              r=['e_mg%d' % b, 'e_wo'], w=[pyk])
                    kb.op('dve', lambda e, half=half: e.tensor_tensor(
                        out=yt[xb][:, half * 512:(half + 1) * 512], in0=py[:],
                        in1=mod_bc[:, 2 * D + half * 512:2 * D + (half + 1) * 512], op=ALU.mult),
                        r=[pyk, 'mod_bc'], w=['e_yt%d' % xb])
                kb.op('dve', lambda e: e.scalar_tensor_tensor(out=yt[xb][:], in0=xt[xb][:], scalar=ALPHA, in1=yt[xb][:],
                                                              op0=ALU.mult, op1=ALU.add),
                      r=['e_xt%d' % xb, 'e_yt%d' % xb], w=['e_yt%d' % xb])
                ln_affine_store(kb, nc, yt[xb], 'e_yt%d' % xb, lb[xb], lng[:], lnb[:],
                                x_dst[t * 128:(t + 1) * 128, :], 'e%d' % xb, 'xdst')
        kb.barrier()


def stage_moe(kb, nc, l, A, SC, x_src, x_dst, mod_bc, cst):
    ident = cst['ident_f']
    with ExitStack() as st:
        hT = sbt(nc, st, "hT2", [128, 8, S], BF16)
        gate_all = sbt(nc, st, "f_gate", [128, NT, NE], F32)
        with ExitStack() as s2:
            xts = [sbt(nc, s2, "f_xt%d" % i, [128, D], F32) for i in range(2)]
            hbs = [sbt(nc, s2, "f_hb%d" % i, [128, D], BF16) for i in range(2)]
            bufs = [(sbt(nc, s2, "f_st%d" % i, [128, 2, 6], F32), sbt(nc, s2, "f_mv%d" % i, [128, 2], F32),
                     sbt(nc, s2, "f_rs%d" % i, [128, 1], F32), sbt(nc, s2, "f_nb%d" % i, [128, 1], F32),
                     sbt(nc, s2, "f_xn%d" % i, [128, D], F32)) for i in range(2)]
            h32 = [sbt(nc, s2, "f_h32%d" % i, [128, 8, 128], F32) for i in range(2)]
            rw = sbt(nc, s2, "f_rw", [128, 8, NE], F32)
            rb = sbt(nc, s2, "f_rb", [128, NE], F32)
            lg = [sbt(nc, s2, "f_lg%d" % i, [128, NE], F32) for i in range(2)]
            mk = [sbt(nc, s2, "f_mk%d" % i, [128, NE], F32) for i in range(2)]
            m8 = [sbt(nc, s2, "f_m8%d" % i, [128, 12], F32) for i in range(2)]
            pT = [pst(nc, s2, "f_pT%d" % i, [128, 8, 128], BF16) for i in range(2)]
            pR = [pst(nc, s2, "f_pR%d" % i, [128, 2, 512], F32) for i in range(2)]
            pL = [pst(nc, s2, "f_pL%d" % i, [128, 512], F32) for i in range(2)]
            kb.dma('sp', rw[:], A['router_w'][l].rearrange("(kc p) n -> p kc n", p=128), w=['f_rw'])
            kb.dma('sp', rb[:], A['rb_bc'][l], w=['f_rb'])
            for t in range(NT):
                b = t % 2
                sx = 'f' + str(b)
                kb.dma('sp', xts[b][:], x_src[t * 128:(t + 1) * 128, :], r=['xsrc5'], w=['f_xt%d' % b])
                stt, mv, rstd, nb, xn = bufs[b]
                ln_mod_tile(kb, nc, xts[b], 'f_xt%d' % b, bufs[b], mod_bc[:, 4 * D:5 * D], mod_bc[:, 3 * D:4 * D],
                            xn[:], 'ln_xn' + sx, sx)
                cp(kb, 'act', hbs[b][:], xn[:], ['ln_xn' + sx], ['f_hb%d' % b])
                for kc in range(8):
                    kb.op('pe', lambda e, kc=kc: e.transpose(out=pT[b][:, kc, :], in_=hbs[b][:, kc * 128:(kc + 1) * 128],
                                                             identity=cst['ident_b'][:, :]),
                          r=['f_hb%d' % b, 'cst'], w=['f_pT%d' % b])
                cp(kb, 'act', hT[:, :, t * 128:(t + 1) * 128], pT[b][:], ['f_pT%d' % b], ['hT2'])
                for kc in range(8):
                    kb.op('pe', lambda e, kc=kc: e.matmul(pR[b][:, kc // 4, (kc % 4) * 128:(kc % 4 + 1) * 128],
                                                          lhsT=xn[:, kc * 128:(kc + 1) * 128], rhs=ident,
                                                          start=True, stop=True),
                          r=['ln_xn' + sx, 'cst'], w=['f_pR%d' % b])
                cp(kb, 'dve', h32[b][:].rearrange("p (a c) n -> p a (c n)", a=2), pR[b][:], ['f_pR%d' % b],
                   ['f_h32%d' % b])
                for kc in range(8):
                    kb.op('pe', lambda e, kc=kc: e.matmul(pL[b][:, 0:NE], lhsT=h32[b][:, kc, :], rhs=rw[:, kc, :],
                                                          start=(kc == 0), stop=(kc == 7)),
                          r=['f_h32%d' % b, 'f_rw'], w=['f_pL%d' % b])
                kb.op('dve', lambda e: e.tensor_tensor(out=lg[b][:], in0=pL[b][:, 0:NE], in1=rb[:], op=ALU.add),
                      r=['f_pL%d' % b, 'f_rb'], w=['f_lg%d' % b])
                kb.op('dve', lambda e: e.max(out=m8[b][:, 0:8], in_=lg[b][:]), r=['f_lg%d' % b], w=['f_m8%d' % b])
                kb.op('dve', lambda e: e.tensor_scalar(out=mk[b][:], in0=lg[b][:], scalar1=m8[b][:, 3:4], scalar2=None,
                                                       op0=ALU.is_ge), r=['f_lg%d' % b, 'f_m8%d' % b], w=['f_mk%d' % b])
                kb.op('dve', lambda e: e.tensor_scalar(out=m8[b][:, 8:9], in0=m8[b][:, 0:1], scalar1=-1.0, scalar2=None,
                                                       op0=ALU.mult), r=['f_m8%d' % b], w=['f_m8%d' % b])
                kb.op('act', lambda e: e.activation(out=lg[b][:], in_=lg[b][:], func=AF.Exp, bias=m8[b][:, 8:9],
                                                    scale=1.0), r=['f_lg%d' % b, 'f_m8%d' % b], w=['f_lg%d' % b])
                kb.op('dve', lambda e: e.tensor_tensor(out=lg[b][:], in0=lg[b][:], in1=mk[b][:], op=ALU.mult),
                      r=['f_lg%d' % b, 'f_mk%d' % b], w=['f_lg%d' % b])
                kb.op('dve', lambda e: e.reduce_sum(out=m8[b][:, 9:10], in_=lg[b][:], axis=AX.X),
                      r=['f_lg%d' % b], w=['f_m8%d' % b])
                kb.op('dve', lambda e: e.reciprocal(out=m8[b][:, 10:11], in_=m8[b][:, 9:10]),
                      r=['f_m8%d' % b], w=['f_m8%d' % b])
                kb.op('dve', lambda e: e.tensor_scalar(out=gate_all[:, t, :], in0=lg[b][:], scalar1=m8[b][:, 10:11],
                                                       scalar2=None, op0=ALU.mult),
                      r=['f_lg%d' % b, 'f_m8%d' % b], w=['f_gate'])
            kb.barrier()
        PT_ = 8
        acc = sbt(nc, st, "f_acc", [128, PT_, D], F32)
        w1t = [sbt(nc, st, "f_w1%d" % i, [128, 8, 512], BF16) for i in range(2)]
        w2t = sbt(nc, st, "f_w2", [128, 8, D], BF16)
        b1t = [sbt(nc, st, "f_b1%d" % i, [128, 16], F32) for i in range(2)]
        b2t = [sbt(nc, st, "f_b2%d" % i, [1, D], BF16) for i in range(2)]
        onesb = sbt(nc, st, "f_onesb", [1, 128], BF16)
        actT = sbt(nc, st, "f_actT", [128, 8, PT_ * 128], BF16)
        gl = [sbt(nc, st, "f_gl%d" % i, [128, 512], F32) for i in range(2)]
        sg = [sbt(nc, st, "f_sg%d" % i, [128, 512], F32) for i in range(2)]
        li = [sbt(nc, st, "f_li%d" % i, [128, 512], F32) for i in range(2)]
        xt = [sbt(nc, st, "f_x%d" % i, [128, D], F32) for i in range(2)]
        lng = sbt(nc, st, "f_lng", [128, D], F32)
        lnb = sbt(nc, st, "f_lnb", [128, D], F32)
        lb = [(sbt(nc, st, "f_lst%d" % i, [128, 2, 6], F32), sbt(nc, st, "f_lmv%d" % i, [128, 2], F32),
               sbt(nc, st, "f_lrs%d" % i, [128, 1], F32), sbt(nc, st, "f_lnb%d" % i, [128, 1], F32)) for i in range(2)]
        PG = PsRot(kb, nc, st, 4, "f_pg")
        PY = PsRot(kb, nc, st, 3, "f_py")
        kb.op('dve', lambda e: e.tensor_copy(out=onesb[:], in_=cst['ones_f'][0:1, :]), r=['cst'], w=['f_onesb'])
        kb.dma('sp', lng[:], A['ln2g_bc'][l], w=['lngb'])
        kb.dma('sp', lnb[:], A['ln2b_bc'][l], w=['lngb'])
        W1, W2 = A['exp_w1'], A['exp_w2']
        gcount = 0
        u = 0
        n_exp = DBG.get('n_exp', NE)
        for ps_ in range(NT // PT_):
            kb.op('pool', lambda e: e.memset(acc[:], 0.0), w=['f_acc'])
            for ex in range(n_exp):
                eb = ex % 2
                kb.dma('sp', b1t[eb][:], A['exp_b1l'][l, ex], w=['f_b1%d' % eb])
                kb.dma('pool', b2t[eb][:], A['exp_b2'][l, ex:ex + 1, :], w=['f_b2%d' % eb])
                for g in range(4):
                    wb = gcount % 2
                    gcount += 1
                    kb.dma('pool', w1t[wb][:, :, 0:256],
                           W1[l, ex, :, g * 256:(g + 1) * 256].rearrange("(kc p) n -> p kc n", p=128), w=['f_w1%d' % wb])
                    kb.dma('pool', w1t[wb][:, :, 256:512],
                           W1[l, ex, :, DFF + g * 256:DFF + (g + 1) * 256].rearrange("(kc p) n -> p kc n", p=128),
                           w=['f_w1%d' % wb])
                    for fl in range(2):
                        fc = g * 2 + fl
                        for tcl in range(PT_ // 4):
                            tb = u % 2
                            u += 1
                            tok = slice((ps_ * PT_ + tcl * 4) * 128, (ps_ * PT_ + tcl * 4 + 4) * 128)
                            pg, pgk = PG.next()
                            for kc in range(8):
                                kb.op('pe', lambda e, kc=kc: e.matmul(pg[:], lhsT=w1t[wb][:, kc, fl * 128:(fl + 1) * 128],
                                                                      rhs=hT[:, kc, tok], start=(kc == 0), stop=(kc == 7)),
                                      r=['f_w1%d' % wb, 'hT2'], w=[pgk])
                            pl, plk = PG.next()
                            for kc in range(8):
                                kb.op('pe', lambda e, kc=kc: e.matmul(
                                    pl[:], lhsT=w1t[wb][:, kc, 256 + fl * 128:256 + (fl + 1) * 128],
                                    rhs=hT[:, kc, tok], start=(kc == 0), stop=(kc == 7)),
                                    r=['f_w1%d' % wb, 'hT2'], w=[plk])
                            kb.op('dve', lambda e: e.tensor_scalar(out=gl[tb][:], in0=pg[:], scalar1=b1t[eb][:, fc:fc + 1],
                                                                   scalar2=7.0, op0=ALU.add, op1=ALU.min),
                                  r=[pgk, 'f_b1%d' % eb], w=['f_gl%d' % tb])
                            kb.op('act', lambda e: e.activation(out=sg[tb][:], in_=gl[tb][:], func=AF.Sigmoid,
                                                                scale=1.702), r=['f_gl%d' % tb], w=['f_sg%d' % tb])
                            kb.op('dve', lambda e: e.tensor_scalar(out=li[tb][:], in0=pl[:],
                                                                   scalar1=b1t[eb][:, 8 + fc:9 + fc], scalar2=7.0,
                                                                   op0=ALU.add, op1=ALU.min),
                                  r=[plk, 'f_b1%d' % eb], w=['f_li%d' % tb])
                            kb.op('dve', lambda e: e.tensor_scalar(out=li[tb][:], in0=li[tb][:], scalar1=-7.0,
                                                                   scalar2=1.0, op0=ALU.max, op1=ALU.add),
                                  r=['f_li%d' % tb], w=['f_li%d' % tb])
                            kb.op('pool', lambda e: e.tensor_tensor(out=gl[tb][:], in0=gl[tb][:], in1=sg[tb][:],
                                                                    op=ALU.mult),
                                  r=['f_gl%d' % tb, 'f_sg%d' % tb], w=['f_gl%d' % tb])
                            kb.op('pool', lambda e: e.tensor_tensor(out=actT[:, fc, tcl * 512:(tcl + 1) * 512],
                                                                    in0=gl[tb][:], in1=li[tb][:], op=ALU.mult),
                                  r=['f_gl%d' % tb, 'f_li%d' % tb], w=['f_actT'])
                kb.dma('pool', w2t[:], W2[l, ex].rearrange("(kc p) n -> p kc n", p=128), w=['f_w2'],
                       max_dma_last_dim=4096)
                for tt in range(PT_):
                    T = ps_ * PT_ + tt
                    for half in range(2):
                        py, pyk = PY.next()
                        for fc in range(8):
                            kb.op('pe', lambda e, fc=fc: e.matmul(py[:], lhsT=actT[:, fc, tt * 128:(tt + 1) * 128],
                                                                  rhs=w2t[:, fc, half * 512:(half + 1) * 512],
                                                                  start=(fc == 0), stop=False),
                                  r=['f_actT', 'f_w2'], w=[pyk])
                        kb.op('pe', lambda e: e.matmul(py[:], lhsT=onesb[0:1, :], rhs=b2t[eb][0:1, half * 512:(half + 1) * 512],
                                                       start=False, stop=True), r=['f_onesb', 'f_b2%d' % eb], w=[pyk])
                        kb.op('dve', lambda e: e.scalar_tensor_tensor(
                            out=acc[:, tt, half * 512:(half + 1) * 512], in0=py[:], scalar=gate_all[:, T, ex:ex + 1],
                            in1=acc[:, tt, half * 512:(half + 1) * 512], op0=ALU.mult, op1=ALU.add),
                            r=[pyk, 'f_gate', 'f_acc'], w=['f_acc'])
            for tt in range(PT_):
                T = ps_ * PT_ + tt
                xb = T % 2
                kb.dma('sp', xt[xb][:], x_src[T * 128:(T + 1) * 128, :], r=['xsrc5'], w=['f_x%d' % xb])
                kb.op('dve', lambda e: e.tensor_tensor(out=acc[:, tt, :], in0=acc[:, tt, :], in1=mod_bc[:, 5 * D:6 * D],
                                                       op=ALU.mult), r=['f_acc', 'mod_bc'], w=['f_acc'])
                kb.op('dve', lambda e: e.scalar_tensor_tensor(out=xt[xb][:], in0=xt[xb][:], scalar=ALPHA,
                                                              in1=acc[:, tt, :], op0=ALU.mult, op1=ALU.add),
                      r=['f_x%d' % xb, 'f_acc'], w=['f_x%d' % xb])
                ln_affine_store(kb, nc, xt[xb], 'f_x%d' % xb, lb[xb], lng[:], lnb[:],
                                x_dst[T * 128:(T + 1) * 128, :], 'f%d' % xb, 'xdst5')
        kb.barrier()


def input_shapes(L):
    return {
        'x': [S, D], 'c_col': [128, 8], 'ada_w': [L, D, 6 * D], 'ada_b': [L, 6 * D], 'w_in': [L, D, INW],
        'conv_wl': [L, 128, 12, 4], 'alog_bc': [L, 128, 4], 'dtb_bc': [L, 128, 4], 'gnw_bc': [L, 128, 128],
        'sink_bc': [L, 128, 8], 'pool_w': [L, 4, 128, 128], 'pscale_l': [L, 128, 4],
        'w_pa': [L, 512, D], 'w_pb': [L, 512, D], 'w_pc': [L, 512, D], 'w_o': [L, D, D],
        'ln1g_bc': [L, 128, D], 'ln1b_bc': [L, 128, D], 'ln2g_bc': [L, 128, D], 'ln2b_bc': [L, 128, D],
        'router_w': [L, D, NE], 'rb_bc': [L, 128, NE], 'exp_w1': [L, NE, D, 2 * DFF], 'exp_w2': [L, NE, DFF, D],
        'exp_b1l': [L, NE, 128, 16], 'exp_b2': [L, NE, D],
        'cst_f': [128, 2, 128], 'gm': [128, 6, 128], 'swab': [128, 8, 256], 'invcnt': [128, 4, 16], 'moec': [128, 160],
    }


def build_program(L):
    nc = bass.Bass("TRN2", target_bir_lowering=False)
    A = {k: nc.dram_tensor(k, s, F32, kind="ExternalInput").ap() for k, s in input_shapes(L).items()}
    out = nc.dram_tensor("out", [S, D], F32, kind="ExternalOutput").ap()
    SC = alloc_scratch(nc)
    SC['ya'] = nc.dram_tensor("s_ya", [4, 128, S], BF16).ap()
    SC['yb'] = nc.dram_tensor("s_yb", [64, 8, S], BF16).ap()
    SC['yc'] = nc.dram_tensor("s_yc", [4, 128, S], BF16).ap()
    x1 = nc.dram_tensor("s_x1", [S, D], F32).ap()
    SC['hg'] = nc.dram_tensor("s_hg", [NE * CAP, D], BF16).ap()
    SC['yg'] = nc.dram_tensor("s_yg", [NE * CAP, D], F32).ap()
    xs = [A['x']] + [nc.dram_tensor("s_xl%d" % i, [S, D], F32).ap() for i in range(L - 1)] + [out]
    with ExitStack() as es:
        kb = KB(nc, es)
        cf = sbt(nc, es, "cst_sb", [128, 2, 128], F32)
        ib = sbt(nc, es, "ident_b", [128, 128], BF16)
        mod_bc = sbt(nc, es, "mod_bc", [128, 6 * D], F32)
        kb.dma('sp', cf[:], A['cst_f'][:, :, :], w=['cst'])
        kb.op('dve', lambda e: e.tensor_copy(out=ib[:], in_=cf[:, 1, :]), r=['cst'], w=['cst'])
        cst = {'ones_f': cf[:, 0, :], 'ident_f': cf[:, 1, :], 'ident_b': ib, 'breg': nc.gpsimd.to_reg(NE * CAP - 1)}
        with ExitStack() as zs:
            zt = sbt(nc, zs, "zero_t", [128, 8 * D], BF16)
            kb.op('dve', lambda e: e.memset(zt[:], 0.0), w=['zero_t'])
            rows_per = 128 * 8
            for i in range(NE * CAP // rows_per):
                kb.dma('sp' if i % 2 == 0 else 'act', SC['hg'][i * rows_per:(i + 1) * rows_per, :].rearrange(
                    "(p r) d -> p (r d)", p=128), zt[:], r=['zero_t'], w=['s_hg_z%d' % (i % 8)])
            kb.barrier()
        for l in range(L):
            stage_mod(kb, nc, l, A, mod_bc, cst)
            stage_proj(kb, nc, l, A, SC, xs[l], mod_bc, cst)
            stage_gdn(kb, nc, l, A, SC, cst)
            stage_swa(kb, nc, l, A, SC, cst)
            stage_merge(kb, nc, l, A, SC, xs[l], x1, mod_bc, cst)
            stage_moe_sparse(kb, nc, l, A, SC, x1, xs[l + 1], mod_bc, cst)
        kb.barrier()
    return nc


def host_consts():
    i = np.arange(128)[:, None]
    j = np.arange(128)[None, :]
    same = (i // 64) == (j // 64)
    gm = np.stack([((i <= j) & same).astype(np.float32), same.astype(np.float32),
                   np.broadcast_to(i < 64, (128, 128)).astype(np.float32),
                   np.broadcast_to(i >= 64, (128, 128)).astype(np.float32),
                   np.where((j >= i) & same, 0.0, NEG).astype(np.float32),
                   (1.0 - np.eye(128)).astype(np.float32)], 1)
    q = np.arange(128)[:, None]
    k = np.arange(256)[None, :]
    dist = q + 128 - k
    valid = (dist >= 0) & (dist < 128)
    slopes = 2.0 ** (-8.0 * np.arange(1, 9, dtype=np.float32) / 8)
    swab = np.where(valid[:, None, :], -slopes[None, :, None] * dist[:, None, :].astype(np.float32), NEG)
    ic = np.zeros((4, 16), np.float32)
    for g, win in enumerate((2, 4, 8, 16)):
        ic[g] = 1.0 / np.minimum(np.arange(16) + 1, win)
    moec = np.concatenate([(i <= j).astype(np.float32),
                           np.broadcast_to((np.arange(NE) * CAP - 1).astype(np.float32)[None, :], (128, NE))], 1)
    return {'moec': np.ascontiguousarray(moec), 'cst_f': np.stack([np.ones((128, 128), np.float32), np.eye(128, dtype=np.float32)], 1),
            'gm': np.ascontiguousarray(gm), 'swab': np.ascontiguousarray(swab.astype(np.float32)),
            'invcnt': np.ascontiguousarray(np.broadcast_to(ic[None], (128, 4, 16)))}


def host_layer_inputs(p, ls):
    f = lambda a: np.ascontiguousarray(np.asarray(a, np.float32))
    n = len(range(*ls.indices(DEPTH)))
    bc = lambda a, w: f(np.broadcast_to(np.asarray(a)[ls][:, None, :], (n, 128, w)))
    W = {}
    for k in ['ada_w', 'ada_b', 'w_in', 'pool_w', 'w_pa', 'w_pb', 'w_pc', 'w_o', 'router_w', 'exp_w1', 'exp_w2', 'exp_b2']:
        W[k] = f(np.asarray(p[k])[ls])
    W['conv_wl'] = f(np.asarray(p['conv_w'])[ls].reshape(n, 4, 12, 128).transpose(0, 3, 2, 1))
    W['alog_bc'] = bc(p['a_log'], 4)
    W['dtb_bc'] = bc(p['dt_bias'], 4)
    W['gnw_bc'] = bc(p['gdn_norm_w'], 128)
    W['sink_bc'] = bc(p['sinks'], 8)
    W['pscale_l'] = f(np.asarray(p['pool_scale'])[ls].reshape(n, 4, 128).transpose(0, 2, 1))
    W['ln1g_bc'] = bc(p['ln1_g'], D)
    W['ln1b_bc'] = bc(p['ln1_b'], D)
    W['ln2g_bc'] = bc(p['ln2_g'], D)
    W['ln2b_bc'] = bc(p['ln2_b'], D)
    W['rb_bc'] = bc(p['router_b'], NE)
    W['exp_b1l'] = f(np.asarray(p['exp_b1'])[ls].reshape(n, NE, 16, 128).transpose(0, 1, 3, 2))
    return W


LAYERS_PER_LAUNCH = 4


def kernel(**inputs):
    x = np.asarray(inputs['x'], np.float32)
    c = np.asarray(inputs['c'], np.float32)
    nb = x.shape[0]
    consts = host_consts()
    Lp = LAYERS_PER_LAUNCH
    nc = build_program(Lp)
    cur = [np.ascontiguousarray(x[b]) for b in range(nb)]
    for l0 in range(0, DEPTH, Lp):
        W = host_layer_inputs(inputs, slice(l0, l0 + Lp))
        in_maps = []
        for b in range(nb):
            m = dict(W)
            m.update(consts)
            m['x'] = cur[b]
            m['c_col'] = np.ascontiguousarray(c[b].reshape(8, 128).T)
            in_maps.append(m)
        res = run_bass_kernel_spmd(nc, in_maps, core_ids=list(range(nb)))
        cur = [np.asarray(res.results[b]['out'], np.float32) for b in range(nb)]
    return np.stack(cur, 0).astype(np.float32)


def stage_moe_sparse(kb, nc, l, A, SC, x_src, x_dst, mod_bc, cst):
    ident = cst['ident_f']
    C = CAP
    NB = C // 128
    hg, yg = SC['hg'], SC['yg']
    with ExitStack() as st:
        dest_all = sbt(nc, st, "f_dest", [128, NT * 4], I32)
        gk_all = sbt(nc, st, "f_gk", [128, NT, 4], F32)
        with ExitStack() as s2:
            moec = sbt(nc, s2, "f_moec", [128, 160], F32)
            xts = [sbt(nc, s2, "f_xt%d" % i, [128, D], F32) for i in range(2)]
            hbs = [sbt(nc, s2, "f_hb%d" % i, [128, D], BF16) for i in range(2)]
            bufs = [(sbt(nc, s2, "f_st%d" % i, [128, 2, 6], F32), sbt(nc, s2, "f_mv%d" % i, [128, 2], F32),
                     sbt(nc, s2, "f_rs%d" % i, [128, 1], F32), sbt(nc, s2, "f_nb%d" % i, [128, 1], F32),
                     sbt(nc, s2, "f_xn%d" % i, [128, D], F32)) for i in range(2)]
            h32 = [sbt(nc, s2, "f_h32%d" % i, [128, 8, 128], F32) for i in range(2)]
            rw = sbt(nc, s2, "f_rw", [128, 8, NE], F32)
            rb = sbt(nc, s2, "f_rb", [128, NE], F32)
            lg = [sbt(nc, s2, "f_lg%d" % i, [128, NE], F32) for i in range(2)]
            mk = [sbt(nc, s2, "f_mk%d" % i, [128, NE], F32) for i in range(2)]
            gt_ = [sbt(nc, s2, "f_gt%d" % i, [128, NE], F32) for i in range(2)]
            ngp = [sbt(nc, s2, "f_ngp%d" % i, [128, NE], F32) for i in range(2)]
            oh = [sbt(nc, s2, "f_oh%d" % i, [128, NE], F32) for i in range(2)]
            jk = [sbt(nc, s2, "f_jk%d" % i, [128, NE], F32) for i in range(2)]
            m8 = [sbt(nc, s2, "f_m8%d" % i, [128, 12], F32) for i in range(2)]
            t8 = [sbt(nc, s2, "f_t8%d" % i, [128, 8], F32) for i in range(2)]
            msum = sbt(nc, s2, "f_msum", [128, NE], F32)
            pR = [pst(nc, s2, "f_pR%d" % i, [128, 2, 512], F32) for i in range(2)]
            pL = [pst(nc, s2, "f_pL%d" % i, [128, 512], F32) for i in range(2)]
            pC = [pst(nc, s2, "f_pC%d" % i, [128, 512], F32) for i in range(2)]
            kb.dma('sp', moec[:], A['moec'][:, :], w=['f_moec'])
            kb.dma('sp', rw[:], A['router_w'][l].rearrange("(kc p) n -> p kc n", p=128), w=['f_rw'])
            kb.dma('sp', rb[:], A['rb_bc'][l], w=['f_rb'])
            kb.op('dve', lambda e: e.memset(msum[:], 0.0), w=['f_msum'])
            triu = moec[:, 0:128]
            ecb = moec[:, 128:160]
            for t in range(NT):
                b = t % 2
                sx = 'f' + str(b)
                B_ = str(b)
                kb.dma('sp', xts[b][:], x_src[t * 128:(t + 1) * 128, :], r=['xsrc5'], w=['f_xt' + B_])
                stt, mv, rstd, nb, xn = bufs[b]
                ln_mod_tile(kb, nc, xts[b], 'f_xt' + B_, bufs[b], mod_bc[:, 4 * D:5 * D], mod_bc[:, 3 * D:4 * D],
                            xn[:], 'ln_xn' + sx, sx)
                cp(kb, 'act', hbs[b][:], xn[:], ['ln_xn' + sx], ['f_hb' + B_])
                for kc in range(8):
                    kb.op('pe', lambda e, kc=kc: e.matmul(pR[b][:, kc // 4, (kc % 4) * 128:(kc % 4 + 1) * 128],
                                                          lhsT=xn[:, kc * 128:(kc + 1) * 128], rhs=ident,
                                                          start=True, stop=True),
                          r=['ln_xn' + sx, 'cst'], w=['f_pR' + B_])
                cp(kb, 'dve', h32[b][:].rearrange("p (a c) n -> p a (c n)", a=2), pR[b][:], ['f_pR' + B_],
                   ['f_h32' + B_])
                for kc in range(8):
                    kb.op('pe', lambda e, kc=kc: e.matmul(pL[b][:, 0:NE], lhsT=h32[b][:, kc, :], rhs=rw[:, kc, :],
                                                          start=(kc == 0), stop=(kc == 7)),
                          r=['f_h32' + B_, 'f_rw'], w=['f_pL' + B_])
                kb.op('dve', lambda e: e.tensor_tensor(out=lg[b][:], in0=pL[b][:, 0:NE], in1=rb[:], op=ALU.add),
                      r=['f_pL' + B_, 'f_rb'], w=['f_lg' + B_])
                kb.op('dve', lambda e: e.max(out=m8[b][:, 0:8], in_=lg[b][:]), r=['f_lg' + B_], w=['f_m8' + B_])
                kb.op('dve', lambda e: e.tensor_scalar(out=mk[b][:], in0=lg[b][:], scalar1=m8[b][:, 3:4], scalar2=None,
                                                       op0=ALU.is_ge), r=['f_lg' + B_, 'f_m8' + B_], w=['f_mk' + B_])
                kb.op('dve', lambda e: e.tensor_scalar(out=m8[b][:, 8:9], in0=m8[b][:, 0:1], scalar1=-1.0, scalar2=None,
                                                       op0=ALU.mult), r=['f_m8' + B_], w=['f_m8' + B_])
                kb.op('act', lambda e: e.activation(out=lg[b][:], in_=lg[b][:], func=AF.Exp, bias=m8[b][:, 8:9],
                                                    scale=1.0), r=['f_lg' + B_, 'f_m8' + B_], w=['f_lg' + B_])
                kb.op('dve', lambda e: e.tensor_tensor(out=lg[b][:], in0=lg[b][:], in1=mk[b][:], op=ALU.mult),
                      r=['f_lg' + B_, 'f_mk' + B_], w=['f_lg' + B_])
                kb.op('dve', lambda e: e.reduce_sum(out=m8[b][:, 9:10], in_=lg[b][:], axis=AX.X),
                      r=['f_lg' + B_], w=['f_m8' + B_])
                kb.op('dve', lambda e: e.reciprocal(out=m8[b][:, 10:11], in_=m8[b][:, 9:10]),
                      r=['f_m8' + B_], w=['f_m8' + B_])
                kb.op('dve', lambda e: e.tensor_scalar(out=gt_[b][:], in0=lg[b][:], scalar1=m8[b][:, 10:11],
                                                       scalar2=None, op0=ALU.mult),
                      r=['f_lg' + B_, 'f_m8' + B_], w=['f_gt' + B_])
                kb.op('pe', lambda e: e.matmul(pC[b][:, 0:NE], lhsT=triu, rhs=mk[b][:], start=True, stop=False),
                      r=['f_moec', 'f_mk' + B_], w=['f_pC' + B_])
                kb.op('pe', lambda e: e.matmul(pC[b][:, 0:NE], lhsT=cst['ones_f'], rhs=msum[:], start=False, stop=True),
                      r=['cst', 'f_msum'], w=['f_pC' + B_])
                kb.op('dve', lambda e: e.tensor_tensor(out=msum[:], in0=msum[:], in1=mk[b][:], op=ALU.add),
                      r=['f_msum', 'f_mk' + B_], w=['f_msum'])
                kb.op('dve', lambda e: e.tensor_tensor(out=ngp[b][:], in0=pC[b][:, 0:NE], in1=ecb, op=ALU.add),
                      r=['f_pC' + B_, 'f_moec'], w=['f_ngp' + B_])
                kb.op('dve', lambda e: e.tensor_scalar(out=ngp[b][:], in0=ngp[b][:], scalar1=-1.0, scalar2=BIG,
                                                       op0=ALU.mult, op1=ALU.add), r=['f_ngp' + B_], w=['f_ngp' + B_])
                kb.op('dve', lambda e: e.tensor_tensor(out=ngp[b][:], in0=ngp[b][:], in1=mk[b][:], op=ALU.mult),
                      r=['f_ngp' + B_, 'f_mk' + B_], w=['f_ngp' + B_])
                kb.op('dve', lambda e: e.tensor_scalar(out=ngp[b][:], in0=ngp[b][:], scalar1=-BIG, scalar2=None,
                                                       op0=ALU.add), r=['f_ngp' + B_], w=['f_ngp' + B_])
                kb.op('dve', lambda e: e.max(out=t8[b][:], in_=ngp[b][:]), r=['f_ngp' + B_], w=['f_t8' + B_])
                dk = 'f_dest%d' % t
                kb.op('dve', lambda e: e.tensor_scalar(out=dest_all[:, t * 4:(t + 1) * 4], in0=t8[b][:, 0:4], scalar1=-1.0,
                                                       scalar2=None, op0=ALU.mult), r=['f_t8' + B_], w=[dk])
                for k in range(4):
                    kb.op('dve', lambda e, k=k: e.tensor_scalar(out=oh[b][:], in0=ngp[b][:], scalar1=t8[b][:, k:k + 1],
                                                               scalar2=None, op0=ALU.is_equal),
                          r=['f_ngp' + B_, 'f_t8' + B_], w=['f_oh' + B_])
                    kb.op('dve', lambda e, k=k: e.scalar_tensor_tensor(out=jk[b][:], in0=oh[b][:], scalar=1.0,
                                                                      in1=gt_[b][:], op0=ALU.mult, op1=ALU.mult,
                                                                      accum_out=gk_all[:, t, k:k + 1]),
                          r=['f_oh' + B_, 'f_gt' + B_], w=['f_jk' + B_, 'f_gk'])
                for k in range(4):
                    kb.ind('pool', ['f_hb' + B_, dk], ['s_hg_%d' % (t * 4 + k)], out=hg[:, :],
                           out_offset=bass.IndirectOffsetOnAxis(ap=dest_all[:, t * 4 + k:t * 4 + k + 1], axis=0),
                           in_=hbs[b][:, :], in_offset=None, bounds_check=cst['breg'], oob_is_err=False)
            kb.barrier()
        with ExitStack() as s3:
            hgT = [sbt(nc, s3, "f_hgT%d" % i, [128, 8, C], BF16) for i in range(2)]
            hgb = [sbt(nc, s3, "f_hgb%d" % i, [128, D], BF16) for i in range(2)]
            actT = sbt(nc, s3, "f_actT", [128, 8, C], BF16)
            w1t = [sbt(nc, s3, "f_w1%d" % i, [128, 8, 512], BF16) for i in range(2)]
            w2t = [sbt(nc, s3, "f_w2%d" % i, [128, 8, D], BF16) for i in range(2)]
            b1t = [sbt(nc, s3, "f_b1%d" % i, [128, 16], F32) for i in range(2)]
            b2t = [sbt(nc, s3, "f_b2%d" % i, [1, D], BF16) for i in range(2)]
            onesb = sbt(nc, s3, "f_onesb", [1, 128], BF16)
            gl = [sbt(nc, s3, "f_gl%d" % i, [128, 512], F32) for i in range(2)]
            sg = [sbt(nc, s3, "f_sg%d" % i, [128, 512], F32) for i in range(2)]
            li = [sbt(nc, s3, "f_li%d" % i, [128, 512], F32) for i in range(2)]
            yrow = [sbt(nc, s3, "f_yr%d" % i, [128, D], F32) for i in range(2)]
            PTr = PsRot(kb, nc, s3, 2, "f_ptr", dt=BF16, shape=(128, 8, 128))
            PG = PsRot(kb, nc, s3, 4, "f_pg")
            PY = PsRot(kb, nc, s3, 2, "f_py")
            kb.op('dve', lambda e: e.tensor_copy(out=onesb[:], in_=cst['ones_f'][0:1, :]), r=['cst'], w=['f_onesb'])
            W1, W2 = A['exp_w1'], A['exp_w2']
            st5 = {'u': 0, 'yc': 0}

            def load_exp(ex):
                eb = ex % 2
                EB = str(eb)
                kb.dma('sp', b1t[eb][:], A['exp_b1l'][l, ex], w=['f_b1' + EB])
                kb.dma('pool', b2t[eb][:], A['exp_b2'][l, ex:ex + 1, :], w=['f_b2' + EB])
                kb.dma('pool', w2t[eb][:], W2[l, ex].rearrange("(kc p) n -> p kc n", p=128), w=['f_w2' + EB],
                       max_dma_last_dim=4096)

            def load_w1(i):
                ex, g = i // 4, i % 4
                wb = i % 2
                kb.dma('pool', w1t[wb][:, :, 0:256],
                       W1[l, ex, :, g * 256:(g + 1) * 256].rearrange("(kc p) n -> p kc n", p=128), w=['f_w1%d' % wb])
                kb.dma('pool', w1t[wb][:, :, 256:512],
                       W1[l, ex, :, DFF + g * 256:DFF + (g + 1) * 256].rearrange("(kc p) n -> p kc n", p=128),
                       w=['f_w1%d' % wb])

            def prelude(ex):
                eb = ex % 2
                EB = str(eb)
                for blk in range(NB):
                    hb_ = blk % 2
                    kb.dma('sp', hgb[hb_][:], hg[ex * C + blk * 128:ex * C + (blk + 1) * 128, :], w=['f_hgb%d' % hb_])
                    ptr, ptrk = PTr.next()
                    for kc in range(8):
                        kb.op('pe', lambda e, kc=kc: e.transpose(out=ptr[:, kc, :], in_=hgb[hb_][:, kc * 128:(kc + 1) * 128],
                                                                 identity=cst['ident_b'][:, :]),
                              r=['f_hgb%d' % hb_, 'cst'], w=[ptrk])
                    cp(kb, 'act' if blk % 2 == 0 else 'dve', hgT[eb][:, :, blk * 128:(blk + 1) * 128], ptr[:], [ptrk],
                       ['f_hgT' + EB])

            def w1_group(i):
                ex, g = i // 4, i % 4
                wb = i % 2
                eb = ex % 2
                EB = str(eb)
                for fl in range(2):
                    fc = g * 2 + fl
                    for scn in range(C // 512):
                        tb = st5['u'] % 2
                        st5['u'] += 1
                        TB = str(tb)
                        sl = slice(scn * 512, (scn + 1) * 512)
                        pg, pgk = PG.next()
                        for kc in range(8):
                            kb.op('pe', lambda e, kc=kc: e.matmul(pg[:], lhsT=w1t[wb][:, kc, fl * 128:(fl + 1) * 128],
                                                                  rhs=hgT[eb][:, kc, sl], start=(kc == 0), stop=(kc == 7)),
                                  r=['f_w1%d' % wb, 'f_hgT' + EB], w=[pgk])
                        pl, plk = PG.next()
                        for kc in range(8):
                            kb.op('pe', lambda e, kc=kc: e.matmul(
                                pl[:], lhsT=w1t[wb][:, kc, 256 + fl * 128:256 + (fl + 1) * 128],
                                rhs=hgT[eb][:, kc, sl], start=(kc == 0), stop=(kc == 7)),
                                r=['f_w1%d' % wb, 'f_hgT' + EB], w=[plk])
                        kb.op('dve', lambda e: e.tensor_scalar(out=gl[tb][:], in0=pg[:], scalar1=b1t[eb][:, fc:fc + 1],
                                                               scalar2=7.0, op0=ALU.add, op1=ALU.min),
                              r=[pgk, 'f_b1' + EB], w=['f_gl' + TB])
                        kb.op('act', lambda e: e.activation(out=sg[tb][:], in_=gl[tb][:], func=AF.Sigmoid,
                                                            scale=1.702), r=['f_gl' + TB], w=['f_sg' + TB])
                        kb.op('dve', lambda e: e.tensor_scalar(out=li[tb][:], in0=pl[:],
                                                               scalar1=b1t[eb][:, 8 + fc:9 + fc], scalar2=7.0,
                                                               op0=ALU.add, op1=ALU.min),
                              r=[plk, 'f_b1' + EB], w=['f_li' + TB])
                        kb.op('dve', lambda e: e.tensor_scalar(out=li[tb][:], in0=li[tb][:], scalar1=-7.0,
                                                               scalar2=1.0, op0=ALU.max, op1=ALU.add),
                              r=['f_li' + TB], w=['f_li' + TB])
                        kb.op('dve', lambda e: e.tensor_tensor(out=gl[tb][:], in0=gl[tb][:], in1=sg[tb][:],
                                                               op=ALU.mult),
                              r=['f_gl' + TB, 'f_sg' + TB], w=['f_gl' + TB])
                        kb.op('dve', lambda e: e.tensor_tensor(out=actT[:, fc, sl], in0=gl[tb][:], in1=li[tb][:],
                                                               op=ALU.mult),
                              r=['f_gl' + TB, 'f_li' + TB], w=['f_actT'])

            def w2_phase(ex):
                eb = ex % 2
                EB = str(eb)
                for blk in range(NB):
                    yb_ = st5['yc'] % 2
                    st5['yc'] += 1
                    for half in range(2):
                        py, pyk = PY.next()
                        for fc in range(8):
                            kb.op('pe', lambda e, fc=fc: e.matmul(py[:], lhsT=actT[:, fc, blk * 128:(blk + 1) * 128],
                                                                  rhs=w2t[eb][:, fc, half * 512:(half + 1) * 512],
                                                                  start=(fc == 0), stop=False),
                                  r=['f_actT', 'f_w2' + EB], w=[pyk])
                        kb.op('pe', lambda e: e.matmul(py[:], lhsT=onesb[0:1, :],
                                                       rhs=b2t[eb][0:1, half * 512:(half + 1) * 512],
                                                       start=False, stop=True), r=['f_onesb', 'f_b2' + EB], w=[pyk])
                        cp(kb, 'act', yrow[yb_][:, half * 512:(half + 1) * 512], py[:], [pyk], ['f_yr%d' % yb_])
                    kb.dma('sp', yg[ex * C + blk * 128:ex * C + (blk + 1) * 128, :], yrow[yb_][:], r=['f_yr%d' % yb_],
                           w=['s_yg_%d' % (ex * NB + blk)])

            load_exp(0)
            prelude(0)
            load_w1(0)
            for ex in range(NE):
                if ex + 1 < NE:
                    load_exp(ex + 1)
                for g in range(4):
                    i = ex * 4 + g
                    if i + 1 < NE * 4:
                        load_w1(i + 1)
                    w1_group(i)
                if ex + 1 < NE:
                    prelude(ex + 1)
                w2_phase(ex)
            kb.barrier()
        with ExitStack() as s4:
            rows = [sbt(nc, s4, "f_row%d" % i, [128, D], F32) for i in range(4)]
            accs = [sbt(nc, s4, "f_acc%d" % i, [128, D], F32) for i in range(2)]
            xt = [sbt(nc, s4, "f_x%d" % i, [128, D], F32) for i in range(2)]
            lng = sbt(nc, s4, "f_lng", [128, D], F32)
            lnb = sbt(nc, s4, "f_lnb", [128, D], F32)
            lb = [(sbt(nc, s4, "f_lst%d" % i, [128, 2, 6], F32), sbt(nc, s4, "f_lmv%d" % i, [128, 2], F32),
                   sbt(nc, s4, "f_lrs%d" % i, [128, 1], F32), sbt(nc, s4, "f_lnb%d" % i, [128, 1], F32))
                  for i in range(2)]
            kb.dma('sp', lng[:], A['ln2g_bc'][l], w=['lngb'])
            kb.dma('sp', lnb[:], A['ln2b_bc'][l], w=['lngb'])
            for T in range(NT):
                xb = T % 2
                XB = str(xb)
                kb.dma('sp', xt[xb][:], x_src[T * 128:(T + 1) * 128, :], r=['xsrc5'], w=['f_x' + XB])
                for k in range(4):
                    kb.ind('pool', ['f_dest%d' % T], ['f_row%d' % k], out=rows[k][:, :], out_offset=None,
                           in_=yg[:, :], in_offset=bass.IndirectOffsetOnAxis(ap=dest_all[:, T * 4 + k:T * 4 + k + 1], axis=0),
                           bounds_check=cst['breg'], oob_is_err=False)
                kb.op('dve', lambda e: e.tensor_scalar(out=accs[xb][:], in0=rows[0][:], scalar1=gk_all[:, T, 0:1],
                                                       scalar2=None, op0=ALU.mult),
                      r=['f_row0', 'f_gk'], w=['f_acc' + XB])
                for k in range(1, 4):
                    kb.op('dve', lambda e, k=k: e.scalar_tensor_tensor(out=accs[xb][:], in0=rows[k][:],
                                                                      scalar=gk_all[:, T, k:k + 1], in1=accs[xb][:],
                                                                      op0=ALU.mult, op1=ALU.add),
                          r=['f_row%d' % k, 'f_gk', 'f_acc' + XB], w=['f_acc' + XB])
                kb.op('pool', lambda e: e.tensor_tensor(out=accs[xb][:], in0=accs[xb][:], in1=mod_bc[:, 5 * D:6 * D],
                                                        op=ALU.mult), r=['f_acc' + XB, 'mod_bc'], w=['f_acc' + XB])
                kb.op('dve', lambda e: e.scalar_tensor_tensor(out=xt[xb][:], in0=xt[xb][:], scalar=ALPHA,
                                                              in1=accs[xb][:], op0=ALU.mult, op1=ALU.add),
                      r=['f_x' + XB, 'f_acc' + XB], w=['f_x' + XB])
                ln_affine_store(kb, nc, xt[xb], 'f_x' + XB, lb[xb], lng[:], lnb[:],
                                x_dst[T * 128:(T + 1) * 128, :], 'f' + XB, 'xdst5')
            kb.barrier()
```

```python
import numpy as np
from contextlib import ExitStack
import concourse.bass as bass
import concourse.mybir as mybir
from concourse.bass_utils import run_bass_kernel_spmd
from concourse.alu_op_type import AluOpType as ALU

F32, BF16 = mybir.dt.float32, mybir.dt.bfloat16
I32 = mybir.dt.int32
CAP = 1536
BIG = 1.0e6
AF = mybir.ActivationFunctionType
AX = mybir.AxisListType

D = 1024
S = 4096
NT = S // 128
DEPTH = 4
INW = 6408
NE = 32
DFF = 1024
ALPHA = (2 * DEPTH) ** 0.25
NEG = -30000.0
DBG = {}


class KB:
    def __init__(self, nc, es, ndma=48):
        self.nc = nc
        self.eng = {'pe': nc.tensor, 'dve': nc.vector, 'act': nc.scalar, 'pool': nc.gpsimd, 'sp': nc.sync}
        self.sems = []
        self.psid = {}
        for e in self.eng:
            self.psid[e] = len(self.sems)
            self.sems.append(es.enter_context(nc.semaphore("ps_" + e)))
        self.cnt = {e: 0 for e in self.eng}
        self.dsid = []
        for i in range(ndma):
            self.dsid.append(len(self.sems))
            self.sems.append(es.enter_context(nc.semaphore("ds%d" % i)))
        self.dcum = [0] * ndma
        self.dnext = 0
        self.seen = {e: {} for e in self.eng}
        self.lastw = {}
        self.reads = {}
        self.nins = 0
        self.excl = set()

    def need(self, E, tok):
        sid, val, _ = tok
        if self.seen[E].get(sid, 0) < val:
            self.eng[E].wait_ge(self.sems[sid], val)
            self.seen[E][sid] = val
            if 'log' in DBG:
                DBG['log'].append("%s WAIT sem%d>=%d" % (E, sid, val))

    def _deps(self, E, r, w, isdma):
        for k in r:
            t = self.lastw.get(k)
            if t is not None:
                self.need(E, t)
            if k in self.excl:
                for t in self.reads.get(k, {}).values():
                    if t[2] != E:
                        self.need(E, t)
        for k in w:
            t = self.lastw.get(k)
            if t is not None and (isdma or t[2] != E):
                self.need(E, t)
            for t in self.reads.get(k, {}).values():
                if isdma or t[2] != E:
                    self.need(E, t)

    def _commit(self, tok, r, w):
        for k in r:
            self.reads.setdefault(k, {})[tok[0]] = tok
        for k in w:
            self.lastw[k] = tok
            self.reads[k] = {}

    def op(self, E, fn, r=(), w=()):
        self._deps(E, r, w, False)
        ins = fn(self.eng[E])
        ins.then_inc(self.sems[self.psid[E]], 1)
        self.cnt[E] += 1
        self.nins += 1
        tok = (self.psid[E], self.cnt[E], E)
        if 'log' in DBG:
            DBG['log'].append("%s OP#%d r=%s w=%s" % (E, self.cnt[E], list(r), list(w)))
        self._commit(tok, r, w)
        return tok

    def dma(self, Q, out, in_, r=(), w=(), **kw):
        self._deps(Q, r, w, True)
        i = self.dnext
        self.dnext = (i + 1) % len(self.dsid)
        if self.dcum[i] > 0:
            self.need(Q, (self.dsid[i], self.dcum[i], None))
        self.eng[Q].dma_start(out=out, in_=in_, **kw).then_inc(self.sems[self.dsid[i]], 16)
        self.dcum[i] += 16
        self.nins += 1
        tok = (self.dsid[i], self.dcum[i], None)
        if 'log' in DBG:
            DBG['log'].append("%s DMA sem%d->%d r=%s w=%s" % (Q, self.dsid[i], self.dcum[i], list(r), list(w)))
        self._commit(tok, r, w)
        return tok

    def ind(self, Q, r, w, **kw):
        self._deps(Q, r, w, True)
        i = self.dnext
        self.dnext = (i + 1) % len(self.dsid)
        if self.dcum[i] > 0:
            self.need(Q, (self.dsid[i], self.dcum[i], None))
        self.eng[Q].indirect_dma_start(**kw).then_inc(self.sems[self.dsid[i]], 16)
        self.dcum[i] += 16
        self.nins += 1
        tok = (self.dsid[i], self.dcum[i], None)
        self._commit(tok, r, w)
        return tok

    def barrier(self):
        for E in self.eng:
            for F in self.eng:
                if F != E and self.cnt[F] > 0:
                    self.need(E, (self.psid[F], self.cnt[F], F))
            for i, s in enumerate(self.dsid):
                if self.dcum[i] > 0:
                    self.need(E, (s, self.dcum[i], None))


_UID = [0]


def _uname(name):
    _UID[0] += 1
    return "%s_u%d" % (name, _UID[0])


def sbt(nc, st, name, shape, dt):
    return st.enter_context(nc.sbuf_tensor(_uname(name), shape, dt))


def pst(nc, st, name, shape, dt):
    return st.enter_context(nc.psum_tensor(_uname(name), shape, dt))


def stage_mod(kb, nc, l, A, mod_bc, cst):
    with ExitStack() as st:
        cc = sbt(nc, st, "m_cc", [128, 8], F32)
        cond = sbt(nc, st, "m_cond", [128, 8], F32)
        crep = sbt(nc, st, "m_crep", [128, 8, 128], F32)
        brow = sbt(nc, st, "m_brow", [1, 6144], F32)
        wa = [sbt(nc, st, "m_wa%d" % i, [128, 8, 512], F32) for i in range(2)]
        ps = [pst(nc, st, "m_ps%d" % i, [128, 512], F32) for i in range(2)]
        kb.dma('sp', cc[:], A['c_col'][:, :], w=['m_cc'])
        kb.dma('sp', brow[:], A['ada_b'][l:l + 1, :], w=['m_brow'])
        kb.op('act', lambda e: e.activation(out=cond[:], in_=cc[:], func=AF.Silu), r=['m_cc'], w=['m_cond'])
        for kc in range(8):
            kb.op('dve', lambda e, kc=kc: e.tensor_scalar(out=crep[:, kc, :], in0=cst['ones_f'][:, :],
                                                         scalar1=cond[:, kc:kc + 1], scalar2=None, op0=ALU.mult),
                  r=['m_cond', 'cst'], w=['m_crep'])
        for j in range(12):
            b = j % 2
            kb.dma('sp' if j % 2 == 0 else 'pool', wa[b][:],
                   A['ada_w'][l, :, j * 512:(j + 1) * 512].rearrange("(kc p) n -> p kc n", p=128),
                   w=['m_wa%d' % b])
            for kc in range(8):
                kb.op('pe', lambda e, kc=kc, b=b: e.matmul(ps[b][:], lhsT=crep[:, kc, :], rhs=wa[b][:, kc, :],
                                                           start=(kc == 0), stop=False),
                      r=['m_crep', 'm_wa%d' % b], w=['m_ps%d' % b])
            kb.op('pe', lambda e, b=b, j=j: e.matmul(ps[b][:], lhsT=cst['ones_f'][0:1, :],
                                                     rhs=brow[0:1, j * 512:(j + 1) * 512], start=False, stop=True),
                  r=['m_brow', 'cst'], w=['m_ps%d' % b])
            addone = 1.0 if (j // 2) in (1, 2, 4, 5) else 0.0
            kb.op('act', lambda e, b=b, j=j, addone=addone: e.activation(
                out=mod_bc[:, j * 512:(j + 1) * 512], in_=ps[b][:], func=AF.Identity, bias=addone, scale=1.0),
                r=['m_ps%d' % b], w=['mod_bc'])
        kb.barrier()


def ln_mod_tile_g(kb, nc, xt, xk, bufs, sc_ap, sh_ap, h_out, hk, sfx):
    stt, mv, rstd, nb, xn = bufs
    kb.op('dve', lambda e: e.bn_stats(out=stt[:, 0, :], in_=xt[:, 0:512]), r=[xk], w=['ln_st' + sfx])
    yield
    kb.op('dve', lambda e: e.bn_stats(out=stt[:, 1, :], in_=xt[:, 512:1024]), r=[xk], w=['ln_st' + sfx])
    yield
    kb.op('dve', lambda e: e.bn_aggr(out=mv[:], in_=stt[:].rearrange("p a b -> p (a b)")),
          r=['ln_st' + sfx], w=['ln_mv' + sfx])
    yield
    kb.op('act', lambda e: e.activation(out=rstd[:], in_=mv[:, 1:2], func=AF.Sqrt, bias=1e-5, scale=1.0),
          r=['ln_mv' + sfx], w=['ln_rstd' + sfx])
    yield
    kb.op('dve', lambda e: e.reciprocal(out=rstd[:], in_=rstd[:]), r=['ln_rstd' + sfx], w=['ln_rstd' + sfx])
    yield
    kb.op('dve', lambda e: e.scalar_tensor_tensor(out=nb[:], in0=mv[:, 0:1], scalar=-1.0, in1=rstd[:],
                                                  op0=ALU.mult, op1=ALU.mult),
          r=['ln_mv' + sfx, 'ln_rstd' + sfx], w=['ln_nb' + sfx])
    yield
    kb.op('act', lambda e: e.activation(out=xn[:], in_=xt[:], func=AF.Identity, bias=nb[:], scale=rstd[:]),
          r=[xk, 'ln_nb' + sfx, 'ln_rstd' + sfx], w=['ln_xn' + sfx])
    yield
    kb.op('dve', lambda e: e.tensor_tensor(out=xn[:], in0=xn[:], in1=sc_ap, op=ALU.mult),
          r=['ln_xn' + sfx, 'mod_bc'], w=['ln_xn' + sfx])
    yield
    kb.op('dve', lambda e: e.tensor_tensor(out=h_out, in0=xn[:], in1=sh_ap, op=ALU.add),
          r=['ln_xn' + sfx, 'mod_bc'], w=[hk])
    yield


def ln_mod_tile(kb, nc, xt, xk, bufs, sc_ap, sh_ap, h_out, hk, sfx):
    for _ in ln_mod_tile_g(kb, nc, xt, xk, bufs, sc_ap, sh_ap, h_out, hk, sfx):
        pass


def build_hT(kb, nc, st, x_src, mod_bc, sc_off, sh_off, hT, cst, pfx):
    xts = [sbt(nc, st, pfx + "xt%d" % i, [128, D], F32) for i in range(2)]
    hbs = [sbt(nc, st, pfx + "hb%d" % i, [128, D], BF16) for i in range(2)]
    bufs = []
    for i in range(2):
        bufs.append((sbt(nc, st, pfx + "st%d" % i, [128, 2, 6], F32), sbt(nc, st, pfx + "mv%d" % i, [128, 2], F32),
                     sbt(nc, st, pfx + "rs%d" % i, [128, 1], F32), sbt(nc, st, pfx + "nb%d" % i, [128, 1], F32),
                     sbt(nc, st, pfx + "xn%d" % i, [128, D], F32)))
    pT = [pst(nc, st, pfx + "pT%d" % i, [128, 8, 128], BF16) for i in range(2)]
    def tile_gen(par):
        for t in range(par, NT, 2):
            b = t % 2
            kb.dma('sp', xts[b][:], x_src[t * 128:(t + 1) * 128, :], r=[pfx + 'xsrc'], w=[pfx + 'xt%d' % b])
            yield
            yield from ln_mod_tile_g(kb, nc, xts[b], pfx + 'xt%d' % b, bufs[b], mod_bc[:, sc_off:sc_off + D],
                        mod_bc[:, sh_off:sh_off + D], hbs[b][:], pfx + 'hb%d' % b, pfx + str(b))
            for kc in range(8):
                kb.op('pe', lambda e, kc=kc, b=b: e.transpose(out=pT[b][:, kc, :], in_=hbs[b][:, kc * 128:(kc + 1) * 128],
                                                              identity=cst['ident_b'][:, :]),
                      r=[pfx + 'hb%d' % b, 'cst'], w=[pfx + 'pT%d' % b])
                yield
            eng = 'act' if t % 2 == 0 else 'dve'
            if eng == 'act':
                kb.op('act', lambda e, b=b, t=t: e.copy(out=hT[:, :, t * 128:(t + 1) * 128], in_=pT[b][:]),
                      r=[pfx + 'pT%d' % b], w=['hT'])
                yield
            else:
                kb.op('dve', lambda e, b=b, t=t: e.tensor_copy(out=hT[:, :, t * 128:(t + 1) * 128], in_=pT[b][:]),
                      r=[pfx + 'pT%d' % b], w=['hT'])
                yield

    gens = [tile_gen(0), tile_gen(1)]
    while gens:
        for g_ in list(gens):
            try:
                next(g_)
            except StopIteration:
                gens.remove(g_)


def stage_proj(kb, nc, l, A, SC, x_src, mod_bc, cst):
    with ExitStack() as st:
        hT = sbt(nc, st, "hT", [128, 8, S], BF16)
        with ExitStack() as st2:
            build_hT(kb, nc, st2, x_src, mod_bc, 1 * D, 0, hT, cst, "p1_")
            kb.barrier()
        wts = [sbt(nc, st, "wt%d" % i, [128, 8, 512], BF16) for i in range(2)]
        raw = sbt(nc, st, "raw", [128, S + 4], F32)
        acc = sbt(nc, st, "acc", [128, S], F32)
        tmp = [sbt(nc, st, "tmp%d" % i, [128, 512], F32) for i in range(2)]
        gb = [sbt(nc, st, "gb%d" % i, [128, S], BF16) for i in range(2)]
        cw = sbt(nc, st, "cw", [128, 12, 4], F32)
        alog = sbt(nc, st, "alog", [128, 4], F32)
        dtb = sbt(nc, st, "dtb", [128, 4], F32)
        sm = sbt(nc, st, "sm", [128, 6, 4], F32)
        g_all = sbt(nc, st, "g_all", [128, NT, 4], F32)
        b_all = sbt(nc, st, "b_all", [128, NT, 4], F32)
        vb_all = sbt(nc, st, "vb_all", [128, NT, 128], BF16)
        zt = [sbt(nc, st, "zt%d" % i, [128, 512], F32) for i in range(2)]
        ps = [pst(nc, st, "pj_ps%d" % i, [128, 512], F32) for i in range(4)]
        kb.dma('sp', cw[:], A['conv_wl'][l], w=['cw'])
        kb.dma('sp', alog[:], A['alog_bc'][l], w=['alog'])
        kb.dma('sp', dtb[:], A['dtb_bc'][l], w=['dtb'])
        kb.op('act', lambda e: e.activation(out=alog[:], in_=alog[:], func=AF.Exp), r=['alog'], w=['alog'])
        kb.op('dve', lambda e: e.memset(raw[:, 0:4], 0.0), w=['raw'])
        W = A['w_in']
        state = {'g': 0, 'ps': 0, 'gb': 0}

        def load_group(pieces):
            b = state['g'] % 2
            state['g'] += 1
            for (off, c0, n) in pieces:
                kb.dma('pool', wts[b][:, :, off:off + n],
                       W[l, :, c0:c0 + n].rearrange("(kc p) n -> p kc n", p=128), w=['wt%d' % b])
            return b

        def fm_chunk(b, off, evac):
            for tc in range(8):
                p = state['ps'] % 4
                state['ps'] += 1
                for kc in range(8):
                    kb.op('pe', lambda e, kc=kc, p=p, tc=tc: e.matmul(
                        ps[p][:], lhsT=wts[b][:, kc, off:off + 128], rhs=hT[:, kc, tc * 512:(tc + 1) * 512],
                        start=(kc == 0), stop=(kc == 7)), r=['wt%d' % b, 'hT'], w=['pj_ps%d' % p])
                evac(tc, ps[p], 'pj_ps%d' % p)

        def conv_chunk(b, off, ch):
            def ev(tc, p, pk):
                eng = 'act' if tc % 2 == 0 else 'dve'
                if eng == 'act':
                    kb.op('act', lambda e: e.copy(out=raw[:, 4 + tc * 512: 4 + (tc + 1) * 512], in_=p[:]),
                          r=[pk], w=['raw'])
                else:
                    kb.op('dve', lambda e: e.tensor_copy(out=raw[:, 4 + tc * 512: 4 + (tc + 1) * 512], in_=p[:]),
                          r=[pk], w=['raw'])
            fm_chunk(b, off, ev)
            kb.op('dve', lambda e: e.tensor_scalar(out=acc[:], in0=raw[:, 1:1 + S], scalar1=cw[:, ch, 0:1],
                                                   scalar2=None, op0=ALU.mult), r=['raw', 'cw'], w=['acc'])
            for j in range(1, 4):
                kb.op('dve', lambda e, j=j: e.scalar_tensor_tensor(out=acc[:], in0=raw[:, 1 + j:1 + j + S],
                                                                  scalar=cw[:, ch, j:j + 1], in1=acc[:],
                                                                  op0=ALU.mult, op1=ALU.add),
                      r=['raw', 'cw', 'acc'], w=['acc'])
            kb.op('act', lambda e: e.activation(out=acc[:], in_=acc[:], func=AF.Silu), r=['acc'], w=['acc'])
            if ch < 8:
                kb.op('act', lambda e: e.activation(out=raw[:, 4:4 + S], in_=acc[:], func=AF.Square),
                      r=['acc'], w=['raw'])
                qs = (128 ** -0.5) if ch < 4 else 1.0
                for tc in range(8):
                    p = state['ps'] % 4
                    state['ps'] += 1
                    tb = tc % 2
                    kb.op('pe', lambda e, p=p, tc=tc: e.matmul(ps[p][:], lhsT=cst['ones_f'][:, :],
                                                               rhs=raw[:, 4 + tc * 512:4 + (tc + 1) * 512],
                                                               start=True, stop=True),
                          r=['raw', 'cst'], w=['pj_ps%d' % p])
                    kb.op('act', lambda e, p=p, tb=tb: e.activation(out=tmp[tb][:], in_=ps[p][:], func=AF.Sqrt,
                                                                    bias=1e-6, scale=1.0),
                          r=['pj_ps%d' % p], w=['tmp%d' % tb])
                    kb.op('dve', lambda e, tb=tb: e.reciprocal(out=tmp[tb][:], in_=tmp[tb][:]),
                          r=['tmp%d' % tb], w=['tmp%d' % tb])
                    kb.op('dve', lambda e, tb=tb, tc=tc: e.scalar_tensor_tensor(
                        out=acc[:, tc * 512:(tc + 1) * 512], in0=acc[:, tc * 512:(tc + 1) * 512], scalar=qs,
                        in1=tmp[tb][:], op0=ALU.mult, op1=ALU.mult), r=['tmp%d' % tb, 'acc'], w=['acc'])
            kb.dma('sp', SC['qkv'][ch], acc[:], r=['acc'], w=['s_qkv'])

        for gi in range(3):
            b = load_group([(0, gi * 512, 512)])
            for j in range(4):
                conv_chunk(b, j * 128, gi * 4 + j)

        b = load_group([(0, 1536, 512)])
        for t in range(NT):
            p = state['ps'] % 4
            state['ps'] += 1
            for kc in range(8):
                kb.op('pe', lambda e, kc=kc, p=p, t=t: e.matmul(ps[p][:], lhsT=hT[:, kc, t * 128:(t + 1) * 128],
                                                                rhs=wts[b][:, kc, 0:512], start=(kc == 0),
                                                                stop=(kc == 7)),
                      r=['wt%d' % b, 'hT'], w=['pj_ps%d' % p])
            zb = t % 2
            kb.op('act', lambda e, p=p, zb=zb: e.activation(out=zt[zb][:], in_=ps[p][:], func=AF.Silu),
                  r=['pj_ps%d' % p], w=['zt%d' % zb])
            kb.dma('sp', SC['sz'][t * 128:(t + 1) * 128, :], zt[zb][:], r=['zt%d' % zb], w=['s_sz'])

        b = load_group([(0, 2048, 8), (128, 2568, 256)])
        for t in range(NT):
            p = state['ps'] % 4
            state['ps'] += 1
            for kc in range(8):
                kb.op('pe', lambda e, kc=kc, p=p, t=t: e.matmul(ps[p][:, 0:8], lhsT=hT[:, kc, t * 128:(t + 1) * 128],
                                                                rhs=wts[b][:, kc, 0:8], start=(kc == 0),
                                                                stop=(kc == 7)),
                      r=['wt%d' % b, 'hT'], w=['pj_ps%d' % p])
            pk = 'pj_ps%d' % p
            P = ps[p]
            kb.op('dve', lambda e, P=P: e.tensor_tensor(out=sm[:, 0, :], in0=P[:, 0:4], in1=dtb[:], op=ALU.add),
                  r=[pk, 'dtb'], w=['sm0'])
            kb.op('dve', lambda e: e.scalar_tensor_tensor(out=sm[:, 1, :], in0=sm[:, 0, :], scalar=-1.0,
                                                          in1=sm[:, 0, :], op0=ALU.mult, op1=ALU.min),
                  r=['sm0'], w=['sm1'])
            kb.op('act', lambda e: e.activation(out=sm[:, 2, :], in_=sm[:, 1, :], func=AF.Exp, scale=1.0),
                  r=['sm1'], w=['sm2'])
            kb.op('act', lambda e: e.activation(out=sm[:, 3, :], in_=sm[:, 2, :], func=AF.Ln, bias=1.0, scale=1.0),
                  r=['sm2'], w=['sm3'])
            kb.op('dve', lambda e: e.scalar_tensor_tensor(out=sm[:, 4, :], in0=sm[:, 0, :], scalar=0.0,
                                                          in1=sm[:, 3, :], op0=ALU.max, op1=ALU.add),
                  r=['sm0', 'sm3'], w=['sm4'])
            kb.op('dve', lambda e, t=t: e.scalar_tensor_tensor(out=g_all[:, t, :], in0=sm[:, 4, :], scalar=-1.0,
                                                              in1=alog[:], op0=ALU.mult, op1=ALU.mult),
                  r=['sm4', 'alog'], w=['g_all'])
            kb.op('act', lambda e, P=P, t=t: e.activation(out=b_all[:, t, :], in_=P[:, 4:8], func=AF.Sigmoid),
                  r=[pk], w=['b_all'])
        kb.dma('sp', SC['g'].rearrange("(t p) h -> p t h", p=128), g_all[:], r=['g_all'], w=['s_g'])
        kb.dma('sp', SC['beta'].rearrange("(t p) h -> p t h", p=128), b_all[:], r=['b_all'], w=['s_beta'])
        for t in range(NT):
            p = state['ps'] % 4
            state['ps'] += 1
            for kc in range(8):
                kb.op('pe', lambda e, kc=kc, p=p, t=t: e.matmul(ps[p][:, 0:128], lhsT=hT[:, kc, t * 128:(t + 1) * 128],
                                                                rhs=wts[b][:, kc, 256:384], start=(kc == 0),
                                                                stop=(kc == 7)),
                      r=['wt%d' % b, 'hT'], w=['pj_ps%d' % p])
            kb.op('dve', lambda e, p=p, t=t: e.tensor_copy(out=vb_all[:, t, :], in_=ps[p][:, 0:128]),
                  r=['pj_ps%d' % p], w=['vb_all'])
        kb.dma('sp', SC['vb'].rearrange("(t p) d -> p t d", p=128), vb_all[:], r=['vb_all'], w=['s_vb'])

        def bf_chunk(b, off, dst, fn=None):
            g = state['gb'] % 2
            state['gb'] += 1

            def ev(tc, p, pk):
                if fn is not None:
                    kb.op('act', lambda e: e.activation(out=gb[g][:, tc * 512:(tc + 1) * 512], in_=p[:], func=fn),
                          r=[pk], w=['gb%d' % g])
                elif tc % 2 == 0:
                    kb.op('act', lambda e: e.copy(out=gb[g][:, tc * 512:(tc + 1) * 512], in_=p[:]),
                          r=[pk], w=['gb%d' % g])
                else:
                    kb.op('dve', lambda e: e.tensor_copy(out=gb[g][:, tc * 512:(tc + 1) * 512], in_=p[:]),
                          r=[pk], w=['gb%d' % g])
            fm_chunk(b, off, ev)
            kb.dma('sp', dst, gb[g][:], r=['gb%d' % g], w=['s_misc'])

        bf_chunk(b, 128, SC['kb'])
        pieces = []
        for j in range(4):
            pieces.append((j * 128, 2056 + j * 64, 64))
            pieces.append((j * 128 + 64, 2056 + (j + 4) * 64, 64))
        b = load_group(pieces)
        for j in range(4):
            bf_chunk(b, j * 128, SC['qb'][j])
        b = load_group([(0, 2824, 512)])
        for j in range(4):
            def ev(tc, p, pk):
                if tc % 2 == 0:
                    kb.op('act', lambda e: e.copy(out=acc[:, tc * 512:(tc + 1) * 512], in_=p[:]), r=[pk], w=['acc'])
                else:
                    kb.op('dve', lambda e: e.tensor_copy(out=acc[:, tc * 512:(tc + 1) * 512], in_=p[:]),
                          r=[pk], w=['acc'])
            fm_chunk(b, j * 128, ev)
            kb.dma('sp', SC['uc'][j], acc[:], r=['acc'], w=['s_uc'])
        for gi in range(6):
            b = load_group([(0, 3336 + gi * 512, 512)])
            for j in range(4):
                bf_chunk(b, j * 128, SC['gate'][gi * 4 + j], fn=AF.Sigmoid)
        kb.barrier()


def alloc_scratch(nc):
    SC = {}
    SC['qkv'] = nc.dram_tensor("s_qkv", [12, 128, S], F32).ap()
    SC['sz'] = nc.dram_tensor("s_sz", [S, 512], F32).ap()
    SC['g'] = nc.dram_tensor("s_g", [S, 4], F32).ap()
    SC['beta'] = nc.dram_tensor("s_beta", [S, 4], F32).ap()
    SC['qb'] = nc.dram_tensor("s_qb", [4, 128, S], BF16).ap()
    SC['kb'] = nc.dram_tensor("s_kb", [128, S], BF16).ap()
    SC['vb'] = nc.dram_tensor("s_vb", [S, 128], BF16).ap()
    SC['uc'] = nc.dram_tensor("s_uc", [4, 128, S], F32).ap()
    SC['gate'] = nc.dram_tensor("s_gate", [24, 128, S], BF16).ap()
    return SC


def cp(kb, eng, out, in_, r, w):
    if eng == 'act':
        return kb.op('act', lambda e: e.copy(out=out, in_=in_), r=r, w=w)
    return kb.op(eng, lambda e: e.tensor_copy(out=out, in_=in_), r=r, w=w)


class PsRot:
    def __init__(self, kb, nc, st, n, pfx, dt=F32, shape=(128, 512)):
        self.t = [pst(nc, st, "%s%d" % (pfx, i), list(shape), dt) for i in range(n)]
        self.k = ["%s%d" % (pfx, i) for i in range(n)]
        self.i = 0
        kb.excl.update(self.k)

    def next(self):
        i = self.i
        self.i = (i + 1) % len(self.t)
        return self.t[i], self.k[i]


def stage_gdn(kb, nc, l, A, SC, cst):
    ones, ident = cst['ones_f'], cst['ident_f']
    with ExitStack() as st:
        gm = sbt(nc, st, "g_gm", [128, 6, 128], F32)
        kb.dma('sp', gm[:], A['gm'][:, :, :], w=['cst'])
        g_all = sbt(nc, st, "g_gall", [128, 128], F32)
        b_all = sbt(nc, st, "g_ball", [128, 128], F32)
        gc = sbt(nc, st, "g_gc", [128, 128], F32)
        ngc = sbt(nc, st, "g_ngc", [128, 128], F32)
        egc = sbt(nc, st, "g_egc", [128, 128], F32)
        ekl = sbt(nc, st, "g_ekl", [128, 128], F32)
        gl0e = sbt(nc, st, "g_gl0e", [128, 128], F32)
        gl1e = sbt(nc, st, "g_gl1e", [128, 128], F32)
        negb = sbt(nc, st, "g_negb", [128, 128], F32)
        nw = sbt(nc, st, "g_nw", [128, 128], F32)
        pst0 = ExitStack()
        PS = PsRot(kb, nc, pst0, 4, "g_ps")
        kb.dma('sp', g_all[:].rearrange("p (t h) -> p t h", h=4), SC['g'].rearrange("(t p) h -> p t h", p=128),
               r=['s_g'], w=['g_gall'])
        kb.dma('sp', b_all[:].rearrange("p (t h) -> p t h", h=4), SC['beta'].rearrange("(t p) h -> p t h", p=128),
               r=['s_beta'], w=['g_ball'])
        kb.dma('sp', nw[:], A['gnw_bc'][l], w=['g_nw'])
        p, pk = PS.next()
        kb.op('pe', lambda e: e.matmul(p[:, 0:128], lhsT=gm[:, 0, :], rhs=g_all[:], start=True, stop=True),
              r=['g_gall', 'cst'], w=[pk])
        cp(kb, 'dve', gc[:], p[:, 0:128], [pk], ['g_gc'])
        kb.op('act', lambda e: e.activation(out=egc[:], in_=gc[:], func=AF.Exp), r=['g_gc'], w=['g_egc'])
        kb.op('dve', lambda e: e.tensor_scalar(out=ngc[:], in0=gc[:], scalar1=-1.0, scalar2=None, op0=ALU.mult),
              r=['g_gc'], w=['g_ngc'])
        p2, pk2 = PS.next()
        kb.op('pe', lambda e: e.matmul(p2[:, 0:128], lhsT=gm[:, 1, :], rhs=g_all[:], start=True, stop=True),
              r=['g_gall', 'cst'], w=[pk2])
        kb.op('dve', lambda e: e.tensor_tensor(out=ekl[:], in0=p2[:, 0:128], in1=gc[:], op=ALU.subtract),
              r=[pk2, 'g_gc'], w=['g_ekl'])
        kb.op('act', lambda e: e.activation(out=ekl[:], in_=ekl[:], func=AF.Exp), r=['g_ekl'], w=['g_ekl'])
        for idx, dst, dk in ((2, gl0e, 'g_gl0e'), (3, gl1e, 'g_gl1e')):
            p3, pk3 = PS.next()
            kb.op('pe', lambda e, p3=p3, idx=idx: e.matmul(p3[:, 0:128], lhsT=gm[:, idx, :], rhs=g_all[:],
                                                           start=True, stop=True), r=['g_gall', 'cst'], w=[pk3])
            kb.op('act', lambda e, p3=p3, dst=dst: e.activation(out=dst[:], in_=p3[:, 0:128], func=AF.Exp),
                  r=[pk3], w=[dk])
        kb.op('dve', lambda e: e.tensor_scalar(out=negb[:], in0=b_all[:], scalar1=-1.0, scalar2=None, op0=ALU.mult),
              r=['g_ball'], w=['g_negb'])

        kb.barrier()
        pst0.close()
        HT = 1024
        ecnt = [0]

        def mm(out_p, pk, lhsT, rhs, r, start=True, stop=True):
            return kb.op('pe', lambda e: e.matmul(out_p, lhsT=lhsT, rhs=rhs, start=start, stop=stop), r=r, w=[pk])

        names = ['ke', 'kg', 'vtok', 'dg', 'tmpm', 'decT', 'EB', 'qg', 'attnT', 'PT0', 'P0', 'PT1', 'P1', 'GT0', 'GT1',
                 'bu', 'wT', 'vnA', 'vnB', 'Sa', 'Sb', 'otok', 'junk', 'ytok']

        def head_gen(h, ch):
            GP = 'g%d_' % ch
            qkvb = [sbt(nc, st, "g_qkv%d" % i, [128, 3, HT], F32) for i in range(2)]
            szb = sbt(nc, st, "g_sz", [128, NT, 128], F32)
            yaT = sbt(nc, st, "g_yaT", [128, S], BF16)
            B = {n: sbt(nc, st, "g_" + n, [128, 128], F32) for n in names}
            ss = sbt(nc, st, "g_ss", [128, 1], F32)
            PS = PsRot(kb, nc, st, 4, "g%d_ps" % ch)
            for h in (h, h + 2):
                kb.op('dve', lambda e: e.memset(B['vnA'][:], 0.0), w=[GP + 'vnA'])
                kb.op('dve', lambda e: e.memset(B['vnB'][:], 0.0), w=[GP + 'vnB'])
                kb.dma('pool', szb[:], SC['sz'][:, h * 128:(h + 1) * 128].rearrange("(t p) d -> p t d", p=128),
                       r=['s_sz'], w=[GP + 'sz'])
                kb.op('dve', lambda e: e.memset(B['Sa'][:], 0.0), w=[GP + 'Sa'])
                Scur, Snxt = 'Sa', 'Sb'
                for half in range(S // HT):
                    qb_ = half % 2
                    for i3 in range(3):
                        kb.dma('sp', qkvb[qb_][:, i3, :], SC['qkv'][i3 * 4 + h, :, half * HT:(half + 1) * HT],
                               r=['s_qkv'], w=[GP + 'qkv%d' % qb_])
                    qk = GP + 'qkv%d' % qb_
                    for tt in range(HT // 128):
                        t = half * (HT // 128) + tt
                        c = t * 4 + h
                        if 'gdn_tiles' in DBG and (h * 32 + t) >= DBG['gdn_tiles']:
                            continue
                        qT = qkvb[qb_][:, 0, tt * 128:(tt + 1) * 128]
                        kT = qkvb[qb_][:, 1, tt * 128:(tt + 1) * 128]
                        vT = qkvb[qb_][:, 2, tt * 128:(tt + 1) * 128]
                        sub = DBG.get('gdn_sub', 31)
                        pa, pka = PS.next()
                        if sub & 1:
                            kb.op('pe', lambda e: e.matmul(pa[:, 0:128], lhsT=kT, rhs=ident, start=True, stop=True),
                                  r=[qk, 'cst'], w=[pka])
                            yield
                        if sub & 2:
                            kb.op('act', lambda e: e.activation(out=B['ke'][:], in_=pa[:, 0:128], func=AF.Identity,
                                                                scale=egc[:, c:c + 1]),
                                  r=[pka, 'g_egc'], w=[GP + 'ke'])
                            yield
                        if sub & 4:
                            kb.op('dve', lambda e: e.tensor_scalar(out=B['kg'][:], in0=pa[:, 0:128],
                                                                   scalar1=ekl[:, c:c + 1], scalar2=None, op0=ALU.mult),
                                  r=[pka, 'g_ekl'], w=[GP + 'kg'])
                            yield
                        pb, pkb = PS.next()
                        if sub & 8:
                            kb.op('pe', lambda e: e.matmul(pb[:, 0:128], lhsT=vT, rhs=ident, start=True, stop=True),
                                  r=[qk, 'cst'], w=[pkb])
                            yield
                        if sub & 16:
                            cp(kb, 'act', B['vtok'][:], pb[:, 0:128], [pkb], [GP + 'vtok'])
                            yield
                        if DBG.get('gdn_step', 99) < 1:
                            continue
                        pc, pkc = PS.next()
                        mm(pc[:, 0:128], pkc, kT, kT, [qk])
                        yield
                        pd, pkd = PS.next()
                        mm(pd[:, 0:128], pkd, kT, qT, [qk])
                        yield
                        if DBG.get('gdn_step', 99) < 2:
                            continue
                        kb.op('dve', lambda e, c=c: e.tensor_scalar(out=B['dg'][:], in0=ident, scalar1=gc[:, c:c + 1],
                                                                    scalar2=None, op0=ALU.mult),
                              r=['cst', 'g_gc'], w=[GP + 'dg'])
                        yield
                        pe_, pke = PS.next()
                        mm(pe_[:, 0:128], pke, ones, B['dg'][:], ['cst', GP + 'dg'])
                        yield
                        kb.op('dve', lambda e, pe_=pe_: e.tensor_tensor(out=B['tmpm'][:], in0=pe_[:, 0:128],
                                                                        in1=gm[:, 4, :], op=ALU.add),
                              r=[pke, 'cst'], w=[GP + 'tmpm'])
                        yield
                        if DBG.get('gdn_step', 99) < 3:
                            continue
                        kb.op('act', lambda e, c=c: e.activation(out=B['decT'][:], in_=B['tmpm'][:], func=AF.Exp,
                                                                 bias=ngc[:, c:c + 1], scale=1.0),
                              r=[GP + 'tmpm', 'g_ngc'], w=[GP + 'decT'])
                        yield
                        kb.op('act', lambda e, pe_=pe_: e.activation(out=B['EB'][:], in_=pe_[:, 0:128], func=AF.Exp),
                              r=[pke], w=[GP + 'EB'])
                        yield
                        kb.op('dve', lambda e, qT=qT: e.tensor_tensor(out=B['qg'][:], in0=qT, in1=B['EB'][:], op=ALU.mult),
                              r=[qk, GP + 'EB'], w=[GP + 'qg'])
                        yield
                        kb.op('dve', lambda e, pd=pd: e.tensor_tensor(out=B['attnT'][:], in0=pd[:, 0:128],
                                                                      in1=B['decT'][:], op=ALU.mult),
                              r=[pkd, GP + 'decT'], w=[GP + 'attnT'])
                        yield
                        kb.op('dve', lambda e: e.tensor_tensor(out=B['tmpm'][:], in0=B['decT'][:], in1=gm[:, 5, :],
                                                               op=ALU.mult), r=[GP + 'decT', 'cst'], w=[GP + 'tmpm'])
                        yield
                        kb.op('dve', lambda e, pc=pc, c=c: e.scalar_tensor_tensor(
                            out=B['PT0'][:], in0=pc[:, 0:128], scalar=negb[:, c:c + 1], in1=B['tmpm'][:],
                            op0=ALU.mult, op1=ALU.mult), r=[pkc, 'g_negb', GP + 'tmpm'], w=[GP + 'PT0'])
                        yield
                        if DBG.get('gdn_step', 99) < 4:
                            continue
                        pf, pkf = PS.next()
                        kb.op('pe', lambda e, pf=pf: e.matmul(pf[:, 0:128], lhsT=B['PT0'][:], rhs=ident, start=True, stop=True),
                              r=[GP + 'PT0', 'cst'], w=[pkf])
                        yield
                        cp(kb, 'act', B['P0'][:], pf[:, 0:128], [pkf], [GP + 'P0'])
                        yield
                        kb.op('dve', lambda e: e.tensor_tensor(out=B['GT0'][:], in0=B['PT0'][:], in1=ident, op=ALU.add),
                              r=[GP + 'PT0', 'cst'], w=[GP + 'GT0'])
                        yield
                        if DBG.get('gdn_step', 99) < 5:
                            continue
                        Pc, PTc, Gc = 'P0', 'PT0', 'GT0'
                        for lv in range(1, 6):
                            Pn = 'P1' if Pc == 'P0' else 'P0'
                            PTn = 'PT1' if PTc == 'PT0' else 'PT0'
                            Gn = 'GT1' if Gc == 'GT0' else 'GT0'
                            p1, pk1 = PS.next()
                            mm(p1[:, 0:128], pk1, B[PTc][:], B[Pc][:], [GP + PTc, GP + Pc])
                            yield
                            cp(kb, 'act', B[Pn][:], p1[:, 0:128], [pk1], [GP + Pn])
                            yield
                            if lv < 5:
                                p2_, pk2_ = PS.next()
                                mm(p2_[:, 0:128], pk2_, B[Pc][:], B[PTc][:], [GP + PTc, GP + Pc])
                                yield
                                cp(kb, 'dve', B[PTn][:], p2_[:, 0:128], [pk2_], [GP + PTn])
                                yield
                            p3_, pk3_ = PS.next()
                            mm(p3_[:, 0:128], pk3_, B[Pn][:], B[Gc][:], [GP + Pn, GP + Gc])
                            yield
                            kb.op('dve', lambda e, p3_=p3_, Gn=Gn, Gc=Gc: e.tensor_tensor(
                                out=B[Gn][:], in0=p3_[:, 0:128], in1=B[Gc][:], op=ALU.add),
                                r=[pk3_, GP + Gc], w=[GP + Gn])
                            yield
                            Pc, PTc, Gc = Pn, PTn, Gn
                        if DBG.get('gdn_step', 99) < 6:
                            continue
                        G = B[Gc]
                        Gk = GP + Gc
                        pu, pku = PS.next()
                        mm(pu[:, 0:128], pku, G[:], B['vtok'][:], [Gk, GP + 'vtok'])
                        yield
                        kb.op('act', lambda e, pu=pu, c=c: e.activation(out=B['bu'][:], in_=pu[:, 0:128], func=AF.Identity,
                                                                        scale=b_all[:, c:c + 1]),
                              r=[pku, 'g_ball'], w=[GP + 'bu'])
                        yield
                        pw, pkw = PS.next()
                        mm(pw[:, 0:128], pkw, B['ke'][:], G[:], [Gk, GP + 'ke'])
                        yield
                        cp(kb, 'dve', B['wT'][:], pw[:, 0:128], [pkw], [GP + 'wT'])
                        yield
                        if DBG.get('gdn_step', 99) < 7:
                            continue
                        for ci, vn, gle, gk in ((0, 'vnA', gl0e, 'g_gl0e'), (1, 'vnB', gl1e, 'g_gl1e')):
                            lo, hi = ci * 64, ci * 64 + 64
                            Sc = B[Scur]
                            Sk = GP + Scur
                            p4, pk4 = PS.next()
                            mm(p4[:, 0:128], pk4, B['wT'][:], Sc[:], [GP + 'wT', Sk])
                            yield
                            kb.op('dve', lambda e, p4=p4, vn=vn, lo=lo, hi=hi, c=c: e.scalar_tensor_tensor(
                                out=B[vn][lo:hi, :], in0=p4[lo:hi, 0:128], scalar=negb[lo:hi, c:c + 1],
                                in1=B['bu'][lo:hi, :], op0=ALU.mult, op1=ALU.add),
                                r=[pk4, 'g_negb', GP + 'bu'], w=[GP + vn])
                            yield
                            p5, pk5 = PS.next()
                            mm(p5[:, 0:128], pk5, B['qg'][:], Sc[:], [GP + 'qg', Sk], start=True, stop=False)
                            yield
                            mm(p5[:, 0:128], pk5, B['attnT'][:], B[vn][:], [GP + 'attnT', GP + vn], start=False, stop=True)
                            yield
                            cp(kb, 'act', B['otok'][lo:hi, :], p5[lo:hi, 0:128], [pk5], [GP + 'otok'])
                            yield
                            p6, pk6 = PS.next()
                            mm(p6[:, 0:128], pk6, B['kg'][:], B[vn][:], [GP + 'kg', GP + vn])
                            yield
                            kb.op('dve', lambda e, p6=p6, Sc=Sc, gle=gle, c=c, Snxt=Snxt: e.scalar_tensor_tensor(
                                out=B[Snxt][:], in0=Sc[:], scalar=gle[:, c:c + 1], in1=p6[:, 0:128],
                                op0=ALU.mult, op1=ALU.add), r=[pk6, Sk, gk], w=[GP + Snxt])
                            yield
                            Scur, Snxt = Snxt, Scur
                        if DBG.get('gdn_step', 99) < 8:
                            continue
                        kb.op('act', lambda e: e.activation(out=B['junk'][:], in_=B['otok'][:], func=AF.Square,
                                                            accum_out=ss[:]), r=[GP + 'otok'], w=[GP + 'junk', GP + 'ss'])
                        yield
                        kb.op('act', lambda e: e.activation(out=ss[:], in_=ss[:], func=AF.Sqrt, bias=1e-6,
                                                            scale=1.0 / 128), r=[GP + 'ss'], w=[GP + 'ss'])
                        yield
                        kb.op('dve', lambda e: e.reciprocal(out=ss[:], in_=ss[:]), r=[GP + 'ss'], w=[GP + 'ss'])
                        yield
                        kb.op('dve', lambda e: e.scalar_tensor_tensor(out=B['ytok'][:], in0=B['otok'][:], scalar=ss[:],
                                                                      in1=nw[:], op0=ALU.mult, op1=ALU.mult),
                              r=[GP + 'otok', GP + 'ss', 'g_nw'], w=[GP + 'ytok'])
                        yield
                        kb.op('dve', lambda e, t=t: e.tensor_tensor(out=B['ytok'][:], in0=B['ytok'][:], in1=szb[:, t, :],
                                                                    op=ALU.mult), r=[GP + 'ytok', GP + 'sz'], w=[GP + 'ytok'])
                        yield
                        if DBG.get('gdn_step', 99) < 9:
                            continue
                        p7, pk7 = PS.next()
                        kb.op('pe', lambda e, p7=p7: e.matmul(p7[:, 0:128], lhsT=B['ytok'][:], rhs=ident, start=True, stop=True),
                              r=[GP + 'ytok', 'cst'], w=[pk7])
                        yield
                        cp(kb, 'act', yaT[:, t * 128:(t + 1) * 128], p7[:, 0:128], [pk7], [GP + 'yaT'])
                        yield
                        if 'gdn_dump' in DBG and h == 0 and t == 0:
                            for bi, n_ in enumerate(names):
                                kb.dma('sp', DBG['gdn_dump'][bi], B[n_][:], r=[GP + n_], w=['dump'])
                kb.dma('sp', SC['ya'][h], yaT[:], r=[GP + 'yaT'], w=['s_ya'])


        gens = [head_gen(0, 0), head_gen(1, 1)]
        while gens:
            for g_ in list(gens):
                try:
                    next(g_)
                except StopIteration:
                    gens.remove(g_)
        kb.barrier()


def stage_swa(kb, nc, l, A, SC, cst):
    with ExitStack() as st:
        swab = sbt(nc, st, "a_swab", [128, 8, 256], F32)
        kb.dma('sp', swab[:], A['swab'][:, :, :], w=['cst'])
        qb = sbt(nc, st, "a_qb", [128, 4, S], BF16)
        kbt = sbt(nc, st, "a_kb", [128, S], BF16)
        vb = sbt(nc, st, "a_vb", [128, NT, 128], BF16)
        ybT = sbt(nc, st, "a_ybT", [64, 8, S], BF16)
        snk = sbt(nc, st, "a_snk", [128, 8], F32)
        for j in range(4):
            kb.dma('sp', qb[:, j, :], SC['qb'][j], r=['s_misc'], w=['a_qb'])
        kb.dma('sp', kbt[:], SC['kb'], r=['s_misc'], w=['a_kb'])
        kb.dma('sp', vb[:], SC['vb'].rearrange("(t p) d -> p t d", p=128), r=['s_vb'], w=['a_vb'])
        kb.dma('sp', snk[:], A['sink_bc'][l], w=['a_snk'])
        def swa_gen(heads, ch):
            CP = 'a%d_' % ch
            sc = [sbt(nc, st, "a_sc%d" % i, [128, 256], F32) for i in range(2)]
            pb_ = [sbt(nc, st, "a_p%d" % i, [128, 256], BF16) for i in range(2)]
            pT = [sbt(nc, st, "a_pT%d" % i, [128, 2, 128], BF16) for i in range(2)]
            sm = [sbt(nc, st, "a_sm%d" % i, [128, 8], F32) for i in range(2)]
            PS = PsRot(kb, nc, st, 2, "a%d_ps" % ch)
            PT = PsRot(kb, nc, st, 1, "a%d_pt" % ch, dt=BF16, shape=(128, 2, 128))
            PO = PsRot(kb, nc, st, 1, "a%d_po" % ch)
            u = 0
            for n in range(NT):
                for hq in heads:
                    kv = hq // 4
                    j = hq % 4
                    lo = kv * 64
                    W = 128 if n == 0 else 256
                    k0 = 0 if n == 0 else (n - 1) * 128
                    b = u % 2
                    u += 1
                    p, pk = PS.next()
                    kb.op('pe', lambda e: e.matmul(p[:, 0:W], lhsT=qb[lo:lo + 64, j, n * 128:(n + 1) * 128],
                                                   rhs=kbt[lo:lo + 64, k0:k0 + W], start=True, stop=True),
                          r=['a_qb', 'a_kb'], w=[pk])
                    yield
                    kb.op('dve', lambda e: e.scalar_tensor_tensor(out=sc[b][:, 0:W], in0=p[:, 0:W], scalar=0.125,
                                                                  in1=swab[:, hq, 256 - W:256], op0=ALU.mult,
                                                                  op1=ALU.add), r=[pk, 'cst'], w=[CP + 'sc%d' % b])
                    yield
                    s_ = sm[b]
                    sk = CP + 'sm%d' % b
                    kb.op('dve', lambda e: e.reduce_max(out=s_[:, 0:1], in_=sc[b][:, 0:W], axis=AX.X),
                          r=[CP + 'sc%d' % b], w=[sk])
                    yield
                    kb.op('dve', lambda e: e.tensor_tensor(out=s_[:, 1:2], in0=s_[:, 0:1], in1=snk[:, hq:hq + 1],
                                                           op=ALU.max), r=[sk, 'a_snk'], w=[sk])
                    yield
                    kb.op('dve', lambda e: e.tensor_scalar(out=s_[:, 2:3], in0=s_[:, 1:2], scalar1=-1.0, scalar2=None,
                                                           op0=ALU.mult), r=[sk], w=[sk])
                    yield
                    kb.op('act', lambda e: e.activation(out=sc[b][:, 0:W], in_=sc[b][:, 0:W], func=AF.Exp,
                                                        bias=s_[:, 2:3], scale=1.0, accum_out=s_[:, 3:4]),
                          r=[CP + 'sc%d' % b, sk], w=[CP + 'sc%d' % b, sk])
                    yield
                    kb.op('act', lambda e: e.activation(out=s_[:, 4:5], in_=snk[:, hq:hq + 1], func=AF.Exp,
                                                        bias=s_[:, 2:3], scale=1.0), r=[sk, 'a_snk'], w=[sk])
                    yield
                    kb.op('dve', lambda e: e.tensor_tensor(out=s_[:, 5:6], in0=s_[:, 3:4], in1=s_[:, 4:5], op=ALU.add),
                          r=[sk], w=[sk])
                    yield
                    kb.op('dve', lambda e: e.reciprocal(out=s_[:, 6:7], in_=s_[:, 5:6]), r=[sk], w=[sk])
                    yield
                    kb.op('dve', lambda e: e.tensor_scalar(out=pb_[b][:, 0:W], in0=sc[b][:, 0:W], scalar1=s_[:, 6:7],
                                                           scalar2=None, op0=ALU.mult),
                          r=[CP + 'sc%d' % b, sk], w=[CP + 'p%d' % b])
                    yield
                    nk = W // 128
                    pt, ptk = PT.next()
                    for q_ in range(nk):
                        kb.op('pe', lambda e, q_=q_: e.transpose(out=pt[:, q_, :], in_=pb_[b][:, q_ * 128:(q_ + 1) * 128],
                                                                identity=cst['ident_b'][:, :]),
                              r=[CP + 'p%d' % b, 'cst'], w=[ptk])
                        yield
                    cp(kb, 'act', pT[b][:, 0:nk, :], pt[:, 0:nk, :], [ptk], [CP + 'pT%d' % b])
                    yield
                    po, pok = PO.next()
                    for q_ in range(nk):
                        tk = n if n == 0 else n - 1 + q_
                        kb.op('pe', lambda e, q_=q_, tk=tk: e.matmul(po[0:64, 0:128], lhsT=vb[:, tk, kv * 64:(kv + 1) * 64],
                                                                    rhs=pT[b][:, q_, :], start=(q_ == 0),
                                                                    stop=(q_ == nk - 1)),
                              r=['a_vb', CP + 'pT%d' % b], w=[pok])
                        yield
                    cp(kb, 'act' if u % 2 else 'dve', ybT[:, hq, n * 128:(n + 1) * 128], po[0:64, 0:128], [pok], ['a_ybT%d' % hq])
                    yield


        gens = [swa_gen((0, 1, 2, 3), 0), swa_gen((4, 5, 6, 7), 1)]
        while gens:
            for g_ in list(gens):
                try:
                    next(g_)
                except StopIteration:
                    gens.remove(g_)
        kb.dma('sp', SC['yb'], ybT[:], r=['a_ybT%d' % i for i in range(8)], w=['s_yb'])
        kb.barrier()


def ln_affine_store_g(kb, nc, xin, xk, bufs, g_bc, b_bc, dst, sfx, dk):
    stt, mv, rstd, nb = bufs
    kb.op('dve', lambda e: e.bn_stats(out=stt[:, 0, :], in_=xin[:, 0:512]), r=[xk], w=['la_st' + sfx])
    yield
    kb.op('dve', lambda e: e.bn_stats(out=stt[:, 1, :], in_=xin[:, 512:1024]), r=[xk], w=['la_st' + sfx])
    yield
    kb.op('dve', lambda e: e.bn_aggr(out=mv[:], in_=stt[:].rearrange("p a b -> p (a b)")),
          r=['la_st' + sfx], w=['la_mv' + sfx])
    yield
    kb.op('act', lambda e: e.activation(out=rstd[:], in_=mv[:, 1:2], func=AF.Sqrt, bias=1e-5, scale=1.0),
          r=['la_mv' + sfx], w=['la_rstd' + sfx])
    yield
    kb.op('dve', lambda e: e.reciprocal(out=rstd[:], in_=rstd[:]), r=['la_rstd' + sfx], w=['la_rstd' + sfx])
    yield
    kb.op('dve', lambda e: e.scalar_tensor_tensor(out=nb[:], in0=mv[:, 0:1], scalar=-1.0, in1=rstd[:],
                                                  op0=ALU.mult, op1=ALU.mult),
          r=['la_mv' + sfx, 'la_rstd' + sfx], w=['la_nb' + sfx])
    yield
    kb.op('act', lambda e: e.activation(out=xin[:], in_=xin[:], func=AF.Identity, bias=nb[:], scale=rstd[:]),
          r=[xk, 'la_nb' + sfx, 'la_rstd' + sfx], w=[xk])
    yield
    kb.op('pool', lambda e: e.tensor_tensor(out=xin[:], in0=xin[:], in1=g_bc, op=ALU.mult), r=[xk, 'lngb'], w=[xk])
    yield
    kb.op('pool', lambda e: e.tensor_tensor(out=xin[:], in0=xin[:], in1=b_bc, op=ALU.add), r=[xk, 'lngb'], w=[xk])
    yield
    kb.dma('sp', dst, xin[:], r=[xk], w=[dk])
    yield


def ln_affine_store(kb, nc, xin, xk, bufs, g_bc, b_bc, dst, sfx, dk):
    for _ in ln_affine_store_g(kb, nc, xin, xk, bufs, g_bc, b_bc, dst, sfx, dk):
        pass


def stage_merge(kb, nc, l, A, SC, x_src, x_dst, mod_bc, cst):
    with ExitStack() as st:
        U = [sbt(nc, st, "c_u%d" % i, [128, 16 + S], F32) for i in range(3)]
        pw = sbt(nc, st, "c_pw", [128, 4, 128], F32)
        pscl = sbt(nc, st, "c_ps", [128, 4], F32)
        ic = sbt(nc, st, "c_ic", [128, 4, 16], F32)
        yc = [sbt(nc, st, "c_yc%d" % i, [128, S], BF16) for i in range(2)]
        PS = PsRot(kb, nc, st, 4, "c_pp")
        kb.dma('sp', pw[:], A['pool_w'][l].rearrange("g c d -> c g d"), w=['c_pw'])
        kb.dma('sp', pscl[:], A['pscale_l'][l], w=['c_ps'])
        kb.dma('sp', ic[:], A['invcnt'][:, :, :], w=['c_ic'])
        for i in range(3):
            kb.op('dve', lambda e, i=i: e.memset(U[i][:, 0:16], 0.0), w=['c_u%d' % i])
        for g, win in enumerate((2, 4, 8, 16)):
            kb.dma('sp', U[0][:, 16:], SC['uc'][g], r=['s_uc'], w=['c_u0'])
            cur = 0
            sh = 1
            while sh < win:
                nxt = 1 if cur != 1 else 2
                kb.op('dve' if sh % 4 == 1 else 'pool', lambda e, cur=cur, nxt=nxt, sh=sh: e.tensor_tensor(
                    out=U[nxt][:, 16:], in0=U[cur][:, 16:], in1=U[cur][:, 16 - sh:16 - sh + S], op=ALU.add),
                    r=['c_u%d' % cur], w=['c_u%d' % nxt])
                cur = nxt
                sh *= 2
            dn = 1 if cur != 1 else 2
            kb.op('dve', lambda e, cur=cur, dn=dn: e.scalar_tensor_tensor(
                out=U[dn][:, 16:], in0=U[cur][:, 16:], scalar=1.0 / win, in1=U[0][:, 16:], op0=ALU.mult,
                op1=ALU.subtract), r=['c_u%d' % cur, 'c_u0'], w=['c_u%d' % dn])
            kb.op('dve', lambda e, cur=cur, dn=dn: e.tensor_tensor(out=U[dn][:, 16:32], in0=U[cur][:, 16:32],
                                                                  in1=ic[:, g, :], op=ALU.mult),
                  r=['c_u%d' % cur, 'c_ic'], w=['c_u%d' % dn])
            kb.op('dve', lambda e, dn=dn: e.tensor_tensor(out=U[dn][:, 16:32], in0=U[dn][:, 16:32],
                                                          in1=U[0][:, 16:32], op=ALU.subtract),
                  r=['c_u%d' % dn, 'c_u0'], w=['c_u%d' % dn])
            yb_ = g % 2
            for tc in range(8):
                p, pk = PS.next()
                kb.op('pe', lambda e, tc=tc, dn=dn: e.matmul(p[:], lhsT=pw[:, g, :],
                                                             rhs=U[dn][:, 16 + tc * 512:16 + (tc + 1) * 512],
                                                             start=True, stop=True), r=['c_pw', 'c_u%d' % dn], w=[pk])
                kb.op('act', lambda e, tc=tc: e.activation(out=yc[yb_][:, tc * 512:(tc + 1) * 512], in_=p[:],
                                                           func=AF.Identity, scale=pscl[:, g:g + 1]),
                      r=[pk, 'c_ps'], w=['c_yc%d' % yb_])
            kb.dma('sp', SC['yc'][g], yc[yb_][:], r=['c_yc%d' % yb_], w=['s_yc'])
        kb.barrier()
    with ExitStack() as st:
        wp = sbt(nc, st, "e_wp", [128, 12, D], BF16)
        wpb = sbt(nc, st, "e_wpb", [64, 8, D], BF16)
        wo = sbt(nc, st, "e_wo", [128, 8, D], BF16)
        lng = sbt(nc, st, "e_lng", [128, D], F32)
        lnb = sbt(nc, st, "e_lnb", [128, D], F32)
        gt = [sbt(nc, st, "e_gt0", [128, 24, 512], BF16)] * 2
        ya = [sbt(nc, st, "e_ya%d" % i, [128, 4, 512], BF16) for i in range(2)]
        ycb = [sbt(nc, st, "e_yc%d" % i, [128, 4, 512], BF16) for i in range(2)]
        ybb = [sbt(nc, st, "e_yb%d" % i, [64, 8, 512], BF16) for i in range(2)]
        mg = [sbt(nc, st, "e_mg%d" % i, [128, 8, 512], BF16) for i in range(2)]
        t1 = [sbt(nc, st, "e_t1%d" % i, [128, 512], F32) for i in range(2)]
        t2 = [sbt(nc, st, "e_t2%d" % i, [128, 512], F32) for i in range(2)]
        t3 = [sbt(nc, st, "e_t3%d" % i, [128, 512], F32) for i in range(2)]
        xt = [sbt(nc, st, "e_xt%d" % i, [128, D], F32) for i in range(2)]
        yt = [sbt(nc, st, "e_yt%d" % i, [128, D], F32) for i in range(2)]
        lb = [(sbt(nc, st, "e_st%d" % i, [128, 2, 6], F32), sbt(nc, st, "e_mv%d" % i, [128, 2], F32),
               sbt(nc, st, "e_rs%d" % i, [128, 1], F32), sbt(nc, st, "e_nb%d" % i, [128, 1], F32)) for i in range(2)]
        PA = PsRot(kb, nc, st, 6, "e_pa")
        PY = PsRot(kb, nc, st, 2, "e_py")
        kb.dma('pool', wp[:, 0:4, :], A['w_pa'][l].rearrange("(kc p) n -> p kc n", p=128), w=['e_wp'])
        kb.dma('pool', wp[:, 4:8, :], A['w_pc'][l].rearrange("(kc p) n -> p kc n", p=128), w=['e_wp'])
        kb.dma('pool', wpb[:], A['w_pb'][l].rearrange("(h p) n -> p h n", p=64), w=['e_wpb'])
        kb.dma('pool', wo[:], A['w_o'][l].rearrange("(kc p) n -> p kc n", p=128), w=['e_wo'])
        kb.dma('sp', lng[:], A['ln1g_bc'][l], w=['lngb'])
        kb.dma('sp', lnb[:], A['ln1b_bc'][l], w=['lngb'])
        u = 0
        for tc in range(8):
            b = tc % 2
            ts = slice(tc * 512, (tc + 1) * 512)
            kb.dma('sp', gt[b][:], SC['gate'][:, :, ts].rearrange("c p t -> p c t"), r=['s_misc'], w=['e_gt0'])
            kb.dma('sp', ya[b][:], SC['ya'][:, :, ts].rearrange("c p t -> p c t"), r=['s_ya'], w=['e_ya%d' % b])
            kb.dma('sp', ycb[b][:], SC['yc'][:, :, ts].rearrange("c p t -> p c t"), r=['s_yc'], w=['e_yc%d' % b])
            kb.dma('sp', ybb[b][:], SC['yb'][:, :, ts], r=['s_yb'], w=['e_yb%d' % b])
            for m in range(8):
                ms = slice(m * 128, (m + 1) * 128)
                tb = u % 2
                u += 1
                pa, pka = PA.next()
                for kc in range(4):
                    kb.op('pe', lambda e, kc=kc: e.matmul(pa[:], lhsT=wp[:, kc, ms], rhs=ya[b][:, kc, :],
                                                          start=(kc == 0), stop=(kc == 3)),
                          r=['e_wp', 'e_ya%d' % b], w=[pka])
                kb.op('dve', lambda e: e.tensor_tensor(out=t1[tb][:], in0=pa[:], in1=gt[b][:, m, :], op=ALU.mult),
                      r=[pka, 'e_gt0'], w=['e_t1%d' % tb])
                pb2, pkb2 = PA.next()
                for hh in range(8):
                    kb.op('pe', lambda e, hh=hh: e.matmul(pb2[:], lhsT=wpb[:, hh, ms], rhs=ybb[b][:, hh, :],
                                                          start=(hh == 0), stop=(hh == 7)),
                          r=['e_wpb', 'e_yb%d' % b], w=[pkb2])
                kb.op('dve', lambda e: e.tensor_tensor(out=t2[tb][:], in0=pb2[:], in1=gt[b][:, 8 + m, :], op=ALU.mult),
                      r=[pkb2, 'e_gt0'], w=['e_t2%d' % tb])
                pc2, pkc2 = PA.next()
                for kc in range(4):
                    kb.op('pe', lambda e, kc=kc: e.matmul(pc2[:], lhsT=wp[:, 4 + kc, ms], rhs=ycb[b][:, kc, :],
                                                          start=(kc == 0), stop=(kc == 3)),
                          r=['e_wp', 'e_yc%d' % b], w=[pkc2])
                kb.op('dve', lambda e: e.tensor_tensor(out=t3[tb][:], in0=pc2[:], in1=gt[b][:, 16 + m, :], op=ALU.mult),
                      r=[pkc2, 'e_gt0'], w=['e_t3%d' % tb])
                kb.op('pool', lambda e: e.tensor_tensor(out=t1[tb][:], in0=t1[tb][:], in1=t2[tb][:], op=ALU.add),
                      r=['e_t1%d' % tb, 'e_t2%d' % tb], w=['e_t1%d' % tb])
                kb.op('pool', lambda e: e.tensor_tensor(out=mg[b][:, m, :], in0=t1[tb][:], in1=t3[tb][:], op=ALU.add),
                      r=['e_t1%d' % tb, 'e_t3%d' % tb], w=['e_mg%d' % b])
            for tt in range(4):
                t = tc * 4 + tt
                xb = t % 2
                kb.dma('sp', xt[xb][:], x_src[t * 128:(t + 1) * 128, :], r=['xsrc'], w=['e_xt%d' % xb])
                for half in range(2):
                    py, pyk = PY.next()
                    for kc in range(8):
                        kb.op('pe', lambda e, kc=kc: e.matmul(py[:], lhsT=mg[b][:, kc, tt * 128:(tt + 1) * 128],
                                                              rhs=wo[:, kc, half * 512:(half + 1) * 512],
                                                              start=(kc == 0), stop=(kc == 7)),
                              r=['e_mg%d' % b, 'e_wo'], w=[pyk])
                    kb.op('dve', lambda e, half=half: e.tensor_tensor(
                        out=yt[xb][:, half * 512:(half + 1) * 512], in0=py[:],
                        in1=mod_bc[:, 2 * D + half * 512:2 * D + (half + 1) * 512], op=ALU.mult),
                        r=[pyk, 'mod_bc'], w=['e_yt%d' % xb])
                kb.op('dve', lambda e: e.scalar_tensor_tensor(out=yt[xb][:], in0=xt[xb][:], scalar=ALPHA, in1=yt[xb][:],
                                                              op0=ALU.mult, op1=ALU.add),
                      r=['e_xt%d' % xb, 'e_yt%d' % xb], w=['e_yt%d' % xb])
                ln_affine_store(kb, nc, yt[xb], 'e_yt%d' % xb, lb[xb], lng[:], lnb[:],
                                x_dst[t * 128:(t + 1) * 128, :], 'e%d' % xb, 'xdst_%d' % t)
        kb.barrier()


def stage_moe(kb, nc, l, A, SC, x_src, x_dst, mod_bc, cst):
    ident = cst['ident_f']
    with ExitStack() as st:
        hT = sbt(nc, st, "hT2", [128, 8, S], BF16)
        gate_all = sbt(nc, st, "f_gate", [128, NT, NE], F32)
        with ExitStack() as s2:
            xts = [sbt(nc, s2, "f_xt%d" % i, [128, D], F32) for i in range(2)]
            hbs = [sbt(nc, s2, "f_hb%d" % i, [128, D], BF16) for i in range(2)]
            bufs = [(sbt(nc, s2, "f_st%d" % i, [128, 2, 6], F32), sbt(nc, s2, "f_mv%d" % i, [128, 2], F32),
                     sbt(nc, s2, "f_rs%d" % i, [128, 1], F32), sbt(nc, s2, "f_nb%d" % i, [128, 1], F32),
                     sbt(nc, s2, "f_xn%d" % i, [128, D], F32)) for i in range(2)]
            h32 = [sbt(nc, s2, "f_h32%d" % i, [128, 8, 128], F32) for i in range(2)]
            rw = sbt(nc, s2, "f_rw", [128, 8, NE], F32)
            rb = sbt(nc, s2, "f_rb", [128, NE], F32)
            lg = [sbt(nc, s2, "f_lg%d" % i, [128, NE], F32) for i in range(2)]
            mk = [sbt(nc, s2, "f_mk%d" % i, [128, NE], F32) for i in range(2)]
            m8 = [sbt(nc, s2, "f_m8%d" % i, [128, 12], F32) for i in range(2)]
            pT = [pst(nc, s2, "f_pT%d" % i, [128, 8, 128], BF16) for i in range(2)]
            pR = [pst(nc, s2, "f_pR%d" % i, [128, 2, 512], F32) for i in range(2)]
            pL = [pst(nc, s2, "f_pL%d" % i, [128, 512], F32) for i in range(2)]
            kb.dma('sp', rw[:], A['router_w'][l].rearrange("(kc p) n -> p kc n", p=128), w=['f_rw'])
            kb.dma('sp', rb[:], A['rb_bc'][l], w=['f_rb'])
            for t in range(NT):
                b = t % 2
                sx = 'f' + str(b)
                kb.dma('sp', xts[b][:], x_src[t * 128:(t + 1) * 128, :], r=['xsrc5'], w=['f_xt%d' % b])
                stt, mv, rstd, nb, xn = bufs[b]
                ln_mod_tile(kb, nc, xts[b], 'f_xt%d' % b, bufs[b], mod_bc[:, 4 * D:5 * D], mod_bc[:, 3 * D:4 * D],
                            xn[:], 'ln_xn' + sx, sx)
                cp(kb, 'act', hbs[b][:], xn[:], ['ln_xn' + sx], ['f_hb%d' % b])
                for kc in range(8):
                    kb.op('pe', lambda e, kc=kc: e.transpose(out=pT[b][:, kc, :], in_=hbs[b][:, kc * 128:(kc + 1) * 128],
                                                             identity=cst['ident_b'][:, :]),
                          r=['f_hb%d' % b, 'cst'], w=['f_pT%d' % b])
                cp(kb, 'act', hT[:, :, t * 128:(t + 1) * 128], pT[b][:], ['f_pT%d' % b], ['hT2'])
                for kc in range(8):
                    kb.op('pe', lambda e, kc=kc: e.matmul(pR[b][:, kc // 4, (kc % 4) * 128:(kc % 4 + 1) * 128],
                                                          lhsT=xn[:, kc * 128:(kc + 1) * 128], rhs=ident,
                                                          start=True, stop=True),
                          r=['ln_xn' + sx, 'cst'], w=['f_pR%d' % b])
                cp(kb, 'dve', h32[b][:].rearrange("p (a c) n -> p a (c n)", a=2), pR[b][:], ['f_pR%d' % b],
                   ['f_h32%d' % b])
                for kc in range(8):
                    kb.op('pe', lambda e, kc=kc: e.matmul(pL[b][:, 0:NE], lhsT=h32[b][:, kc, :], rhs=rw[:, kc, :],
                                                          start=(kc == 0), stop=(kc == 7)),
                          r=['f_h32%d' % b, 'f_rw'], w=['f_pL%d' % b])
                kb.op('dve', lambda e: e.tensor_tensor(out=lg[b][:], in0=pL[b][:, 0:NE], in1=rb[:], op=ALU.add),
                      r=['f_pL%d' % b, 'f_rb'], w=['f_lg%d' % b])
                kb.op('dve', lambda e: e.max(out=m8[b][:, 0:8], in_=lg[b][:]), r=['f_lg%d' % b], w=['f_m8%d' % b])
                kb.op('dve', lambda e: e.tensor_scalar(out=mk[b][:], in0=lg[b][:], scalar1=m8[b][:, 3:4], scalar2=None,
                                                       op0=ALU.is_ge), r=['f_lg%d' % b, 'f_m8%d' % b], w=['f_mk%d' % b])
                kb.op('dve', lambda e: e.tensor_scalar(out=m8[b][:, 8:9], in0=m8[b][:, 0:1], scalar1=-1.0, scalar2=None,
                                                       op0=ALU.mult), r=['f_m8%d' % b], w=['f_m8%d' % b])
                kb.op('act', lambda e: e.activation(out=lg[b][:], in_=lg[b][:], func=AF.Exp, bias=m8[b][:, 8:9],
                                                    scale=1.0), r=['f_lg%d' % b, 'f_m8%d' % b], w=['f_lg%d' % b])
                kb.op('dve', lambda e: e.tensor_tensor(out=lg[b][:], in0=lg[b][:], in1=mk[b][:], op=ALU.mult),
                      r=['f_lg%d' % b, 'f_mk%d' % b], w=['f_lg%d' % b])
                kb.op('dve', lambda e: e.reduce_sum(out=m8[b][:, 9:10], in_=lg[b][:], axis=AX.X),
                      r=['f_lg%d' % b], w=['f_m8%d' % b])
                kb.op('dve', lambda e: e.reciprocal(out=m8[b][:, 10:11], in_=m8[b][:, 9:10]),
                      r=['f_m8%d' % b], w=['f_m8%d' % b])
                kb.op('dve', lambda e: e.tensor_scalar(out=gate_all[:, t, :], in0=lg[b][:], scalar1=m8[b][:, 10:11],
                                                       scalar2=None, op0=ALU.mult),
                      r=['f_lg%d' % b, 'f_m8%d' % b], w=['f_gate'])
            kb.barrier()
        PT_ = 8
        acc = sbt(nc, st, "f_acc", [128, PT_, D], F32)
        w1t = [sbt(nc, st, "f_w1%d" % i, [128, 8, 512], BF16) for i in range(2)]
        w2t = sbt(nc, st, "f_w2", [128, 8, D], BF16)
        b1t = [sbt(nc, st, "f_b1%d" % i, [128, 16], F32) for i in range(2)]
        b2t = [sbt(nc, st, "f_b2%d" % i, [1, D], BF16) for i in range(2)]
        onesb = sbt(nc, st, "f_onesb", [1, 128], BF16)
        actT = sbt(nc, st, "f_actT", [128, 8, PT_ * 128], BF16)
        gl = [sbt(nc, st, "f_gl%d" % i, [128, 512], F32) for i in range(2)]
        sg = [sbt(nc, st, "f_sg%d" % i, [128, 512], F32) for i in range(2)]
        li = [sbt(nc, st, "f_li%d" % i, [128, 512], F32) for i in range(2)]
        xt = [sbt(nc, st, "f_x%d" % i, [128, D], F32) for i in range(2)]
        lng = sbt(nc, st, "f_lng", [128, D], F32)
        lnb = sbt(nc, st, "f_lnb", [128, D], F32)
        lb = [(sbt(nc, st, "f_lst%d" % i, [128, 2, 6], F32), sbt(nc, st, "f_lmv%d" % i, [128, 2], F32),
               sbt(nc, st, "f_lrs%d" % i, [128, 1], F32), sbt(nc, st, "f_lnb%d" % i, [128, 1], F32)) for i in range(2)]
        PG = PsRot(kb, nc, st, 4, "f_pg")
        PY = PsRot(kb, nc, st, 3, "f_py")
        kb.op('dve', lambda e: e.tensor_copy(out=onesb[:], in_=cst['ones_f'][0:1, :]), r=['cst'], w=['f_onesb'])
        kb.dma('sp', lng[:], A['ln2g_bc'][l], w=['lngb'])
        kb.dma('sp', lnb[:], A['ln2b_bc'][l], w=['lngb'])
        W1, W2 = A['exp_w1'], A['exp_w2']
        gcount = 0
        u = 0
        n_exp = DBG.get('n_exp', NE)
        for ps_ in range(NT // PT_):
            kb.op('pool', lambda e: e.memset(acc[:], 0.0), w=['f_acc'])
            for ex in range(n_exp):
                eb = ex % 2
                kb.dma('sp', b1t[eb][:], A['exp_b1l'][l, ex], w=['f_b1%d' % eb])
                kb.dma('pool', b2t[eb][:], A['exp_b2'][l, ex:ex + 1, :], w=['f_b2%d' % eb])
                for g in range(4):
                    wb = gcount % 2
                    gcount += 1
                    kb.dma('pool', w1t[wb][:, :, 0:256],
                           W1[l, ex, :, g * 256:(g + 1) * 256].rearrange("(kc p) n -> p kc n", p=128), w=['f_w1%d' % wb])
                    kb.dma('pool', w1t[wb][:, :, 256:512],
                           W1[l, ex, :, DFF + g * 256:DFF + (g + 1) * 256].rearrange("(kc p) n -> p kc n", p=128),
                           w=['f_w1%d' % wb])
                    for fl in range(2):
                        fc = g * 2 + fl
                        for tcl in range(PT_ // 4):
                            tb = u % 2
                            u += 1
                            tok = slice((ps_ * PT_ + tcl * 4) * 128, (ps_ * PT_ + tcl * 4 + 4) * 128)
                            pg, pgk = PG.next()
                            for kc in range(8):
                                kb.op('pe', lambda e, kc=kc: e.matmul(pg[:], lhsT=w1t[wb][:, kc, fl * 128:(fl + 1) * 128],
                                                                      rhs=hT[:, kc, tok], start=(kc == 0), stop=(kc == 7)),
                                      r=['f_w1%d' % wb, 'hT2'], w=[pgk])
                            pl, plk = PG.next()
                            for kc in range(8):
                                kb.op('pe', lambda e, kc=kc: e.matmul(
                                    pl[:], lhsT=w1t[wb][:, kc, 256 + fl * 128:256 + (fl + 1) * 128],
                                    rhs=hT[:, kc, tok], start=(kc == 0), stop=(kc == 7)),
                                    r=['f_w1%d' % wb, 'hT2'], w=[plk])
                            kb.op('dve', lambda e: e.tensor_scalar(out=gl[tb][:], in0=pg[:], scalar1=b1t[eb][:, fc:fc + 1],
                                                                   scalar2=7.0, op0=ALU.add, op1=ALU.min),
                                  r=[pgk, 'f_b1%d' % eb], w=['f_gl%d' % tb])
                            kb.op('act', lambda e: e.activation(out=sg[tb][:], in_=gl[tb][:], func=AF.Sigmoid,
                                                                scale=1.702), r=['f_gl%d' % tb], w=['f_sg%d' % tb])
                            kb.op('dve', lambda e: e.tensor_scalar(out=li[tb][:], in0=pl[:],
                                                                   scalar1=b1t[eb][:, 8 + fc:9 + fc], scalar2=7.0,
                                                                   op0=ALU.add, op1=ALU.min),
                                  r=[plk, 'f_b1%d' % eb], w=['f_li%d' % tb])
                            kb.op('dve', lambda e: e.tensor_scalar(out=li[tb][:], in0=li[tb][:], scalar1=-7.0,
                                                                   scalar2=1.0, op0=ALU.max, op1=ALU.add),
                                  r=['f_li%d' % tb], w=['f_li%d' % tb])
                            kb.op('pool', lambda e: e.tensor_tensor(out=gl[tb][:], in0=gl[tb][:], in1=sg[tb][:],
                                                                    op=ALU.mult),
                                  r=['f_gl%d' % tb, 'f_sg%d' % tb], w=['f_gl%d' % tb])
                            kb.op('pool', lambda e: e.tensor_tensor(out=actT[:, fc, tcl * 512:(tcl + 1) * 512],
                                                                    in0=gl[tb][:], in1=li[tb][:], op=ALU.mult),
                                  r=['f_gl%d' % tb, 'f_li%d' % tb], w=['f_actT'])
                kb.dma('pool', w2t[:], W2[l, ex].rearrange("(kc p) n -> p kc n", p=128), w=['f_w2'],
                       max_dma_last_dim=4096)
                for tt in range(PT_):
                    T = ps_ * PT_ + tt
                    for half in range(2):
                        py, pyk = PY.next()
                        for fc in range(8):
                            kb.op('pe', lambda e, fc=fc: e.matmul(py[:], lhsT=actT[:, fc, tt * 128:(tt + 1) * 128],
                                                                  rhs=w2t[:, fc, half * 512:(half + 1) * 512],
                                                                  start=(fc == 0), stop=False),
                                  r=['f_actT', 'f_w2'], w=[pyk])
                        kb.op('pe', lambda e: e.matmul(py[:], lhsT=onesb[0:1, :], rhs=b2t[eb][0:1, half * 512:(half + 1) * 512],
                                                       start=False, stop=True), r=['f_onesb', 'f_b2%d' % eb], w=[pyk])
                        kb.op('dve', lambda e: e.scalar_tensor_tensor(
                            out=acc[:, tt, half * 512:(half + 1) * 512], in0=py[:], scalar=gate_all[:, T, ex:ex + 1],
                            in1=acc[:, tt, half * 512:(half + 1) * 512], op0=ALU.mult, op1=ALU.add),
                            r=[pyk, 'f_gate', 'f_acc'], w=['f_acc'])
            for tt in range(PT_):
                T = ps_ * PT_ + tt
                xb = T % 2
                kb.dma('sp', xt[xb][:], x_src[T * 128:(T + 1) * 128, :], r=['xsrc5'], w=['f_x%d' % xb])
                kb.op('dve', lambda e: e.tensor_tensor(out=acc[:, tt, :], in0=acc[:, tt, :], in1=mod_bc[:, 5 * D:6 * D],
                                                       op=ALU.mult), r=['f_acc', 'mod_bc'], w=['f_acc'])
                kb.op('dve', lambda e: e.scalar_tensor_tensor(out=xt[xb][:], in0=xt[xb][:], scalar=ALPHA,
                                                              in1=acc[:, tt, :], op0=ALU.mult, op1=ALU.add),
                      r=['f_x%d' % xb, 'f_acc'], w=['f_x%d' % xb])
                ln_affine_store(kb, nc, xt[xb], 'f_x%d' % xb, lb[xb], lng[:], lnb[:],
                                x_dst[T * 128:(T + 1) * 128, :], 'f%d' % xb, 'xdst5')
        kb.barrier()


def input_shapes(L):
    return {
        'x': [S, D], 'c_col': [128, 8], 'ada_w': [L, D, 6 * D], 'ada_b': [L, 6 * D], 'w_in': [L, D, INW],
        'conv_wl': [L, 128, 12, 4], 'alog_bc': [L, 128, 4], 'dtb_bc': [L, 128, 4], 'gnw_bc': [L, 128, 128],
        'sink_bc': [L, 128, 8], 'pool_w': [L, 4, 128, 128], 'pscale_l': [L, 128, 4],
        'w_pa': [L, 512, D], 'w_pb': [L, 512, D], 'w_pc': [L, 512, D], 'w_o': [L, D, D],
        'ln1g_bc': [L, 128, D], 'ln1b_bc': [L, 128, D], 'ln2g_bc': [L, 128, D], 'ln2b_bc': [L, 128, D],
        'router_w': [L, D, NE], 'rb_bc': [L, 128, NE], 'exp_w1': [L, NE, D, 2 * DFF], 'exp_w2': [L, NE, DFF, D],
        'exp_b1l': [L, NE, 128, 16], 'exp_b2': [L, NE, D],
        'cst_f': [128, 2, 128], 'gm': [128, 6, 128], 'swab': [128, 8, 256], 'invcnt': [128, 4, 16], 'moec': [128, 160],
    }


def build_program(L):
    nc = bass.Bass("TRN2", target_bir_lowering=False)
    A = {k: nc.dram_tensor(k, s, F32, kind="ExternalInput").ap() for k, s in input_shapes(L).items()}
    out = nc.dram_tensor("out", [S, D], F32, kind="ExternalOutput").ap()
    SC = alloc_scratch(nc)
    SC['ya'] = nc.dram_tensor("s_ya", [4, 128, S], BF16).ap()
    SC['yb'] = nc.dram_tensor("s_yb", [64, 8, S], BF16).ap()
    SC['yc'] = nc.dram_tensor("s_yc", [4, 128, S], BF16).ap()
    x1 = nc.dram_tensor("s_x1", [S, D], F32).ap()
    SC['hg'] = nc.dram_tensor("s_hg", [NE * CAP, D], BF16).ap()
    SC['yg'] = nc.dram_tensor("s_yg", [NE * CAP, D], F32).ap()
    xs = [A['x']] + [nc.dram_tensor("s_xl%d" % i, [S, D], F32).ap() for i in range(L - 1)] + [out]
    with ExitStack() as es:
        kb = KB(nc, es)
        cf = sbt(nc, es, "cst_sb", [128, 2, 128], F32)
        ib = sbt(nc, es, "ident_b", [128, 128], BF16)
        mod_bc = sbt(nc, es, "mod_bc", [128, 6 * D], F32)
        kb.dma('sp', cf[:], A['cst_f'][:, :, :], w=['cst'])
        kb.op('dve', lambda e: e.tensor_copy(out=ib[:], in_=cf[:, 1, :]), r=['cst'], w=['cst'])
        cst = {'ones_f': cf[:, 0, :], 'ident_f': cf[:, 1, :], 'ident_b': ib, 'breg': nc.gpsimd.to_reg(NE * CAP - 1)}
        with ExitStack() as zs:
            zt = sbt(nc, zs, "zero_t", [128, 8 * D], BF16)
            kb.op('dve', lambda e: e.memset(zt[:], 0.0), w=['zero_t'])
            rows_per = 128 * 8
            for i in range(NE * CAP // rows_per):
                kb.dma('sp' if i % 2 == 0 else 'act', SC['hg'][i * rows_per:(i + 1) * rows_per, :].rearrange(
                    "(p r) d -> p (r d)", p=128), zt[:], r=['zero_t'], w=['s_hg_z%d' % (i % 8)])
            kb.barrier()
        for l in range(L):
            stage_mod(kb, nc, l, A, mod_bc, cst)
            stage_proj(kb, nc, l, A, SC, xs[l], mod_bc, cst)
            stage_gdn(kb, nc, l, A, SC, cst)
            stage_swa(kb, nc, l, A, SC, cst)
            stage_merge(kb, nc, l, A, SC, xs[l], x1, mod_bc, cst)
            stage_moe_sparse(kb, nc, l, A, SC, x1, xs[l + 1], mod_bc, cst)
        kb.barrier()
    return nc


def host_consts():
    i = np.arange(128)[:, None]
    j = np.arange(128)[None, :]
    same = (i // 64) == (j // 64)
    gm = np.stack([((i <= j) & same).astype(np.float32), same.astype(np.float32),
                   np.broadcast_to(i < 64, (128, 128)).astype(np.float32),
                   np.broadcast_to(i >= 64, (128, 128)).astype(np.float32),
                   np.where((j >= i) & same, 0.0, NEG).astype(np.float32),
                   (1.0 - np.eye(128)).astype(np.float32)], 1)
    q = np.arange(128)[:, None]
    k = np.arange(256)[None, :]
    dist = q + 128 - k
    valid = (dist >= 0) & (dist < 128)
    slopes = 2.0 ** (-8.0 * np.arange(1, 9, dtype=np.float32) / 8)
    swab = np.where(valid[:, None, :], -slopes[None, :, None] * dist[:, None, :].astype(np.float32), NEG)
    ic = np.zeros((4, 16), np.float32)
    for g, win in enumerate((2, 4, 8, 16)):
        ic[g] = 1.0 / np.minimum(np.arange(16) + 1, win)
    moec = np.concatenate([(i <= j).astype(np.float32),
                           np.broadcast_to((np.arange(NE) * CAP - 1).astype(np.float32)[None, :], (128, NE))], 1)
    return {'moec': np.ascontiguousarray(moec), 'cst_f': np.stack([np.ones((128, 128), np.float32), np.eye(128, dtype=np.float32)], 1),
            'gm': np.ascontiguousarray(gm), 'swab': np.ascontiguousarray(swab.astype(np.float32)),
            'invcnt': np.ascontiguousarray(np.broadcast_to(ic[None], (128, 4, 16)))}


def host_layer_inputs(p, ls):
    f = lambda a: np.ascontiguousarray(np.asarray(a, np.float32))
    n = len(range(*ls.indices(DEPTH)))
    bc = lambda a, w: f(np.broadcast_to(np.asarray(a)[ls][:, None, :], (n, 128, w)))
    W = {}
    for k in ['ada_w', 'ada_b', 'w_in', 'pool_w', 'w_pa', 'w_pb', 'w_pc', 'w_o', 'router_w', 'exp_w1', 'exp_w2', 'exp_b2']:
        W[k] = f(np.asarray(p[k])[ls])
    W['conv_wl'] = f(np.asarray(p['conv_w'])[ls].reshape(n, 4, 12, 128).transpose(0, 3, 2, 1))
    W['alog_bc'] = bc(p['a_log'], 4)
    W['dtb_bc'] = bc(p['dt_bias'], 4)
    W['gnw_bc'] = bc(p['gdn_norm_w'], 128)
    W['sink_bc'] = bc(p['sinks'], 8)
    W['pscale_l'] = f(np.asarray(p['pool_scale'])[ls].reshape(n, 4, 128).transpose(0, 2, 1))
    W['ln1g_bc'] = bc(p['ln1_g'], D)
    W['ln1b_bc'] = bc(p['ln1_b'], D)
    W['ln2g_bc'] = bc(p['ln2_g'], D)
    W['ln2b_bc'] = bc(p['ln2_b'], D)
    W['rb_bc'] = bc(p['router_b'], NE)
    W['exp_b1l'] = f(np.asarray(p['exp_b1'])[ls].reshape(n, NE, 16, 128).transpose(0, 1, 3, 2))
    return W


LAYERS_PER_LAUNCH = 4


def kernel(**inputs):
    x = np.asarray(inputs['x'], np.float32)
    c = np.asarray(inputs['c'], np.float32)
    nb = x.shape[0]
    consts = host_consts()
    Lp = LAYERS_PER_LAUNCH
    nc = build_program(Lp)
    cur = [np.ascontiguousarray(x[b]) for b in range(nb)]
    for l0 in range(0, DEPTH, Lp):
        W = host_layer_inputs(inputs, slice(l0, l0 + Lp))
        in_maps = []
        for b in range(nb):
            m = dict(W)
            m.update(consts)
            m['x'] = cur[b]
            m['c_col'] = np.ascontiguousarray(c[b].reshape(8, 128).T)
            in_maps.append(m)
        res = run_bass_kernel_spmd(nc, in_maps, core_ids=list(range(nb)))
        cur = [np.asarray(res.results[b]['out'], np.float32) for b in range(nb)]
    return np.stack(cur, 0).astype(np.float32)


def stage_moe_sparse(kb, nc, l, A, SC, x_src, x_dst, mod_bc, cst):
    ident = cst['ident_f']
    C = CAP
    NB = C // 128
    hg, yg = SC['hg'], SC['yg']
    with ExitStack() as st:
        dest_all = sbt(nc, st, "f_dest", [128, NT * 4], I32)
        gk_all = sbt(nc, st, "f_gk", [128, NT, 4], F32)
        with ExitStack() as s2:
            moec = sbt(nc, s2, "f_moec", [128, 160], F32)
            xts = [sbt(nc, s2, "f_xt%d" % i, [128, D], F32) for i in range(2)]
            hbs = [sbt(nc, s2, "f_hb%d" % i, [128, D], BF16) for i in range(2)]
            bufs = [(sbt(nc, s2, "f_st%d" % i, [128, 2, 6], F32), sbt(nc, s2, "f_mv%d" % i, [128, 2], F32),
                     sbt(nc, s2, "f_rs%d" % i, [128, 1], F32), sbt(nc, s2, "f_nb%d" % i, [128, 1], F32),
                     sbt(nc, s2, "f_xn%d" % i, [128, D], F32)) for i in range(2)]
            h32 = [sbt(nc, s2, "f_h32%d" % i, [128, 8, 128], F32) for i in range(2)]
            rw = sbt(nc, s2, "f_rw", [128, 8, NE], F32)
            rb = sbt(nc, s2, "f_rb", [128, NE], F32)
            lg = [sbt(nc, s2, "f_lg%d" % i, [128, NE], F32) for i in range(2)]
            mk = [sbt(nc, s2, "f_mk%d" % i, [128, NE], F32) for i in range(2)]
            gt_ = [sbt(nc, s2, "f_gt%d" % i, [128, NE], F32) for i in range(2)]
            ngp = [sbt(nc, s2, "f_ngp%d" % i, [128, NE], F32) for i in range(2)]
            oh = [sbt(nc, s2, "f_oh%d" % i, [128, NE], F32) for i in range(2)]
            jk = [sbt(nc, s2, "f_jk%d" % i, [128, NE], F32) for i in range(2)]
            m8 = [sbt(nc, s2, "f_m8%d" % i, [128, 12], F32) for i in range(2)]
            t8 = [sbt(nc, s2, "f_t8%d" % i, [128, 8], F32) for i in range(2)]
            msum = sbt(nc, s2, "f_msum", [128, NE], F32)
            pR = [pst(nc, s2, "f_pR%d" % i, [128, 2, 512], F32) for i in range(2)]
            pL = [pst(nc, s2, "f_pL%d" % i, [128, 512], F32) for i in range(2)]
            pC = [pst(nc, s2, "f_pC%d" % i, [128, 512], F32) for i in range(2)]
            kb.dma('sp', moec[:], A['moec'][:, :], w=['f_moec'])
            kb.dma('sp', rw[:], A['router_w'][l].rearrange("(kc p) n -> p kc n", p=128), w=['f_rw'])
            kb.dma('sp', rb[:], A['rb_bc'][l], w=['f_rb'])
            kb.op('dve', lambda e: e.memset(msum[:], 0.0), w=['f_msum'])
            triu = moec[:, 0:128]
            ecb = moec[:, 128:160]
            for t in range(NT):
                b = t % 2
                sx = 'f' + str(b)
                B_ = str(b)
                kb.dma('sp', xts[b][:], x_src[t * 128:(t + 1) * 128, :], r=['xsrc5'], w=['f_xt' + B_])
                stt, mv, rstd, nb, xn = bufs[b]
                ln_mod_tile(kb, nc, xts[b], 'f_xt' + B_, bufs[b], mod_bc[:, 4 * D:5 * D], mod_bc[:, 3 * D:4 * D],
                            xn[:], 'ln_xn' + sx, sx)
                cp(kb, 'act', hbs[b][:], xn[:], ['ln_xn' + sx], ['f_hb' + B_])
                for kc in range(8):
                    kb.op('pe', lambda e, kc=kc: e.matmul(pR[b][:, kc // 4, (kc % 4) * 128:(kc % 4 + 1) * 128],
                                                          lhsT=xn[:, kc * 128:(kc + 1) * 128], rhs=ident,
                                                          start=True, stop=True),
                          r=['ln_xn' + sx, 'cst'], w=['f_pR' + B_])
                cp(kb, 'dve', h32[b][:].rearrange("p (a c) n -> p a (c n)", a=2), pR[b][:], ['f_pR' + B_],
                   ['f_h32' + B_])
                for kc in range(8):
                    kb.op('pe', lambda e, kc=kc: e.matmul(pL[b][:, 0:NE], lhsT=h32[b][:, kc, :], rhs=rw[:, kc, :],
                                                          start=(kc == 0), stop=(kc == 7)),
                          r=['f_h32' + B_, 'f_rw'], w=['f_pL' + B_])
                kb.op('dve', lambda e: e.tensor_tensor(out=lg[b][:], in0=pL[b][:, 0:NE], in1=rb[:], op=ALU.add),
                      r=['f_pL' + B_, 'f_rb'], w=['f_lg' + B_])
                kb.op('dve', lambda e: e.max(out=m8[b][:, 0:8], in_=lg[b][:]), r=['f_lg' + B_], w=['f_m8' + B_])
                kb.op('dve', lambda e: e.tensor_scalar(out=mk[b][:], in0=lg[b][:], scalar1=m8[b][:, 3:4], scalar2=None,
                                                       op0=ALU.is_ge), r=['f_lg' + B_, 'f_m8' + B_], w=['f_mk' + B_])
                kb.op('dve', lambda e: e.tensor_scalar(out=m8[b][:, 8:9], in0=m8[b][:, 0:1], scalar1=-1.0, scalar2=None,
                                                       op0=ALU.mult), r=['f_m8' + B_], w=['f_m8' + B_])
                kb.op('act', lambda e: e.activation(out=lg[b][:], in_=lg[b][:], func=AF.Exp, bias=m8[b][:, 8:9],
                                                    scale=1.0), r=['f_lg' + B_, 'f_m8' + B_], w=['f_lg' + B_])
                kb.op('dve', lambda e: e.tensor_tensor(out=lg[b][:], in0=lg[b][:], in1=mk[b][:], op=ALU.mult),
                      r=['f_lg' + B_, 'f_mk' + B_], w=['f_lg' + B_])
                kb.op('dve', lambda e: e.reduce_sum(out=m8[b][:, 9:10], in_=lg[b][:], axis=AX.X),
                      r=['f_lg' + B_], w=['f_m8' + B_])
                kb.op('dve', lambda e: e.reciprocal(out=m8[b][:, 10:11], in_=m8[b][:, 9:10]),
                      r=['f_m8' + B_], w=['f_m8' + B_])
                kb.op('dve', lambda e: e.tensor_scalar(out=gt_[b][:], in0=lg[b][:], scalar1=m8[b][:, 10:11],
                                                       scalar2=None, op0=ALU.mult),
                      r=['f_lg' + B_, 'f_m8' + B_], w=['f_gt' + B_])
                kb.op('pe', lambda e: e.matmul(pC[b][:, 0:NE], lhsT=triu, rhs=mk[b][:], start=True, stop=False),
                      r=['f_moec', 'f_mk' + B_], w=['f_pC' + B_])
                kb.op('pe', lambda e: e.matmul(pC[b][:, 0:NE], lhsT=cst['ones_f'], rhs=msum[:], start=False, stop=True),
                      r=['cst', 'f_msum'], w=['f_pC' + B_])
                kb.op('dve', lambda e: e.tensor_tensor(out=msum[:], in0=msum[:], in1=mk[b][:], op=ALU.add),
                      r=['f_msum', 'f_mk' + B_], w=['f_msum'])
                kb.op('dve', lambda e: e.tensor_tensor(out=ngp[b][:], in0=pC[b][:, 0:NE], in1=ecb, op=ALU.add),
                      r=['f_pC' + B_, 'f_moec'], w=['f_ngp' + B_])
                kb.op('dve', lambda e: e.tensor_scalar(out=ngp[b][:], in0=ngp[b][:], scalar1=-1.0, scalar2=BIG,
                                                       op0=ALU.mult, op1=ALU.add), r=['f_ngp' + B_], w=['f_ngp' + B_])
                kb.op('dve', lambda e: e.tensor_tensor(out=ngp[b][:], in0=ngp[b][:], in1=mk[b][:], op=ALU.mult),
                      r=['f_ngp' + B_, 'f_mk' + B_], w=['f_ngp' + B_])
                kb.op('dve', lambda e: e.tensor_scalar(out=ngp[b][:], in0=ngp[b][:], scalar1=-BIG, scalar2=None,
                                                       op0=ALU.add), r=['f_ngp' + B_], w=['f_ngp' + B_])
                kb.op('dve', lambda e: e.max(out=t8[b][:], in_=ngp[b][:]), r=['f_ngp' + B_], w=['f_t8' + B_])
                dk = 'f_dest%d' % t
                kb.op('dve', lambda e: e.tensor_scalar(out=dest_all[:, t * 4:(t + 1) * 4], in0=t8[b][:, 0:4], scalar1=-1.0,
                                                       scalar2=None, op0=ALU.mult), r=['f_t8' + B_], w=[dk])
                for k in range(4):
                    kb.op('dve', lambda e, k=k: e.tensor_scalar(out=oh[b][:], in0=ngp[b][:], scalar1=t8[b][:, k:k + 1],
                                                               scalar2=None, op0=ALU.is_equal),
                          r=['f_ngp' + B_, 'f_t8' + B_], w=['f_oh' + B_])
                    kb.op('dve', lambda e, k=k: e.scalar_tensor_tensor(out=jk[b][:], in0=oh[b][:], scalar=1.0,
                                                                      in1=gt_[b][:], op0=ALU.mult, op1=ALU.mult,
                                                                      accum_out=gk_all[:, t, k:k + 1]),
                          r=['f_oh' + B_, 'f_gt' + B_], w=['f_jk' + B_, 'f_gk'])
                for k in range(4):
                    kb.ind('pool', ['f_hb' + B_, dk], ['s_hg_%d' % (t * 4 + k)], out=hg[:, :],
                           out_offset=bass.IndirectOffsetOnAxis(ap=dest_all[:, t * 4 + k:t * 4 + k + 1], axis=0),
                           in_=hbs[b][:, :], in_offset=None, bounds_check=cst['breg'], oob_is_err=False)
            kb.barrier()
        with ExitStack() as s3:
            hgT = [sbt(nc, s3, "f_hgT%d" % i, [128, 8, C], BF16) for i in range(2)]
            hgb = [sbt(nc, s3, "f_hgb%d" % i, [128, D], BF16) for i in range(2)]
            actT = sbt(nc, s3, "f_actT", [128, 8, C], BF16)
            w1t = [sbt(nc, s3, "f_w1%d" % i, [128, 8, 512], BF16) for i in range(2)]
            w2t = [sbt(nc, s3, "f_w2%d" % i, [128, 8, D], BF16) for i in range(2)]
            b1t = [sbt(nc, s3, "f_b1%d" % i, [128, 16], F32) for i in range(2)]
            b2t = [sbt(nc, s3, "f_b2%d" % i, [1, D], BF16) for i in range(2)]
            onesb = sbt(nc, s3, "f_onesb", [1, 128], BF16)
            gl = [sbt(nc, s3, "f_gl%d" % i, [128, 512], F32) for i in range(2)]
            sg = [sbt(nc, s3, "f_sg%d" % i, [128, 512], F32) for i in range(2)]
            li = [sbt(nc, s3, "f_li%d" % i, [128, 512], F32) for i in range(2)]
            yrow = [sbt(nc, s3, "f_yr%d" % i, [128, D], F32) for i in range(2)]
            PTr = PsRot(kb, nc, s3, 2, "f_ptr", dt=BF16, shape=(128, 8, 128))
            PG = PsRot(kb, nc, s3, 4, "f_pg")
            PY = PsRot(kb, nc, s3, 2, "f_py")
            kb.op('dve', lambda e: e.tensor_copy(out=onesb[:], in_=cst['ones_f'][0:1, :]), r=['cst'], w=['f_onesb'])
            W1, W2 = A['exp_w1'], A['exp_w2']
            st5 = {'u': 0, 'yc': 0}

            def load_exp(ex):
                eb = ex % 2
                EB = str(eb)
                kb.dma('sp', b1t[eb][:], A['exp_b1l'][l, ex], w=['f_b1' + EB])
                kb.dma('pool', b2t[eb][:], A['exp_b2'][l, ex:ex + 1, :], w=['f_b2' + EB])
                kb.dma('pool', w2t[eb][:], W2[l, ex].rearrange("(kc p) n -> p kc n", p=128), w=['f_w2' + EB],
                       max_dma_last_dim=4096)

            def load_w1(i):
                ex, g = i // 4, i % 4
                wb = i % 2
                kb.dma('pool', w1t[wb][:, :, 0:256],
                       W1[l, ex, :, g * 256:(g + 1) * 256].rearrange("(kc p) n -> p kc n", p=128), w=['f_w1%d' % wb])
                kb.dma('pool', w1t[wb][:, :, 256:512],
                       W1[l, ex, :, DFF + g * 256:DFF + (g + 1) * 256].rearrange("(kc p) n -> p kc n", p=128),
                       w=['f_w1%d' % wb])

            def prelude(ex):
                eb = ex % 2
                EB = str(eb)
                for blk in range(NB):
                    hb_ = blk % 2
                    kb.dma('sp', hgb[hb_][:], hg[ex * C + blk * 128:ex * C + (blk + 1) * 128, :], w=['f_hgb%d' % hb_])
                    ptr, ptrk = PTr.next()
                    for kc in range(8):
                        kb.op('pe', lambda e, kc=kc: e.transpose(out=ptr[:, kc, :], in_=hgb[hb_][:, kc * 128:(kc + 1) * 128],
                                                                 identity=cst['ident_b'][:, :]),
                              r=['f_hgb%d' % hb_, 'cst'], w=[ptrk])
                    cp(kb, 'act' if blk % 2 == 0 else 'dve', hgT[eb][:, :, blk * 128:(blk + 1) * 128], ptr[:], [ptrk],
                       ['f_hgT' + EB])

            def w1_group(i):
                ex, g = i // 4, i % 4
                wb = i % 2
                eb = ex % 2
                EB = str(eb)
                for fl in range(2):
                    fc = g * 2 + fl
                    for scn in range(C // 512):
                        tb = st5['u'] % 2
                        st5['u'] += 1
                        TB = str(tb)
                        sl = slice(scn * 512, (scn + 1) * 512)
                        pg, pgk = PG.next()
                        for kc in range(8):
                            kb.op('pe', lambda e, kc=kc: e.matmul(pg[:], lhsT=w1t[wb][:, kc, fl * 128:(fl + 1) * 128],
                                                                  rhs=hgT[eb][:, kc, sl], start=(kc == 0), stop=(kc == 7)),
                                  r=['f_w1%d' % wb, 'f_hgT' + EB], w=[pgk])
                        pl, plk = PG.next()
                        for kc in range(8):
                            kb.op('pe', lambda e, kc=kc: e.matmul(
                                pl[:], lhsT=w1t[wb][:, kc, 256 + fl * 128:256 + (fl + 1) * 128],
                                rhs=hgT[eb][:, kc, sl], start=(kc == 0), stop=(kc == 7)),
                                r=['f_w1%d' % wb, 'f_hgT' + EB], w=[plk])
                        kb.op('dve', lambda e: e.tensor_scalar(out=gl[tb][:], in0=pg[:], scalar1=b1t[eb][:, fc:fc + 1],
                                                               scalar2=7.0, op0=ALU.add, op1=ALU.min),
                              r=[pgk, 'f_b1' + EB], w=['f_gl' + TB])
                        kb.op('act', lambda e: e.activation(out=sg[tb][:], in_=gl[tb][:], func=AF.Sigmoid,
                                                            scale=1.702), r=['f_gl' + TB], w=['f_sg' + TB])
                        kb.op('dve', lambda e: e.tensor_scalar(out=li[tb][:], in0=pl[:],
                                                               scalar1=b1t[eb][:, 8 + fc:9 + fc], scalar2=7.0,
                                                               op0=ALU.add, op1=ALU.min),
                              r=[plk, 'f_b1' + EB], w=['f_li' + TB])
                        kb.op('dve', lambda e: e.tensor_scalar(out=li[tb][:], in0=li[tb][:], scalar1=-7.0,
                                                               scalar2=1.0, op0=ALU.max, op1=ALU.add),
                              r=['f_li' + TB], w=['f_li' + TB])
                        kb.op('dve', lambda e: e.tensor_tensor(out=gl[tb][:], in0=gl[tb][:], in1=sg[tb][:],
                                                               op=ALU.mult),
                              r=['f_gl' + TB, 'f_sg' + TB], w=['f_gl' + TB])
                        kb.op('dve', lambda e: e.tensor_tensor(out=actT[:, fc, sl], in0=gl[tb][:], in1=li[tb][:],
                                                               op=ALU.mult),
                              r=['f_gl' + TB, 'f_li' + TB], w=['f_actT'])

            def w2_phase(ex):
                eb = ex % 2
                EB = str(eb)
                for blk in range(NB):
                    yb_ = st5['yc'] % 2
                    st5['yc'] += 1
                    for half in range(2):
                        py, pyk = PY.next()
                        for fc in range(8):
                            kb.op('pe', lambda e, fc=fc: e.matmul(py[:], lhsT=actT[:, fc, blk * 128:(blk + 1) * 128],
                                                                  rhs=w2t[eb][:, fc, half * 512:(half + 1) * 512],
                                                                  start=(fc == 0), stop=False),
                                  r=['f_actT', 'f_w2' + EB], w=[pyk])
                        kb.op('pe', lambda e: e.matmul(py[:], lhsT=onesb[0:1, :],
                                                       rhs=b2t[eb][0:1, half * 512:(half + 1) * 512],
                                                       start=False, stop=True), r=['f_onesb', 'f_b2' + EB], w=[pyk])
                        cp(kb, 'act', yrow[yb_][:, half * 512:(half + 1) * 512], py[:], [pyk], ['f_yr%d' % yb_])
                    kb.dma('sp', yg[ex * C + blk * 128:ex * C + (blk + 1) * 128, :], yrow[yb_][:], r=['f_yr%d' % yb_],
                           w=['s_yg_%d' % (ex * NB + blk)])

            load_exp(0)
            prelude(0)
            load_w1(0)
            for ex in range(NE):
                if ex + 1 < NE:
                    load_exp(ex + 1)
                for g in range(4):
                    i = ex * 4 + g
                    if i + 1 < NE * 4:
                        load_w1(i + 1)
                    w1_group(i)
                if ex + 1 < NE:
                    prelude(ex + 1)
                w2_phase(ex)
            kb.barrier()
        with ExitStack() as s4:
            rows = [[sbt(nc, s4, "f_row%d_%d" % (j, i), [128, D], F32) for i in range(4)] for j in range(2)]
            accs = [sbt(nc, s4, "f_acc%d" % i, [128, D], F32) for i in range(2)]
            xt = [sbt(nc, s4, "f_x%d" % i, [128, D], F32) for i in range(2)]
            lng = sbt(nc, s4, "f_lng", [128, D], F32)
            lnb = sbt(nc, s4, "f_lnb", [128, D], F32)
            lb = [(sbt(nc, s4, "f_lst%d" % i, [128, 2, 6], F32), sbt(nc, s4, "f_lmv%d" % i, [128, 2], F32),
                   sbt(nc, s4, "f_lrs%d" % i, [128, 1], F32), sbt(nc, s4, "f_lnb%d" % i, [128, 1], F32))
                  for i in range(2)]
            kb.dma('sp', lng[:], A['ln2g_bc'][l], w=['lngb'])
            kb.dma('sp', lnb[:], A['ln2b_bc'][l], w=['lngb'])
            def comb_gen(par):
                for T in range(par, NT, 2):
                    xb = T % 2
                    XB = str(xb)
                    kb.dma('sp', xt[xb][:], x_src[T * 128:(T + 1) * 128, :], r=['xsrc5'], w=['f_x' + XB])
                    yield
                    for k in range(4):
                        kb.ind('pool', ['f_dest%d' % T], ['f_row%d_%d' % (xb, k)], out=rows[xb][k][:, :], out_offset=None,
                               in_=yg[:, :], in_offset=bass.IndirectOffsetOnAxis(ap=dest_all[:, T * 4 + k:T * 4 + k + 1], axis=0),
                               bounds_check=cst['breg'], oob_is_err=False)
                        yield
                    kb.op('dve', lambda e: e.tensor_scalar(out=accs[xb][:], in0=rows[xb][0][:], scalar1=gk_all[:, T, 0:1],
                                                           scalar2=None, op0=ALU.mult),
                          r=['f_row%d_0' % xb, 'f_gk'], w=['f_acc' + XB])
                    yield
                    for k in range(1, 4):
                        kb.op('dve', lambda e, k=k: e.scalar_tensor_tensor(out=accs[xb][:], in0=rows[xb][k][:],
                                                                          scalar=gk_all[:, T, k:k + 1], in1=accs[xb][:],
                                                                          op0=ALU.mult, op1=ALU.add),
                              r=['f_row%d_%d' % (xb, k), 'f_gk', 'f_acc' + XB], w=['f_acc' + XB])
                        yield
                    kb.op('pool', lambda e: e.tensor_tensor(out=accs[xb][:], in0=accs[xb][:], in1=mod_bc[:, 5 * D:6 * D],
                                                            op=ALU.mult), r=['f_acc' + XB, 'mod_bc'], w=['f_acc' + XB])
                    yield
                    kb.op('dve', lambda e: e.scalar_tensor_tensor(out=xt[xb][:], in0=xt[xb][:], scalar=ALPHA,
                                                                  in1=accs[xb][:], op0=ALU.mult, op1=ALU.add),
                          r=['f_x' + XB, 'f_acc' + XB], w=['f_x' + XB])
                    yield
                    yield from ln_affine_store_g(kb, nc, xt[xb], 'f_x' + XB, lb[xb], lng[:], lnb[:],
                                    x_dst[T * 128:(T + 1) * 128, :], 'f' + XB, 'xdst5_%d' % T)

            gens = [comb_gen(0), comb_gen(1)]
            while gens:
                for g_ in list(gens):
                    try:
                        next(g_)
                    except StopIteration:
                        gens.remove(g_)
            kb.barrier()
```
